# Optimizing a Trainium2 kernel written in Bass

```python
import jax, jax.numpy as jnp
from jax import lax
import numpy as np

D_MODEL = 1024
BATCH = 16
SEQ = 2048
DEPTH = 2

CTX_LEN = 256
GRID_W = 64
N_MOD = 6
EPS = 1e-6
D_RNN = 1024
RNN_BLOCKS = 16
RNN_BW = D_RNN // RNN_BLOCKS
CONV_W = 4
CONV_PAD = (2, 1)
LRU_C = 8.0
NA_HEADS = 16
NA_HEAD_DIM = 64
D_ATT = NA_HEADS * NA_HEAD_DIM
WIN_H = 8
WIN_W = 16
Q_COL_BLOCK = WIN_W
K_COL_BLOCK = 2 * WIN_W
IN_SIZES = (D_RNN, D_ATT, D_ATT, D_RNN, D_ATT, D_MODEL, D_MODEL)
IN_COLS = 2 * D_RNN + 3 * D_ATT + 2 * D_MODEL
CTX_STATE_COLS = D_RNN + 2 * D_ATT
D_FF = 2816
N_EXPERTS = 8
TOP_K = 2
D_FF_EXPERT = 3584

kernel_name = 'hybrid_rglru_natten_moe_dit_block'


def rms_norm(x):
    xf = x.astype(jnp.float32)
    return (xf * lax.rsqrt(jnp.mean(xf * xf, axis=-1, keepdims=True) + EPS)).astype(x.dtype)


def head_rms_norm(x, gain):
    return rms_norm(x) * gain


def modulate(x, shift, scale):
    return rms_norm(x) * (1 + scale) + shift


def adaln(cond, w_mod, b_mod):
    return jnp.split(jax.nn.silu(cond) @ w_mod + b_mod, N_MOD, axis=-1)


def split_columns(z):
    parts, off = [], 0
    for size in IN_SIZES:
        if off >= z.shape[-1]:
            break
        parts.append(z[..., off:off + size])
        off += size
    return parts


def heads(t):
    return t.reshape(*t.shape[:-1], NA_HEADS, NA_HEAD_DIM)


def dw_conv(x, w, b):
    y = lax.conv_general_dilated(x, w[:, None, :].astype(x.dtype), window_strides=(1,), padding=(CONV_PAD,),
                                 dimension_numbers=('NWC', 'WIO', 'NWC'), feature_group_count=x.shape[-1])
    return y + b.astype(x.dtype)


def rglru_coeffs(xc, lam, w_gates, b_gates):
    xb = xc.reshape(*xc.shape[:-1], RNN_BLOCKS, RNN_BW)
    g = jnp.einsum('blnc,gncd->gblnd', xb, w_gates).reshape(2, *xc.shape) + b_gates[:, None, None, :]
    r = jax.nn.sigmoid(g[0].astype(jnp.float32))
    i = jax.nn.sigmoid(g[1].astype(jnp.float32))
    log_a = LRU_C * r * jax.nn.log_sigmoid(lam.astype(jnp.float32))
    a = jnp.exp(log_a)
    u = jnp.sqrt(-jnp.expm1(2.0 * log_a)) * (i * xc.astype(jnp.float32))
    return a, u


def linear_scan(a, u, h0, reverse, return_seq):
    def step(h, au):
        h = au[0] * h + au[1]
        return h, (h if return_seq else None)
    h_last, hs = lax.scan(step, h0, (jnp.swapaxes(a, 0, 1), jnp.swapaxes(u, 0, 1)), reverse=reverse)
    return (jnp.swapaxes(hs, 0, 1) if return_seq else None), h_last


def bidirectional_rglru(xc_ctx, xc_lat, rg_lambda, rg_w, rg_b, ctx_out):
    h0 = jnp.zeros((xc_lat.shape[0], D_RNN), jnp.float32)
    ys_ctx, ys_lat = [], []
    for d, reverse in enumerate((False, True)):
        a_c, u_c = rglru_coeffs(xc_ctx, rg_lambda[d], rg_w[d], rg_b[d])
        hs_c, h_c = linear_scan(a_c, u_c, h0, reverse, ctx_out)
        a_l, u_l = rglru_coeffs(xc_lat, rg_lambda[d], rg_w[d], rg_b[d])
        hs_l, _ = linear_scan(a_l, u_l, h_c, reverse, True)
        ys_ctx.append(hs_c)
        ys_lat.append(hs_l)
    y_lat = (ys_lat[0] + ys_lat[1]).astype(xc_lat.dtype)
    y_ctx = (ys_ctx[0] + ys_ctx[1]).astype(xc_ctx.dtype) if ctx_out else None
    return y_ctx, y_lat


def na_tables(rows):
    kr = min(WIN_H, rows)
    n_cb = GRID_W // Q_COL_BLOCK
    qcol = np.arange(GRID_W).reshape(n_cb, Q_COL_BLOCK)
    kstart = np.clip(np.arange(n_cb) * Q_COL_BLOCK - WIN_W // 2, 0, GRID_W - K_COL_BLOCK)
    kcol = kstart[:, None] + np.arange(K_COL_BLOCK)[None, :]
    wstart = np.clip(qcol - WIN_W // 2, 0, GRID_W - WIN_W)[..., None]
    kc = kcol[:, None, :]
    col_valid = (kc >= wstart) & (kc < wstart + WIN_W)
    col_off = np.clip(kc - qcol[..., None] + WIN_W - 1, 0, 2 * WIN_W - 2)
    return kr, kcol, col_valid, col_off


def neighbourhood_attention(q, k, v, k_ctx, v_ctx, rpb):
    b, s, h, dh = q.shape
    rows = s // GRID_W
    kr, kcol, col_valid, col_off = na_tables(rows)
    n_cb = kcol.shape[0]
    n_lat = kr * K_COL_BLOCK
    scale = dh ** -0.5
    qg = q.reshape(b, rows, n_cb, Q_COL_BLOCK, h, dh)
    kg = k.reshape(b, rows, GRID_W, h, dh)
    vg = v.reshape(b, rows, GRID_W, h, dh)
    col_bias = rpb[:, :, col_off].astype(jnp.float32)
    mask = jnp.asarray(col_valid)[:, :, None, :]

    def row_block(r):
        start = jnp.clip(r - kr // 2, 0, rows - kr)
        k_blk = lax.dynamic_slice_in_dim(kg, start, kr, axis=1)[:, :, kcol]
        v_blk = lax.dynamic_slice_in_dim(vg, start, kr, axis=1)[:, :, kcol]
        q_blk = lax.dynamic_index_in_dim(qg, r, axis=1, keepdims=False)
        bias = jnp.take(col_bias, start + jnp.arange(kr) - r + WIN_H - 1, axis=1)
        s_lat = (jnp.einsum('bjqhd,brjkhd->bhjqrk', q_blk, k_blk).astype(jnp.float32) * scale
                 + jnp.transpose(bias, (0, 2, 3, 1, 4)))
        s_lat = jnp.where(mask, s_lat, -jnp.inf).reshape(b, h, n_cb, Q_COL_BLOCK, n_lat)
        s_ctx = jnp.einsum('bjqhd,bchd->bhjqc', q_blk, k_ctx).astype(jnp.float32) * scale
        p = jax.nn.softmax(jnp.concatenate([s_lat, s_ctx], axis=-1), axis=-1).astype(v.dtype)
        p_lat = p[..., :n_lat].reshape(b, h, n_cb, Q_COL_BLOCK, kr, K_COL_BLOCK)
        o = (jnp.einsum('bhjqrk,brjkhd->bjqhd', p_lat, v_blk)
             + jnp.einsum('bhjqc,bchd->bjqhd', p[..., n_lat:], v_ctx))
        return o.reshape(b, GRID_W, h, dh)

    out = lax.map(row_block, jnp.arange(rows))
    return jnp.moveaxis(out, 0, 1).reshape(b, s, h * dh)


def context_attention(q, k, v):
    s = jnp.einsum('bqhd,bkhd->bhqk', q, k).astype(jnp.float32) * q.shape[-1] ** -0.5
    p = jax.nn.softmax(s, axis=-1).astype(v.dtype)
    o = jnp.einsum('bhqk,bkhd->bqhd', p, v)
    return o.reshape(*o.shape[:2], -1)


def merge_branches(y_rnn, y_na, g_rnn, g_na, w_rnn_o, w_na_o, w_out):
    return (jax.nn.sigmoid(g_rnn) * (y_rnn @ w_rnn_o) + jax.nn.sigmoid(g_na) * (y_na @ w_na_o)) @ w_out


def token_mixer(h_ctx, h_lat, w_in, conv_w, conv_b, rg_lambda, rg_w, rg_b, q_gain, k_gain, rpb,
                w_rnn_o, w_na_o, w_out, ctx_out):
    x_l, k_l, v_l, y_l, q_l, gr_l, gn_l = split_columns(h_lat @ w_in)
    z_c = split_columns(h_ctx @ (w_in if ctx_out else w_in[:, :CTX_STATE_COLS]))
    x_c, k_c, v_c = z_c[:3]
    rnn_ctx, rnn_lat = bidirectional_rglru(dw_conv(x_c, conv_w, conv_b), dw_conv(x_l, conv_w, conv_b),
                                           rg_lambda, rg_w, rg_b, ctx_out)
    k_c = head_rms_norm(heads(k_c), k_gain)
    v_c = heads(v_c)
    na_lat = neighbourhood_attention(head_rms_norm(heads(q_l), q_gain), head_rms_norm(heads(k_l), k_gain),
                                     heads(v_l), k_c, v_c, rpb)
    out_lat = merge_branches(rnn_lat * jax.nn.gelu(y_l), na_lat, gr_l, gn_l, w_rnn_o, w_na_o, w_out)
    out_ctx = None
    if ctx_out:
        y_c, q_c, gr_c, gn_c = z_c[3:]
        na_ctx = context_attention(head_rms_norm(heads(q_c), q_gain), k_c, v_c)
        out_ctx = merge_branches(rnn_ctx * jax.nn.gelu(y_c), na_ctx, gr_c, gn_c, w_rnn_o, w_na_o, w_out)
    return out_ctx, out_lat


def swiglu(h, w1, w3, w2):
    return (jax.nn.silu(h @ w1) * (h @ w3)) @ w2


def moe_swiglu(h, router, w1, w3, w2):
    logits = (h @ router).astype(jnp.float32)
    top_val, top_idx = lax.top_k(logits, TOP_K)
    top_w = jax.nn.softmax(top_val, axis=-1)
    gates = jnp.sum(jax.nn.one_hot(top_idx, N_EXPERTS, dtype=jnp.float32) * top_w[..., None], axis=-2).astype(h.dtype)
    out = jnp.zeros_like(h)
    for e in range(N_EXPERTS):
        out = out + gates[..., e:e + 1] * swiglu(h, w1[e], w3[e], w2[e])
    return out


def setup_inputs(seed: int = 0) -> dict:
    key = jax.random.key(seed)
    ks = jax.random.split(key, 25)
    f32 = jnp.float32
    n_dense = (DEPTH + 1) // 2
    n_moe = DEPTH // 2

    def nrm(k, shape, scale):
        return jax.random.normal(k, shape, f32) * scale

    a0 = jax.random.uniform(ks[9], (DEPTH, 2, D_RNN), f32, 0.9, 0.999) ** (1.0 / LRU_C)
    return {
        'x': nrm(ks[0], (BATCH, SEQ, D_MODEL), 1.0),
        'c': nrm(ks[1], (BATCH, D_MODEL), 1.0),
        'ctx': nrm(ks[2], (BATCH, CTX_LEN, D_MODEL), 1.0),
        'c_ctx': nrm(ks[3], (D_MODEL,), 1.0),
        'w_mod': nrm(ks[4], (DEPTH, D_MODEL, N_MOD * D_MODEL), D_MODEL ** -0.5),
        'b_mod': nrm(ks[5], (DEPTH, N_MOD * D_MODEL), 0.02),
        'w_in': nrm(ks[6], (DEPTH, D_MODEL, IN_COLS), D_MODEL ** -0.5),
        'conv_w': nrm(ks[7], (DEPTH, CONV_W, D_RNN), CONV_W ** -0.5),
        'conv_b': nrm(ks[8], (DEPTH, D_RNN), 0.02),
        'rg_lambda': jnp.log(a0) - jnp.log1p(-a0),
        'rg_w': nrm(ks[10], (DEPTH, 2, 2, RNN_BLOCKS, RNN_BW, RNN_BW), RNN_BW ** -0.5),
        'rg_b': nrm(ks[11], (DEPTH, 2, 2, D_RNN), 0.02),
        'q_gain': 1.0 + nrm(ks[12], (DEPTH, NA_HEAD_DIM), 0.02),
        'k_gain': 1.0 + nrm(ks[13], (DEPTH, NA_HEAD_DIM), 0.02),
        'rpb': nrm(ks[14], (DEPTH, NA_HEADS, 2 * WIN_H - 1, 2 * WIN_W - 1), 0.1),
        'w_rnn_o': nrm(ks[15], (DEPTH, D_RNN, D_MODEL), D_RNN ** -0.5),
        'w_na_o': nrm(ks[16], (DEPTH, D_ATT, D_MODEL), D_ATT ** -0.5),
        'w_out': nrm(ks[17], (DEPTH, D_MODEL, D_MODEL), D_MODEL ** -0.5),
        'ffn_w1': nrm(ks[18], (n_dense, D_MODEL, D_FF), D_MODEL ** -0.5),
        'ffn_w3': nrm(ks[19], (n_dense, D_MODEL, D_FF), D_MODEL ** -0.5),
        'ffn_w2': nrm(ks[20], (n_dense, D_FF, D_MODEL), D_FF ** -0.5),
        'router': nrm(ks[21], (n_moe, D_MODEL, N_EXPERTS), D_MODEL ** -0.5),
        'moe_w1': nrm(ks[22], (n_moe, N_EXPERTS, D_MODEL, D_FF_EXPERT), D_MODEL ** -0.5),
        'moe_w3': nrm(ks[23], (n_moe, N_EXPERTS, D_MODEL, D_FF_EXPERT), D_MODEL ** -0.5),
        'moe_w2': nrm(ks[24], (n_moe, N_EXPERTS, D_FF_EXPERT, D_MODEL), D_FF_EXPERT ** -0.5),
    }


def reference(x, c, ctx, c_ctx, w_mod, b_mod, w_in, conv_w, conv_b, rg_lambda, rg_w, rg_b, q_gain, k_gain,
              rpb, w_rnn_o, w_na_o, w_out, ffn_w1, ffn_w3, ffn_w2, router, moe_w1, moe_w3, moe_w2):
    xc = ctx
    for l in range(DEPTH):
        ctx_out = l < DEPTH - 1
        sh1, sc1, g1, sh2, sc2, g2 = [m[:, None, :] for m in adaln(c, w_mod[l], b_mod[l])]
        csh1, csc1, cg1, csh2, csc2, cg2 = adaln(c_ctx, w_mod[l], b_mod[l])
        mix_ctx, mix_lat = token_mixer(modulate(xc, csh1, csc1), modulate(x, sh1, sc1), w_in[l], conv_w[l],
                                       conv_b[l], rg_lambda[l], rg_w[l], rg_b[l], q_gain[l], k_gain[l], rpb[l],
                                       w_rnn_o[l], w_na_o[l], w_out[l], ctx_out)
        x = x + g1 * mix_lat
        if ctx_out:
            xc = xc + cg1 * mix_ctx
        if l % 2 == 0:
            ffn = lambda h, j=l // 2: swiglu(h, ffn_w1[j], ffn_w3[j], ffn_w2[j])
        else:
            ffn = lambda h, j=l // 2: moe_swiglu(h, router[j], moe_w1[j], moe_w3[j], moe_w2[j])
        x = x + g2 * ffn(modulate(x, sh2, sc2))
        if ctx_out:
            xc = xc + cg2 * ffn(modulate(xc, csh2, csc2))
    return x
```

```python
import contextlib
import numpy as np
import concourse.bass as bass
import concourse.mybir as mybir
from concourse.bass_utils import run_bass_kernel_spmd

F32 = mybir.dt.float32
BF16 = mybir.dt.bfloat16
AF = mybir.ActivationFunctionType
ALU = mybir.AluOpType
AX = mybir.AxisListType

NCORES = 8
D = 1024
NCH = 8
CTX = 256
SEQ = 2048
NT = CTX + SEQ
TILES = [(0, 256), (256, 512), (768, 512), (1280, 512), (1792, 512)]
D_FF = 2816
D_FFE = 3584
NEXP = 8
EPS = 1e-6
NSLOT = 8
NRING = 5
SLAB = 4096
WCH = 16
SPARSE_MOE = True
MOE_TSZ = 512
JCLAMP = None
MERGED_MOE = True
ZERO_HG = True
SB_BASE = 16384 + 128


class Ins:
    __slots__ = ("eng", "fn", "reads", "writes", "dma", "deps", "sig", "semkey", "val", "slot", "cond")


class Prog:
    ENGS = ["pe", "act", "dve", "pool", "sp"]

    def __init__(self):
        self.ins = []
        self.cur_cond = None
        self.ncond = 0
        self.cond_thr = {}

    def op(self, eng, fn, reads=(), writes=()):
        i = Ins()
        i.eng, i.fn, i.reads, i.writes, i.dma = eng, fn, tuple(reads), tuple(writes), False
        i.sig, i.deps, i.semkey, i.val, i.slot = False, (), None, 0, 0
        i.cond = self.cur_cond
        self.ins.append(i)
        return i

    def cond_begin(self, thr):
        self.ncond += 1
        self.cur_cond = self.ncond
        self.cond_thr[self.ncond] = thr

    def cond_end(self):
        self.cur_cond = None

    def load_reg(self, ap, key, engines=("pe", "act", "dve")):
        for e in engines:
            self.op(e, ("REGLOAD", ap), reads=[key])

    def dma(self, q, fn, reads=(), writes=()):
        i = self.op(q, fn, reads, writes)
        i.dma = True
        return i

    def barrier(self):
        for e in ("pe", "act", "dve", "sp"):
            self.op(e, lambda en: en.nop(), writes=("_bar",))

    def resolve(self):
        last_w = {}
        readers = {}
        ndma = {e: 0 for e in self.ENGS}
        slot_last = {e: {} for e in self.ENGS}
        for idx, I in enumerate(self.ins):
            reads = I.reads
            if I.eng != "pool" and "_bar" not in I.writes:
                reads = reads + ("_bar",)
            deps = {}
            for k in reads:
                j = last_w.get(k)
                if j is not None:
                    deps[j] = True
            for k in I.writes:
                j = last_w.get(k)
                if j is not None:
                    deps.setdefault(j, False)
                r = readers.get(k)
                if r:
                    for j2 in r[0].values():
                        deps.setdefault(j2, False)
                    for j2 in r[1]:
                        deps.setdefault(j2, False)
            final = []
            for j, raw in deps.items():
                J = self.ins[j]
                if J.dma:
                    final.append(j)
                elif J.eng == I.eng:
                    if I.dma or (raw and I.eng != "pe"):
                        final.append(j)
                else:
                    final.append(j)
            if I.dma:
                q = I.eng
                slot = ndma[q] % NSLOT
                prev = slot_last[q].get(slot)
                if prev is not None:
                    final.append(prev)
                slot_last[q][slot] = idx
                I.slot = slot
                ndma[q] += 1
            I.deps = final
            for j in final:
                self.ins[j].sig = True
            for k in reads:
                r = readers.setdefault(k, ({}, []))
                if I.dma:
                    r[1].append(idx)
                else:
                    r[0][I.eng] = idx
            for k in I.writes:
                last_w[k] = idx
                readers[k] = ({}, [])
        cnt = {e: 0 for e in self.ENGS}
        dcnt = {}
        for I in self.ins:
            if I.dma:
                key = ("d", I.eng, I.slot)
                dcnt[key] = dcnt.get(key, 0) + 16
                I.semkey, I.val = key, dcnt[key]
            elif I.sig:
                cnt[I.eng] += 1
                I.semkey, I.val = ("e", I.eng), cnt[I.eng]
        self.final_dma = dict(dcnt)

    def emit(self, nc, final_waits_on="sp"):
        self.resolve()
        keys = [("e", e) for e in self.ENGS]
        for e in self.ENGS:
            if any(I.dma and I.eng == e for I in self.ins):
                keys += [("d", e, s) for s in range(NSLOT)]
        with contextlib.ExitStack() as st:
            sems = {}
            for k in keys:
                sems[k] = st.enter_context(nc.semaphore("s_" + "_".join(str(x) for x in k)))
            block = st.enter_context(nc.Block())
            per = {e: [I for I in self.ins if I.eng == e] for e in self.ENGS}

            def replay(ename, eng):
                seen = {}
                reg = {}

                def do_waits(I, seen, only_external=None):
                    waits = {}
                    for j in I.deps:
                        J = self.ins[j]
                        if only_external is not None and J.cond == only_external:
                            continue
                        if waits.get(J.semkey, 0) < J.val:
                            waits[J.semkey] = J.val
                    for sk, v in waits.items():
                        if seen.get(sk, 0) < v:
                            eng.wait_ge(sems[sk], v)
                            seen[sk] = v

                def run(I):
                    if isinstance(I.fn, tuple):
                        if "r" not in reg:
                            reg["r"] = eng.alloc_register("rj_" + ename)
                        r = eng.reg_load(reg["r"], I.fn[1])
                    else:
                        r = I.fn(eng)
                    if I.dma:
                        r.then_inc(sems[I.semkey], 16)
                    elif I.sig:
                        r.then_inc(sems[I.semkey], 1)

                lst = per[ename]
                n = len(lst)
                p = 0
                while p < n:
                    I = lst[p]
                    if I.cond is None:
                        do_waits(I, seen)
                        run(I)
                        p += 1
                        continue
                    cid = I.cond
                    q = p
                    while q < n and lst[q].cond == cid:
                        q += 1
                    body = lst[p:q]
                    for B in body:
                        do_waits(B, seen, only_external=cid)
                    snap = dict(seen)
                    k = sum(1 for B in body if B.sig and not B.dma)
                    with eng.If_lt(reg["r"], self.cond_thr[cid]):
                        if k > 0:
                            eng.drain().then_inc(sems[("e", ename)], k)
                        for B in body:
                            if B.dma:
                                eng.nop().then_inc(sems[B.semkey], 16)
                        if k == 0 and not any(B.dma for B in body):
                            eng.nop()
                    with eng.Else():
                        inner = dict(snap)
                        for B in body:
                            do_waits(B, inner)
                            run(B)
                    seen = snap
                    p = q
                if ename == final_waits_on:
                    for sk, v in self.final_dma.items():
                        if seen.get(sk, 0) < v:
                            eng.wait_ge(sems[sk], v)

            @block.tensor
            def _(e):
                replay("pe", e)

            @block.scalar
            def _(e):
                replay("act", e)

            @block.vector
            def _(e):
                replay("dve", e)

            @block.gpsimd
            def _(e):
                replay("pool", e)

            @block.sync
            def _(e):
                replay("sp", e)


def na_plan():
    pats = []
    groups = {}
    plan = []
    for qp in range(16):
        r0 = 2 * qp
        s = [min(max(r - 4, 0), 24) for r in (r0, r0 + 1)]
        first = s[0] // 2
        last = (s[1] + 7) // 2
        keys = []
        for m in range(first, last + 1):
            key = []
            for kl in range(2):
                kr = 2 * m + kl
                for ql in range(2):
                    r = r0 + ql
                    valid = s[ql] <= kr < s[ql] + 8
                    key.append(kr - r + 7 if valid else None)
            keys.append(tuple(key))
        gk = tuple(keys)
        if gk not in groups:
            groups[gk] = len(pats)
            pats.extend(keys)
        base = groups[gk]
        plan.append([(first + i, base + i) for i in range(len(keys))])
    return pats, plan


NA_PATS, NA_PLAN = na_plan()
NPAT = len(NA_PATS)


def slab_order():
    pro = [("mod", l, g) for l in range(2) for g in range(12)]
    seq = []
    for l in range(2):
        ntile = 5 if l == 0 else 4
        seq += [("rnn", l, i) for i in range(4)]
        for g in range(2):
            seq += [("rnno", l, g), ("gr", l, g)]
        seq += [("kvq", l, c) for c in range(8)]
        for t in range(ntile):
            for g in range(2):
                seq += [("nao", l, g), ("gn", l, g)]
            for g in range(2):
                seq += [("out", l, g)]
        if l == 0:
            for g in range(6):
                seq += [("f1", g), ("f3", g), ("f2", g)]
        else:
            for e in range(NEXP):
                for g in range(7):
                    seq += [("m1", e, g), ("m3", e, g), ("m2", e, g)]
    return pro, seq


def unique_slabs():
    pro, seq = slab_order()
    names = []
    seen = set()
    for n in pro + seq:
        if n not in seen:
            seen.add(n)
            names.append(n)
    return names


SLAB_NAMES = unique_slabs()
SLAB_IDX = {n: i for i, n in enumerate(SLAB_NAMES)}


def _slab_cols(W, cols):
    S = W[:, cols]
    return np.ascontiguousarray(S.reshape(8, 128, 512).transpose(1, 0, 2)).reshape(128, SLAB)


def _slab_rows(W2, r0):
    blk = np.zeros((512, 1024), np.float32)
    n = max(0, min(512, W2.shape[0] - r0))
    blk[:n] = W2[r0:r0 + n]
    return np.ascontiguousarray(blk.reshape(4, 128, 1024).transpose(1, 0, 2)).reshape(128, SLAB)


def _cols_pad(W, c0, n):
    idx = np.full(512, c0, np.int64)
    idx[:n] = np.arange(c0, c0 + n)
    return idx


def build_wstream(inp):
    ar = np.arange
    out = np.empty((len(SLAB_NAMES), 128, SLAB), np.float32)
    for i, nm in enumerate(SLAB_NAMES):
        k = nm[0]
        if k == "mod":
            _, l, g = nm
            out[i] = _slab_cols(inp["w_mod"][l], ar(g * 512, g * 512 + 512))
        elif k == "rnn":
            _, l, j = nm
            cols = np.concatenate([ar(c * 128, c * 128 + 128) if which == 0 else ar(3072 + c * 128, 3072 + c * 128 + 128)
                                   for c in (2 * j, 2 * j + 1) for which in (0, 1)])
            out[i] = _slab_cols(inp["w_in"][l], cols)
        elif k == "rnno":
            _, l, g = nm
            out[i] = _slab_cols(inp["w_rnn_o"][l], ar(g * 512, g * 512 + 512))
        elif k == "gr":
            _, l, g = nm
            out[i] = _slab_cols(inp["w_in"][l], ar(5120 + g * 512, 5120 + g * 512 + 512))
        elif k == "kvq":
            _, l, c = nm
            cols = np.concatenate([ar(1024 + c * 128, 1024 + c * 128 + 128), ar(2048 + c * 128, 2048 + c * 128 + 128),
                                   ar(4096 + c * 128, 4096 + c * 128 + 128), ar(4096 + c * 128, 4096 + c * 128 + 128)])
            out[i] = _slab_cols(inp["w_in"][l], cols)
        elif k == "nao":
            _, l, g = nm
            out[i] = _slab_cols(inp["w_na_o"][l], ar(g * 512, g * 512 + 512))
        elif k == "gn":
            _, l, g = nm
            out[i] = _slab_cols(inp["w_in"][l], ar(6144 + g * 512, 6144 + g * 512 + 512))
        elif k == "out":
            _, l, g = nm
            out[i] = _slab_cols(inp["w_out"][l], ar(g * 512, g * 512 + 512))
        elif k in ("f1", "f3"):
            _, g = nm
            W = inp["ffn_w1"][0] if k == "f1" else inp["ffn_w3"][0]
            n = min(512, D_FF - g * 512)
            out[i] = _slab_cols(W, _cols_pad(W, g * 512, n))
        elif k == "f2":
            _, g = nm
            out[i] = _slab_rows(inp["ffn_w2"][0], g * 512)
        elif k in ("m1", "m3"):
            _, e, g = nm
            W = inp["moe_w1"][0][e] if k == "m1" else inp["moe_w3"][0][e]
            out[i] = _slab_cols(W, ar(g * 512, g * 512 + 512))
        elif k == "m2":
            _, e, g = nm
            out[i] = _slab_rows(inp["moe_w2"][0][e], g * 512)
        else:
            raise KeyError(nm)
    return out


def _pm(v):
    v = np.asarray(v, np.float32)
    lead = v.shape[:-1]
    return np.ascontiguousarray(np.moveaxis(v.reshape(*lead, 8, 128), -1, 0))


def build_shared(inp):
    sh = {}
    wsr = build_wstream(inp)
    for i in range((len(SLAB_NAMES) + WCH - 1) // WCH):
        sh[f"wstream{i}"] = wsr[i * WCH:(i + 1) * WCH]
    sh["bmodT"] = np.ascontiguousarray(np.moveaxis(inp["b_mod"].reshape(2, 48, 128), -1, 0))
    sh["convw"] = np.ascontiguousarray(np.moveaxis(inp["conv_w"].reshape(2, 4, 8, 128), -1, 0).transpose(0, 1, 3, 2))
    sh["convb"] = _pm(inp["conv_b"])
    sh["lam"] = _pm(inp["rg_lambda"])
    sh["rgb"] = _pm(inp["rg_b"])
    g = np.stack([inp["q_gain"], inp["k_gain"]], 1)
    sh["gains"] = np.ascontiguousarray(np.concatenate([g, g], -1).transpose(2, 0, 1))
    rgw = inp["rg_w"]
    bd = np.zeros((2, 128, 2, 2, 8, 128), np.float32)
    for hb in range(2):
        blk = rgw[:, :, :, hb::2]
        bd[:, hb * 64:(hb + 1) * 64, :, :, :, hb * 64:(hb + 1) * 64] = np.moveaxis(blk, 4, 1)
    sh["rgw"] = bd.reshape(2, 128, 32 * 128)
    kp = np.arange(128)
    kl, kc = kp // 64, kp % 64
    ql, qc = kp // 64, kp % 64
    wstart = np.clip(qc - 8, 0, 48)
    colv = (kc[:, None] >= wstart[None, :]) & (kc[:, None] < wstart[None, :] + 16)
    coff = np.clip(kc[:, None] - qc[None, :] + 15, 0, 30)
    bias = np.zeros((2, 8, 128, 2, NPAT, 128), np.float32)
    mask = np.zeros((128, NPAT, 128), np.float32)
    rpb = inp["rpb"]
    for pc, key in enumerate(NA_PATS):
        drm = np.full((128, 128), -1, np.int64)
        for a in range(2):
            for b in range(2):
                dr = key[a * 2 + b]
                if dr is not None:
                    sel = (kl[:, None] == a) & (ql[None, :] == b)
                    drm[sel] = dr
        valid = (drm >= 0) & colv
        mask[:, pc, :] = valid
        drc = np.where(drm >= 0, drm, 0)
        gathered = rpb[:, :, drc, coff]
        gathered = np.where(valid[None, None], gathered, np.float32(0))
        bias[:, :, :, :, pc, :] = gathered.reshape(2, 8, 2, 128, 128).transpose(0, 1, 3, 2, 4)
    sh["biasG"] = bias.reshape(2, 8, 128, 2 * NPAT * 128)
    sh["maskG"] = mask.reshape(128, NPAT * 128)
    sh["router"] = np.ascontiguousarray(inp["router"][0].reshape(8, 128, 8).transpose(1, 0, 2)).reshape(128, 64)
    sh["ident"] = np.eye(128, dtype=np.float32)
    sh["ltri"] = np.triu(np.ones((128, 128), np.float32), 1)
    return sh


def build_core_inputs(inp, core):
    b0 = 2 * core
    toks = np.concatenate([inp["ctx"][b0:b0 + 2], inp["x"][b0:b0 + 2]], axis=1)
    xT = np.ascontiguousarray(toks.reshape(2, NT, 8, 128).transpose(0, 3, 2, 1))
    cv = np.zeros((4, 1024), np.float32)
    cv[0:2] = inp["c"][b0:b0 + 2]
    cv[2] = inp["c_ctx"]
    scT = np.ascontiguousarray(cv.reshape(4, 8, 128).transpose(2, 1, 0))
    return {"xT": xT, "scT": scT}


def _nbytes(dt):
    return 2 if dt == BF16 else 4


class SBAlloc:
    def __init__(self, nc, limit):
        self.nc, self.off, self.limit, self.n = nc, SB_BASE, SB_BASE + limit - 256, 0

    def __call__(self, name, free_shape, dt, off=None):
        size = int(np.prod(free_shape)) * _nbytes(dt)
        size = (size + 31) // 32 * 32
        if off is None:
            off = self.off
            self.off += size
        assert off + size <= self.limit, (name, off, size, self.limit)
        self.n += 1
        return self.nc.alloc_sbuf_tensor_at(f"{name}_{self.n}", [128] + list(free_shape), dt, offset=off), off + size


def build_program(stage="full", debug=None, nslabs=None):
    nc = bass.Bass("TRN2", target_bir_lowering=False)
    P = Prog()
    limit = nc.sbuf_bytes_remaining
    A = SBAlloc(nc, limit)

    def din(name, shape):
        return nc.dram_tensor(name, list(shape), F32, kind="ExternalInput").ap()

    xT_d = din("xT", [2, 128, 8, NT])
    scT_d = din("scT", [128, 8, 4])
    nsl = nslabs or len(SLAB_NAMES)
    ws_d = [din(f"wstream{i}", [min(WCH, nsl - i * WCH), 128, SLAB]) for i in range((nsl + WCH - 1) // WCH)]
    bmod_d = din("bmodT", [128, 2, 48])
    convw_d = din("convw", [128, 2, 8, 4])
    convb_d = din("convb", [128, 2, 8])
    lam_d = din("lam", [128, 2, 2, 8])
    rgb_d = din("rgb", [128, 2, 2, 2, 8])
    gains_d = din("gains", [128, 2, 2])
    rgw_d = din("rgw", [2, 128, 32 * 128])
    biasG_d = din("biasG", [2, 8, 128, 2 * NPAT * 128])
    maskG_d = din("maskG", [128, NPAT * 128])
    router_d = din("router", [128, 64])
    ident_d = din("ident", [128, 128])
    ltri_d = din("ltri", [128, 128])
    outT_d = nc.dram_tensor("outT", [2, 128, 8, SEQ], F32, kind="ExternalOutput").ap()
    xres_d = nc.dram_tensor("xres", [2, 128, 8, NT], F32, kind="Internal").ap()
    mrnn_d = nc.dram_tensor("mrnn", [128, 8, NT], F32, kind="Internal").ap()
    hg_d = nc.dram_tensor("hg", [NEXP * SEQ * 2, D], BF16, kind="Internal").ap()
    yb_d = nc.dram_tensor("yb", [NEXP * SEQ * 2, D], F32, kind="Internal").ap()
    dbg_d = None
    if debug is not None:
        dbg_d = nc.dram_tensor("dbg", list(debug), F32, kind="ExternalOutput").ap()

    ones_bf, _ = A("ones_bf", [128], BF16)
    blk_bf, _ = A("blk_bf", [128], BF16)
    onesV, _ = A("onesV", [192], BF16)
    ident_f, _ = A("ident_f", [128], F32)
    ones_f, _ = A("ones_f", [128], F32)
    ident_bf, _ = A("ident_bf", [128], BF16)
    ltri_bf, _ = A("ltri_bf", [128], BF16)
    ZT, _ = A("ZT", [D], BF16)
    scT, _ = A("scT", [8, 4], F32)
    scb, _ = A("scb", [8, 4], BF16)
    bmodT, _ = A("bmodT", [2, 48], F32)
    mod, _ = A("mod", [2, 48, 4], F32)
    convw, _ = A("convw", [2, 8, 4], F32)
    convb, _ = A("convb", [2, 8], F32)
    lam, _ = A("lam", [2, 2, 8], F32)
    c1, _ = A("c1", [2, 2, 8], F32)
    c2, _ = A("c2", [2, 2, 8], F32)
    rgb, _ = A("rgb", [2, 2, 2, 8], F32)
    gains, _ = A("gains", [2, 2], F32)
    qg, _ = A("qg", [2], F32)
    rgw, _ = A("rgw", [32, 128], BF16)
    maskG, _ = A("maskG", [NPAT, 128], BF16)
    router, _ = A("router", [8, 8], F32)
    routb, _ = A("routb", [2], F32)
    WS = [A(f"ws{i}", [SLAB], BF16)[0] for i in range(NRING)]
    HT_OFF = A.off
    HT, _ = A("HT", [8, NT], BF16)
    NXR = 4
    for i_ in range(NXR):
        WS.append(A(f"wsx{i_}", [SLAB], BF16, HT_OFF + i_ * SLAB * 2)[0])
    XBASE = A.off

    ps = [nc.alloc_psum_tensor(f"ps{i}", [128, 512], F32) for i in range(8)]

    def PSK(i):
        return ("ps", i)

    ring = {"n": 0}

    def load_slab(name, big=False):
        i = ring["n"] % (NRING + NXR if big else NRING)
        ring["n"] += 1
        src = ws_d[SLAB_IDX[name] // WCH][SLAB_IDX[name] % WCH]
        dst = WS[i]
        wr = [("ws", i)] + ([("HT", t_) for t_ in range(5)] if i >= NRING else [])
        P.dma("pool", lambda e, dst=dst, src=src: e.dma_start(out=dst[:, :], in_=src), writes=wr)
        return i

    def wk(i, k, c0, n):
        return WS[i][:, k * 512 + c0: k * 512 + c0 + n]

    def wk2(i, j, c0, n):
        return WS[i][:, j * 1024 + c0: j * 1024 + c0 + n]

    def mm(out, lhsT, rhs, start, stop, reads, pk):
        P.op("pe", lambda e: e.matmul(out, lhsT, rhs, start=start, stop=stop), reads=reads, writes=[pk])

    def act(out, in_, func, reads, writes, bias=None, scale=None):
        kw = {}
        if bias is not None:
            kw["bias"] = bias
        if scale is not None:
            kw["scale"] = scale
        P.op("act", lambda e: e.activation(out, in_, func, **kw), reads=reads, writes=writes)

    def tt(out, in0, in1, op, reads, writes, eng="dve"):
        P.op(eng, lambda e: e.tensor_tensor(out, in0, in1, op), reads=reads, writes=writes)

    def ts(out, in0, s1, s2, op0, op1, reads, writes, eng="dve"):
        if s2 is None:
            P.op(eng, lambda e: e.tensor_scalar(out, in0, s1, None, op0), reads=reads, writes=writes)
        else:
            P.op(eng, lambda e: e.tensor_scalar(out, in0, s1, s2, op0, op1), reads=reads, writes=writes)

    def stt(out, in0, scalar, in1, op0, op1, reads, writes):
        P.op("dve", lambda e: e.scalar_tensor_tensor(out, in0, scalar, in1, op0, op1), reads=reads, writes=writes)

    def sp_dma(out, in_, reads, writes):
        P.dma("sp", lambda e: e.dma_start(out=out, in_=in_), reads=reads, writes=writes)

    def pool_dma(out, in_, reads, writes):
        P.dma("pool", lambda e: e.dma_start(out=out, in_=in_), reads=reads, writes=writes)

    P.op("dve", lambda e: e.memset(ones_bf[:, :], 1.0), writes=["ones_bf"])
    P.op("dve", lambda e: e.memset(ones_f[:, :], 1.0), writes=["ones_f"])
    P.op("dve", lambda e: e.memset(blk_bf[:, :], 0.0), writes=["blk_bf"])
    P.op("dve", lambda e: e.memset(blk_bf[0:64, 0:64], 1.0), writes=["blk_bf"])
    P.op("dve", lambda e: e.memset(blk_bf[64:128, 64:128], 1.0), writes=["blk_bf"])
    P.op("dve", lambda e: e.memset(onesV[:, :], 1.0), writes=["onesV"])
    P.op("dve", lambda e: e.memset(onesV[:, 64:128], 0.0), writes=["onesV"])
    sp_dma(ident_f[:, :], ident_d, [], ["ident_f"])
    pool_dma(ident_bf[:, :], ident_d, [], ["ident_bf"])
    pool_dma(ltri_bf[:, :], ltri_d, [], ["ltri_bf"])
    sp_dma(scT[:, :, :], scT_d, [], ["scT"])
    sp_dma(bmodT[:, :, :], bmod_d, [], ["bmodT"])
    sp_dma(convw[:, :, :, :], convw_d, [], ["convw"])
    sp_dma(convb[:, :, :], convb_d, [], ["convb"])
    sp_dma(lam[:, :, :, :], lam_d, [], ["lam"])
    sp_dma(rgb[:, :, :, :, :], rgb_d, [], ["rgb"])
    sp_dma(gains[:, :, :], gains_d, [], ["gains"])
    sp_dma(router[:, :, :], router_d.rearrange("p (k e) -> p k e", e=8), [], ["router"])
    pool_dma(maskG[:, :, :], maskG_d.rearrange("p (a b) -> p a b", b=128), [], ["maskG"])
    if SPARSE_MOE and MERGED_MOE and stage in ("full", "seq1"):
        P.op("dve", lambda e: e.memset(ZT[:, :], 0.0), writes=["ZT"])
        for blk in range(NEXP * SEQ * 2 // 128):
            sp_dma(hg_d[blk * 128:(blk + 1) * 128, :], ZT[:, :], ["ZT"], [("hgz", blk)])
    act(c1[:, :, :, :], lam[:, :, :, :], AF.Exp, ["lam"], ["c1"], scale=-1.0)
    act(c1[:, :, :, :], c1[:, :, :, :], AF.Ln, ["c1"], ["c1"], bias=1.0)
    ts(c2[:, :, :, :], c1[:, :, :, :], -16.0, None, ALU.mult, None, ["c1"], ["c2"])
    ts(c1[:, :, :, :], c1[:, :, :, :], -8.0, None, ALU.mult, None, ["c1", "c2"], ["c1"])
    act(scb[:, :, :], scT[:, :, :], AF.Silu, ["scT"], ["scb"])
    for l in range(2):
        for g in range(12):
            i = load_slab(("mod", l, g))
            for j in range(4):
                col = (g * 4 + j) * 4
                for k in range(8):
                    mm(ps[0][:, col:col + 4], wk(i, k, j * 128, 128), scb[:, k, :], k == 0, k == 7,
                       [("ws", i), "scb"], PSK(0))
        pv = ps[0][:, 0:192].rearrange("p (a b) -> p a b", b=4)
        for j in range(3):
            tt(mod[:, l, :, j], pv[:, :, j], bmodT[:, l, :], ALU.add, ["bmodT"], [PSK(0), "mod"])
    for l in range(2):
        for m in (1, 4):
            ts(mod[:, l, m * 8:(m + 1) * 8, :], mod[:, l, m * 8:(m + 1) * 8, :], 1.0, None, ALU.add, None, ["mod"], ["mod"])

    def modv(l, m, c, col):
        return mod[:, l, m * 8 + c, col:col + 1]

    stages = ["pro", "A0", "B0", "C0", "D0", "E0", "F0", "G0", "H0", "F1", "G1", "G1a", "H1", "seq1", "full"]
    si = stages.index(stage)

    state = {"x_in_res": False}

    def modulate(l, s, which, tiles, xsrc, X, moe=False, after_tile=None):
        m_sh, m_sc = (0, 1) if which == 0 else (3, 4)
        for ti in tiles:
            t0, w = TILES[ti]
            col = 2 if ti == 0 else s
            xap, xkeys = xsrc(ti)
            SQ, RS = X["SQ"], X["RS"]
            for c in range(8):
                act(SQ[:, c, 0:w], xap(c), AF.Square, xkeys, [("SQ", c)])
            for c in range(8):
                mm(ps[1][:, 0:w], ones_bf[:, :], SQ[:, c, 0:w], c == 0, c == 7, [("SQ", c), "ones_bf"], PSK(1))
            act(RS[:, 0:w], ps[1][:, 0:w], AF.Sqrt, [], [PSK(1), "RS"], bias=EPS, scale=1.0 / D)
            P.op("dve", lambda e, w=w: e.reciprocal(RS[:, 0:w], RS[:, 0:w]), reads=["RS"], writes=["RS"])
            for c in range(8):
                if moe:
                    tmp, tk = X["TMP8"][:, c, 0:w], ("TMP8", c)
                else:
                    tmp, tk = X["TMP"][c % 2][:, 0:w], ("TMP", c % 2)
                stt(tmp, xap(c), modv(l, m_sc, c, col), RS[:, 0:w], ALU.mult, ALU.mult, list(xkeys) + ["mod", "RS"], [tk])
                act(HT[:, c, t0:t0 + w], tmp, AF.Identity, [tk, "mod"], [("HT", ti)], bias=modv(l, m_sh, c, col))
            if after_tile is not None:
                after_tile(ti)

    def token_mixer(l, s):
        ctx_out = (l == 0)
        tiles = [0, 1, 2, 3, 4]
        otiles = tiles if ctx_out else [1, 2, 3, 4]
        off = XBASE
        YB, off = A("YB", [8, NT], BF16, off)
        TB = off
        pool_dma(rgw[:, :, :], rgw_d[l].rearrange("p (a b) -> p a b", b=128), [], ["rgw"])
        ts(qg[:, 0:1], gains[:, l, 0:1], 0.125, None, ALU.mult, None, ["gains"], ["qg"])

        off = TB
        XL = []
        for i in range(2):
            t_, off = A(f"XL{i}", [8, 512], F32, off)
            XL.append(t_)
        X = {}
        X["SQ"], off = A("SQ", [8, 512], BF16, off)
        X["RS"], off = A("RS", [512], F32, off)
        X["TMP"] = []
        for i in range(2):
            t_, off = A(f"TMP{i}", [512], F32, off)
            X["TMP"].append(t_)
        xd = xres_d if state["x_in_res"] else xT_d

        def xsrc(ti):
            t0, w = TILES[ti]
            b = ti % 2
            sp_dma(XL[b][:, :, 0:w], xd[s, :, :, t0:t0 + w], [("xd", ti)], [("XL", b)])
            return (lambda c: XL[b][:, c, 0:w]), [("XL", b)]

        modulate(l, s, 0, tiles, xsrc, X)
        P.barrier()
        if stage == "A0":
            return HT, [("HT", t) for t in range(5)]

        off = TB
        S0, off = A("S0", [2312], F32, off)
        XC, off = A("XC", [NT], F32, off)
        S2, off = A("S2", [NT], F32, off)
        S3, off = A("S3", [NT], F32, off)
        HF, off = A("HF", [NT], F32, off)
        HR, off = A("HR", [NT], F32, off)
        XCb, off = A("XCb", [NT], BF16, off)
        GT, off = A("GT", [512], F32, off)

        def xrp_pos(t0):
            return 2 + t0 if t0 < CTX else 261 + (t0 - CTX)

        for c in range(8):
            if c % 2 == 0:
                wsi = load_slab(("rnn", l, c // 2))
            cb = (c % 2) * 256
            for a, b in ((0, 2), (258, 261), (2309, 2312)):
                P.op("dve", lambda e, a=a, b=b: e.memset(S0[:, a:b], 0.0), writes=["S0"])
            for ti in tiles:
                t0, w = TILES[ti]
                pb = 2 + (ti % 2)
                for k in range(8):
                    mm(ps[pb][:, 0:w], wk(wsi, k, cb, 128), HT[:, k, t0:t0 + w], k == 0, k == 7,
                       [("ws", wsi), ("HT", ti)], PSK(pb))
                p0 = xrp_pos(t0)
                act(S0[:, p0:p0 + w], ps[pb][:, 0:w], AF.Identity, [], [PSK(pb), "S0"])
            for (d0, n, base) in ((0, CTX, 2), (CTX, SEQ, 261)):
                ts(XC[:, d0:d0 + n], S0[:, base - 2:base - 2 + n], convw[:, l, c, 0:1], convb[:, l, c:c + 1],
                   ALU.mult, ALU.add, ["S0", "convw", "convb"], [("XC", d0)])
                for j in range(1, 4):
                    stt(XC[:, d0:d0 + n], S0[:, base - 2 + j:base - 2 + j + n], convw[:, l, c, j:j + 1], XC[:, d0:d0 + n],
                        ALU.mult, ALU.add, ["S0", "convw", ("XC", d0)], [("XC", d0)])
            act(XCb[:, :], XC[:, :], AF.Identity, [("XC", 0), ("XC", CTX)], ["XCb"])
            for dr in range(2):
                for ti in tiles:
                    t0, w = TILES[ti]
                    for gt in range(2):
                        pb = 4 + gt + 2 * (ti % 2)
                        mm(ps[pb][:, 0:w], rgw[:, (dr * 2 + gt) * 8 + c, :], XCb[:, t0:t0 + w], True, True,
                           ["rgw", "XCb"], PSK(pb))
                        dst = S2 if gt == 0 else S3
                        act(dst[:, t0:t0 + w], ps[pb][:, 0:w], AF.Sigmoid, ["rgb"], [PSK(pb), ("S2" if gt == 0 else "S3")],
                            bias=rgb[:, l, dr, gt, c:c + 1])
                act(S0[:, 0:NT], S2[:, :], AF.Exp, ["S2", "c1"], ["S0"], scale=c1[:, l, dr, c:c + 1])
                act(S2[:, :], S2[:, :], AF.Exp, ["S2", "c2"], ["S2"], scale=c2[:, l, dr, c:c + 1])
                act(S2[:, :], S2[:, :], AF.Sqrt, ["S2"], ["S2"], scale=-1.0, bias=1.0)
                tt(S3[:, :], S3[:, :], XC[:, :], ALU.mult, ["S3", ("XC", 0), ("XC", CTX)], ["S3"])
                tt(S3[:, :], S3[:, :], S2[:, :], ALU.mult, ["S3", "S2"], ["S3"])
                if dr == 0:
                    P.op("dve", lambda e: e.tensor_tensor_scan(HF[:, :], S0[:, 0:NT], S3[:, :], 0.0, ALU.mult, ALU.add),
                         reads=["S0", "S3"], writes=["HF"])
                else:
                    P.op("dve", lambda e: e.tensor_tensor_scan(HR[:, CTX - 1::-1], S0[:, CTX - 1::-1], S3[:, CTX - 1::-1], 0.0,
                                                               ALU.mult, ALU.add),
                         reads=["S0", "S3"], writes=["HR"])
                    P.op("dve", lambda e: e.tensor_tensor_scan(HR[:, NT - 1:CTX - 1:-1], S0[:, NT - 1:CTX - 1:-1],
                                                               S3[:, NT - 1:CTX - 1:-1], HR[:, 0:1], ALU.mult, ALU.add),
                         reads=["S0", "S3", "HR"], writes=["HR"])
            tt(HF[:, :], HF[:, :], HR[:, :], ALU.add, ["HF", "HR"], ["HF"])
            for ti in otiles:
                t0, w = TILES[ti]
                pb = 2 + (ti % 2)
                for k in range(8):
                    mm(ps[pb][:, 0:w], wk(wsi, k, cb + 128, 128), HT[:, k, t0:t0 + w], k == 0, k == 7,
                       [("ws", wsi), ("HT", ti)], PSK(pb))
                act(GT[:, 0:w], ps[pb][:, 0:w], AF.Gelu_apprx_tanh, [], [PSK(pb), "GT"])
                tt(YB[:, c, t0:t0 + w], HF[:, t0:t0 + w], GT[:, 0:w], ALU.mult, ["HF", "GT"], [("YB", ti)])
            if stage == "B0" and debug is not None and c == 0:
                pass
        P.barrier()
        if stage == "B0":
            return YB, [("YB", t) for t in range(5)]

        off = TB
        SG, MR = [], []
        for i in range(2):
            t_, off = A(f"SG{i}", [512], F32, off)
            SG.append(t_)
            t_, off = A(f"MR{i}", [512], F32, off)
            MR.append(t_)
        cnt = 0
        for g in range(2):
            wa = load_slab(("rnno", l, g))
            wb = load_slab(("gr", l, g))
            for ti in otiles:
                t0, w = TILES[ti]
                for j in range(4):
                    dc = g * 4 + j
                    b = cnt % 2
                    cnt += 1
                    p1, p2 = 2 + b, 4 + b
                    for k in range(8):
                        mm(ps[p1][:, 0:w], wk(wa, k, j * 128, 128), YB[:, k, t0:t0 + w], k == 0, k == 7,
                           [("ws", wa), ("YB", ti)], PSK(p1))
                    for k in range(8):
                        mm(ps[p2][:, 0:w], wk(wb, k, j * 128, 128), HT[:, k, t0:t0 + w], k == 0, k == 7,
                           [("ws", wb), ("HT", ti)], PSK(p2))
                    act(SG[b][:, 0:w], ps[p2][:, 0:w], AF.Sigmoid, [], [PSK(p2), ("SG", b)])
                    tt(MR[b][:, 0:w], ps[p1][:, 0:w], SG[b][:, 0:w], ALU.mult, [("SG", b)], [PSK(p1), ("MR", b)])
                    sp_dma(mrnn_d[:, dc, t0:t0 + w], MR[b][:, 0:w], [("MR", b)], [("mrnn", dc, ti)])
        P.barrier()

        off = TB
        KT, QT, Vz, Eb = [], [], [], []
        for i in range(2):
            t_, off = A(f"KT{i}", [NT], BF16, off)
            KT.append(t_)
            t_, off = A(f"QT{i}", [NT], BF16, off)
            QT.append(t_)
            t_, off = A(f"Vz{i}", [18, 192], BF16, off)
            Vz.append(t_)
            t_, off = A(f"Eb{i}", [2, NPAT, 128], BF16, off)
            Eb.append(t_)
        SQh, RSh, RD = [], [], []
        for i in range(2):
            t_, off = A(f"SQh{i}", [512], BF16, off)
            SQh.append(t_)
            t_, off = A(f"RSh{i}", [512], F32, off)
            RSh.append(t_)
            t_, off = A(f"RD{i}", [128], F32, off)
            RD.append(t_)
        PT = []
        for hh in range(2):
            row = []
            for i in range(2):
                t_, off = A(f"PT{hh}{i}", [7, 128], BF16, off)
                row.append(t_)
            PT.append(row)
        for i in range(2):
            P.op("dve", lambda e, i=i: e.memset(Vz[i][:, :, 64:128], 0.0), writes=[("Vz", i)])
        acnt = {"n": 0}

        def attend_S(c, qtok0, chunks, out_ti):
            cb_ = c % 2
            i = acnt["n"] % 2
            acnt["n"] += 1
            clist = [(0, None), (1, None)] + [(2 + m, pc) for (m, pc) in chunks]
            nchunk = len(clist)
            for hh in range(2):
                pbase = hh * 64
                bX, bY = 2 + 2 * hh, 3 + 2 * hh
                ptk = ("PT", hh, i)
                pt = PT[hh][i]
                for ci, (jt, pc) in enumerate(clist):
                    bank = bX if ci < 4 else bY
                    col = (ci % 4) * 128
                    mm(ps[bank][:, col:col + 128], KT[cb_][pbase:pbase + 64, jt * 128:(jt + 1) * 128],
                       QT[cb_][pbase:pbase + 64, qtok0:qtok0 + 128], True, True, [("KT", cb_), ("QT", cb_)], PSK(bank))
                n1 = min(4, nchunk)
                act(pt[:, 0:n1, :], ps[bX][:, 0:n1 * 128].rearrange("p (a b) -> p a b", b=128), AF.Exp, [], [PSK(bX), ptk])
                if nchunk > 4:
                    n2 = nchunk - 4
                    act(pt[:, 4:nchunk, :], ps[bY][:, 0:n2 * 128].rearrange("p (a b) -> p a b", b=128), AF.Exp, [],
                        [PSK(bY), ptk])
                if chunks:
                    pc0, n = chunks[0][1], len(chunks)
                    tt(pt[:, 2:2 + n, :], pt[:, 2:2 + n, :], Eb[cb_][:, hh, pc0:pc0 + n, :], ALU.mult, [ptk, ("Eb", cb_)], [ptk])
            return (c, qtok0, clist, out_ti, i)

        def attend_PV(stt_):
            c, qtok0, clist, out_ti, i = stt_
            cb_ = c % 2
            nchunk = len(clist)
            total = 2 * nchunk
            idx = 0
            for hh in range(2):
                ptk = ("PT", hh, i)
                for ci, (jt, pc) in enumerate(clist):
                    lv = Vz[cb_][:, jt, 0:128] if hh == 0 else Vz[cb_][:, jt, 64:192]
                    lo = onesV[:, 0:128] if hh == 0 else onesV[:, 64:192]
                    mm(ps[6][:, 0:128], lv, PT[hh][i][:, ci, :], idx == 0, idx == total - 1, [("Vz", cb_), ptk], PSK(6))
                    mm(ps[7][:, 0:128], lo, PT[hh][i][:, ci, :], idx == 0, idx == total - 1, ["onesV", ptk], PSK(7))
                    idx += 1
            P.op("dve", lambda e: e.reciprocal(RD[i][:, :], ps[7][:, 0:128]), reads=[], writes=[PSK(7), ("RD", i)])
            tt(YB[:, c, qtok0:qtok0 + 128], ps[6][:, 0:128], RD[i][:, :], ALU.mult, [("RD", i)], [PSK(6), ("YB", out_ti)])

        pcnt = {"n": 0}

        def inproj_items(c):
            cb_ = c % 2
            items = []
            st_ = {}

            def first():
                st_["ws"] = load_slab(("kvq", l, c))
                pool_dma(Eb[cb_][:, :, :, :], biasG_d[l, c].rearrange("p (h a b) -> p h a b", h=2, b=128), [], [("Eb", cb_)])
                act(Eb[cb_][:, :, :, :], Eb[cb_][:, :, :, :], AF.Exp, [("Eb", cb_)], [("Eb", cb_)])
                for hh in range(2):
                    tt(Eb[cb_][:, hh, :, :], Eb[cb_][:, hh, :, :], maskG[:, :, :], ALU.mult, [("Eb", cb_), "maskG"], [("Eb", cb_)])
            items.append(first)
            for (colbase, dst, dkey, gain, gkey, tl) in ((0, KT[cb_], ("KT", cb_), gains[:, l, 1:2], "gains", tiles),
                                                       (256, QT[cb_], ("QT", cb_), qg[:, 0:1], "qg", otiles)):
                for ti in tl:
                    def proj(colbase=colbase, dst=dst, dkey=dkey, gain=gain, gkey=gkey, ti=ti):
                        wsi = st_["ws"]
                        t0, w = TILES[ti]
                        b = pcnt["n"] % 2
                        pcnt["n"] += 1
                        pP, pS = (0, 1) if b == 0 else (6, 7)
                        for k in range(8):
                            mm(ps[pP][:, 0:w], wk(wsi, k, colbase, 128), HT[:, k, t0:t0 + w], k == 0, k == 7,
                               [("ws", wsi), ("HT", ti)], PSK(pP))
                        act(SQh[b][:, 0:w], ps[pP][:, 0:w], AF.Square, [], [PSK(pP), ("SQh", b)])
                        mm(ps[pS][:, 0:w], blk_bf[:, :], SQh[b][:, 0:w], True, True, [("SQh", b), "blk_bf"], PSK(pS))
                        act(RSh[b][:, 0:w], ps[pS][:, 0:w], AF.Sqrt, [], [PSK(pS), ("RSh", b)], bias=EPS, scale=1.0 / 64)
                        P.op("dve", lambda e, b=b, w=w: e.reciprocal(RSh[b][:, 0:w], RSh[b][:, 0:w]), reads=[("RSh", b)],
                             writes=[("RSh", b)])
                        stt(dst[:, t0:t0 + w], ps[pP][:, 0:w], gain, RSh[b][:, 0:w], ALU.mult, ALU.mult, [("RSh", b), gkey],
                            [PSK(pP), dkey])
                    items.append(proj)
            for j0 in range(0, 18, 4):
                def vproj(j0=j0):
                    wsi = st_["ws"]
                    n = min(4, 18 - j0)
                    bank = 0 if (j0 // 4) % 2 == 0 else 1
                    for jj in range(n):
                        jt = j0 + jj
                        ti = 0 if jt < 2 else 1 + (jt - 2) // 4
                        for k in range(8):
                            mm(ps[bank][:, jj * 128:(jj + 1) * 128], HT[:, k, jt * 128:(jt + 1) * 128], wk(wsi, k, 128, 128),
                               k == 0, k == 7, [("ws", wsi), ("HT", ti)], PSK(bank))
                    pv3 = ps[bank][:, 0:n * 128].rearrange("p (a b) -> p a b", b=128)
                    act(Vz[cb_][:, j0:j0 + n, 0:64], pv3[:, :, 0:64], AF.Identity, [], [PSK(bank), ("Vz", cb_)])
                    P.op("dve", lambda e, j0=j0, n=n, pv3=pv3: e.tensor_copy(Vz[cb_][:, j0:j0 + n, 128:192], pv3[:, :, 64:128]),
                         reads=[], writes=[PSK(bank), ("Vz", cb_)])
                items.append(vproj)
            return items

        for c in range(8):
            for it in inproj_items(c):
                it()
            calls = []
            if ctx_out:
                for qt in range(2):
                    calls.append((qt * 128, [], 0))
            for qp in range(16):
                calls.append((CTX + qp * 128, NA_PLAN[qp], 1 + qp // 4))
            pend = None
            for (q0, ch, oti) in calls:
                cur = attend_S(c, q0, ch, oti)
                if pend is not None:
                    attend_PV(pend)
                pend = cur
            attend_PV(pend)
        P.barrier()
        if stage == "D0":
            return YB, [("YB", t) for t in range(5)]

        off = TB
        MTt = []
        for i in range(2):
            t_, off = A(f"MTt{i}", [8, 512], BF16, off)
            MTt.append(t_)
        XL2, MRL, T1, SG2 = [], [], [], []
        for i in range(2):
            t_, off = A(f"XL2{i}", [8, 512], F32, off)
            XL2.append(t_)
        for i in range(2):
            t_, off = A(f"MRL{i}", [512], F32, off)
            MRL.append(t_)
            t_, off = A(f"T1{i}", [512], F32, off)
            T1.append(t_)
            t_, off = A(f"SG2{i}", [512], F32, off)
            SG2.append(t_)
        cnt = 0
        for tn, ti in enumerate(otiles):
            t0, w = TILES[ti]
            mb = tn % 2
            col = 2 if ti == 0 else s
            sp_dma(XL2[mb][:, :, 0:w], xd[s, :, :, t0:t0 + w], [("xd", ti)], [("XL2", mb)])
            for g in range(2):
                wa = load_slab(("nao", l, g))
                wb = load_slab(("gn", l, g))
                for j in range(4):
                    dc = g * 4 + j
                    b = cnt % 2
                    cnt += 1
                    p1, p2 = 2 + b, 4 + b
                    for k in range(8):
                        mm(ps[p1][:, 0:w], wk(wa, k, j * 128, 128), YB[:, k, t0:t0 + w], k == 0, k == 7,
                           [("ws", wa), ("YB", ti)], PSK(p1))
                    for k in range(8):
                        mm(ps[p2][:, 0:w], wk(wb, k, j * 128, 128), HT[:, k, t0:t0 + w], k == 0, k == 7,
                           [("ws", wb), ("HT", ti)], PSK(p2))
                    sp_dma(MRL[b][:, 0:w], mrnn_d[:, dc, t0:t0 + w], [("mrnn", dc, ti)], [("MRL", b)])
                    act(SG2[b][:, 0:w], ps[p2][:, 0:w], AF.Sigmoid, [], [PSK(p2), ("SG2", b)])
                    tt(T1[b][:, 0:w], ps[p1][:, 0:w], SG2[b][:, 0:w], ALU.mult, [("SG2", b)], [PSK(p1), ("T1", b)])
                    tt(MTt[mb][:, dc, 0:w], T1[b][:, 0:w], MRL[b][:, 0:w], ALU.add, [("T1", b), ("MRL", b)], [("MTt", mb)])
            for g in range(2):
                wo = load_slab(("out", l, g))
                for j in range(4):
                    dc = g * 4 + j
                    b = cnt % 2
                    cnt += 1
                    p1 = 6 + b
                    for k in range(8):
                        mm(ps[p1][:, 0:w], wk(wo, k, j * 128, 128), MTt[mb][:, k, 0:w], k == 0, k == 7,
                           [("ws", wo), ("MTt", mb)], PSK(p1))
                    stt(XL2[mb][:, dc, 0:w], ps[p1][:, 0:w], modv(l, 2, dc, col), XL2[mb][:, dc, 0:w], ALU.mult, ALU.add,
                        ["mod", ("XL2", mb)], [PSK(p1), ("XL2", mb)])
            sp_dma(xres_d[s, :, :, t0:t0 + w], XL2[mb][:, :, 0:w], [("XL2", mb)], [("xd", ti)])
        state["x_in_res"] = True
        P.barrier()
        return None

    def ffn(l, s):
        ctx_out = (l == 0)
        moe = (l == 1)
        otiles = [0, 1, 2, 3, 4] if ctx_out else [1, 2, 3, 4]
        off = XBASE
        XTs, off = A("XTs", [8, NT], F32, off)
        Gt, off = A("Gt", [16, 8], F32, off)
        OV = off
        X = {}
        X["SQ"], off = A("SQ2", [8, 512], BF16, off)
        X["RS"], off = A("RS2", [512], F32, off)
        if moe:
            X["TMP8"], off = A("TMP8", [8, 512], F32, off)
            LG, off = A("LG", [512], F32, off)
            LT, off = A("LT", [16, 8], F32, off)
            EQ1, off = A("EQ1", [16, 8], F32, off)
            L2, off = A("L2", [16, 8], F32, off)
            EQ2, off = A("EQ2", [16, 8], F32, off)
            TG, off = A("TG", [16, 8], F32, off)
            M1, off = A("M1", [16, 1], F32, off)
            M2, off = A("M2", [16, 1], F32, off)
            W1, off = A("W1", [16, 1], F32, off)
            W2, off = A("W2", [16, 1], F32, off)
        else:
            X["TMP"] = []
            for i in range(2):
                t_, off = A(f"TMPf{i}", [512], F32, off)
                X["TMP"].append(t_)
        for ti in otiles:
            t0, w = TILES[ti]
            sp_dma(XTs[:, :, t0:t0 + w], xres_d[s, :, :, t0:t0 + w], [("xd", ti)], [("XTs", ti)])

        def xsrc(ti):
            t0, w = TILES[ti]
            return (lambda c: XTs[:, c, t0:t0 + w]), [("XTs", ti)]

        hook = None
        if moe:
            for c in range(8):
                mm(ps[2][0:8, 0:2], router[:, c, :], mod[:, l, 24 + c, s:s + 2], c == 0, c == 7, ["router", "mod"], PSK(2))
            act(routb[0:8, 0:2], ps[2][0:8, 0:2], AF.Identity, [], [PSK(2), "routb"])

            def hook(ti):
                t0, w = TILES[ti]
                for c in range(8):
                    mm(ps[2][0:8, 0:w], router[:, c, :], X["TMP8"][:, c, 0:w], c == 0, c == 7, ["router", ("TMP8", c)], PSK(2))
                act(LG[0:8, 0:w], ps[2][0:8, 0:w], AF.Identity, ["routb"], [PSK(2), "LG"], bias=routb[0:8, 0:1])
                for j in range(w // 128):
                    jt = (t0 - CTX) // 128 + j
                    P.op("pe", lambda e, jt=jt, j=j: e.transpose(ps[3][:, jt * 8:(jt + 1) * 8], LG[0:8, j * 128:(j + 1) * 128],
                                                                 ident_f[0:8, 0:8]),
                         reads=["LG", "ident_f"], writes=[PSK(3)])

        modulate(l, s, 1, otiles, xsrc, X, moe=moe, after_tile=hook)
        if moe:
            P.op("dve", lambda e: e.tensor_copy(LT[:, :, :], ps[3][:, 0:128].rearrange("p (a b) -> p a b", b=8)),
                 reads=[], writes=[PSK(3), "LT"])
            P.op("dve", lambda e: e.tensor_reduce(M1[:, :, 0], LT[:, :, :], AX.X, ALU.max), reads=["LT"], writes=["M1"])
            tt(EQ1[:, :, :], LT[:, :, :], M1[:, :, 0:1].to_broadcast([128, 16, 8]), ALU.is_equal, ["LT", "M1"], ["EQ1"])
            stt(L2[:, :, :], EQ1[:, :, :], -1.0e30, LT[:, :, :], ALU.mult, ALU.add, ["EQ1", "LT"], ["L2"])
            P.op("dve", lambda e: e.tensor_reduce(M2[:, :, 0], L2[:, :, :], AX.X, ALU.max), reads=["L2"], writes=["M2"])
            tt(EQ2[:, :, :], L2[:, :, :], M2[:, :, 0:1].to_broadcast([128, 16, 8]), ALU.is_equal, ["L2", "M2"], ["EQ2"])
            tt(W2[:, :, :], M2[:, :, :], M1[:, :, :], ALU.subtract, ["M1", "M2"], ["W2"])
            act(W2[:, :, :], W2[:, :, :], AF.Exp, ["W2"], ["W2"])
            ts(W1[:, :, :], W2[:, :, :], 1.0, None, ALU.add, None, ["W2"], ["W1"])
            P.op("dve", lambda e: e.reciprocal(W1[:, :, :], W1[:, :, :]), reads=["W1"], writes=["W1"])
            tt(W2[:, :, :], W2[:, :, :], W1[:, :, :], ALU.mult, ["W1", "W2"], ["W2"])
            tt(Gt[:, :, :], EQ1[:, :, :], W1[:, :, 0:1].to_broadcast([128, 16, 8]), ALU.mult, ["EQ1", "W1"], ["Gt"])
            tt(TG[:, :, :], EQ2[:, :, :], W2[:, :, 0:1].to_broadcast([128, 16, 8]), ALU.mult, ["EQ2", "W2"], ["TG"])
            tt(Gt[:, :, :], Gt[:, :, :], TG[:, :, :], ALU.add, ["Gt", "TG"], ["Gt"])
        P.barrier()
        if stage == "G0":
            return HT, [("HT", t) for t in range(5)]

        off = OV
        SIL, TT, ACTT, GE, DG = [], [], [], [], []
        for i in range(2):
            t_, off = A(f"SIL{i}", [512], BF16, off)
            SIL.append(t_)
            t_, off = A(f"TT{i}", [512], F32, off)
            TT.append(t_)
            t_, off = A(f"ACTT{i}", [4, 512], BF16, off)
            ACTT.append(t_)
            if moe:
                t_, off = A(f"GE{i}", [SEQ], F32, off)
                GE.append(t_)
                t_, off = A(f"DG{i}", [128], F32, off)
                DG.append(t_)
        cnt = {"h": 0, "a": 0, "o": 0, "d": 0}

        def swiglu_group(names, nj, tl, ge):
            w1 = load_slab(names[0])
            w3 = load_slab(names[1])
            w2 = load_slab(names[2])
            for ti in tl:
                t0, w = TILES[ti]
                col = 2 if ti == 0 else s
                ab = cnt["a"] % 2
                cnt["a"] += 1
                for j in range(nj):
                    b = cnt["h"] % 2
                    cnt["h"] += 1
                    b1, b3 = 2 + b, 4 + b
                    for k in range(8):
                        mm(ps[b1][:, 0:w], wk(w1, k, j * 128, 128), HT[:, k, t0:t0 + w], k == 0, k == 7,
                           [("ws", w1), ("HT", ti)], PSK(b1))
                    for k in range(8):
                        mm(ps[b3][:, 0:w], wk(w3, k, j * 128, 128), HT[:, k, t0:t0 + w], k == 0, k == 7,
                           [("ws", w3), ("HT", ti)], PSK(b3))
                    act(SIL[b][:, 0:w], ps[b1][:, 0:w], AF.Silu, [], [PSK(b1), ("SIL", b)])
                    if ge is None:
                        tt(ACTT[ab][:, j, 0:w], ps[b3][:, 0:w], SIL[b][:, 0:w], ALU.mult, [("SIL", b)], [PSK(b3), ("ACTT", ab)])
                    else:
                        tt(TT[b][:, 0:w], ps[b3][:, 0:w], SIL[b][:, 0:w], ALU.mult, [("SIL", b)], [PSK(b3), ("TT", b)])
                        tt(ACTT[ab][:, j, 0:w], TT[b][:, 0:w], GE[ge][:, t0 - CTX:t0 - CTX + w], ALU.mult,
                           [("TT", b), ("GE", ge)], [("ACTT", ab)])
                for dc in range(8):
                    ob = 6 + cnt["o"] % 2
                    cnt["o"] += 1
                    for j in range(nj):
                        mm(ps[ob][:, 0:w], wk2(w2, j, dc * 128, 128), ACTT[ab][:, j, 0:w], j == 0, j == nj - 1,
                           [("ws", w2), ("ACTT", ab)], PSK(ob))
                    stt(XTs[:, dc, t0:t0 + w], ps[ob][:, 0:w], modv(l, 5, dc, col), XTs[:, dc, t0:t0 + w], ALU.mult, ALU.add,
                        ["mod", ("XTs", ti)], [PSK(ob), ("XTs", ti)])

        if not moe:
            for g in range(6):
                swiglu_group([("f1", g), ("f3", g), ("f2", g)], 4 if g < 5 else 2, otiles, None)
        else:
            for ex in range(NEXP):
                ge = ex % 2
                for j0 in range(0, 16, 4):
                    bank = 0 if (j0 // 4) % 2 == 0 else 1
                    for jj in range(4):
                        jt = j0 + jj
                        d = cnt["d"] % 2
                        cnt["d"] += 1
                        ts(DG[d][:, :], ident_f[:, :], Gt[:, jt, ex:ex + 1], None, ALU.mult, None, ["ident_f", "Gt"], [("DG", d)])
                        mm(ps[bank][:, jj * 128:(jj + 1) * 128], ones_f[:, :], DG[d][:, :], True, True, ["ones_f", ("DG", d)],
                           PSK(bank))
                    act(GE[ge][:, j0 * 128:(j0 + 4) * 128], ps[bank][:, 0:512], AF.Identity, [], [PSK(bank), ("GE", ge)])
                for g in range(7):
                    swiglu_group([("m1", ex, g), ("m3", ex, g), ("m2", ex, g)], 4, [1, 2, 3, 4], ge)
        for ti in otiles:
            t0, w = TILES[ti]
            if l == 0:
                sp_dma(xres_d[s, :, :, t0:t0 + w], XTs[:, :, t0:t0 + w], [("XTs", ti)], [("xd", ti)])
            else:
                sp_dma(outT_d[s, :, :, t0 - CTX:t0 - CTX + w], XTs[:, :, t0:t0 + w], [("XTs", ti)], [("out", s, ti)])
        P.barrier()
        return None

    def ffn_moe_sparse(l, s):
        I32 = mybir.dt.int32
        TSZ = MOE_TSZ
        NQ = TSZ // 128
        NBLK = SEQ // TSZ
        lat = [1, 2, 3, 4]
        off = XBASE
        Gsm = {}
        for nm, shp, dt in (("W1", [16, 1], F32), ("W2", [16, 1], F32), ("SI", [16, 2], I32), ("JI", [8], I32)):
            Gsm[nm], off = A("m_" + nm, shp, dt, off)
        OV = off
        for nm, shp, dt in (("LT", [16, 8], F32), ("EQ1", [16, 8], F32), ("L2", [16, 8], F32), ("EQ2", [16, 8], F32),
                            ("M1", [16, 1], F32), ("M2", [16, 1], F32),
                            ("AB", [16, 8], BF16), ("PW", [16, 8], F32), ("TOT", [16, 8], F32), ("CS", [16, 8], F32),
                            ("EOFF", [16, 8], F32), ("TQ", [16, 8], F32), ("S12", [16, 2], F32),
                            ("NE", [8, 1], F32), ("THR", [8, 8], F32), ("CMP", [8, 8], F32), ("JF", [8], F32)):
            Gsm[nm], off = A("m_" + nm, shp, dt, off)
        LT, EQ1, L2, EQ2, M1, M2, W1, W2 = (Gsm[k] for k in ("LT", "EQ1", "L2", "EQ2", "M1", "M2", "W1", "W2"))
        AB, PW, TOT, CS, EOFF, TQ, S12, SI = (Gsm[k] for k in ("AB", "PW", "TOT", "CS", "EOFF", "TQ", "S12", "SI"))
        NE, THR, CMP, JF, JI = (Gsm[k] for k in ("NE", "THR", "CMP", "JF", "JI"))
        LG, off = A("mLG", [512], F32, off)
        XL = []
        for i in range(2):
            t_, off = A(f"mXL{i}", [8, 512], F32, off)
            XL.append(t_)
        X = {}
        X["SQ"], off = A("mSQ", [8, 512], BF16, off)
        X["RS"], off = A("mRS", [512], F32, off)
        X["TMP8"], off = A("mTMP8", [8, 512], F32, off)
        HTok = []
        for i in range(2):
            t_, off = A(f"HTok{i}", [D], BF16, off)
            HTok.append(t_)

        def xsrc(ti):
            t0, w = TILES[ti]
            b = ti % 2
            sp_dma(XL[b][:, :, 0:w], xres_d[s, :, :, t0:t0 + w], [("xd", ti)], [("XL", b)])
            return (lambda c: XL[b][:, c, 0:w]), [("XL", b)]

        for c in range(8):
            mm(ps[2][0:8, 0:2], router[:, c, :], mod[:, l, 24 + c, s:s + 2], c == 0, c == 7, ["router", "mod"], PSK(2))
        act(routb[0:8, 0:2], ps[2][0:8, 0:2], AF.Identity, [], [PSK(2), "routb"])

        def hook(ti):
            t0, w = TILES[ti]
            for c in range(8):
                mm(ps[2][0:8, 0:w], router[:, c, :], X["TMP8"][:, c, 0:w], c == 0, c == 7, ["router", ("TMP8", c)], PSK(2))
            act(LG[0:8, 0:w], ps[2][0:8, 0:w], AF.Identity, ["routb"], [PSK(2), "LG"], bias=routb[0:8, 0:1])
            for j in range(w // 128):
                jt = (t0 - CTX) // 128 + j
                P.op("pe", lambda e, jt=jt, j=j: e.transpose(ps[3][:, jt * 8:(jt + 1) * 8], LG[0:8, j * 128:(j + 1) * 128],
                                                             ident_f[0:8, 0:8]),
                     reads=["LG", "ident_f"], writes=[PSK(3)])

        modulate(l, s, 1, lat, xsrc, X, moe=True, after_tile=hook)
        P.op("dve", lambda e: e.tensor_copy(LT[:, :, :], ps[3][:, 0:128].rearrange("p (a b) -> p a b", b=8)),
             reads=[], writes=[PSK(3), "LT"])
        P.op("dve", lambda e: e.tensor_reduce(M1[:, :, 0], LT[:, :, :], AX.X, ALU.max), reads=["LT"], writes=["M1"])
        tt(EQ1[:, :, :], LT[:, :, :], M1[:, :, 0:1].to_broadcast([128, 16, 8]), ALU.is_equal, ["LT", "M1"], ["EQ1"])
        stt(L2[:, :, :], EQ1[:, :, :], -1.0e30, LT[:, :, :], ALU.mult, ALU.add, ["EQ1", "LT"], ["L2"])
        P.op("dve", lambda e: e.tensor_reduce(M2[:, :, 0], L2[:, :, :], AX.X, ALU.max), reads=["L2"], writes=["M2"])
        tt(EQ2[:, :, :], L2[:, :, :], M2[:, :, 0:1].to_broadcast([128, 16, 8]), ALU.is_equal, ["L2", "M2"], ["EQ2"])
        tt(W2[:, :, :], M2[:, :, :], M1[:, :, :], ALU.subtract, ["M1", "M2"], ["W2"])
        act(W2[:, :, :], W2[:, :, :], AF.Exp, ["W2"], ["W2"])
        ts(W1[:, :, :], W2[:, :, :], 1.0, None, ALU.add, None, ["W2"], ["W1"])
        P.op("dve", lambda e: e.reciprocal(W1[:, :, :], W1[:, :, :]), reads=["W1"], writes=["W1"])
        tt(W2[:, :, :], W2[:, :, :], W1[:, :, :], ALU.mult, ["W1", "W2"], ["W2"])
        tt(TQ[:, :, :], EQ1[:, :, :], EQ2[:, :, :], ALU.add, ["EQ1", "EQ2"], ["TQ"])
        P.op("dve", lambda e: e.tensor_copy(AB[:, :, :], TQ[:, :, :]), reads=["TQ"], writes=["AB"])
        ABf = AB[:, :, :].rearrange("p a b -> p (a b)")
        mm(ps[0][:, 0:128], ltri_bf[:, :], ABf, True, True, ["AB", "ltri_bf"], PSK(0))
        mm(ps[0][:, 128:256], ones_bf[:, :], ABf, True, True, ["AB", "ones_bf"], PSK(0))
        P.op("dve", lambda e: e.tensor_copy(PW[:, :, :], ps[0][:, 0:128].rearrange("p (a b) -> p a b", b=8)),
             reads=[], writes=[PSK(0), "PW"])
        P.op("dve", lambda e: e.tensor_copy(TOT[:, :, :], ps[0][:, 128:256].rearrange("p (a b) -> p a b", b=8)),
             reads=[], writes=[PSK(0), "TOT"])
        P.op("dve", lambda e: e.memset(CS[:, 0, :], 0.0), writes=["CS"])
        for j in range(1, 16):
            tt(CS[:, j, :], CS[:, j - 1, :], TOT[:, j - 1, :], ALU.add, ["CS", "TOT"], ["CS"])
        tt(NE[:, :, 0], CS[:, 15, :], TOT[:, 15, :], ALU.add, ["CS", "TOT"], ["NE"])
        for ex in range(NEXP):
            P.op("dve", lambda e, ex=ex: e.memset(EOFF[:, :, ex], float(ex * SEQ)), writes=["EOFF"])
            P.op("dve", lambda e, ex=ex: e.memset(THR[:, :, ex], float(ex * TSZ)), writes=["THR"])
        tt(PW[:, :, :], PW[:, :, :], CS[:, :, :], ALU.add, ["PW", "CS"], ["PW"])
        tt(PW[:, :, :], PW[:, :, :], EOFF[:, :, :], ALU.add, ["PW", "EOFF"], ["PW"])
        tt(TQ[:, :, :], EQ1[:, :, :], PW[:, :, :], ALU.mult, ["EQ1", "PW"], ["TQ"])
        P.op("dve", lambda e: e.tensor_reduce(S12[:, :, 0], TQ[:, :, :], AX.X, ALU.add), reads=["TQ"], writes=["S12"])
        tt(TQ[:, :, :], EQ2[:, :, :], PW[:, :, :], ALU.mult, ["EQ2", "PW", "S12"], ["TQ"])
        P.op("dve", lambda e: e.tensor_reduce(S12[:, :, 1], TQ[:, :, :], AX.X, ALU.add), reads=["TQ"], writes=["S12"])
        P.op("dve", lambda e: e.tensor_copy(SI[:, :, :], S12[:, :, :]), reads=["S12"], writes=["SI"])
        tt(CMP[:, :, :], NE[:, :, 0:1].to_broadcast([128, 8, 8]), THR[:, :, :], ALU.is_gt, ["NE", "THR"], ["CMP"])
        P.op("dve", lambda e: e.tensor_reduce(JF[:, :], CMP[:, :, :], AX.X, ALU.add), reads=["CMP"], writes=["JF"])
        if JCLAMP is not None:
            ts(JF[:, :], JF[:, :], float(JCLAMP), None, ALU.min, None, ["JF"], ["JF"])
        P.op("dve", lambda e: e.tensor_copy(JI[:, :], JF[:, :]), reads=["JF"], writes=["JI"])
        if stage == "G1a":
            DB, _ = A("DBG1", [64], F32, off)
            P.op("dve", lambda e: e.memset(DB[:, :], 0.0), writes=["DB"])
            P.op("dve", lambda e: e.tensor_copy(DB[:, 0:8], NE[:, :, 0]), reads=["NE"], writes=["DB"])
            P.op("dve", lambda e: e.tensor_copy(DB[:, 8:16], JF[:, :]), reads=["JF"], writes=["DB"])
            P.op("dve", lambda e: e.tensor_copy(DB[:, 16:48], S12[:, :, :].rearrange("p a b -> p (a b)")), reads=["S12"], writes=["DB"])
            P.op("dve", lambda e: e.tensor_copy(DB[:, 48:64], M1[:, :, 0]), reads=["M1"], writes=["DB"])
            sp_dma(dbg_d, DB[:, :], ["DB"], ["dbg"])
            return "done"
        psb = [ps[i][:, :].bitcast(BF16) for i in range(8)]
        if ZERO_HG:
            P.op("dve", lambda e: e.memset(HTok[0][:, :], 0.0), writes=[("HTok", 0)])
            for blk in range(NEXP * SEQ // 128):
                sp_dma(hg_d[blk * 128:(blk + 1) * 128, :], HTok[0][:, :], [("HTok", 0)], [("hgz", blk)])
        for jt in range(16):
            b = jt % 2
            ti = 1 + jt // 4
            for c in range(8):
                P.op("pe", lambda e, b=b, c=c, jt=jt: e.transpose(psb[b][:, c * 128:(c + 1) * 128],
                                                                  HT[:, c, CTX + jt * 128:CTX + (jt + 1) * 128], ident_bf[:, :]),
                     reads=[("HT", ti), "ident_bf"], writes=[PSK(b)])
            act(HTok[b][:, :], psb[b][:, :], AF.Identity, [], [PSK(b), ("HTok", b)])
            for k in range(2):
                P.dma("pool", lambda e, b=b, jt=jt, k=k: e.indirect_dma_start(
                    out=hg_d, out_offset=bass.IndirectOffsetOnAxis(ap=SI[:, jt, k:k + 1], axis=0), in_=HTok[b][:, :],
                    in_offset=None), reads=[("HTok", b), "SI"] + ([("hgz", q_) for q_ in range(128)] if ZERO_HG else []), writes=[("hg", jt, k)])
        P.barrier()
        if stage == "G1":
            return None

        off = OV
        Yacc, off = A("Yacc", [16, D], F32, off)
        HTg, off = A("HTg", [8, SEQ], BF16, off)
        HGs, SIL, ACTT = [], [], []
        t_, off = A("HGs0", [D], BF16, off)
        HGs = [t_, t_]
        for i in range(2):
            t_, off = A(f"mSIL{i}", [TSZ], BF16, off)
            SIL.append(t_)
            t_, off = A(f"mACTT{i}", [4, TSZ], BF16, off)
            ACTT.append(t_)
        cnt = {"h": 0, "a": 0, "o": 0, "g": 0}
        for ex in range(NEXP):
            P.load_reg(JI[0:1, ex:ex + 1], "JI")
            for k in range(16):
                b = cnt["g"] % 2
                cnt["g"] += 1
                r0 = ex * SEQ + k * 128
                sp_dma(HGs[b][:, :], hg_d[r0:r0 + 128, :], [("hg", a_, b_) for a_ in range(16) for b_ in range(2)], [("HGs", 0)])
                P.cond_begin(k // NQ + 1)
                for c in range(8):
                    P.op("pe", lambda e, b=b, c=c: e.transpose(psb[b][:, c * 128:(c + 1) * 128], HGs[b][:, c * 128:(c + 1) * 128],
                                                               ident_bf[:, :]),
                         reads=[("HGs", 0), "ident_bf"], writes=[PSK(b)])
                act(HTg[:, :, k * 128:(k + 1) * 128], psb[b][:, :].rearrange("p (a b) -> p a b", b=128), AF.Identity, [],
                    [PSK(b), ("HTg", k // NQ)])
                P.cond_end()
            for g in range(7):
                w1 = load_slab(("m1", ex, g), big=True)
                w3 = load_slab(("m3", ex, g), big=True)
                w2 = load_slab(("m2", ex, g), big=True)
                for j in range(NBLK):
                    P.cond_begin(j + 1)
                    ab = cnt["a"] % 2
                    cnt["a"] += 1
                    s0 = j * TSZ
                    for jj in range(4):
                        b = cnt["h"] % 2
                        cnt["h"] += 1
                        b1, b3 = 2 + b, 4 + b
                        for k in range(8):
                            mm(ps[b1][:, 0:TSZ], wk(w1, k, jj * 128, 128), HTg[:, k, s0:s0 + TSZ], k == 0, k == 7,
                               [("ws", w1), ("HTg", j)], PSK(b1))
                        for k in range(8):
                            mm(ps[b3][:, 0:TSZ], wk(w3, k, jj * 128, 128), HTg[:, k, s0:s0 + TSZ], k == 0, k == 7,
                               [("ws", w3), ("HTg", j)], PSK(b3))
                        act(SIL[b][:, :], ps[b1][:, 0:TSZ], AF.Silu, [], [PSK(b1), ("SIL", b)])
                        tt(ACTT[ab][:, jj, :], ps[b3][:, 0:TSZ], SIL[b][:, :], ALU.mult, [("SIL", b)], [PSK(b3), ("ACTT", ab)])
                    for h2 in range(NQ):
                        kt = NQ * j + h2
                        for dh in range(2):
                            ob = 6 + cnt["o"] % 2
                            cnt["o"] += 1
                            for jj in range(4):
                                mm(ps[ob][:, 0:512], ACTT[ab][:, jj, h2 * 128:(h2 + 1) * 128], wk2(w2, jj, dh * 512, 512),
                                   jj == 0, jj == 3, [("ws", w2), ("ACTT", ab)], PSK(ob))
                            ya = Yacc[:, kt, dh * 512:(dh + 1) * 512]
                            if g == 0:
                                P.op("dve", lambda e, ya=ya, ob=ob: e.tensor_copy(ya, ps[ob][:, 0:512]), reads=[],
                                     writes=[PSK(ob), ("Yacc", kt)])
                            else:
                                tt(ya, ps[ob][:, 0:512], ya, ALU.add, [("Yacc", kt)], [PSK(ob), ("Yacc", kt)])
                    P.cond_end()
            for k in range(16):
                r0 = ex * SEQ + k * 128
                sp_dma(yb_d[r0:r0 + 128, :], Yacc[:, k, :], [("Yacc", k)], [("yb", ex, k)])
        P.barrier()

        off = OV
        YA, YB2, OO, XC8 = [], [], [], []
        for i in range(2):
            t_, off = A(f"YA{i}", [D], F32, off)
            YA.append(t_)
            t_, off = A(f"YB2{i}", [D], F32, off)
            YB2.append(t_)
            t_, off = A(f"OO{i}", [D], F32, off)
            OO.append(t_)
            t_, off = A(f"XC8{i}", [8, 128], F32, off)
            XC8.append(t_)
        for jt in range(16):
            b = jt % 2
            ti = 1 + jt // 4
            c0 = CTX + jt * 128
            for k, dst, dk in ((0, YA, "YA"), (1, YB2, "YB2")):
                P.dma("pool", lambda e, b=b, jt=jt, k=k, dst=dst: e.indirect_dma_start(
                    out=dst[b][:, :], out_offset=None, in_=yb_d,
                    in_offset=bass.IndirectOffsetOnAxis(ap=SI[:, jt, k:k + 1], axis=0)),
                    reads=[("yb", a_, b_) for a_ in range(NEXP) for b_ in range(16)] + ["SI"], writes=[(dk, b)])
            sp_dma(XC8[b][:, :, :], xres_d[s, :, :, c0:c0 + 128], [("xd", ti)], [("XC8", b)])
            ts(OO[b][:, :], YA[b][:, :], W1[:, jt, 0:1], None, ALU.mult, None, [("YA", b), "W1"], [("OO", b)])
            stt(OO[b][:, :], YB2[b][:, :], W2[:, jt, 0:1], OO[b][:, :], ALU.mult, ALU.add, [("YB2", b), "W2", ("OO", b)], [("OO", b)])
            for c in range(8):
                pbk = 2 + 2 * b + c // 4
                P.op("pe", lambda e, b=b, c=c, pbk=pbk: e.transpose(ps[pbk][:, (c % 4) * 128:(c % 4 + 1) * 128],
                                                                    OO[b][:, c * 128:(c + 1) * 128], ident_f[:, :]),
                     reads=[("OO", b), "ident_f"], writes=[PSK(pbk)])
            for c in range(8):
                pbk = 2 + 2 * b + c // 4
                stt(XC8[b][:, c, :], ps[pbk][:, (c % 4) * 128:(c % 4 + 1) * 128], modv(l, 5, c, s), XC8[b][:, c, :],
                    ALU.mult, ALU.add, ["mod", ("XC8", b)], [PSK(pbk), ("XC8", b)])
            sp_dma(outT_d[s, :, :, jt * 128:(jt + 1) * 128], XC8[b][:, :, :], [("XC8", b)], [("out", s, jt)])
        P.barrier()
        return None

    def moe_merged(l, seqs):
        I32 = mybir.dt.int32
        TSZ = 512
        NQ = TSZ // 128
        CAP = SEQ * len(seqs)
        NPASS = len(seqs)
        off = XBASE
        PS_ = {}
        for s in seqs:
            for nm, shp, dt in (("W1", [16, 1], F32), ("W2", [16, 1], F32), ("SI", [16, 2], I32)):
                PS_[(nm, s)], off = A(f"mm_{nm}{s}", shp, dt, off)
        NEacc, off = A("mm_NEacc", [8, 1], F32, off)
        JI, off = A("mm_JI", [8], I32, off)
        OV = off
        G = {}
        for nm, shp, dt in (("LT", [16, 8], F32), ("EQ1", [16, 8], F32), ("L2", [16, 8], F32), ("EQ2", [16, 8], F32),
                            ("M1", [16, 1], F32), ("M2", [16, 1], F32),
                            ("AB", [16, 8], BF16), ("PW", [16, 8], F32), ("TOT", [16, 8], F32), ("CS", [16, 8], F32),
                            ("EOFF", [16, 8], F32), ("TQ", [16, 8], F32), ("S12", [16, 2], F32),
                            ("THR", [8, 8], F32), ("CMP", [8, 8], F32), ("JF", [8], F32)):
            G[nm], off = A("mm_" + nm, shp, dt, off)
        LT, EQ1, L2, EQ2, M1, M2 = (G[k] for k in ("LT", "EQ1", "L2", "EQ2", "M1", "M2"))
        AB, PW, TOT, CS, EOFF, TQ, S12 = (G[k] for k in ("AB", "PW", "TOT", "CS", "EOFF", "TQ", "S12"))
        THR, CMP, JF = (G[k] for k in ("THR", "CMP", "JF"))
        LG, off = A("mm_LG", [512], F32, off)
        XL = []
        for i in range(2):
            t_, off = A(f"mm_XL{i}", [8, 512], F32, off)
            XL.append(t_)
        X = {}
        X["SQ"], off = A("mm_SQ", [8, 512], BF16, off)
        X["RS"], off = A("mm_RS", [512], F32, off)
        X["TMP8"], off = A("mm_TMP8", [8, 512], F32, off)
        HTok = []
        for i in range(2):
            t_, off = A(f"mm_HTok{i}", [D], BF16, off)
            HTok.append(t_)
        psb = [ps[i][:, :].bitcast(BF16) for i in range(8)]
        lat = [1, 2, 3, 4]
        P.op("dve", lambda e: e.memset(NEacc[:, :, :], 0.0), writes=["NEacc"])
        for ex in range(NEXP):
            P.op("dve", lambda e, ex=ex: e.memset(EOFF[:, :, ex], float(ex * CAP)), writes=["EOFF"])
            P.op("dve", lambda e, ex=ex: e.memset(THR[:, :, ex], float(ex * TSZ)), writes=["THR"])

        for s in seqs:
            W1, W2, SI = PS_[("W1", s)], PS_[("W2", s)], PS_[("SI", s)]

            def xsrc(ti, s=s):
                t0, w = TILES[ti]
                b = ti % 2
                sp_dma(XL[b][:, :, 0:w], xres_d[s, :, :, t0:t0 + w], [("xd", ti)], [("XL", b)])
                return (lambda c: XL[b][:, c, 0:w]), [("XL", b)]

            for c in range(8):
                mm(ps[2][0:8, 0:2], router[:, c, :], mod[:, l, 24 + c, s:s + 2], c == 0, c == 7, ["router", "mod"], PSK(2))
            act(routb[0:8, 0:2], ps[2][0:8, 0:2], AF.Identity, [], [PSK(2), "routb"])

            def hook(ti):
                t0, w = TILES[ti]
                for c in range(8):
                    mm(ps[2][0:8, 0:w], router[:, c, :], X["TMP8"][:, c, 0:w], c == 0, c == 7, ["router", ("TMP8", c)], PSK(2))
                act(LG[0:8, 0:w], ps[2][0:8, 0:w], AF.Identity, ["routb"], [PSK(2), "LG"], bias=routb[0:8, 0:1])
                for j in range(w // 128):
                    jt = (t0 - CTX) // 128 + j
                    P.op("pe", lambda e, jt=jt, j=j: e.transpose(ps[3][:, jt * 8:(jt + 1) * 8], LG[0:8, j * 128:(j + 1) * 128],
                                                                 ident_f[0:8, 0:8]),
                         reads=["LG", "ident_f"], writes=[PSK(3)])

            modulate(l, s, 1, lat, xsrc, X, moe=True, after_tile=hook)
            P.op("dve", lambda e: e.tensor_copy(LT[:, :, :], ps[3][:, 0:128].rearrange("p (a b) -> p a b", b=8)),
                 reads=[], writes=[PSK(3), "LT"])
            P.op("dve", lambda e: e.tensor_reduce(M1[:, :, 0], LT[:, :, :], AX.X, ALU.max), reads=["LT"], writes=["M1"])
            tt(EQ1[:, :, :], LT[:, :, :], M1[:, :, 0:1].to_broadcast([128, 16, 8]), ALU.is_equal, ["LT", "M1"], ["EQ1"])
            stt(L2[:, :, :], EQ1[:, :, :], -1.0e30, LT[:, :, :], ALU.mult, ALU.add, ["EQ1", "LT"], ["L2"])
            P.op("dve", lambda e: e.tensor_reduce(M2[:, :, 0], L2[:, :, :], AX.X, ALU.max), reads=["L2"], writes=["M2"])
            tt(EQ2[:, :, :], L2[:, :, :], M2[:, :, 0:1].to_broadcast([128, 16, 8]), ALU.is_equal, ["L2", "M2"], ["EQ2"])
            tt(W2[:, :, :], M2[:, :, :], M1[:, :, :], ALU.subtract, ["M1", "M2"], [("W2", s)])
            act(W2[:, :, :], W2[:, :, :], AF.Exp, [("W2", s)], [("W2", s)])
            ts(W1[:, :, :], W2[:, :, :], 1.0, None, ALU.add, None, [("W2", s)], [("W1", s)])
            P.op("dve", lambda e, W1=W1: e.reciprocal(W1[:, :, :], W1[:, :, :]), reads=[("W1", s)], writes=[("W1", s)])
            tt(W2[:, :, :], W2[:, :, :], W1[:, :, :], ALU.mult, [("W1", s), ("W2", s)], [("W2", s)])
            tt(TQ[:, :, :], EQ1[:, :, :], EQ2[:, :, :], ALU.add, ["EQ1", "EQ2"], ["TQ"])
            P.op("dve", lambda e: e.tensor_copy(AB[:, :, :], TQ[:, :, :]), reads=["TQ"], writes=["AB"])
            ABf = AB[:, :, :].rearrange("p a b -> p (a b)")
            mm(ps[0][:, 0:128], ltri_bf[:, :], ABf, True, True, ["AB", "ltri_bf"], PSK(0))
            mm(ps[0][:, 128:256], ones_bf[:, :], ABf, True, True, ["AB", "ones_bf"], PSK(0))
            P.op("dve", lambda e: e.tensor_copy(PW[:, :, :], ps[0][:, 0:128].rearrange("p (a b) -> p a b", b=8)),
                 reads=[], writes=[PSK(0), "PW"])
            P.op("dve", lambda e: e.tensor_copy(TOT[:, :, :], ps[0][:, 128:256].rearrange("p (a b) -> p a b", b=8)),
                 reads=[], writes=[PSK(0), "TOT"])
            P.op("dve", lambda e: e.tensor_copy(CS[:, 0, :], NEacc[:, :, 0]), reads=["NEacc"], writes=["CS"])
            for j in range(1, 16):
                tt(CS[:, j, :], CS[:, j - 1, :], TOT[:, j - 1, :], ALU.add, ["CS", "TOT"], ["CS"])
            tt(NEacc[:, :, 0], CS[:, 15, :], TOT[:, 15, :], ALU.add, ["CS", "TOT"], ["NEacc"])
            tt(PW[:, :, :], PW[:, :, :], CS[:, :, :], ALU.add, ["PW", "CS"], ["PW"])
            tt(PW[:, :, :], PW[:, :, :], EOFF[:, :, :], ALU.add, ["PW", "EOFF"], ["PW"])
            tt(TQ[:, :, :], EQ1[:, :, :], PW[:, :, :], ALU.mult, ["EQ1", "PW"], ["TQ"])
            P.op("dve", lambda e: e.tensor_reduce(S12[:, :, 0], TQ[:, :, :], AX.X, ALU.add), reads=["TQ"], writes=["S12"])
            tt(TQ[:, :, :], EQ2[:, :, :], PW[:, :, :], ALU.mult, ["EQ2", "PW", "S12"], ["TQ"])
            P.op("dve", lambda e: e.tensor_reduce(S12[:, :, 1], TQ[:, :, :], AX.X, ALU.add), reads=["TQ"], writes=["S12"])
            P.op("dve", lambda e, SI=SI: e.tensor_copy(SI[:, :, :], S12[:, :, :]), reads=["S12"], writes=[("SI", s)])
            for jt in range(16):
                b = jt % 2
                ti = 1 + jt // 4
                for c in range(8):
                    P.op("pe", lambda e, b=b, c=c, jt=jt: e.transpose(psb[b][:, c * 128:(c + 1) * 128],
                                                                      HT[:, c, CTX + jt * 128:CTX + (jt + 1) * 128], ident_bf[:, :]),
                         reads=[("HT", ti), "ident_bf"], writes=[PSK(b)])
                act(HTok[b][:, :], psb[b][:, :], AF.Identity, [], [PSK(b), ("HTok", b)])
                for k in range(2):
                    P.dma("pool", lambda e, b=b, jt=jt, k=k, SI=SI: e.indirect_dma_start(
                        out=hg_d, out_offset=bass.IndirectOffsetOnAxis(ap=SI[:, jt, k:k + 1], axis=0), in_=HTok[b][:, :],
                        in_offset=None), reads=[("HTok", b), ("SI", s)] + [("hgz", q_) for q_ in range(NEXP * SEQ * 2 // 128)], writes=[("hg", s, jt, k)])
        tt(CMP[:, :, :], NEacc[:, :, 0:1].to_broadcast([128, 8, 8]), THR[:, :, :], ALU.is_gt, ["NEacc", "THR"], ["CMP"])
        P.op("dve", lambda e: e.tensor_reduce(JF[:, :], CMP[:, :, :], AX.X, ALU.add), reads=["CMP"], writes=["JF"])
        P.op("dve", lambda e: e.tensor_copy(JI[:, :], JF[:, :]), reads=["JF"], writes=["JI"])
        P.barrier()

        off = OV
        Yacc, off = A("mm_Yacc", [16, D], F32, off)
        HTg, off = A("mm_HTg", [8, SEQ], BF16, off)
        t_, off = A("mm_HGs", [D], BF16, off)
        HGs = t_
        SIL, ACTT = [], []
        for i in range(2):
            t_, off = A(f"mm_SIL{i}", [TSZ], BF16, off)
            SIL.append(t_)
            t_, off = A(f"mm_ACTT{i}", [4, TSZ], BF16, off)
            ACTT.append(t_)
        cnt = {"h": 0, "a": 0, "o": 0, "g": 0}
        allhg = [("hg", s_, a_, b_) for s_ in seqs for a_ in range(16) for b_ in range(2)]
        for ex in range(NEXP):
            P.load_reg(JI[0:1, ex:ex + 1], "JI", engines=("pe", "act", "dve", "sp", "pool"))
            for p_ in range(NPASS):
                jb = 4 * p_
                for k in range(16):
                    b = cnt["g"] % 2
                    cnt["g"] += 1
                    r0 = ex * CAP + p_ * SEQ + k * 128
                    thr = jb + k // NQ + 1
                    P.cond_begin(thr)
                    sp_dma(HGs[:, :], hg_d[r0:r0 + 128, :], allhg, ["HGs"])
                    for c in range(8):
                        P.op("pe", lambda e, b=b, c=c: e.transpose(psb[b][:, c * 128:(c + 1) * 128], HGs[:, c * 128:(c + 1) * 128],
                                                                   ident_bf[:, :]),
                             reads=["HGs", "ident_bf"], writes=[PSK(b)])
                    act(HTg[:, :, k * 128:(k + 1) * 128], psb[b][:, :].rearrange("p (a b) -> p a b", b=128), AF.Identity, [],
                        [PSK(b), ("HTg", k // NQ)])
                    P.cond_end()
                for g in range(7):
                    P.cond_begin(jb + 1)
                    w1 = load_slab(("m1", ex, g), big=True)
                    w3 = load_slab(("m3", ex, g), big=True)
                    w2 = load_slab(("m2", ex, g), big=True)
                    P.cond_end()
                    for j in range(4):
                        P.cond_begin(jb + j + 1)
                        ab = cnt["a"] % 2
                        cnt["a"] += 1
                        s0 = j * TSZ
                        for jj in range(4):
                            b = cnt["h"] % 2
                            cnt["h"] += 1
                            b1, b3 = 2 + b, 4 + b
                            for k in range(8):
                                mm(ps[b1][:, 0:TSZ], wk(w1, k, jj * 128, 128), HTg[:, k, s0:s0 + TSZ], k == 0, k == 7,
                                   [("ws", w1), ("HTg", j)], PSK(b1))
                            for k in range(8):
                                mm(ps[b3][:, 0:TSZ], wk(w3, k, jj * 128, 128), HTg[:, k, s0:s0 + TSZ], k == 0, k == 7,
                                   [("ws", w3), ("HTg", j)], PSK(b3))
                            act(SIL[b][:, :], ps[b1][:, 0:TSZ], AF.Silu, [], [PSK(b1), ("SIL", b)])
                            tt(ACTT[ab][:, jj, :], ps[b3][:, 0:TSZ], SIL[b][:, :], ALU.mult, [("SIL", b)], [PSK(b3), ("ACTT", ab)])
                        for h2 in range(NQ):
                            kt = NQ * j + h2
                            for dh in range(2):
                                ob = 6 + cnt["o"] % 2
                                cnt["o"] += 1
                                for jj in range(4):
                                    mm(ps[ob][:, 0:512], ACTT[ab][:, jj, h2 * 128:(h2 + 1) * 128], wk2(w2, jj, dh * 512, 512),
                                       jj == 0, jj == 3, [("ws", w2), ("ACTT", ab)], PSK(ob))
                                ya = Yacc[:, kt, dh * 512:(dh + 1) * 512]
                                if g == 0:
                                    P.op("dve", lambda e, ya=ya, ob=ob: e.tensor_copy(ya, ps[ob][:, 0:512]), reads=[],
                                         writes=[PSK(ob), ("Yacc", kt)])
                                else:
                                    tt(ya, ps[ob][:, 0:512], ya, ALU.add, [("Yacc", kt)], [PSK(ob), ("Yacc", kt)])
                        P.cond_end()
                for k in range(16):
                    r0 = ex * CAP + p_ * SEQ + k * 128
                    P.cond_begin(jb + k // NQ + 1)
                    sp_dma(yb_d[r0:r0 + 128, :], Yacc[:, k, :], [("Yacc", k)], [("yb", ex, p_, k)])
                    P.cond_end()
        P.barrier()

        off = OV
        YA, YB2, OO, XC8 = [], [], [], []
        for i in range(2):
            t_, off = A(f"mm_YA{i}", [D], F32, off)
            YA.append(t_)
            t_, off = A(f"mm_YB2{i}", [D], F32, off)
            YB2.append(t_)
            t_, off = A(f"mm_OO{i}", [D], F32, off)
            OO.append(t_)
            t_, off = A(f"mm_XC8{i}", [8, 128], F32, off)
            XC8.append(t_)
        allyb = [("yb", a_, p_, b_) for a_ in range(NEXP) for p_ in range(NPASS) for b_ in range(16)]
        n_ = 0
        for s in seqs:
            W1, W2, SI = PS_[("W1", s)], PS_[("W2", s)], PS_[("SI", s)]
            for jt in range(16):
                b = n_ % 2
                n_ += 1
                ti = 1 + jt // 4
                c0 = CTX + jt * 128
                for k, dst, dk in ((0, YA, "YA"), (1, YB2, "YB2")):
                    P.dma("pool", lambda e, b=b, jt=jt, k=k, dst=dst, SI=SI: e.indirect_dma_start(
                        out=dst[b][:, :], out_offset=None, in_=yb_d,
                        in_offset=bass.IndirectOffsetOnAxis(ap=SI[:, jt, k:k + 1], axis=0)),
                        reads=allyb + [("SI", s)], writes=[(dk, b)])
                sp_dma(XC8[b][:, :, :], xres_d[s, :, :, c0:c0 + 128], [("xd", ti)], [("XC8", b)])
                ts(OO[b][:, :], YA[b][:, :], W1[:, jt, 0:1], None, ALU.mult, None, [("YA", b), ("W1", s)], [("OO", b)])
                stt(OO[b][:, :], YB2[b][:, :], W2[:, jt, 0:1], OO[b][:, :], ALU.mult, ALU.add, [("YB2", b), ("W2", s), ("OO", b)],
                    [("OO", b)])
                for c in range(8):
                    pbk = 2 + 2 * b + c // 4
                    P.op("pe", lambda e, b=b, c=c, pbk=pbk: e.transpose(ps[pbk][:, (c % 4) * 128:(c % 4 + 1) * 128],
                                                                        OO[b][:, c * 128:(c + 1) * 128], ident_f[:, :]),
                         reads=[("OO", b), "ident_f"], writes=[PSK(pbk)])
                for c in range(8):
                    pbk = 2 + 2 * b + c // 4
                    stt(XC8[b][:, c, :], ps[pbk][:, (c % 4) * 128:(c % 4 + 1) * 128], modv(l, 5, c, s), XC8[b][:, c, :],
                        ALU.mult, ALU.add, ["mod", ("XC8", b)], [PSK(pbk), ("XC8", b)])
                sp_dma(outT_d[s, :, :, jt * 128:(jt + 1) * 128], XC8[b][:, :, :], [("XC8", b)], [("out", s, jt)])
        P.barrier()
        return None

    result = None
    if stage in ("full", "seq1"):
        seqs = (1,) if stage == "seq1" else (0, 1)
        for s in seqs:
            state["x_in_res"] = False
            for l in range(2):
                token_mixer(l, s)
                if l == 1 and SPARSE_MOE:
                    if not MERGED_MOE:
                        ffn_moe_sparse(l, s)
                else:
                    ffn(l, s)
        if SPARSE_MOE and MERGED_MOE:
            moe_merged(1, seqs)
    elif si >= 1:
        result = token_mixer(0, 0)
        if result is None and stage in ("G0", "H0", "F1", "G1", "G1a", "H1"):
            result = ffn(0, 0)
            if result is None and stage in ("F1", "G1", "G1a", "H1"):
                result = token_mixer(1, 0)
                if result is None and stage in ("G1", "G1a", "H1"):
                    result = ffn_moe_sparse(1, 0) if SPARSE_MOE else ffn(1, 0)

    if debug is not None:
        if stage == "pro":
            sp_dma(dbg_d.rearrange("p (a b) -> p a b", b=4), mod[:, :, :, :].rearrange("p l a b -> p (l a) b"), ["mod"], ["dbg"])
        elif stage in ("F0", "H0", "F1"):
            sp_dma(dbg_d, xres_d[0], [("xd", t) for t in range(5)], ["dbg"])
        elif result == "done":
            pass
        elif result is not None:
            src_t, keys = result
            DT, _ = A("DT", [NT], F32, (A.limit - NT * 4 - 64) // 32 * 32)
            for c in range(8):
                act(DT[:, :], src_t[:, c, :], AF.Identity, keys, ["DT"])
                sp_dma(dbg_d[:, c, :], DT[:, :], ["DT"], ["dbg"])
    P.emit(nc)
    return nc


def kernel(**inputs):
    inp = {k: np.asarray(v, np.float32) for k, v in inputs.items()}
    shared = build_shared(inp)
    nc = build_program("full")
    in_maps = []
    for core in range(NCORES):
        m = dict(shared)
        m.update(build_core_inputs(inp, core))
        in_maps.append(m)
    res = run_bass_kernel_spmd(nc, in_maps, core_ids=list(range(NCORES)))
    out = np.empty((2 * NCORES, SEQ, D), np.float32)
    for core in range(NCORES):
        oT = np.asarray(res.results[core]["outT"])
        out[2 * core:2 * core + 2] = oT.transpose(0, 3, 2, 1).reshape(2, SEQ, D)
    return out
```

```python
import contextlib
import numpy as np
import concourse.bass as bass
import concourse.mybir as mybir
from concourse.bass_utils import run_bass_kernel_spmd

F32 = mybir.dt.float32
BF16 = mybir.dt.bfloat16
AF = mybir.ActivationFunctionType
ALU = mybir.AluOpType
AX = mybir.AxisListType

NCORES = 8
D = 1024
NCH = 8
CTX = 256
SEQ = 2048
NT = CTX + SEQ
TILES = [(0, 256), (256, 512), (768, 512), (1280, 512), (1792, 512)]
D_FF = 2816
D_FFE = 3584
NEXP = 8
EPS = 1e-6
NSLOT = 8
NRING = 5
SLAB = 4096
WCH = 16
SPARSE_MOE = True
MOE_TSZ = 512
JCLAMP = None
MERGED_MOE = True
ZERO_HG = True
SB_BASE = 16384 + 128


class Ins:
    __slots__ = ("eng", "fn", "reads", "writes", "dma", "deps", "sig", "semkey", "val", "slot", "cond")


class Prog:
    ENGS = ["pe", "act", "dve", "pool", "sp"]

    def __init__(self):
        self.ins = []
        self.cur_cond = None
        self.ncond = 0
        self.cond_thr = {}

    def op(self, eng, fn, reads=(), writes=()):
        i = Ins()
        i.eng, i.fn, i.reads, i.writes, i.dma = eng, fn, tuple(reads), tuple(writes), False
        i.sig, i.deps, i.semkey, i.val, i.slot = False, (), None, 0, 0
        i.cond = self.cur_cond
        self.ins.append(i)
        return i

    def cond_begin(self, thr):
        self.ncond += 1
        self.cur_cond = self.ncond
        self.cond_thr[self.ncond] = thr

    def cond_end(self):
        self.cur_cond = None

    def load_reg(self, ap, key, engines=("pe", "act", "dve")):
        for e in engines:
            self.op(e, ("REGLOAD", ap), reads=[key])

    def dma(self, q, fn, reads=(), writes=()):
        i = self.op(q, fn, reads, writes)
        i.dma = True
        return i

    def barrier(self):
        for e in ("pe", "act", "dve", "sp"):
            self.op(e, lambda en: en.nop(), writes=("_bar",))

    def resolve(self):
        last_w = {}
        readers = {}
        ndma = {e: 0 for e in self.ENGS}
        slot_last = {e: {} for e in self.ENGS}
        for idx, I in enumerate(self.ins):
            reads = I.reads
            if I.eng != "pool" and "_bar" not in I.writes:
                reads = reads + ("_bar",)
            deps = {}
            for k in reads:
                j = last_w.get(k)
                if j is not None:
                    deps[j] = True
            for k in I.writes:
                j = last_w.get(k)
                if j is not None:
                    deps.setdefault(j, False)
                r = readers.get(k)
                if r:
                    for j2 in r[0].values():
                        deps.setdefault(j2, False)
                    for j2 in r[1]:
                        deps.setdefault(j2, False)
            final = []
            for j, raw in deps.items():
                J = self.ins[j]
                if J.dma:
                    final.append(j)
                elif J.eng == I.eng:
                    if I.dma or (raw and I.eng != "pe"):
                        final.append(j)
                else:
                    final.append(j)
            if I.dma:
                q = I.eng
                slot = ndma[q] % NSLOT
                prev = slot_last[q].get(slot)
                if prev is not None:
                    final.append(prev)
                slot_last[q][slot] = idx
                I.slot = slot
                ndma[q] += 1
            I.deps = final
            for j in final:
                self.ins[j].sig = True
            for k in reads:
                r = readers.setdefault(k, ({}, []))
                if I.dma:
                    r[1].append(idx)
                else:
                    r[0][I.eng] = idx
            for k in I.writes:
                last_w[k] = idx
                readers[k] = ({}, [])
        cnt = {e: 0 for e in self.ENGS}
        dcnt = {}
        for I in self.ins:
            if I.dma:
                key = ("d", I.eng, I.slot)
                dcnt[key] = dcnt.get(key, 0) + 16
                I.semkey, I.val = key, dcnt[key]
            elif I.sig:
                cnt[I.eng] += 1
                I.semkey, I.val = ("e", I.eng), cnt[I.eng]
        self.final_dma = dict(dcnt)

    def emit(self, nc, final_waits_on="sp"):
        self.resolve()
        keys = [("e", e) for e in self.ENGS]
        for e in self.ENGS:
            if any(I.dma and I.eng == e for I in self.ins):
                keys += [("d", e, s) for s in range(NSLOT)]
        with contextlib.ExitStack() as st:
            sems = {}
            for k in keys:
                sems[k] = st.enter_context(nc.semaphore("s_" + "_".join(str(x) for x in k)))
            block = st.enter_context(nc.Block())
            per = {e: [I for I in self.ins if I.eng == e] for e in self.ENGS}

            def replay(ename, eng):
                seen = {}
                reg = {}

                def do_waits(I, seen, only_external=None):
                    waits = {}
                    for j in I.deps:
                        J = self.ins[j]
                        if only_external is not None and J.cond == only_external:
                            continue
                        if waits.get(J.semkey, 0) < J.val:
                            waits[J.semkey] = J.val
                    for sk, v in waits.items():
                        if seen.get(sk, 0) < v:
                            eng.wait_ge(sems[sk], v)
                            seen[sk] = v

                def run(I):
                    if isinstance(I.fn, tuple):
                        if "r" not in reg:
                            reg["r"] = eng.alloc_register("rj_" + ename)
                        r = eng.reg_load(reg["r"], I.fn[1])
                    else:
                        r = I.fn(eng)
                    if I.dma:
                        r.then_inc(sems[I.semkey], 16)
                    elif I.sig:
                        r.then_inc(sems[I.semkey], 1)

                lst = per[ename]
                n = len(lst)
                p = 0
                while p < n:
                    I = lst[p]
                    if I.cond is None:
                        do_waits(I, seen)
                        run(I)
                        p += 1
                        continue
                    cid = I.cond
                    q = p
                    while q < n and lst[q].cond == cid:
                        q += 1
                    body = lst[p:q]
                    for B in body:
                        do_waits(B, seen, only_external=cid)
                    snap = dict(seen)
                    k = sum(1 for B in body if B.sig and not B.dma)
                    with eng.If_lt(reg["r"], self.cond_thr[cid]):
                        if k > 0:
                            eng.drain().then_inc(sems[("e", ename)], k)
                        for B in body:
                            if B.dma:
                                eng.nop().then_inc(sems[B.semkey], 16)
                        if k == 0 and not any(B.dma for B in body):
                            eng.nop()
                    with eng.Else():
                        inner = dict(snap)
                        for B in body:
                            do_waits(B, inner)
                            run(B)
                    seen = snap
                    p = q
                if ename == final_waits_on:
                    for sk, v in self.final_dma.items():
                        if seen.get(sk, 0) < v:
                            eng.wait_ge(sems[sk], v)

            @block.tensor
            def _(e):
                replay("pe", e)

            @block.scalar
            def _(e):
                replay("act", e)

            @block.vector
            def _(e):
                replay("dve", e)

            @block.gpsimd
            def _(e):
                replay("pool", e)

            @block.sync
            def _(e):
                replay("sp", e)


def na_plan():
    pats = []
    groups = {}
    plan = []
    for qp in range(16):
        r0 = 2 * qp
        s = [min(max(r - 4, 0), 24) for r in (r0, r0 + 1)]
        first = s[0] // 2
        last = (s[1] + 7) // 2
        keys = []
        for m in range(first, last + 1):
            key = []
            for kl in range(2):
                kr = 2 * m + kl
                for ql in range(2):
                    r = r0 + ql
                    valid = s[ql] <= kr < s[ql] + 8
                    key.append(kr - r + 7 if valid else None)
            keys.append(tuple(key))
        gk = tuple(keys)
        if gk not in groups:
            groups[gk] = len(pats)
            pats.extend(keys)
        base = groups[gk]
        plan.append([(first + i, base + i) for i in range(len(keys))])
    return pats, plan


NA_PATS, NA_PLAN = na_plan()
NPAT = len(NA_PATS)


def slab_order():
    pro = [("mod", l, g) for l in range(2) for g in range(12)]
    seq = []
    for l in range(2):
        ntile = 5 if l == 0 else 4
        seq += [("rnn", l, i) for i in range(4)]
        for g in range(2):
            seq += [("rnno", l, g), ("gr", l, g)]
        seq += [("kvq", l, c) for c in range(8)]
        for t in range(ntile):
            for g in range(2):
                seq += [("nao", l, g), ("gn", l, g)]
            for g in range(2):
                seq += [("out", l, g)]
        if l == 0:
            for g in range(6):
                seq += [("f1", g), ("f3", g), ("f2", g)]
        else:
            for e in range(NEXP):
                for g in range(7):
                    seq += [("m1", e, g), ("m3", e, g), ("m2", e, g)]
    return pro, seq


def unique_slabs():
    pro, seq = slab_order()
    names = []
    seen = set()
    for n in pro + seq:
        if n not in seen:
            seen.add(n)
            names.append(n)
    return names


SLAB_NAMES = unique_slabs()
SLAB_IDX = {n: i for i, n in enumerate(SLAB_NAMES)}


def _slab_cols(W, cols):
    S = W[:, cols]
    return np.ascontiguousarray(S.reshape(8, 128, 512).transpose(1, 0, 2)).reshape(128, SLAB)


def _slab_rows(W2, r0):
    blk = np.zeros((512, 1024), np.float32)
    n = max(0, min(512, W2.shape[0] - r0))
    blk[:n] = W2[r0:r0 + n]
    return np.ascontiguousarray(blk.reshape(4, 128, 1024).transpose(1, 0, 2)).reshape(128, SLAB)


def _cols_pad(W, c0, n):
    idx = np.full(512, c0, np.int64)
    idx[:n] = np.arange(c0, c0 + n)
    return idx


def build_wstream(inp):
    ar = np.arange
    out = np.empty((len(SLAB_NAMES), 128, SLAB), np.float32)
    for i, nm in enumerate(SLAB_NAMES):
        k = nm[0]
        if k == "mod":
            _, l, g = nm
            out[i] = _slab_cols(inp["w_mod"][l], ar(g * 512, g * 512 + 512))
        elif k == "rnn":
            _, l, j = nm
            cols = np.concatenate([ar(c * 128, c * 128 + 128) if which == 0 else ar(3072 + c * 128, 3072 + c * 128 + 128)
                                   for c in (2 * j, 2 * j + 1) for which in (0, 1)])
            out[i] = _slab_cols(inp["w_in"][l], cols)
        elif k == "rnno":
            _, l, g = nm
            out[i] = _slab_cols(inp["w_rnn_o"][l], ar(g * 512, g * 512 + 512))
        elif k == "gr":
            _, l, g = nm
            out[i] = _slab_cols(inp["w_in"][l], ar(5120 + g * 512, 5120 + g * 512 + 512))
        elif k == "kvq":
            _, l, c = nm
            cols = np.concatenate([ar(1024 + c * 128, 1024 + c * 128 + 128), ar(2048 + c * 128, 2048 + c * 128 + 128),
                                   ar(4096 + c * 128, 4096 + c * 128 + 128), ar(4096 + c * 128, 4096 + c * 128 + 128)])
            out[i] = _slab_cols(inp["w_in"][l], cols)
        elif k == "nao":
            _, l, g = nm
            out[i] = _slab_cols(inp["w_na_o"][l], ar(g * 512, g * 512 + 512))
        elif k == "gn":
            _, l, g = nm
            out[i] = _slab_cols(inp["w_in"][l], ar(6144 + g * 512, 6144 + g * 512 + 512))
        elif k == "out":
            _, l, g = nm
            out[i] = _slab_cols(inp["w_out"][l], ar(g * 512, g * 512 + 512))
        elif k in ("f1", "f3"):
            _, g = nm
            W = inp["ffn_w1"][0] if k == "f1" else inp["ffn_w3"][0]
            n = min(512, D_FF - g * 512)
            out[i] = _slab_cols(W, _cols_pad(W, g * 512, n))
        elif k == "f2":
            _, g = nm
            out[i] = _slab_rows(inp["ffn_w2"][0], g * 512)
        elif k in ("m1", "m3"):
            _, e, g = nm
            W = inp["moe_w1"][0][e] if k == "m1" else inp["moe_w3"][0][e]
            out[i] = _slab_cols(W, ar(g * 512, g * 512 + 512))
        elif k == "m2":
            _, e, g = nm
            out[i] = _slab_rows(inp["moe_w2"][0][e], g * 512)
        else:
            raise KeyError(nm)
    return out


def _pm(v):
    v = np.asarray(v, np.float32)
    lead = v.shape[:-1]
    return np.ascontiguousarray(np.moveaxis(v.reshape(*lead, 8, 128), -1, 0))


def build_shared(inp):
    sh = {}
    wsr = build_wstream(inp)
    for i in range((len(SLAB_NAMES) + WCH - 1) // WCH):
        sh[f"wstream{i}"] = wsr[i * WCH:(i + 1) * WCH]
    sh["bmodT"] = np.ascontiguousarray(np.moveaxis(inp["b_mod"].reshape(2, 48, 128), -1, 0))
    sh["convw"] = np.ascontiguousarray(np.moveaxis(inp["conv_w"].reshape(2, 4, 8, 128), -1, 0).transpose(0, 1, 3, 2))
    sh["convb"] = _pm(inp["conv_b"])
    sh["lam"] = _pm(inp["rg_lambda"])
    sh["rgb"] = _pm(inp["rg_b"])
    g = np.stack([inp["q_gain"], inp["k_gain"]], 1)
    sh["gains"] = np.ascontiguousarray(np.concatenate([g, g], -1).transpose(2, 0, 1))
    rgw = inp["rg_w"]
    bd = np.zeros((2, 128, 2, 2, 8, 128), np.float32)
    for hb in range(2):
        blk = rgw[:, :, :, hb::2]
        bd[:, hb * 64:(hb + 1) * 64, :, :, :, hb * 64:(hb + 1) * 64] = np.moveaxis(blk, 4, 1)
    sh["rgw"] = bd.reshape(2, 128, 32 * 128)
    kp = np.arange(128)
    kl, kc = kp // 64, kp % 64
    ql, qc = kp // 64, kp % 64
    wstart = np.clip(qc - 8, 0, 48)
    colv = (kc[:, None] >= wstart[None, :]) & (kc[:, None] < wstart[None, :] + 16)
    coff = np.clip(kc[:, None] - qc[None, :] + 15, 0, 30)
    bias = np.zeros((2, 8, 128, 2, NPAT, 128), np.float32)
    mask = np.zeros((128, NPAT, 128), np.float32)
    rpb = inp["rpb"]
    for pc, key in enumerate(NA_PATS):
        drm = np.full((128, 128), -1, np.int64)
        for a in range(2):
            for b in range(2):
                dr = key[a * 2 + b]
                if dr is not None:
                    sel = (kl[:, None] == a) & (ql[None, :] == b)
                    drm[sel] = dr
        valid = (drm >= 0) & colv
        mask[:, pc, :] = valid
        drc = np.where(drm >= 0, drm, 0)
        gathered = rpb[:, :, drc, coff]
        gathered = np.where(valid[None, None], gathered, np.float32(0))
        bias[:, :, :, :, pc, :] = gathered.reshape(2, 8, 2, 128, 128).transpose(0, 1, 3, 2, 4)
    sh["biasG"] = bias.reshape(2, 8, 128, 2 * NPAT * 128)
    sh["maskG"] = mask.reshape(128, NPAT * 128)
    sh["router"] = np.ascontiguousarray(inp["router"][0].reshape(8, 128, 8).transpose(1, 0, 2)).reshape(128, 64)
    sh["ident"] = np.eye(128, dtype=np.float32)
    sh["ltri"] = np.triu(np.ones((128, 128), np.float32), 1)
    return sh


def build_core_inputs(inp, core):
    b0 = 2 * core
    toks = np.concatenate([inp["ctx"][b0:b0 + 2], inp["x"][b0:b0 + 2]], axis=1)
    xT = np.ascontiguousarray(toks.reshape(2, NT, 8, 128).transpose(0, 3, 2, 1))
    cv = np.zeros((4, 1024), np.float32)
    cv[0:2] = inp["c"][b0:b0 + 2]
    cv[2] = inp["c_ctx"]
    scT = np.ascontiguousarray(cv.reshape(4, 8, 128).transpose(2, 1, 0))
    return {"xT": xT, "scT": scT}


def _nbytes(dt):
    return 2 if dt == BF16 else 4


class SBAlloc:
    def __init__(self, nc, limit):
        self.nc, self.off, self.limit, self.n = nc, SB_BASE, SB_BASE + limit - 256, 0

    def __call__(self, name, free_shape, dt, off=None):
        size = int(np.prod(free_shape)) * _nbytes(dt)
        size = (size + 31) // 32 * 32
        if off is None:
            off = self.off
            self.off += size
        assert off + size <= self.limit, (name, off, size, self.limit)
        self.n += 1
        return self.nc.alloc_sbuf_tensor_at(f"{name}_{self.n}", [128] + list(free_shape), dt, offset=off), off + size


def build_program(stage="full", debug=None, nslabs=None):
    nc = bass.Bass("TRN2", target_bir_lowering=False)
    P = Prog()
    limit = nc.sbuf_bytes_remaining
    A = SBAlloc(nc, limit)

    def din(name, shape):
        return nc.dram_tensor(name, list(shape), F32, kind="ExternalInput").ap()

    xT_d = din("xT", [2, 128, 8, NT])
    scT_d = din("scT", [128, 8, 4])
    nsl = nslabs or len(SLAB_NAMES)
    ws_d = [din(f"wstream{i}", [min(WCH, nsl - i * WCH), 128, SLAB]) for i in range((nsl + WCH - 1) // WCH)]
    bmod_d = din("bmodT", [128, 2, 48])
    convw_d = din("convw", [128, 2, 8, 4])
    convb_d = din("convb", [128, 2, 8])
    lam_d = din("lam", [128, 2, 2, 8])
    rgb_d = din("rgb", [128, 2, 2, 2, 8])
    gains_d = din("gains", [128, 2, 2])
    rgw_d = din("rgw", [2, 128, 32 * 128])
    biasG_d = din("biasG", [2, 8, 128, 2 * NPAT * 128])
    maskG_d = din("maskG", [128, NPAT * 128])
    router_d = din("router", [128, 64])
    ident_d = din("ident", [128, 128])
    ltri_d = din("ltri", [128, 128])
    outT_d = nc.dram_tensor("outT", [2, 128, 8, SEQ], F32, kind="ExternalOutput").ap()
    xres_d = nc.dram_tensor("xres", [2, 128, 8, NT], F32, kind="Internal").ap()
    mrnn_d = nc.dram_tensor("mrnn", [128, 8, NT], F32, kind="Internal").ap()
    hg_d = nc.dram_tensor("hg", [NEXP * SEQ * 2, D], BF16, kind="Internal").ap()
    yb_d = nc.dram_tensor("yb", [NEXP * SEQ * 2, D], F32, kind="Internal").ap()
    dbg_d = None
    if debug is not None:
        dbg_d = nc.dram_tensor("dbg", list(debug), F32, kind="ExternalOutput").ap()

    ones_bf, _ = A("ones_bf", [128], BF16)
    blk_bf, _ = A("blk_bf", [128], BF16)
    onesV, _ = A("onesV", [192], BF16)
    ident_f, _ = A("ident_f", [128], F32)
    ones_f, _ = A("ones_f", [128], F32)
    ident_bf, _ = A("ident_bf", [128], BF16)
    ltri_bf, _ = A("ltri_bf", [128], BF16)
    ZT, _ = A("ZT", [D], BF16)
    scT, _ = A("scT", [8, 4], F32)
    scb, _ = A("scb", [8, 4], BF16)
    bmodT, _ = A("bmodT", [2, 48], F32)
    mod, _ = A("mod", [2, 48, 4], F32)
    convw, _ = A("convw", [2, 8, 4], F32)
    convb, _ = A("convb", [2, 8], F32)
    lam, _ = A("lam", [2, 2, 8], F32)
    c1, _ = A("c1", [2, 2, 8], F32)
    c2, _ = A("c2", [2, 2, 8], F32)
    rgb, _ = A("rgb", [2, 2, 2, 8], F32)
    gains, _ = A("gains", [2, 2], F32)
    qg, _ = A("qg", [2], F32)
    rgw, _ = A("rgw", [32, 128], BF16)
    maskG, _ = A("maskG", [NPAT, 128], BF16)
    router, _ = A("router", [8, 8], F32)
    routb, _ = A("routb", [2], F32)
    WS = [A(f"ws{i}", [SLAB], BF16)[0] for i in range(NRING)]
    HT_OFF = A.off
    HT, _ = A("HT", [8, NT], BF16)
    NXR = 4
    for i_ in range(NXR):
        WS.append(A(f"wsx{i_}", [SLAB], BF16, HT_OFF + i_ * SLAB * 2)[0])
    XBASE = A.off

    ps = [nc.alloc_psum_tensor(f"ps{i}", [128, 512], F32) for i in range(8)]

    def PSK(i):
        return ("ps", i)

    ring = {"n": 0}

    def load_slab(name, big=False):
        i = ring["n"] % (NRING + NXR if big else NRING)
        ring["n"] += 1
        src = ws_d[SLAB_IDX[name] // WCH][SLAB_IDX[name] % WCH]
        dst = WS[i]
        wr = [("ws", i)] + ([("HT", t_) for t_ in range(5)] if i >= NRING else [])
        P.dma("pool", lambda e, dst=dst, src=src: e.dma_start(out=dst[:, :], in_=src), writes=wr)
        return i

    def wk(i, k, c0, n):
        return WS[i][:, k * 512 + c0: k * 512 + c0 + n]

    def wk2(i, j, c0, n):
        return WS[i][:, j * 1024 + c0: j * 1024 + c0 + n]

    def mm(out, lhsT, rhs, start, stop, reads, pk):
        P.op("pe", lambda e: e.matmul(out, lhsT, rhs, start=start, stop=stop), reads=reads, writes=[pk])

    def act(out, in_, func, reads, writes, bias=None, scale=None):
        kw = {}
        if bias is not None:
            kw["bias"] = bias
        if scale is not None:
            kw["scale"] = scale
        P.op("act", lambda e: e.activation(out, in_, func, **kw), reads=reads, writes=writes)

    def tt(out, in0, in1, op, reads, writes, eng="dve"):
        P.op(eng, lambda e: e.tensor_tensor(out, in0, in1, op), reads=reads, writes=writes)

    def ts(out, in0, s1, s2, op0, op1, reads, writes, eng="dve"):
        if s2 is None:
            P.op(eng, lambda e: e.tensor_scalar(out, in0, s1, None, op0), reads=reads, writes=writes)
        else:
            P.op(eng, lambda e: e.tensor_scalar(out, in0, s1, s2, op0, op1), reads=reads, writes=writes)

    def stt(out, in0, scalar, in1, op0, op1, reads, writes):
        P.op("dve", lambda e: e.scalar_tensor_tensor(out, in0, scalar, in1, op0, op1), reads=reads, writes=writes)

    def sp_dma(out, in_, reads, writes):
        P.dma("sp", lambda e: e.dma_start(out=out, in_=in_), reads=reads, writes=writes)

    def pool_dma(out, in_, reads, writes):
        P.dma("pool", lambda e: e.dma_start(out=out, in_=in_), reads=reads, writes=writes)

    P.op("dve", lambda e: e.memset(ones_bf[:, :], 1.0), writes=["ones_bf"])
    P.op("dve", lambda e: e.memset(ones_f[:, :], 1.0), writes=["ones_f"])
    P.op("dve", lambda e: e.memset(blk_bf[:, :], 0.0), writes=["blk_bf"])
    P.op("dve", lambda e: e.memset(blk_bf[0:64, 0:64], 1.0), writes=["blk_bf"])
    P.op("dve", lambda e: e.memset(blk_bf[64:128, 64:128], 1.0), writes=["blk_bf"])
    P.op("dve", lambda e: e.memset(onesV[:, :], 1.0), writes=["onesV"])
    P.op("dve", lambda e: e.memset(onesV[:, 64:128], 0.0), writes=["onesV"])
    sp_dma(ident_f[:, :], ident_d, [], ["ident_f"])
    pool_dma(ident_bf[:, :], ident_d, [], ["ident_bf"])
    pool_dma(ltri_bf[:, :], ltri_d, [], ["ltri_bf"])
    sp_dma(scT[:, :, :], scT_d, [], ["scT"])
    sp_dma(bmodT[:, :, :], bmod_d, [], ["bmodT"])
    sp_dma(convw[:, :, :, :], convw_d, [], ["convw"])
    sp_dma(convb[:, :, :], convb_d, [], ["convb"])
    sp_dma(lam[:, :, :, :], lam_d, [], ["lam"])
    sp_dma(rgb[:, :, :, :, :], rgb_d, [], ["rgb"])
    sp_dma(gains[:, :, :], gains_d, [], ["gains"])
    sp_dma(router[:, :, :], router_d.rearrange("p (k e) -> p k e", e=8), [], ["router"])
    pool_dma(maskG[:, :, :], maskG_d.rearrange("p (a b) -> p a b", b=128), [], ["maskG"])
    P.op("dve", lambda e: e.memset(ZT[:, :], 0.0), writes=["ZT"])

    def zero_fill_hg():
        for blk in range(NEXP * SEQ * 2 // 128):
            sp_dma(hg_d[blk * 128:(blk + 1) * 128, :], ZT[:, :], ["ZT"], [("hgz", blk)])
    act(c1[:, :, :, :], lam[:, :, :, :], AF.Exp, ["lam"], ["c1"], scale=-1.0)
    act(c1[:, :, :, :], c1[:, :, :, :], AF.Ln, ["c1"], ["c1"], bias=1.0)
    ts(c2[:, :, :, :], c1[:, :, :, :], -16.0, None, ALU.mult, None, ["c1"], ["c2"])
    ts(c1[:, :, :, :], c1[:, :, :, :], -8.0, None, ALU.mult, None, ["c1", "c2"], ["c1"])
    act(scb[:, :, :], scT[:, :, :], AF.Silu, ["scT"], ["scb"])
    for l in range(2):
        for g in range(12):
            i = load_slab(("mod", l, g))
            for j in range(4):
                col = (g * 4 + j) * 4
                for k in range(8):
                    mm(ps[0][:, col:col + 4], wk(i, k, j * 128, 128), scb[:, k, :], k == 0, k == 7,
                       [("ws", i), "scb"], PSK(0))
        pv = ps[0][:, 0:192].rearrange("p (a b) -> p a b", b=4)
        for j in range(3):
            tt(mod[:, l, :, j], pv[:, :, j], bmodT[:, l, :], ALU.add, ["bmodT"], [PSK(0), "mod"])
    for l in range(2):
        for m in (1, 4):
            ts(mod[:, l, m * 8:(m + 1) * 8, :], mod[:, l, m * 8:(m + 1) * 8, :], 1.0, None, ALU.add, None, ["mod"], ["mod"])

    def modv(l, m, c, col):
        return mod[:, l, m * 8 + c, col:col + 1]

    stages = ["pro", "A0", "B0", "C0", "D0", "E0", "F0", "G0", "H0", "F1", "G1", "G1a", "H1", "seq1", "full"]
    si = stages.index(stage)

    state = {"x_in_res": False}

    def modulate(l, s, which, tiles, xsrc, X, moe=False, after_tile=None):
        m_sh, m_sc = (0, 1) if which == 0 else (3, 4)
        for ti in tiles:
            t0, w = TILES[ti]
            col = 2 if ti == 0 else s
            xap, xkeys = xsrc(ti)
            SQ, RS = X["SQ"], X["RS"]
            for c in range(8):
                act(SQ[:, c, 0:w], xap(c), AF.Square, xkeys, [("SQ", c)])
            for c in range(8):
                mm(ps[1][:, 0:w], ones_bf[:, :], SQ[:, c, 0:w], c == 0, c == 7, [("SQ", c), "ones_bf"], PSK(1))
            act(RS[:, 0:w], ps[1][:, 0:w], AF.Sqrt, [], [PSK(1), "RS"], bias=EPS, scale=1.0 / D)
            P.op("dve", lambda e, w=w: e.reciprocal(RS[:, 0:w], RS[:, 0:w]), reads=["RS"], writes=["RS"])
            for c in range(8):
                if moe:
                    tmp, tk = X["TMP8"][:, c, 0:w], ("TMP8", c)
                else:
                    tmp, tk = X["TMP"][c % 2][:, 0:w], ("TMP", c % 2)
                stt(tmp, xap(c), modv(l, m_sc, c, col), RS[:, 0:w], ALU.mult, ALU.mult, list(xkeys) + ["mod", "RS"], [tk])
                act(HT[:, c, t0:t0 + w], tmp, AF.Identity, [tk, "mod"], [("HT", ti)], bias=modv(l, m_sh, c, col))
            if after_tile is not None:
                after_tile(ti)

    def token_mixer(l, s):
        ctx_out = (l == 0)
        tiles = [0, 1, 2, 3, 4]
        otiles = tiles if ctx_out else [1, 2, 3, 4]
        off = XBASE
        YB, off = A("YB", [8, NT], BF16, off)
        TB = off
        pool_dma(rgw[:, :, :], rgw_d[l].rearrange("p (a b) -> p a b", b=128), [], ["rgw"])
        ts(qg[:, 0:1], gains[:, l, 0:1], 0.125, None, ALU.mult, None, ["gains"], ["qg"])

        off = TB
        XL = []
        for i in range(2):
            t_, off = A(f"XL{i}", [8, 512], F32, off)
            XL.append(t_)
        X = {}
        X["SQ"], off = A("SQ", [8, 512], BF16, off)
        X["RS"], off = A("RS", [512], F32, off)
        X["TMP"] = []
        for i in range(2):
            t_, off = A(f"TMP{i}", [512], F32, off)
            X["TMP"].append(t_)
        xd = xres_d if state["x_in_res"] else xT_d

        def xsrc(ti):
            t0, w = TILES[ti]
            b = ti % 2
            sp_dma(XL[b][:, :, 0:w], xd[s, :, :, t0:t0 + w], [("xd", ti)], [("XL", b)])
            return (lambda c: XL[b][:, c, 0:w]), [("XL", b)]

        modulate(l, s, 0, tiles, xsrc, X)
        P.barrier()
        if stage == "A0":
            return HT, [("HT", t) for t in range(5)]

        off = TB
        S0, off = A("S0", [2312], F32, off)
        XC, off = A("XC", [NT], F32, off)
        S2, off = A("S2", [NT], F32, off)
        S3, off = A("S3", [NT], F32, off)
        HF, off = A("HF", [NT], F32, off)
        HR, off = A("HR", [NT], F32, off)
        XCb, off = A("XCb", [NT], BF16, off)
        GT, off = A("GT", [512], F32, off)

        def xrp_pos(t0):
            return 2 + t0 if t0 < CTX else 261 + (t0 - CTX)

        for c in range(8):
            if c % 2 == 0:
                wsi = load_slab(("rnn", l, c // 2))
            cb = (c % 2) * 256
            for a, b in ((0, 2), (258, 261), (2309, 2312)):
                P.op("dve", lambda e, a=a, b=b: e.memset(S0[:, a:b], 0.0), writes=["S0"])
            for ti in tiles:
                t0, w = TILES[ti]
                pb = 2 + (ti % 2)
                for k in range(8):
                    mm(ps[pb][:, 0:w], wk(wsi, k, cb, 128), HT[:, k, t0:t0 + w], k == 0, k == 7,
                       [("ws", wsi), ("HT", ti)], PSK(pb))
                p0 = xrp_pos(t0)
                act(S0[:, p0:p0 + w], ps[pb][:, 0:w], AF.Identity, [], [PSK(pb), "S0"])
            for (d0, n, base) in ((0, CTX, 2), (CTX, SEQ, 261)):
                ts(XC[:, d0:d0 + n], S0[:, base - 2:base - 2 + n], convw[:, l, c, 0:1], convb[:, l, c:c + 1],
                   ALU.mult, ALU.add, ["S0", "convw", "convb"], [("XC", d0)])
                for j in range(1, 4):
                    stt(XC[:, d0:d0 + n], S0[:, base - 2 + j:base - 2 + j + n], convw[:, l, c, j:j + 1], XC[:, d0:d0 + n],
                        ALU.mult, ALU.add, ["S0", "convw", ("XC", d0)], [("XC", d0)])
            act(XCb[:, :], XC[:, :], AF.Identity, [("XC", 0), ("XC", CTX)], ["XCb"])
            for dr in range(2):
                for ti in tiles:
                    t0, w = TILES[ti]
                    for gt in range(2):
                        pb = 4 + gt + 2 * (ti % 2)
                        mm(ps[pb][:, 0:w], rgw[:, (dr * 2 + gt) * 8 + c, :], XCb[:, t0:t0 + w], True, True,
                           ["rgw", "XCb"], PSK(pb))
                        dst = S2 if gt == 0 else S3
                        act(dst[:, t0:t0 + w], ps[pb][:, 0:w], AF.Sigmoid, ["rgb"], [PSK(pb), ("S2" if gt == 0 else "S3")],
                            bias=rgb[:, l, dr, gt, c:c + 1])
                act(S0[:, 0:NT], S2[:, :], AF.Exp, ["S2", "c1"], ["S0"], scale=c1[:, l, dr, c:c + 1])
                act(S2[:, :], S2[:, :], AF.Exp, ["S2", "c2"], ["S2"], scale=c2[:, l, dr, c:c + 1])
                act(S2[:, :], S2[:, :], AF.Sqrt, ["S2"], ["S2"], scale=-1.0, bias=1.0)
                tt(S3[:, :], S3[:, :], XC[:, :], ALU.mult, ["S3", ("XC", 0), ("XC", CTX)], ["S3"])
                tt(S3[:, :], S3[:, :], S2[:, :], ALU.mult, ["S3", "S2"], ["S3"])
                if dr == 0:
                    P.op("dve", lambda e: e.tensor_tensor_scan(HF[:, :], S0[:, 0:NT], S3[:, :], 0.0, ALU.mult, ALU.add),
                         reads=["S0", "S3"], writes=["HF"])
                else:
                    P.op("dve", lambda e: e.tensor_tensor_scan(HR[:, CTX - 1::-1], S0[:, CTX - 1::-1], S3[:, CTX - 1::-1], 0.0,
                                                               ALU.mult, ALU.add),
                         reads=["S0", "S3"], writes=["HR"])
                    P.op("dve", lambda e: e.tensor_tensor_scan(HR[:, NT - 1:CTX - 1:-1], S0[:, NT - 1:CTX - 1:-1],
                                                               S3[:, NT - 1:CTX - 1:-1], HR[:, 0:1], ALU.mult, ALU.add),
                         reads=["S0", "S3", "HR"], writes=["HR"])
            tt(HF[:, :], HF[:, :], HR[:, :], ALU.add, ["HF", "HR"], ["HF"])
            for ti in otiles:
                t0, w = TILES[ti]
                pb = 2 + (ti % 2)
                for k in range(8):
                    mm(ps[pb][:, 0:w], wk(wsi, k, cb + 128, 128), HT[:, k, t0:t0 + w], k == 0, k == 7,
                       [("ws", wsi), ("HT", ti)], PSK(pb))
                act(GT[:, 0:w], ps[pb][:, 0:w], AF.Gelu_apprx_tanh, [], [PSK(pb), "GT"])
                tt(YB[:, c, t0:t0 + w], HF[:, t0:t0 + w], GT[:, 0:w], ALU.mult, ["HF", "GT"], [("YB", ti)])
            if stage == "B0" and debug is not None and c == 0:
                pass
        P.barrier()
        if stage == "B0":
            return YB, [("YB", t) for t in range(5)]

        off = TB
        SG, MR = [], []
        for i in range(2):
            t_, off = A(f"SG{i}", [512], F32, off)
            SG.append(t_)
            t_, off = A(f"MR{i}", [512], F32, off)
            MR.append(t_)
        cnt = 0
        for g in range(2):
            wa = load_slab(("rnno", l, g))
            wb = load_slab(("gr", l, g))
            for ti in otiles:
                t0, w = TILES[ti]
                for j in range(4):
                    dc = g * 4 + j
                    b = cnt % 2
                    cnt += 1
                    p1, p2 = 2 + b, 4 + b
                    for k in range(8):
                        mm(ps[p1][:, 0:w], wk(wa, k, j * 128, 128), YB[:, k, t0:t0 + w], k == 0, k == 7,
                           [("ws", wa), ("YB", ti)], PSK(p1))
                    for k in range(8):
                        mm(ps[p2][:, 0:w], wk(wb, k, j * 128, 128), HT[:, k, t0:t0 + w], k == 0, k == 7,
                           [("ws", wb), ("HT", ti)], PSK(p2))
                    act(SG[b][:, 0:w], ps[p2][:, 0:w], AF.Sigmoid, [], [PSK(p2), ("SG", b)])
                    tt(MR[b][:, 0:w], ps[p1][:, 0:w], SG[b][:, 0:w], ALU.mult, [("SG", b)], [PSK(p1), ("MR", b)])
                    sp_dma(mrnn_d[:, dc, t0:t0 + w], MR[b][:, 0:w], [("MR", b)], [("mrnn", dc, ti)])
        P.barrier()

        off = TB
        KT, QT, Vz, Eb = [], [], [], []
        for i in range(2):
            t_, off = A(f"KT{i}", [NT], BF16, off)
            KT.append(t_)
            t_, off = A(f"QT{i}", [NT], BF16, off)
            QT.append(t_)
            t_, off = A(f"Vz{i}", [18, 192], BF16, off)
            Vz.append(t_)
            t_, off = A(f"Eb{i}", [2, NPAT, 128], BF16, off)
            Eb.append(t_)
        SQh, RSh, RD = [], [], []
        for i in range(2):
            t_, off = A(f"SQh{i}", [512], BF16, off)
            SQh.append(t_)
            t_, off = A(f"RSh{i}", [512], F32, off)
            RSh.append(t_)
            t_, off = A(f"RD{i}", [128], F32, off)
            RD.append(t_)
        PT = []
        for hh in range(2):
            row = []
            for i in range(2):
                t_, off = A(f"PT{hh}{i}", [7, 128], BF16, off)
                row.append(t_)
            PT.append(row)
        for i in range(2):
            P.op("dve", lambda e, i=i: e.memset(Vz[i][:, :, 64:128], 0.0), writes=[("Vz", i)])
        acnt = {"n": 0}

        def attend_S(c, qtok0, chunks, out_ti):
            cb_ = c % 2
            i = acnt["n"] % 2
            acnt["n"] += 1
            clist = [(0, None), (1, None)] + [(2 + m, pc) for (m, pc) in chunks]
            nchunk = len(clist)
            for hh in range(2):
                pbase = hh * 64
                bX, bY = 2 + 2 * hh, 3 + 2 * hh
                ptk = ("PT", hh, i)
                pt = PT[hh][i]
                for ci, (jt, pc) in enumerate(clist):
                    bank = bX if ci < 4 else bY
                    col = (ci % 4) * 128
                    mm(ps[bank][:, col:col + 128], KT[cb_][pbase:pbase + 64, jt * 128:(jt + 1) * 128],
                       QT[cb_][pbase:pbase + 64, qtok0:qtok0 + 128], True, True, [("KT", cb_), ("QT", cb_)], PSK(bank))
                n1 = min(4, nchunk)
                act(pt[:, 0:n1, :], ps[bX][:, 0:n1 * 128].rearrange("p (a b) -> p a b", b=128), AF.Exp, [], [PSK(bX), ptk])
                if nchunk > 4:
                    n2 = nchunk - 4
                    act(pt[:, 4:nchunk, :], ps[bY][:, 0:n2 * 128].rearrange("p (a b) -> p a b", b=128), AF.Exp, [],
                        [PSK(bY), ptk])
                if chunks:
                    pc0, n = chunks[0][1], len(chunks)
                    tt(pt[:, 2:2 + n, :], pt[:, 2:2 + n, :], Eb[cb_][:, hh, pc0:pc0 + n, :], ALU.mult, [ptk, ("Eb", cb_)], [ptk])
            return (c, qtok0, clist, out_ti, i)

        def attend_PV(stt_):
            c, qtok0, clist, out_ti, i = stt_
            cb_ = c % 2
            nchunk = len(clist)
            total = 2 * nchunk
            idx = 0
            for hh in range(2):
                ptk = ("PT", hh, i)
                for ci, (jt, pc) in enumerate(clist):
                    lv = Vz[cb_][:, jt, 0:128] if hh == 0 else Vz[cb_][:, jt, 64:192]
                    lo = onesV[:, 0:128] if hh == 0 else onesV[:, 64:192]
                    mm(ps[6][:, 0:128], lv, PT[hh][i][:, ci, :], idx == 0, idx == total - 1, [("Vz", cb_), ptk], PSK(6))
                    mm(ps[7][:, 0:128], lo, PT[hh][i][:, ci, :], idx == 0, idx == total - 1, ["onesV", ptk], PSK(7))
                    idx += 1
            P.op("dve", lambda e: e.reciprocal(RD[i][:, :], ps[7][:, 0:128]), reads=[], writes=[PSK(7), ("RD", i)])
            tt(YB[:, c, qtok0:qtok0 + 128], ps[6][:, 0:128], RD[i][:, :], ALU.mult, [("RD", i)], [PSK(6), ("YB", out_ti)])

        pcnt = {"n": 0}

        def inproj_items(c):
            cb_ = c % 2
            items = []
            st_ = {}

            def first():
                st_["ws"] = load_slab(("kvq", l, c))
                pool_dma(Eb[cb_][:, :, :, :], biasG_d[l, c].rearrange("p (h a b) -> p h a b", h=2, b=128), [], [("Eb", cb_)])
                act(Eb[cb_][:, :, :, :], Eb[cb_][:, :, :, :], AF.Exp, [("Eb", cb_)], [("Eb", cb_)])
                for hh in range(2):
                    tt(Eb[cb_][:, hh, :, :], Eb[cb_][:, hh, :, :], maskG[:, :, :], ALU.mult, [("Eb", cb_), "maskG"], [("Eb", cb_)])
            items.append(first)
            for (colbase, dst, dkey, gain, gkey, tl) in ((0, KT[cb_], ("KT", cb_), gains[:, l, 1:2], "gains", tiles),
                                                       (256, QT[cb_], ("QT", cb_), qg[:, 0:1], "qg", otiles)):
                for ti in tl:
                    def proj(colbase=colbase, dst=dst, dkey=dkey, gain=gain, gkey=gkey, ti=ti):
                        wsi = st_["ws"]
                        t0, w = TILES[ti]
                        b = pcnt["n"] % 2
                        pcnt["n"] += 1
                        pP, pS = (0, 1) if b == 0 else (6, 7)
                        for k in range(8):
                            mm(ps[pP][:, 0:w], wk(wsi, k, colbase, 128), HT[:, k, t0:t0 + w], k == 0, k == 7,
                               [("ws", wsi), ("HT", ti)], PSK(pP))
                        act(SQh[b][:, 0:w], ps[pP][:, 0:w], AF.Square, [], [PSK(pP), ("SQh", b)])
                        mm(ps[pS][:, 0:w], blk_bf[:, :], SQh[b][:, 0:w], True, True, [("SQh", b), "blk_bf"], PSK(pS))
                        act(RSh[b][:, 0:w], ps[pS][:, 0:w], AF.Sqrt, [], [PSK(pS), ("RSh", b)], bias=EPS, scale=1.0 / 64)
                        P.op("dve", lambda e, b=b, w=w: e.reciprocal(RSh[b][:, 0:w], RSh[b][:, 0:w]), reads=[("RSh", b)],
                             writes=[("RSh", b)])
                        stt(dst[:, t0:t0 + w], ps[pP][:, 0:w], gain, RSh[b][:, 0:w], ALU.mult, ALU.mult, [("RSh", b), gkey],
                            [PSK(pP), dkey])
                    items.append(proj)
            for j0 in range(0, 18, 4):
                def vproj(j0=j0):
                    wsi = st_["ws"]
                    n = min(4, 18 - j0)
                    bank = 0 if (j0 // 4) % 2 == 0 else 1
                    for jj in range(n):
                        jt = j0 + jj
                        ti = 0 if jt < 2 else 1 + (jt - 2) // 4
                        for k in range(8):
                            mm(ps[bank][:, jj * 128:(jj + 1) * 128], HT[:, k, jt * 128:(jt + 1) * 128], wk(wsi, k, 128, 128),
                               k == 0, k == 7, [("ws", wsi), ("HT", ti)], PSK(bank))
                    pv3 = ps[bank][:, 0:n * 128].rearrange("p (a b) -> p a b", b=128)
                    act(Vz[cb_][:, j0:j0 + n, 0:64], pv3[:, :, 0:64], AF.Identity, [], [PSK(bank), ("Vz", cb_)])
                    P.op("dve", lambda e, j0=j0, n=n, pv3=pv3: e.tensor_copy(Vz[cb_][:, j0:j0 + n, 128:192], pv3[:, :, 64:128]),
                         reads=[], writes=[PSK(bank), ("Vz", cb_)])
                items.append(vproj)
            return items

        for c in range(8):
            for it in inproj_items(c):
                it()
            calls = []
            if ctx_out:
                for qt in range(2):
                    calls.append((qt * 128, [], 0))
            for qp in range(16):
                calls.append((CTX + qp * 128, NA_PLAN[qp], 1 + qp // 4))
            pend = None
            for (q0, ch, oti) in calls:
                cur = attend_S(c, q0, ch, oti)
                if pend is not None:
                    attend_PV(pend)
                pend = cur
            attend_PV(pend)
        P.barrier()
        if stage == "D0":
            return YB, [("YB", t) for t in range(5)]

        off = TB
        MTa, off = A("MTa", [8, NT], BF16, off)
        XL2, MRL, T1, SG2 = [], [], [], []
        for i in range(2):
            t_, off = A(f"XL2{i}", [4, 512], F32, off)
            XL2.append(t_)
        for i in range(2):
            t_, off = A(f"MRL{i}", [512], F32, off)
            MRL.append(t_)
            t_, off = A(f"T1{i}", [512], F32, off)
            T1.append(t_)
            t_, off = A(f"SG2{i}", [512], F32, off)
            SG2.append(t_)
        cnt = 0
        for g in range(2):
            wa = load_slab(("nao", l, g))
            wb = load_slab(("gn", l, g))
            for ti in otiles:
                t0, w = TILES[ti]
                for j in range(4):
                    dc = g * 4 + j
                    b = cnt % 2
                    cnt += 1
                    p1, p2 = 2 + b, 4 + b
                    for k in range(8):
                        mm(ps[p1][:, 0:w], wk(wa, k, j * 128, 128), YB[:, k, t0:t0 + w], k == 0, k == 7,
                           [("ws", wa), ("YB", ti)], PSK(p1))
                    for k in range(8):
                        mm(ps[p2][:, 0:w], wk(wb, k, j * 128, 128), HT[:, k, t0:t0 + w], k == 0, k == 7,
                           [("ws", wb), ("HT", ti)], PSK(p2))
                    sp_dma(MRL[b][:, 0:w], mrnn_d[:, dc, t0:t0 + w], [("mrnn", dc, ti)], [("MRL", b)])
                    act(SG2[b][:, 0:w], ps[p2][:, 0:w], AF.Sigmoid, [], [PSK(p2), ("SG2", b)])
                    tt(T1[b][:, 0:w], ps[p1][:, 0:w], SG2[b][:, 0:w], ALU.mult, [("SG2", b)], [PSK(p1), ("T1", b)])
                    tt(MTa[:, dc, t0:t0 + w], T1[b][:, 0:w], MRL[b][:, 0:w], ALU.add, [("T1", b), ("MRL", b)], [("MTa", ti)])
        xn = 0
        for g in range(2):
            wo = load_slab(("out", l, g))
            for ti in otiles:
                t0, w = TILES[ti]
                col = 2 if ti == 0 else s
                xb = xn % 2
                xn += 1
                sp_dma(XL2[xb][:, :, 0:w], xd[s, :, g * 4:(g + 1) * 4, t0:t0 + w], [("xd", ti)], [("XL2", xb)])
                for j in range(4):
                    dc = g * 4 + j
                    b = cnt % 2
                    cnt += 1
                    p1 = 6 + b
                    for k in range(8):
                        mm(ps[p1][:, 0:w], wk(wo, k, j * 128, 128), MTa[:, k, t0:t0 + w], k == 0, k == 7,
                           [("ws", wo), ("MTa", ti)], PSK(p1))
                    stt(XL2[xb][:, j, 0:w], ps[p1][:, 0:w], modv(l, 2, dc, col), XL2[xb][:, j, 0:w], ALU.mult, ALU.add,
                        ["mod", ("XL2", xb)], [PSK(p1), ("XL2", xb)])
                sp_dma(xres_d[s, :, g * 4:(g + 1) * 4, t0:t0 + w], XL2[xb][:, :, 0:w], [("XL2", xb)], [("xdw", ti, g)])
        state["x_in_res"] = True
        P.barrier()
        return None

    def ffn(l, s):
        ctx_out = (l == 0)
        moe = (l == 1)
        otiles = [0, 1, 2, 3, 4] if ctx_out else [1, 2, 3, 4]
        off = XBASE
        XTs, off = A("XTs", [8, NT], F32, off)
        Gt, off = A("Gt", [16, 8], F32, off)
        OV = off
        X = {}
        X["SQ"], off = A("SQ2", [8, 512], BF16, off)
        X["RS"], off = A("RS2", [512], F32, off)
        if moe:
            X["TMP8"], off = A("TMP8", [8, 512], F32, off)
            LG, off = A("LG", [512], F32, off)
            LT, off = A("LT", [16, 8], F32, off)
            EQ1, off = A("EQ1", [16, 8], F32, off)
            L2, off = A("L2", [16, 8], F32, off)
            EQ2, off = A("EQ2", [16, 8], F32, off)
            TG, off = A("TG", [16, 8], F32, off)
            M1, off = A("M1", [16, 1], F32, off)
            M2, off = A("M2", [16, 1], F32, off)
            W1, off = A("W1", [16, 1], F32, off)
            W2, off = A("W2", [16, 1], F32, off)
        else:
            X["TMP"] = []
            for i in range(2):
                t_, off = A(f"TMPf{i}", [512], F32, off)
                X["TMP"].append(t_)
        for ti in otiles:
            t0, w = TILES[ti]
            sp_dma(XTs[:, :, t0:t0 + w], xres_d[s, :, :, t0:t0 + w], [("xd", ti)], [("XTs", ti)])

        def xsrc(ti):
            t0, w = TILES[ti]
            return (lambda c: XTs[:, c, t0:t0 + w]), [("XTs", ti)]

        if SPARSE_MOE and MERGED_MOE and stage in ("full", "seq1") and not state.get("zf"):
            state["zf"] = True
            zero_fill_hg()
        hook = None
        if moe:
            for c in range(8):
                mm(ps[2][0:8, 0:2], router[:, c, :], mod[:, l, 24 + c, s:s + 2], c == 0, c == 7, ["router", "mod"], PSK(2))
            act(routb[0:8, 0:2], ps[2][0:8, 0:2], AF.Identity, [], [PSK(2), "routb"])

            def hook(ti):
                t0, w = TILES[ti]
                for c in range(8):
                    mm(ps[2][0:8, 0:w], router[:, c, :], X["TMP8"][:, c, 0:w], c == 0, c == 7, ["router", ("TMP8", c)], PSK(2))
                act(LG[0:8, 0:w], ps[2][0:8, 0:w], AF.Identity, ["routb"], [PSK(2), "LG"], bias=routb[0:8, 0:1])
                for j in range(w // 128):
                    jt = (t0 - CTX) // 128 + j
                    P.op("pe", lambda e, jt=jt, j=j: e.transpose(ps[3][:, jt * 8:(jt + 1) * 8], LG[0:8, j * 128:(j + 1) * 128],
                                                                 ident_f[0:8, 0:8]),
                         reads=["LG", "ident_f"], writes=[PSK(3)])

        modulate(l, s, 1, otiles, xsrc, X, moe=moe, after_tile=hook)
        if moe:
            P.op("dve", lambda e: e.tensor_copy(LT[:, :, :], ps[3][:, 0:128].rearrange("p (a b) -> p a b", b=8)),
                 reads=[], writes=[PSK(3), "LT"])
            P.op("dve", lambda e: e.tensor_reduce(M1[:, :, 0], LT[:, :, :], AX.X, ALU.max), reads=["LT"], writes=["M1"])
            tt(EQ1[:, :, :], LT[:, :, :], M1[:, :, 0:1].to_broadcast([128, 16, 8]), ALU.is_equal, ["LT", "M1"], ["EQ1"])
            stt(L2[:, :, :], EQ1[:, :, :], -1.0e30, LT[:, :, :], ALU.mult, ALU.add, ["EQ1", "LT"], ["L2"])
            P.op("dve", lambda e: e.tensor_reduce(M2[:, :, 0], L2[:, :, :], AX.X, ALU.max), reads=["L2"], writes=["M2"])
            tt(EQ2[:, :, :], L2[:, :, :], M2[:, :, 0:1].to_broadcast([128, 16, 8]), ALU.is_equal, ["L2", "M2"], ["EQ2"])
            tt(W2[:, :, :], M2[:, :, :], M1[:, :, :], ALU.subtract, ["M1", "M2"], ["W2"])
            act(W2[:, :, :], W2[:, :, :], AF.Exp, ["W2"], ["W2"])
            ts(W1[:, :, :], W2[:, :, :], 1.0, None, ALU.add, None, ["W2"], ["W1"])
            P.op("dve", lambda e: e.reciprocal(W1[:, :, :], W1[:, :, :]), reads=["W1"], writes=["W1"])
            tt(W2[:, :, :], W2[:, :, :], W1[:, :, :], ALU.mult, ["W1", "W2"], ["W2"])
            tt(Gt[:, :, :], EQ1[:, :, :], W1[:, :, 0:1].to_broadcast([128, 16, 8]), ALU.mult, ["EQ1", "W1"], ["Gt"])
            tt(TG[:, :, :], EQ2[:, :, :], W2[:, :, 0:1].to_broadcast([128, 16, 8]), ALU.mult, ["EQ2", "W2"], ["TG"])
            tt(Gt[:, :, :], Gt[:, :, :], TG[:, :, :], ALU.add, ["Gt", "TG"], ["Gt"])
        P.barrier()
        if stage == "G0":
            return HT, [("HT", t) for t in range(5)]

        off = OV
        SIL, TT, ACTT, GE, DG = [], [], [], [], []
        for i in range(2):
            t_, off = A(f"SIL{i}", [512], BF16, off)
            SIL.append(t_)
            t_, off = A(f"TT{i}", [512], F32, off)
            TT.append(t_)
            t_, off = A(f"ACTT{i}", [4, 512], BF16, off)
            ACTT.append(t_)
            if moe:
                t_, off = A(f"GE{i}", [SEQ], F32, off)
                GE.append(t_)
                t_, off = A(f"DG{i}", [128], F32, off)
                DG.append(t_)
        cnt = {"h": 0, "a": 0, "o": 0, "d": 0}

        def swiglu_group(names, nj, tl, ge):
            w1 = load_slab(names[0])
            w3 = load_slab(names[1])
            w2 = load_slab(names[2])
            for ti in tl:
                t0, w = TILES[ti]
                col = 2 if ti == 0 else s
                ab = cnt["a"] % 2
                cnt["a"] += 1
                for j in range(nj):
                    b = cnt["h"] % 2
                    cnt["h"] += 1
                    b1, b3 = 2 + b, 4 + b
                    for k in range(8):
                        mm(ps[b1][:, 0:w], wk(w1, k, j * 128, 128), HT[:, k, t0:t0 + w], k == 0, k == 7,
                           [("ws", w1), ("HT", ti)], PSK(b1))
                    for k in range(8):
                        mm(ps[b3][:, 0:w], wk(w3, k, j * 128, 128), HT[:, k, t0:t0 + w], k == 0, k == 7,
                           [("ws", w3), ("HT", ti)], PSK(b3))
                    act(SIL[b][:, 0:w], ps[b1][:, 0:w], AF.Silu, [], [PSK(b1), ("SIL", b)])
                    if ge is None:
                        tt(ACTT[ab][:, j, 0:w], ps[b3][:, 0:w], SIL[b][:, 0:w], ALU.mult, [("SIL", b)], [PSK(b3), ("ACTT", ab)])
                    else:
                        tt(TT[b][:, 0:w], ps[b3][:, 0:w], SIL[b][:, 0:w], ALU.mult, [("SIL", b)], [PSK(b3), ("TT", b)])
                        tt(ACTT[ab][:, j, 0:w], TT[b][:, 0:w], GE[ge][:, t0 - CTX:t0 - CTX + w], ALU.mult,
                           [("TT", b), ("GE", ge)], [("ACTT", ab)])
                for dc in range(8):
                    ob = 6 + cnt["o"] % 2
                    cnt["o"] += 1
                    for j in range(nj):
                        mm(ps[ob][:, 0:w], wk2(w2, j, dc * 128, 128), ACTT[ab][:, j, 0:w], j == 0, j == nj - 1,
                           [("ws", w2), ("ACTT", ab)], PSK(ob))
                    stt(XTs[:, dc, t0:t0 + w], ps[ob][:, 0:w], modv(l, 5, dc, col), XTs[:, dc, t0:t0 + w], ALU.mult, ALU.add,
                        ["mod", ("XTs", ti)], [PSK(ob), ("XTs", ti)])

        if not moe:
            for g in range(6):
                swiglu_group([("f1", g), ("f3", g), ("f2", g)], 4 if g < 5 else 2, otiles, None)
        else:
            for ex in range(NEXP):
                ge = ex % 2
                for j0 in range(0, 16, 4):
                    bank = 0 if (j0 // 4) % 2 == 0 else 1
                    for jj in range(4):
                        jt = j0 + jj
                        d = cnt["d"] % 2
                        cnt["d"] += 1
                        ts(DG[d][:, :], ident_f[:, :], Gt[:, jt, ex:ex + 1], None, ALU.mult, None, ["ident_f", "Gt"], [("DG", d)])
                        mm(ps[bank][:, jj * 128:(jj + 1) * 128], ones_f[:, :], DG[d][:, :], True, True, ["ones_f", ("DG", d)],
                           PSK(bank))
                    act(GE[ge][:, j0 * 128:(j0 + 4) * 128], ps[bank][:, 0:512], AF.Identity, [], [PSK(bank), ("GE", ge)])
                for g in range(7):
                    swiglu_group([("m1", ex, g), ("m3", ex, g), ("m2", ex, g)], 4, [1, 2, 3, 4], ge)
        for ti in otiles:
            t0, w = TILES[ti]
            if l == 0:
                sp_dma(xres_d[s, :, :, t0:t0 + w], XTs[:, :, t0:t0 + w], [("XTs", ti)], [("xd", ti)])
            else:
                sp_dma(outT_d[s, :, :, t0 - CTX:t0 - CTX + w], XTs[:, :, t0:t0 + w], [("XTs", ti)], [("out", s, ti)])
        P.barrier()
        return None

    def ffn_moe_sparse(l, s):
        I32 = mybir.dt.int32
        TSZ = MOE_TSZ
        NQ = TSZ // 128
        NBLK = SEQ // TSZ
        lat = [1, 2, 3, 4]
        off = XBASE
        Gsm = {}
        for nm, shp, dt in (("W1", [16, 1], F32), ("W2", [16, 1], F32), ("SI", [16, 2], I32), ("JI", [8], I32)):
            Gsm[nm], off = A("m_" + nm, shp, dt, off)
        OV = off
        for nm, shp, dt in (("LT", [16, 8], F32), ("EQ1", [16, 8], F32), ("L2", [16, 8], F32), ("EQ2", [16, 8], F32),
                            ("M1", [16, 1], F32), ("M2", [16, 1], F32),
                            ("AB", [16, 8], BF16), ("PW", [16, 8], F32), ("TOT", [16, 8], F32), ("CS", [16, 8], F32),
                            ("EOFF", [16, 8], F32), ("TQ", [16, 8], F32), ("S12", [16, 2], F32),
                            ("NE", [8, 1], F32), ("THR", [8, 8], F32), ("CMP", [8, 8], F32), ("JF", [8], F32)):
            Gsm[nm], off = A("m_" + nm, shp, dt, off)
        LT, EQ1, L2, EQ2, M1, M2, W1, W2 = (Gsm[k] for k in ("LT", "EQ1", "L2", "EQ2", "M1", "M2", "W1", "W2"))
        AB, PW, TOT, CS, EOFF, TQ, S12, SI = (Gsm[k] for k in ("AB", "PW", "TOT", "CS", "EOFF", "TQ", "S12", "SI"))
        NE, THR, CMP, JF, JI = (Gsm[k] for k in ("NE", "THR", "CMP", "JF", "JI"))
        LG, off = A("mLG", [512], F32, off)
        XL = []
        for i in range(2):
            t_, off = A(f"mXL{i}", [8, 512], F32, off)
            XL.append(t_)
        X = {}
        X["SQ"], off = A("mSQ", [8, 512], BF16, off)
        X["RS"], off = A("mRS", [512], F32, off)
        X["TMP8"], off = A("mTMP8", [8, 512], F32, off)
        HTok = []
        for i in range(2):
            t_, off = A(f"HTok{i}", [D], BF16, off)
            HTok.append(t_)

        def xsrc(ti):
            t0, w = TILES[ti]
            b = ti % 2
            sp_dma(XL[b][:, :, 0:w], xres_d[s, :, :, t0:t0 + w], [("xd", ti)], [("XL", b)])
            return (lambda c: XL[b][:, c, 0:w]), [("XL", b)]

        for c in range(8):
            mm(ps[2][0:8, 0:2], router[:, c, :], mod[:, l, 24 + c, s:s + 2], c == 0, c == 7, ["router", "mod"], PSK(2))
        act(routb[0:8, 0:2], ps[2][0:8, 0:2], AF.Identity, [], [PSK(2), "routb"])

        def hook(ti):
            t0, w = TILES[ti]
            for c in range(8):
                mm(ps[2][0:8, 0:w], router[:, c, :], X["TMP8"][:, c, 0:w], c == 0, c == 7, ["router", ("TMP8", c)], PSK(2))
            act(LG[0:8, 0:w], ps[2][0:8, 0:w], AF.Identity, ["routb"], [PSK(2), "LG"], bias=routb[0:8, 0:1])
            for j in range(w // 128):
                jt = (t0 - CTX) // 128 + j
                P.op("pe", lambda e, jt=jt, j=j: e.transpose(ps[3][:, jt * 8:(jt + 1) * 8], LG[0:8, j * 128:(j + 1) * 128],
                                                             ident_f[0:8, 0:8]),
                     reads=["LG", "ident_f"], writes=[PSK(3)])

        modulate(l, s, 1, lat, xsrc, X, moe=True, after_tile=hook)
        P.op("dve", lambda e: e.tensor_copy(LT[:, :, :], ps[3][:, 0:128].rearrange("p (a b) -> p a b", b=8)),
             reads=[], writes=[PSK(3), "LT"])
        P.op("dve", lambda e: e.tensor_reduce(M1[:, :, 0], LT[:, :, :], AX.X, ALU.max), reads=["LT"], writes=["M1"])
        tt(EQ1[:, :, :], LT[:, :, :], M1[:, :, 0:1].to_broadcast([128, 16, 8]), ALU.is_equal, ["LT", "M1"], ["EQ1"])
        stt(L2[:, :, :], EQ1[:, :, :], -1.0e30, LT[:, :, :], ALU.mult, ALU.add, ["EQ1", "LT"], ["L2"])
        P.op("dve", lambda e: e.tensor_reduce(M2[:, :, 0], L2[:, :, :], AX.X, ALU.max), reads=["L2"], writes=["M2"])
        tt(EQ2[:, :, :], L2[:, :, :], M2[:, :, 0:1].to_broadcast([128, 16, 8]), ALU.is_equal, ["L2", "M2"], ["EQ2"])
        tt(W2[:, :, :], M2[:, :, :], M1[:, :, :], ALU.subtract, ["M1", "M2"], ["W2"])
        act(W2[:, :, :], W2[:, :, :], AF.Exp, ["W2"], ["W2"])
        ts(W1[:, :, :], W2[:, :, :], 1.0, None, ALU.add, None, ["W2"], ["W1"])
        P.op("dve", lambda e: e.reciprocal(W1[:, :, :], W1[:, :, :]), reads=["W1"], writes=["W1"])
        tt(W2[:, :, :], W2[:, :, :], W1[:, :, :], ALU.mult, ["W1", "W2"], ["W2"])
        tt(TQ[:, :, :], EQ1[:, :, :], EQ2[:, :, :], ALU.add, ["EQ1", "EQ2"], ["TQ"])
        P.op("dve", lambda e: e.tensor_copy(AB[:, :, :], TQ[:, :, :]), reads=["TQ"], writes=["AB"])
        ABf = AB[:, :, :].rearrange("p a b -> p (a b)")
        mm(ps[0][:, 0:128], ltri_bf[:, :], ABf, True, True, ["AB", "ltri_bf"], PSK(0))
        mm(ps[0][:, 128:256], ones_bf[:, :], ABf, True, True, ["AB", "ones_bf"], PSK(0))
        P.op("dve", lambda e: e.tensor_copy(PW[:, :, :], ps[0][:, 0:128].rearrange("p (a b) -> p a b", b=8)),
             reads=[], writes=[PSK(0), "PW"])
        P.op("dve", lambda e: e.tensor_copy(TOT[:, :, :], ps[0][:, 128:256].rearrange("p (a b) -> p a b", b=8)),
             reads=[], writes=[PSK(0), "TOT"])
        P.op("dve", lambda e: e.memset(CS[:, 0, :], 0.0), writes=["CS"])
        for j in range(1, 16):
            tt(CS[:, j, :], CS[:, j - 1, :], TOT[:, j - 1, :], ALU.add, ["CS", "TOT"], ["CS"])
        tt(NE[:, :, 0], CS[:, 15, :], TOT[:, 15, :], ALU.add, ["CS", "TOT"], ["NE"])
        for ex in range(NEXP):
            P.op("dve", lambda e, ex=ex: e.memset(EOFF[:, :, ex], float(ex * SEQ)), writes=["EOFF"])
            P.op("dve", lambda e, ex=ex: e.memset(THR[:, :, ex], float(ex * TSZ)), writes=["THR"])
        tt(PW[:, :, :], PW[:, :, :], CS[:, :, :], ALU.add, ["PW", "CS"], ["PW"])
        tt(PW[:, :, :], PW[:, :, :], EOFF[:, :, :], ALU.add, ["PW", "EOFF"], ["PW"])
        tt(TQ[:, :, :], EQ1[:, :, :], PW[:, :, :], ALU.mult, ["EQ1", "PW"], ["TQ"])
        P.op("dve", lambda e: e.tensor_reduce(S12[:, :, 0], TQ[:, :, :], AX.X, ALU.add), reads=["TQ"], writes=["S12"])
        tt(TQ[:, :, :], EQ2[:, :, :], PW[:, :, :], ALU.mult, ["EQ2", "PW", "S12"], ["TQ"])
        P.op("dve", lambda e: e.tensor_reduce(S12[:, :, 1], TQ[:, :, :], AX.X, ALU.add), reads=["TQ"], writes=["S12"])
        P.op("dve", lambda e: e.tensor_copy(SI[:, :, :], S12[:, :, :]), reads=["S12"], writes=["SI"])
        tt(CMP[:, :, :], NE[:, :, 0:1].to_broadcast([128, 8, 8]), THR[:, :, :], ALU.is_gt, ["NE", "THR"], ["CMP"])
        P.op("dve", lambda e: e.tensor_reduce(JF[:, :], CMP[:, :, :], AX.X, ALU.add), reads=["CMP"], writes=["JF"])
        if JCLAMP is not None:
            ts(JF[:, :], JF[:, :], float(JCLAMP), None, ALU.min, None, ["JF"], ["JF"])
        P.op("dve", lambda e: e.tensor_copy(JI[:, :], JF[:, :]), reads=["JF"], writes=["JI"])
        if stage == "G1a":
            DB, _ = A("DBG1", [64], F32, off)
            P.op("dve", lambda e: e.memset(DB[:, :], 0.0), writes=["DB"])
            P.op("dve", lambda e: e.tensor_copy(DB[:, 0:8], NE[:, :, 0]), reads=["NE"], writes=["DB"])
            P.op("dve", lambda e: e.tensor_copy(DB[:, 8:16], JF[:, :]), reads=["JF"], writes=["DB"])
            P.op("dve", lambda e: e.tensor_copy(DB[:, 16:48], S12[:, :, :].rearrange("p a b -> p (a b)")), reads=["S12"], writes=["DB"])
            P.op("dve", lambda e: e.tensor_copy(DB[:, 48:64], M1[:, :, 0]), reads=["M1"], writes=["DB"])
            sp_dma(dbg_d, DB[:, :], ["DB"], ["dbg"])
            return "done"
        psb = [ps[i][:, :].bitcast(BF16) for i in range(8)]
        if ZERO_HG:
            P.op("dve", lambda e: e.memset(HTok[0][:, :], 0.0), writes=[("HTok", 0)])
            for blk in range(NEXP * SEQ // 128):
                sp_dma(hg_d[blk * 128:(blk + 1) * 128, :], HTok[0][:, :], [("HTok", 0)], [("hgz", blk)])
        for jt in range(16):
            b = jt % 2
            ti = 1 + jt // 4
            for c in range(8):
                P.op("pe", lambda e, b=b, c=c, jt=jt: e.transpose(psb[b][:, c * 128:(c + 1) * 128],
                                                                  HT[:, c, CTX + jt * 128:CTX + (jt + 1) * 128], ident_bf[:, :]),
                     reads=[("HT", ti), "ident_bf"], writes=[PSK(b)])
            act(HTok[b][:, :], psb[b][:, :], AF.Identity, [], [PSK(b), ("HTok", b)])
            for k in range(2):
                P.dma("pool", lambda e, b=b, jt=jt, k=k: e.indirect_dma_start(
                    out=hg_d, out_offset=bass.IndirectOffsetOnAxis(ap=SI[:, jt, k:k + 1], axis=0), in_=HTok[b][:, :],
                    in_offset=None), reads=[("HTok", b), "SI"] + ([("hgz", q_) for q_ in range(128)] if ZERO_HG else []), writes=[("hg", jt, k)])
        P.barrier()
        if stage == "G1":
            return None

        off = OV
        Yacc, off = A("Yacc", [16, D], F32, off)
        HTg, off = A("HTg", [8, SEQ], BF16, off)
        HGs, SIL, ACTT = [], [], []
        t_, off = A("HGs0", [D], BF16, off)
        HGs = [t_, t_]
        for i in range(2):
            t_, off = A(f"mSIL{i}", [TSZ], BF16, off)
            SIL.append(t_)
            t_, off = A(f"mACTT{i}", [4, TSZ], BF16, off)
            ACTT.append(t_)
        cnt = {"h": 0, "a": 0, "o": 0, "g": 0}
        for ex in range(NEXP):
            P.load_reg(JI[0:1, ex:ex + 1], "JI")
            for k in range(16):
                b = cnt["g"] % 2
                cnt["g"] += 1
                r0 = ex * SEQ + k * 128
                sp_dma(HGs[b][:, :], hg_d[r0:r0 + 128, :], [("hg", a_, b_) for a_ in range(16) for b_ in range(2)], [("HGs", 0)])
                P.cond_begin(k // NQ + 1)
                for c in range(8):
                    P.op("pe", lambda e, b=b, c=c: e.transpose(psb[b][:, c * 128:(c + 1) * 128], HGs[b][:, c * 128:(c + 1) * 128],
                                                               ident_bf[:, :]),
                         reads=[("HGs", 0), "ident_bf"], writes=[PSK(b)])
                act(HTg[:, :, k * 128:(k + 1) * 128], psb[b][:, :].rearrange("p (a b) -> p a b", b=128), AF.Identity, [],
                    [PSK(b), ("HTg", k // NQ)])
                P.cond_end()
            for g in range(7):
                w1 = load_slab(("m1", ex, g), big=True)
                w3 = load_slab(("m3", ex, g), big=True)
                w2 = load_slab(("m2", ex, g), big=True)
                for j in range(NBLK):
                    P.cond_begin(j + 1)
                    ab = cnt["a"] % 2
                    cnt["a"] += 1
                    s0 = j * TSZ
                    for jj in range(4):
                        b = cnt["h"] % 2
                        cnt["h"] += 1
                        b1, b3 = 2 + b, 4 + b
                        for k in range(8):
                            mm(ps[b1][:, 0:TSZ], wk(w1, k, jj * 128, 128), HTg[:, k, s0:s0 + TSZ], k == 0, k == 7,
                               [("ws", w1), ("HTg", j)], PSK(b1))
                        for k in range(8):
                            mm(ps[b3][:, 0:TSZ], wk(w3, k, jj * 128, 128), HTg[:, k, s0:s0 + TSZ], k == 0, k == 7,
                               [("ws", w3), ("HTg", j)], PSK(b3))
                        act(SIL[b][:, :], ps[b1][:, 0:TSZ], AF.Silu, [], [PSK(b1), ("SIL", b)])
                        tt(ACTT[ab][:, jj, :], ps[b3][:, 0:TSZ], SIL[b][:, :], ALU.mult, [("SIL", b)], [PSK(b3), ("ACTT", ab)])
                    for h2 in range(NQ):
                        kt = NQ * j + h2
                        for dh in range(2):
                            ob = 6 + cnt["o"] % 2
                            cnt["o"] += 1
                            for jj in range(4):
                                mm(ps[ob][:, 0:512], ACTT[ab][:, jj, h2 * 128:(h2 + 1) * 128], wk2(w2, jj, dh * 512, 512),
                                   jj == 0, jj == 3, [("ws", w2), ("ACTT", ab)], PSK(ob))
                            ya = Yacc[:, kt, dh * 512:(dh + 1) * 512]
                            if g == 0:
                                P.op("dve", lambda e, ya=ya, ob=ob: e.tensor_copy(ya, ps[ob][:, 0:512]), reads=[],
                                     writes=[PSK(ob), ("Yacc", kt)])
                            else:
                                tt(ya, ps[ob][:, 0:512], ya, ALU.add, [("Yacc", kt)], [PSK(ob), ("Yacc", kt)])
                    P.cond_end()
            for k in range(16):
                r0 = ex * SEQ + k * 128
                sp_dma(yb_d[r0:r0 + 128, :], Yacc[:, k, :], [("Yacc", k)], [("yb", ex, k)])
        P.barrier()

        off = OV
        YA, YB2, OO, XC8 = [], [], [], []
        for i in range(2):
            t_, off = A(f"YA{i}", [D], F32, off)
            YA.append(t_)
            t_, off = A(f"YB2{i}", [D], F32, off)
            YB2.append(t_)
            t_, off = A(f"OO{i}", [D], F32, off)
            OO.append(t_)
            t_, off = A(f"XC8{i}", [8, 128], F32, off)
            XC8.append(t_)
        for jt in range(16):
            b = jt % 2
            ti = 1 + jt // 4
            c0 = CTX + jt * 128
            for k, dst, dk in ((0, YA, "YA"), (1, YB2, "YB2")):
                P.dma("pool", lambda e, b=b, jt=jt, k=k, dst=dst: e.indirect_dma_start(
                    out=dst[b][:, :], out_offset=None, in_=yb_d,
                    in_offset=bass.IndirectOffsetOnAxis(ap=SI[:, jt, k:k + 1], axis=0)),
                    reads=[("yb", a_, b_) for a_ in range(NEXP) for b_ in range(16)] + ["SI"], writes=[(dk, b)])
            sp_dma(XC8[b][:, :, :], xres_d[s, :, :, c0:c0 + 128], [("xd", ti)], [("XC8", b)])
            ts(OO[b][:, :], YA[b][:, :], W1[:, jt, 0:1], None, ALU.mult, None, [("YA", b), "W1"], [("OO", b)])
            stt(OO[b][:, :], YB2[b][:, :], W2[:, jt, 0:1], OO[b][:, :], ALU.mult, ALU.add, [("YB2", b), "W2", ("OO", b)], [("OO", b)])
            for c in range(8):
                pbk = 2 + 2 * b + c // 4
                P.op("pe", lambda e, b=b, c=c, pbk=pbk: e.transpose(ps[pbk][:, (c % 4) * 128:(c % 4 + 1) * 128],
                                                                    OO[b][:, c * 128:(c + 1) * 128], ident_f[:, :]),
                     reads=[("OO", b), "ident_f"], writes=[PSK(pbk)])
            for c in range(8):
                pbk = 2 + 2 * b + c // 4
                stt(XC8[b][:, c, :], ps[pbk][:, (c % 4) * 128:(c % 4 + 1) * 128], modv(l, 5, c, s), XC8[b][:, c, :],
                    ALU.mult, ALU.add, ["mod", ("XC8", b)], [PSK(pbk), ("XC8", b)])
            sp_dma(outT_d[s, :, :, jt * 128:(jt + 1) * 128], XC8[b][:, :, :], [("XC8", b)], [("out", s, jt)])
        P.barrier()
        return None

    def moe_merged(l, seqs):
        I32 = mybir.dt.int32
        TSZ = 512
        NQ = TSZ // 128
        CAP = SEQ * len(seqs)
        NPASS = len(seqs)
        off = XBASE
        PS_ = {}
        for s in seqs:
            for nm, shp, dt in (("W1", [16, 1], F32), ("W2", [16, 1], F32), ("SI", [16, 2], I32)):
                PS_[(nm, s)], off = A(f"mm_{nm}{s}", shp, dt, off)
        NEacc, off = A("mm_NEacc", [8, 1], F32, off)
        JI, off = A("mm_JI", [8], I32, off)
        OV = off
        G = {}
        for nm, shp, dt in (("LT", [16, 8], F32), ("EQ1", [16, 8], F32), ("L2", [16, 8], F32), ("EQ2", [16, 8], F32),
                            ("M1", [16, 1], F32), ("M2", [16, 1], F32),
                            ("AB", [16, 8], BF16), ("PW", [16, 8], F32), ("TOT", [16, 8], F32), ("CS", [16, 8], F32),
                            ("EOFF", [16, 8], F32), ("TQ", [16, 8], F32), ("S12", [16, 2], F32),
                            ("THR", [8, 8], F32), ("CMP", [8, 8], F32), ("JF", [8], F32)):
            G[nm], off = A("mm_" + nm, shp, dt, off)
        LT, EQ1, L2, EQ2, M1, M2 = (G[k] for k in ("LT", "EQ1", "L2", "EQ2", "M1", "M2"))
        AB, PW, TOT, CS, EOFF, TQ, S12 = (G[k] for k in ("AB", "PW", "TOT", "CS", "EOFF", "TQ", "S12"))
        THR, CMP, JF = (G[k] for k in ("THR", "CMP", "JF"))
        LG, off = A("mm_LG", [512], F32, off)
        XL = []
        for i in range(2):
            t_, off = A(f"mm_XL{i}", [8, 512], F32, off)
            XL.append(t_)
        X = {}
        X["SQ"], off = A("mm_SQ", [8, 512], BF16, off)
        X["RS"], off = A("mm_RS", [512], F32, off)
        X["TMP8"], off = A("mm_TMP8", [8, 512], F32, off)
        NHB = 4
        HTok = []
        for i in range(NHB):
            t_, off = A(f"mm_HTok{i}", [D], BF16, off)
            HTok.append(t_)
        psb = [ps[i][:, :].bitcast(BF16) for i in range(8)]
        lat = [1, 2, 3, 4]
        P.op("dve", lambda e: e.memset(NEacc[:, :, :], 0.0), writes=["NEacc"])
        for ex in range(NEXP):
            P.op("dve", lambda e, ex=ex: e.memset(EOFF[:, :, ex], float(ex * CAP)), writes=["EOFF"])
            P.op("dve", lambda e, ex=ex: e.memset(THR[:, :, ex], float(ex * TSZ)), writes=["THR"])

        for s in seqs:
            W1, W2, SI = PS_[("W1", s)], PS_[("W2", s)], PS_[("SI", s)]

            def xsrc(ti, s=s):
                t0, w = TILES[ti]
                b = ti % 2
                sp_dma(XL[b][:, :, 0:w], xres_d[s, :, :, t0:t0 + w], [("xd", ti)], [("XL", b)])
                return (lambda c: XL[b][:, c, 0:w]), [("XL", b)]

            for c in range(8):
                mm(ps[2][0:8, 0:2], router[:, c, :], mod[:, l, 24 + c, s:s + 2], c == 0, c == 7, ["router", "mod"], PSK(2))
            act(routb[0:8, 0:2], ps[2][0:8, 0:2], AF.Identity, [], [PSK(2), "routb"])

            def hook(ti):
                t0, w = TILES[ti]
                for c in range(8):
                    mm(ps[2][0:8, 0:w], router[:, c, :], X["TMP8"][:, c, 0:w], c == 0, c == 7, ["router", ("TMP8", c)], PSK(2))
                act(LG[0:8, 0:w], ps[2][0:8, 0:w], AF.Identity, ["routb"], [PSK(2), "LG"], bias=routb[0:8, 0:1])
                for j in range(w // 128):
                    jt = (t0 - CTX) // 128 + j
                    P.op("pe", lambda e, jt=jt, j=j: e.transpose(ps[3][:, jt * 8:(jt + 1) * 8], LG[0:8, j * 128:(j + 1) * 128],
                                                                 ident_f[0:8, 0:8]),
                         reads=["LG", "ident_f"], writes=[PSK(3)])

            modulate(l, s, 1, lat, xsrc, X, moe=True, after_tile=hook)
            P.op("dve", lambda e: e.tensor_copy(LT[:, :, :], ps[3][:, 0:128].rearrange("p (a b) -> p a b", b=8)),
                 reads=[], writes=[PSK(3), "LT"])
            P.op("dve", lambda e: e.tensor_reduce(M1[:, :, 0], LT[:, :, :], AX.X, ALU.max), reads=["LT"], writes=["M1"])
            tt(EQ1[:, :, :], LT[:, :, :], M1[:, :, 0:1].to_broadcast([128, 16, 8]), ALU.is_equal, ["LT", "M1"], ["EQ1"])
            stt(L2[:, :, :], EQ1[:, :, :], -1.0e30, LT[:, :, :], ALU.mult, ALU.add, ["EQ1", "LT"], ["L2"])
            P.op("dve", lambda e: e.tensor_reduce(M2[:, :, 0], L2[:, :, :], AX.X, ALU.max), reads=["L2"], writes=["M2"])
            tt(EQ2[:, :, :], L2[:, :, :], M2[:, :, 0:1].to_broadcast([128, 16, 8]), ALU.is_equal, ["L2", "M2"], ["EQ2"])
            tt(W2[:, :, :], M2[:, :, :], M1[:, :, :], ALU.subtract, ["M1", "M2"], [("W2", s)])
            act(W2[:, :, :], W2[:, :, :], AF.Exp, [("W2", s)], [("W2", s)])
            ts(W1[:, :, :], W2[:, :, :], 1.0, None, ALU.add, None, [("W2", s)], [("W1", s)])
            P.op("dve", lambda e, W1=W1: e.reciprocal(W1[:, :, :], W1[:, :, :]), reads=[("W1", s)], writes=[("W1", s)])
            tt(W2[:, :, :], W2[:, :, :], W1[:, :, :], ALU.mult, [("W1", s), ("W2", s)], [("W2", s)])
            tt(TQ[:, :, :], EQ1[:, :, :], EQ2[:, :, :], ALU.add, ["EQ1", "EQ2"], ["TQ"])
            P.op("dve", lambda e: e.tensor_copy(AB[:, :, :], TQ[:, :, :]), reads=["TQ"], writes=["AB"])
            ABf = AB[:, :, :].rearrange("p a b -> p (a b)")
            mm(ps[0][:, 0:128], ltri_bf[:, :], ABf, True, True, ["AB", "ltri_bf"], PSK(0))
            mm(ps[0][:, 128:256], ones_bf[:, :], ABf, True, True, ["AB", "ones_bf"], PSK(0))
            P.op("dve", lambda e: e.tensor_copy(PW[:, :, :], ps[0][:, 0:128].rearrange("p (a b) -> p a b", b=8)),
                 reads=[], writes=[PSK(0), "PW"])
            P.op("dve", lambda e: e.tensor_copy(TOT[:, :, :], ps[0][:, 128:256].rearrange("p (a b) -> p a b", b=8)),
                 reads=[], writes=[PSK(0), "TOT"])
            P.op("dve", lambda e: e.tensor_copy(CS[:, 0, :], NEacc[:, :, 0]), reads=["NEacc"], writes=["CS"])
            for j in range(1, 16):
                tt(CS[:, j, :], CS[:, j - 1, :], TOT[:, j - 1, :], ALU.add, ["CS", "TOT"], ["CS"])
            tt(NEacc[:, :, 0], CS[:, 15, :], TOT[:, 15, :], ALU.add, ["CS", "TOT"], ["NEacc"])
            tt(PW[:, :, :], PW[:, :, :], CS[:, :, :], ALU.add, ["PW", "CS"], ["PW"])
            tt(PW[:, :, :], PW[:, :, :], EOFF[:, :, :], ALU.add, ["PW", "EOFF"], ["PW"])
            tt(TQ[:, :, :], EQ1[:, :, :], PW[:, :, :], ALU.mult, ["EQ1", "PW"], ["TQ"])
            P.op("dve", lambda e: e.tensor_reduce(S12[:, :, 0], TQ[:, :, :], AX.X, ALU.add), reads=["TQ"], writes=["S12"])
            tt(TQ[:, :, :], EQ2[:, :, :], PW[:, :, :], ALU.mult, ["EQ2", "PW", "S12"], ["TQ"])
            P.op("dve", lambda e: e.tensor_reduce(S12[:, :, 1], TQ[:, :, :], AX.X, ALU.add), reads=["TQ"], writes=["S12"])
            P.op("dve", lambda e, SI=SI: e.tensor_copy(SI[:, :, :], S12[:, :, :]), reads=["S12"], writes=[("SI", s)])
            for jt in range(16):
                b = jt % 2
                hb = jt % NHB
                ti = 1 + jt // 4
                for c in range(8):
                    P.op("pe", lambda e, b=b, c=c, jt=jt: e.transpose(psb[b][:, c * 128:(c + 1) * 128],
                                                                      HT[:, c, CTX + jt * 128:CTX + (jt + 1) * 128], ident_bf[:, :]),
                         reads=[("HT", ti), "ident_bf"], writes=[PSK(b)])
                act(HTok[hb][:, :], psb[b][:, :], AF.Identity, [], [PSK(b), ("HTok", hb)])
                for k in range(2):
                    P.dma("pool", lambda e, hb=hb, jt=jt, k=k, SI=SI: e.indirect_dma_start(
                        out=hg_d, out_offset=bass.IndirectOffsetOnAxis(ap=SI[:, jt, k:k + 1], axis=0), in_=HTok[hb][:, :],
                        in_offset=None), reads=[("HTok", hb), ("SI", s)] + [("hgz", q_) for q_ in range(NEXP * SEQ * 2 // 128)], writes=[("hg", s, jt, k)])
        tt(CMP[:, :, :], NEacc[:, :, 0:1].to_broadcast([128, 8, 8]), THR[:, :, :], ALU.is_gt, ["NEacc", "THR"], ["CMP"])
        P.op("dve", lambda e: e.tensor_reduce(JF[:, :], CMP[:, :, :], AX.X, ALU.add), reads=["CMP"], writes=["JF"])
        P.op("dve", lambda e: e.tensor_copy(JI[:, :], JF[:, :]), reads=["JF"], writes=["JI"])
        P.barrier()

        off = OV
        Yacc, off = A("mm_Yacc", [16, D], F32, off)
        HTg, off = A("mm_HTg", [8, SEQ], BF16, off)
        HGs = []
        for i in range(2):
            t_, off = A(f"mm_HGs{i}", [D], BF16, off)
            HGs.append(t_)
        SIL, ACTT = [], []
        for i in range(2):
            t_, off = A(f"mm_SIL{i}", [TSZ], BF16, off)
            SIL.append(t_)
            t_, off = A(f"mm_ACTT{i}", [4, TSZ], BF16, off)
            ACTT.append(t_)
        cnt = {"h": 0, "a": 0, "o": 0, "g": 0}
        allhg = [("hg", s_, a_, b_) for s_ in seqs for a_ in range(16) for b_ in range(2)]
        for ex in range(NEXP):
            P.load_reg(JI[0:1, ex:ex + 1], "JI", engines=("pe", "act", "dve", "sp", "pool"))
            for p_ in range(NPASS):
                jb = 4 * p_
                for k in range(16):
                    b = cnt["g"] % 2
                    cnt["g"] += 1
                    r0 = ex * CAP + p_ * SEQ + k * 128
                    thr = jb + k // NQ + 1
                    P.cond_begin(thr)
                    sp_dma(HGs[b][:, :], hg_d[r0:r0 + 128, :], allhg, [("HGs", b)])
                    for c in range(8):
                        P.op("pe", lambda e, b=b, c=c: e.transpose(psb[b][:, c * 128:(c + 1) * 128], HGs[b][:, c * 128:(c + 1) * 128],
                                                                   ident_bf[:, :]),
                             reads=[("HGs", b), "ident_bf"], writes=[PSK(b)])
                    act(HTg[:, :, k * 128:(k + 1) * 128], psb[b][:, :].rearrange("p (a b) -> p a b", b=128), AF.Identity, [],
                        [PSK(b), ("HTg", k // NQ)])
                    P.cond_end()
                for g in range(7):
                    P.cond_begin(jb + 1)
                    w1 = load_slab(("m1", ex, g), big=True)
                    w3 = load_slab(("m3", ex, g), big=True)
                    w2 = load_slab(("m2", ex, g), big=True)
                    P.cond_end()
                    for j in range(4):
                        P.cond_begin(jb + j + 1)
                        ab = cnt["a"] % 2
                        cnt["a"] += 1
                        s0 = j * TSZ
                        for jj in range(4):
                            b = cnt["h"] % 2
                            cnt["h"] += 1
                            b1, b3 = 2 + b, 4 + b
                            for k in range(8):
                                mm(ps[b1][:, 0:TSZ], wk(w1, k, jj * 128, 128), HTg[:, k, s0:s0 + TSZ], k == 0, k == 7,
                                   [("ws", w1), ("HTg", j)], PSK(b1))
                            for k in range(8):
                                mm(ps[b3][:, 0:TSZ], wk(w3, k, jj * 128, 128), HTg[:, k, s0:s0 + TSZ], k == 0, k == 7,
                                   [("ws", w3), ("HTg", j)], PSK(b3))
                            act(SIL[b][:, :], ps[b1][:, 0:TSZ], AF.Silu, [], [PSK(b1), ("SIL", b)])
                            tt(ACTT[ab][:, jj, :], ps[b3][:, 0:TSZ], SIL[b][:, :], ALU.mult, [("SIL", b)], [PSK(b3), ("ACTT", ab)])
                        for h2 in range(NQ):
                            kt = NQ * j + h2
                            for dh in range(2):
                                ob = 6 + cnt["o"] % 2
                                cnt["o"] += 1
                                for jj in range(4):
                                    mm(ps[ob][:, 0:512], ACTT[ab][:, jj, h2 * 128:(h2 + 1) * 128], wk2(w2, jj, dh * 512, 512),
                                       jj == 0, jj == 3, [("ws", w2), ("ACTT", ab)], PSK(ob))
                                ya = Yacc[:, kt, dh * 512:(dh + 1) * 512]
                                if g == 0:
                                    P.op("dve", lambda e, ya=ya, ob=ob: e.tensor_copy(ya, ps[ob][:, 0:512]), reads=[],
                                         writes=[PSK(ob), ("Yacc", kt)])
                                else:
                                    tt(ya, ps[ob][:, 0:512], ya, ALU.add, [("Yacc", kt)], [PSK(ob), ("Yacc", kt)])
                        P.cond_end()
                for k in range(16):
                    r0 = ex * CAP + p_ * SEQ + k * 128
                    P.cond_begin(jb + k // NQ + 1)
                    sp_dma(yb_d[r0:r0 + 128, :], Yacc[:, k, :], [("Yacc", k)], [("yb", ex, p_, k)])
                    P.cond_end()
        P.barrier()

        off = OV
        NCB = 4
        YA, YB2, OO, XC8 = [], [], [], []
        for i in range(NCB):
            t_, off = A(f"mm_YA{i}", [D], F32, off)
            YA.append(t_)
            t_, off = A(f"mm_YB2{i}", [D], F32, off)
            YB2.append(t_)
            t_, off = A(f"mm_OO{i}", [D], F32, off)
            OO.append(t_)
            t_, off = A(f"mm_XC8{i}", [8, 128], F32, off)
            XC8.append(t_)
        allyb = [("yb", a_, p_, b_) for a_ in range(NEXP) for p_ in range(NPASS) for b_ in range(16)]
        n_ = 0
        for s in seqs:
            W1, W2, SI = PS_[("W1", s)], PS_[("W2", s)], PS_[("SI", s)]
            for jt in range(16):
                b = n_ % NCB
                pb2 = n_ % 2
                n_ += 1
                ti = 1 + jt // 4
                c0 = CTX + jt * 128
                for k, dst, dk in ((0, YA, "YA"), (1, YB2, "YB2")):
                    P.dma("pool", lambda e, b=b, jt=jt, k=k, dst=dst, SI=SI: e.indirect_dma_start(
                        out=dst[b][:, :], out_offset=None, in_=yb_d,
                        in_offset=bass.IndirectOffsetOnAxis(ap=SI[:, jt, k:k + 1], axis=0)),
                        reads=allyb + [("SI", s)], writes=[(dk, b)])
                sp_dma(XC8[b][:, :, :], xres_d[s, :, :, c0:c0 + 128], [("xd", ti)], [("XC8", b)])
                ts(OO[b][:, :], YA[b][:, :], W1[:, jt, 0:1], None, ALU.mult, None, [("YA", b), ("W1", s)], [("OO", b)])
                stt(OO[b][:, :], YB2[b][:, :], W2[:, jt, 0:1], OO[b][:, :], ALU.mult, ALU.add, [("YB2", b), ("W2", s), ("OO", b)],
                    [("OO", b)])
                for c in range(8):
                    pbk = 2 + 2 * pb2 + c // 4
                    P.op("pe", lambda e, b=b, c=c, pbk=pbk: e.transpose(ps[pbk][:, (c % 4) * 128:(c % 4 + 1) * 128],
                                                                        OO[b][:, c * 128:(c + 1) * 128], ident_f[:, :]),
                         reads=[("OO", b), "ident_f"], writes=[PSK(pbk)])
                for c in range(8):
                    pbk = 2 + 2 * pb2 + c // 4
                    stt(XC8[b][:, c, :], ps[pbk][:, (c % 4) * 128:(c % 4 + 1) * 128], modv(l, 5, c, s), XC8[b][:, c, :],
                        ALU.mult, ALU.add, ["mod", ("XC8", b)], [PSK(pbk), ("XC8", b)])
                sp_dma(outT_d[s, :, :, jt * 128:(jt + 1) * 128], XC8[b][:, :, :], [("XC8", b)], [("out", s, jt)])
        P.barrier()
        return None

    result = None
    if stage in ("full", "seq1"):
        seqs = (1,) if stage == "seq1" else (0, 1)
        for s in seqs:
            state["x_in_res"] = False
            for l in range(2):
                token_mixer(l, s)
                if l == 1 and SPARSE_MOE:
                    if not MERGED_MOE:
                        ffn_moe_sparse(l, s)
                else:
                    ffn(l, s)
        if SPARSE_MOE and MERGED_MOE:
            moe_merged(1, seqs)
    elif si >= 1:
        result = token_mixer(0, 0)
        if result is None and stage in ("G0", "H0", "F1", "G1", "G1a", "H1"):
            result = ffn(0, 0)
            if result is None and stage in ("F1", "G1", "G1a", "H1"):
                result = token_mixer(1, 0)
                if result is None and stage in ("G1", "G1a", "H1"):
                    result = ffn_moe_sparse(1, 0) if SPARSE_MOE else ffn(1, 0)

    if debug is not None:
        if stage == "pro":
            sp_dma(dbg_d.rearrange("p (a b) -> p a b", b=4), mod[:, :, :, :].rearrange("p l a b -> p (l a) b"), ["mod"], ["dbg"])
        elif stage in ("F0", "H0", "F1"):
            sp_dma(dbg_d, xres_d[0], [("xd", t) for t in range(5)], ["dbg"])
        elif result == "done":
            pass
        elif result is not None:
            src_t, keys = result
            DT, _ = A("DT", [NT], F32, (A.limit - NT * 4 - 64) // 32 * 32)
            for c in range(8):
                act(DT[:, :], src_t[:, c, :], AF.Identity, keys, ["DT"])
                sp_dma(dbg_d[:, c, :], DT[:, :], ["DT"], ["dbg"])
    P.emit(nc)
    return nc


def kernel(**inputs):
    inp = {k: np.asarray(v, np.float32) for k, v in inputs.items()}
    shared = build_shared(inp)
    nc = build_program("full")
    in_maps = []
    for core in range(NCORES):
        m = dict(shared)
        m.update(build_core_inputs(inp, core))
        in_maps.append(m)
    res = run_bass_kernel_spmd(nc, in_maps, core_ids=list(range(NCORES)))
    out = np.empty((2 * NCORES, SEQ, D), np.float32)
    for core in range(NCORES):
        oT = np.asarray(res.results[core]["outT"])
        out[2 * core:2 * core + 2] = oT.transpose(0, 3, 2, 1).reshape(2, SEQ, D)
    return out
```

```python
import contextlib
import numpy as np
import concourse.bass as bass
import concourse.mybir as mybir
from concourse.bass_utils import run_bass_kernel_spmd

F32 = mybir.dt.float32
BF16 = mybir.dt.bfloat16
AF = mybir.ActivationFunctionType
ALU = mybir.AluOpType
AX = mybir.AxisListType

NCORES = 8
D = 1024
NCH = 8
CTX = 256
SEQ = 2048
NT = CTX + SEQ
TILES = [(0, 256), (256, 512), (768, 512), (1280, 512), (1792, 512)]
D_FF = 2816
D_FFE = 3584
NEXP = 8
EPS = 1e-6
NSLOT = 8
NRING = 5
SLAB = 4096
WCH = 16
SPARSE_MOE = True
MOE_TSZ = 512
JCLAMP = None
MERGED_MOE = True
ZERO_HG = True
SB_BASE = 16384 + 128


class Ins:
    __slots__ = ("eng", "fn", "reads", "writes", "dma", "deps", "sig", "semkey", "val", "slot", "cond")


class Prog:
    ENGS = ["pe", "act", "dve", "pool", "sp"]

    def __init__(self):
        self.ins = []
        self.cur_cond = None
        self.ncond = 0
        self.cond_thr = {}

    def op(self, eng, fn, reads=(), writes=()):
        i = Ins()
        i.eng, i.fn, i.reads, i.writes, i.dma = eng, fn, tuple(reads), tuple(writes), False
        i.sig, i.deps, i.semkey, i.val, i.slot = False, (), None, 0, 0
        i.cond = self.cur_cond
        self.ins.append(i)
        return i

    def cond_begin(self, thr):
        self.ncond += 1
        self.cur_cond = self.ncond
        self.cond_thr[self.ncond] = thr

    def cond_end(self):
        self.cur_cond = None

    def load_reg(self, ap, key, engines=("pe", "act", "dve")):
        for e in engines:
            self.op(e, ("REGLOAD", ap), reads=[key])

    def dma(self, q, fn, reads=(), writes=()):
        i = self.op(q, fn, reads, writes)
        i.dma = True
        return i

    def barrier(self):
        for e in ("pe", "act", "dve", "sp"):
            self.op(e, lambda en: en.nop(), writes=("_bar",))

    def resolve(self):
        last_w = {}
        readers = {}
        ndma = {e: 0 for e in self.ENGS}
        slot_last = {e: {} for e in self.ENGS}
        for idx, I in enumerate(self.ins):
            reads = I.reads
            if I.eng != "pool" and "_bar" not in I.writes:
                reads = reads + ("_bar",)
            deps = {}
            for k in reads:
                j = last_w.get(k)
                if j is not None:
                    deps[j] = True
            for k in I.writes:
                j = last_w.get(k)
                if j is not None:
                    deps.setdefault(j, False)
                r = readers.get(k)
                if r:
                    for j2 in r[0].values():
                        deps.setdefault(j2, False)
                    for j2 in r[1]:
                        deps.setdefault(j2, False)
            final = []
            for j, raw in deps.items():
                J = self.ins[j]
                if J.dma:
                    final.append(j)
                elif J.eng == I.eng:
                    if I.dma or (raw and I.eng != "pe"):
                        final.append(j)
                else:
                    final.append(j)
            if I.dma:
                q = I.eng
                slot = ndma[q] % NSLOT
                prev = slot_last[q].get(slot)
                if prev is not None:
                    final.append(prev)
                slot_last[q][slot] = idx
                I.slot = slot
                ndma[q] += 1
            I.deps = final
            for j in final:
                self.ins[j].sig = True
            for k in reads:
                r = readers.setdefault(k, ({}, []))
                if I.dma:
                    r[1].append(idx)
                else:
                    r[0][I.eng] = idx
            for k in I.writes:
                last_w[k] = idx
                readers[k] = ({}, [])
        cnt = {e: 0 for e in self.ENGS}
        dcnt = {}
        for I in self.ins:
            if I.dma:
                key = ("d", I.eng, I.slot)
                dcnt[key] = dcnt.get(key, 0) + 16
                I.semkey, I.val = key, dcnt[key]
            elif I.sig:
                cnt[I.eng] += 1
                I.semkey, I.val = ("e", I.eng), cnt[I.eng]
        self.final_dma = dict(dcnt)

    def emit(self, nc, final_waits_on="sp"):
        self.resolve()
        keys = [("e", e) for e in self.ENGS]
        for e in self.ENGS:
            if any(I.dma and I.eng == e for I in self.ins):
                keys += [("d", e, s) for s in range(NSLOT)]
        with contextlib.ExitStack() as st:
            sems = {}
            for k in keys:
                sems[k] = st.enter_context(nc.semaphore("s_" + "_".join(str(x) for x in k)))
            block = st.enter_context(nc.Block())
            per = {e: [I for I in self.ins if I.eng == e] for e in self.ENGS}

            def replay(ename, eng):
                seen = {}
                reg = {}

                def do_waits(I, seen, only_external=None):
                    waits = {}
                    for j in I.deps:
                        J = self.ins[j]
                        if only_external is not None and J.cond == only_external:
                            continue
                        if waits.get(J.semkey, 0) < J.val:
                            waits[J.semkey] = J.val
                    for sk, v in waits.items():
                        if seen.get(sk, 0) < v:
                            eng.wait_ge(sems[sk], v)
                            seen[sk] = v

                def run(I):
                    if isinstance(I.fn, tuple):
                        if "r" not in reg:
                            reg["r"] = eng.alloc_register("rj_" + ename)
                        r = eng.reg_load(reg["r"], I.fn[1])
                    else:
                        r = I.fn(eng)
                    if I.dma:
                        r.then_inc(sems[I.semkey], 16)
                    elif I.sig:
                        r.then_inc(sems[I.semkey], 1)

                lst = per[ename]
                n = len(lst)
                p = 0
                while p < n:
                    I = lst[p]
                    if I.cond is None:
                        do_waits(I, seen)
                        run(I)
                        p += 1
                        continue
                    cid = I.cond
                    q = p
                    while q < n and lst[q].cond == cid:
                        q += 1
                    body = lst[p:q]
                    for B in body:
                        do_waits(B, seen, only_external=cid)
                    snap = dict(seen)
                    k = sum(1 for B in body if B.sig and not B.dma)
                    with eng.If_lt(reg["r"], self.cond_thr[cid]):
                        if k > 0:
                            eng.drain().then_inc(sems[("e", ename)], k)
                        for B in body:
                            if B.dma:
                                eng.nop().then_inc(sems[B.semkey], 16)
                        if k == 0 and not any(B.dma for B in body):
                            eng.nop()
                    with eng.Else():
                        inner = dict(snap)
                        for B in body:
                            do_waits(B, inner)
                            run(B)
                    seen = snap
                    p = q
                if ename == final_waits_on:
                    for sk, v in self.final_dma.items():
                        if seen.get(sk, 0) < v:
                            eng.wait_ge(sems[sk], v)

            @block.tensor
            def _(e):
                replay("pe", e)

            @block.scalar
            def _(e):
                replay("act", e)

            @block.vector
            def _(e):
                replay("dve", e)

            @block.gpsimd
            def _(e):
                replay("pool", e)

            @block.sync
            def _(e):
                replay("sp", e)


def na_plan():
    pats = []
    groups = {}
    plan = []
    for qp in range(16):
        r0 = 2 * qp
        s = [min(max(r - 4, 0), 24) for r in (r0, r0 + 1)]
        first = s[0] // 2
        last = (s[1] + 7) // 2
        keys = []
        for m in range(first, last + 1):
            key = []
            for kl in range(2):
                kr = 2 * m + kl
                for ql in range(2):
                    r = r0 + ql
                    valid = s[ql] <= kr < s[ql] + 8
                    key.append(kr - r + 7 if valid else None)
            keys.append(tuple(key))
        gk = tuple(keys)
        if gk not in groups:
            groups[gk] = len(pats)
            pats.extend(keys)
        base = groups[gk]
        plan.append([(first + i, base + i) for i in range(len(keys))])
    return pats, plan


NA_PATS, NA_PLAN = na_plan()
NPAT = len(NA_PATS)


def slab_order():
    pro = [("mod", l, g) for l in range(2) for g in range(12)]
    seq = []
    for l in range(2):
        ntile = 5 if l == 0 else 4
        seq += [("rnn", l, i) for i in range(4)]
        for g in range(2):
            seq += [("rnno", l, g), ("gr", l, g)]
        seq += [("kvq", l, c) for c in range(8)]
        for t in range(ntile):
            for g in range(2):
                seq += [("nao", l, g), ("gn", l, g)]
            for g in range(2):
                seq += [("out", l, g)]
        if l == 0:
            for g in range(6):
                seq += [("f1", g), ("f3", g), ("f2", g)]
        else:
            for e in range(NEXP):
                for g in range(7):
                    seq += [("m1", e, g), ("m3", e, g), ("m2", e, g)]
    return pro, seq


def unique_slabs():
    pro, seq = slab_order()
    names = []
    seen = set()
    for n in pro + seq:
        if n not in seen:
            seen.add(n)
            names.append(n)
    return names


SLAB_NAMES = unique_slabs()
SLAB_IDX = {n: i for i, n in enumerate(SLAB_NAMES)}


def _slab_cols(W, cols):
    S = W[:, cols]
    return np.ascontiguousarray(S.reshape(8, 128, 512).transpose(1, 0, 2)).reshape(128, SLAB)


def _slab_rows(W2, r0):
    blk = np.zeros((512, 1024), np.float32)
    n = max(0, min(512, W2.shape[0] - r0))
    blk[:n] = W2[r0:r0 + n]
    return np.ascontiguousarray(blk.reshape(4, 128, 1024).transpose(1, 0, 2)).reshape(128, SLAB)


def _cols_pad(W, c0, n):
    idx = np.full(512, c0, np.int64)
    idx[:n] = np.arange(c0, c0 + n)
    return idx


def build_wstream(inp):
    ar = np.arange
    out = np.empty((len(SLAB_NAMES), 128, SLAB), np.float32)
    for i, nm in enumerate(SLAB_NAMES):
        k = nm[0]
        if k == "mod":
            _, l, g = nm
            out[i] = _slab_cols(inp["w_mod"][l], ar(g * 512, g * 512 + 512))
        elif k == "rnn":
            _, l, j = nm
            cols = np.concatenate([ar(c * 128, c * 128 + 128) if which == 0 else ar(3072 + c * 128, 3072 + c * 128 + 128)
                                   for c in (2 * j, 2 * j + 1) for which in (0, 1)])
            out[i] = _slab_cols(inp["w_in"][l], cols)
        elif k == "rnno":
            _, l, g = nm
            out[i] = _slab_cols(inp["w_rnn_o"][l], ar(g * 512, g * 512 + 512))
        elif k == "gr":
            _, l, g = nm
            out[i] = _slab_cols(inp["w_in"][l], ar(5120 + g * 512, 5120 + g * 512 + 512))
        elif k == "kvq":
            _, l, c = nm
            cols = np.concatenate([ar(1024 + c * 128, 1024 + c * 128 + 128), ar(2048 + c * 128, 2048 + c * 128 + 128),
                                   ar(4096 + c * 128, 4096 + c * 128 + 128), ar(4096 + c * 128, 4096 + c * 128 + 128)])
            out[i] = _slab_cols(inp["w_in"][l], cols)
        elif k == "nao":
            _, l, g = nm
            out[i] = _slab_cols(inp["w_na_o"][l], ar(g * 512, g * 512 + 512))
        elif k == "gn":
            _, l, g = nm
            out[i] = _slab_cols(inp["w_in"][l], ar(6144 + g * 512, 6144 + g * 512 + 512))
        elif k == "out":
            _, l, g = nm
            out[i] = _slab_cols(inp["w_out"][l], ar(g * 512, g * 512 + 512))
        elif k in ("f1", "f3"):
            _, g = nm
            W = inp["ffn_w1"][0] if k == "f1" else inp["ffn_w3"][0]
            n = min(512, D_FF - g * 512)
            out[i] = _slab_cols(W, _cols_pad(W, g * 512, n))
        elif k == "f2":
            _, g = nm
            out[i] = _slab_rows(inp["ffn_w2"][0], g * 512)
        elif k in ("m1", "m3"):
            _, e, g = nm
            W = inp["moe_w1"][0][e] if k == "m1" else inp["moe_w3"][0][e]
            out[i] = _slab_cols(W, ar(g * 512, g * 512 + 512))
        elif k == "m2":
            _, e, g = nm
            out[i] = _slab_rows(inp["moe_w2"][0][e], g * 512)
        else:
            raise KeyError(nm)
    return out


def _pm(v):
    v = np.asarray(v, np.float32)
    lead = v.shape[:-1]
    return np.ascontiguousarray(np.moveaxis(v.reshape(*lead, 8, 128), -1, 0))


def build_shared(inp):
    sh = {}
    wsr = build_wstream(inp)
    for i in range((len(SLAB_NAMES) + WCH - 1) // WCH):
        sh[f"wstream{i}"] = wsr[i * WCH:(i + 1) * WCH]
    sh["bmodT"] = np.ascontiguousarray(np.moveaxis(inp["b_mod"].reshape(2, 48, 128), -1, 0))
    sh["convw"] = np.ascontiguousarray(np.moveaxis(inp["conv_w"].reshape(2, 4, 8, 128), -1, 0).transpose(0, 1, 3, 2))
    sh["convb"] = _pm(inp["conv_b"])
    sh["lam"] = _pm(inp["rg_lambda"])
    sh["rgb"] = _pm(inp["rg_b"])
    g = np.stack([inp["q_gain"], inp["k_gain"]], 1)
    sh["gains"] = np.ascontiguousarray(np.concatenate([g, g], -1).transpose(2, 0, 1))
    rgw = inp["rg_w"]
    bd = np.zeros((2, 128, 2, 2, 8, 128), np.float32)
    for hb in range(2):
        blk = rgw[:, :, :, hb::2]
        bd[:, hb * 64:(hb + 1) * 64, :, :, :, hb * 64:(hb + 1) * 64] = np.moveaxis(blk, 4, 1)
    sh["rgw"] = bd.reshape(2, 128, 32 * 128)
    kp = np.arange(128)
    kl, kc = kp // 64, kp % 64
    ql, qc = kp // 64, kp % 64
    wstart = np.clip(qc - 8, 0, 48)
    colv = (kc[:, None] >= wstart[None, :]) & (kc[:, None] < wstart[None, :] + 16)
    coff = np.clip(kc[:, None] - qc[None, :] + 15, 0, 30)
    bias = np.zeros((2, 8, 128, 2, NPAT, 128), np.float32)
    mask = np.zeros((128, NPAT, 128), np.float32)
    rpb = inp["rpb"]
    for pc, key in enumerate(NA_PATS):
        drm = np.full((128, 128), -1, np.int64)
        for a in range(2):
            for b in range(2):
                dr = key[a * 2 + b]
                if dr is not None:
                    sel = (kl[:, None] == a) & (ql[None, :] == b)
                    drm[sel] = dr
        valid = (drm >= 0) & colv
        mask[:, pc, :] = valid
        drc = np.where(drm >= 0, drm, 0)
        gathered = rpb[:, :, drc, coff]
        gathered = np.where(valid[None, None], gathered, np.float32(0))
        bias[:, :, :, :, pc, :] = gathered.reshape(2, 8, 2, 128, 128).transpose(0, 1, 3, 2, 4)
    sh["biasG"] = bias.reshape(2, 8, 128, 2 * NPAT * 128)
    sh["maskG"] = mask.reshape(128, NPAT * 128)
    sh["router"] = np.ascontiguousarray(inp["router"][0].reshape(8, 128, 8).transpose(1, 0, 2)).reshape(128, 64)
    sh["ident"] = np.eye(128, dtype=np.float32)
    sh["ltri"] = np.triu(np.ones((128, 128), np.float32), 1)
    return sh


def build_core_inputs(inp, core):
    b0 = 2 * core
    toks = np.concatenate([inp["ctx"][b0:b0 + 2], inp["x"][b0:b0 + 2]], axis=1)
    xT = np.ascontiguousarray(toks.reshape(2, NT, 8, 128).transpose(0, 3, 2, 1))
    cv = np.zeros((4, 1024), np.float32)
    cv[0:2] = inp["c"][b0:b0 + 2]
    cv[2] = inp["c_ctx"]
    scT = np.ascontiguousarray(cv.reshape(4, 8, 128).transpose(2, 1, 0))
    return {"xT": xT, "scT": scT}


def _nbytes(dt):
    return 2 if dt == BF16 else 4


class SBAlloc:
    def __init__(self, nc, limit):
        self.nc, self.off, self.limit, self.n = nc, SB_BASE, SB_BASE + limit - 256, 0

    def __call__(self, name, free_shape, dt, off=None):
        size = int(np.prod(free_shape)) * _nbytes(dt)
        size = (size + 31) // 32 * 32
        if off is None:
            off = self.off
            self.off += size
        assert off + size <= self.limit, (name, off, size, self.limit)
        self.n += 1
        return self.nc.alloc_sbuf_tensor_at(f"{name}_{self.n}", [128] + list(free_shape), dt, offset=off), off + size


def build_program(stage="full", debug=None, nslabs=None):
    nc = bass.Bass("TRN2", target_bir_lowering=False)
    P = Prog()
    limit = nc.sbuf_bytes_remaining
    A = SBAlloc(nc, limit)

    def din(name, shape):
        return nc.dram_tensor(name, list(shape), F32, kind="ExternalInput").ap()

    xT_d = din("xT", [2, 128, 8, NT])
    scT_d = din("scT", [128, 8, 4])
    nsl = nslabs or len(SLAB_NAMES)
    ws_d = [din(f"wstream{i}", [min(WCH, nsl - i * WCH), 128, SLAB]) for i in range((nsl + WCH - 1) // WCH)]
    bmod_d = din("bmodT", [128, 2, 48])
    convw_d = din("convw", [128, 2, 8, 4])
    convb_d = din("convb", [128, 2, 8])
    lam_d = din("lam", [128, 2, 2, 8])
    rgb_d = din("rgb", [128, 2, 2, 2, 8])
    gains_d = din("gains", [128, 2, 2])
    rgw_d = din("rgw", [2, 128, 32 * 128])
    biasG_d = din("biasG", [2, 8, 128, 2 * NPAT * 128])
    maskG_d = din("maskG", [128, NPAT * 128])
    router_d = din("router", [128, 64])
    ident_d = din("ident", [128, 128])
    ltri_d = din("ltri", [128, 128])
    outT_d = nc.dram_tensor("outT", [2, 128, 8, SEQ], F32, kind="ExternalOutput").ap()
    xres_d = nc.dram_tensor("xres", [2, 128, 8, NT], F32, kind="Internal").ap()
    mrnn_d = nc.dram_tensor("mrnn", [128, 8, NT], F32, kind="Internal").ap()
    hg_d = nc.dram_tensor("hg", [NEXP * SEQ * 2, D], BF16, kind="Internal").ap()
    yb_d = nc.dram_tensor("yb", [NEXP * SEQ * 2, D], F32, kind="Internal").ap()
    dbg_d = None
    if debug is not None:
        dbg_d = nc.dram_tensor("dbg", list(debug), F32, kind="ExternalOutput").ap()

    ones_bf, _ = A("ones_bf", [128], BF16)
    blk_bf, _ = A("blk_bf", [128], BF16)
    onesV, _ = A("onesV", [192], BF16)
    ident_f, _ = A("ident_f", [128], F32)
    ones_f, _ = A("ones_f", [128], F32)
    ident_bf, _ = A("ident_bf", [128], BF16)
    ltri_bf, _ = A("ltri_bf", [128], BF16)
    ZT, _ = A("ZT", [D], BF16)
    scT, _ = A("scT", [8, 4], F32)
    scb, _ = A("scb", [8, 4], BF16)
    bmodT, _ = A("bmodT", [2, 48], F32)
    mod, _ = A("mod", [2, 48, 4], F32)
    convw, _ = A("convw", [2, 8, 4], F32)
    convb, _ = A("convb", [2, 8], F32)
    lam, _ = A("lam", [2, 2, 8], F32)
    c1, _ = A("c1", [2, 2, 8], F32)
    c2, _ = A("c2", [2, 2, 8], F32)
    rgb, _ = A("rgb", [2, 2, 2, 8], F32)
    gains, _ = A("gains", [2, 2], F32)
    qg, _ = A("qg", [2], F32)
    rgw, _ = A("rgw", [32, 128], BF16)
    maskG, _ = A("maskG", [NPAT, 128], BF16)
    router, _ = A("router", [8, 8], F32)
    routb, _ = A("routb", [2], F32)
    WS = [A(f"ws{i}", [SLAB], BF16)[0] for i in range(NRING)]
    HT_OFF = A.off
    HT, _ = A("HT", [8, NT], BF16)
    NXR = 4
    for i_ in range(NXR):
        WS.append(A(f"wsx{i_}", [SLAB], BF16, HT_OFF + i_ * SLAB * 2)[0])
    XBASE = A.off

    ps = [nc.alloc_psum_tensor(f"ps{i}", [128, 512], F32) for i in range(8)]

    def PSK(i):
        return ("ps", i)

    ring = {"n": 0}

    def load_slab(name, big=False):
        i = ring["n"] % (NRING + NXR if big else NRING)
        ring["n"] += 1
        src = ws_d[SLAB_IDX[name] // WCH][SLAB_IDX[name] % WCH]
        dst = WS[i]
        wr = [("ws", i)] + ([("HT", t_) for t_ in range(5)] if i >= NRING else [])
        P.dma("pool", lambda e, dst=dst, src=src: e.dma_start(out=dst[:, :], in_=src), writes=wr)
        return i

    def wk(i, k, c0, n):
        return WS[i][:, k * 512 + c0: k * 512 + c0 + n]

    def wk2(i, j, c0, n):
        return WS[i][:, j * 1024 + c0: j * 1024 + c0 + n]

    def mm(out, lhsT, rhs, start, stop, reads, pk):
        P.op("pe", lambda e: e.matmul(out, lhsT, rhs, start=start, stop=stop), reads=reads, writes=[pk])

    def act(out, in_, func, reads, writes, bias=None, scale=None):
        kw = {}
        if bias is not None:
            kw["bias"] = bias
        if scale is not None:
            kw["scale"] = scale
        P.op("act", lambda e: e.activation(out, in_, func, **kw), reads=reads, writes=writes)

    def tt(out, in0, in1, op, reads, writes, eng="dve"):
        P.op(eng, lambda e: e.tensor_tensor(out, in0, in1, op), reads=reads, writes=writes)

    def ts(out, in0, s1, s2, op0, op1, reads, writes, eng="dve"):
        if s2 is None:
            P.op(eng, lambda e: e.tensor_scalar(out, in0, s1, None, op0), reads=reads, writes=writes)
        else:
            P.op(eng, lambda e: e.tensor_scalar(out, in0, s1, s2, op0, op1), reads=reads, writes=writes)

    def stt(out, in0, scalar, in1, op0, op1, reads, writes):
        P.op("dve", lambda e: e.scalar_tensor_tensor(out, in0, scalar, in1, op0, op1), reads=reads, writes=writes)

    def sp_dma(out, in_, reads, writes):
        P.dma("sp", lambda e: e.dma_start(out=out, in_=in_), reads=reads, writes=writes)

    def pool_dma(out, in_, reads, writes):
        P.dma("pool", lambda e: e.dma_start(out=out, in_=in_), reads=reads, writes=writes)

    P.op("dve", lambda e: e.memset(ones_bf[:, :], 1.0), writes=["ones_bf"])
    P.op("dve", lambda e: e.memset(ones_f[:, :], 1.0), writes=["ones_f"])
    P.op("dve", lambda e: e.memset(blk_bf[:, :], 0.0), writes=["blk_bf"])
    P.op("dve", lambda e: e.memset(blk_bf[0:64, 0:64], 1.0), writes=["blk_bf"])
    P.op("dve", lambda e: e.memset(blk_bf[64:128, 64:128], 1.0), writes=["blk_bf"])
    P.op("dve", lambda e: e.memset(onesV[:, :], 1.0), writes=["onesV"])
    P.op("dve", lambda e: e.memset(onesV[:, 64:128], 0.0), writes=["onesV"])
    sp_dma(ident_f[:, :], ident_d, [], ["ident_f"])
    pool_dma(ident_bf[:, :], ident_d, [], ["ident_bf"])
    pool_dma(ltri_bf[:, :], ltri_d, [], ["ltri_bf"])
    sp_dma(scT[:, :, :], scT_d, [], ["scT"])
    sp_dma(bmodT[:, :, :], bmod_d, [], ["bmodT"])
    sp_dma(convw[:, :, :, :], convw_d, [], ["convw"])
    sp_dma(convb[:, :, :], convb_d, [], ["convb"])
    sp_dma(lam[:, :, :, :], lam_d, [], ["lam"])
    sp_dma(rgb[:, :, :, :, :], rgb_d, [], ["rgb"])
    sp_dma(gains[:, :, :], gains_d, [], ["gains"])
    sp_dma(router[:, :, :], router_d.rearrange("p (k e) -> p k e", e=8), [], ["router"])
    pool_dma(maskG[:, :, :], maskG_d.rearrange("p (a b) -> p a b", b=128), [], ["maskG"])
    P.op("dve", lambda e: e.memset(ZT[:, :], 0.0), writes=["ZT"])

    def zero_fill_hg():
        for blk in range(NEXP * SEQ * 2 // 128):
            sp_dma(hg_d[blk * 128:(blk + 1) * 128, :], ZT[:, :], ["ZT"], [("hgz", blk)])
    act(c1[:, :, :, :], lam[:, :, :, :], AF.Exp, ["lam"], ["c1"], scale=-1.0)
    act(c1[:, :, :, :], c1[:, :, :, :], AF.Ln, ["c1"], ["c1"], bias=1.0)
    ts(c2[:, :, :, :], c1[:, :, :, :], -16.0, None, ALU.mult, None, ["c1"], ["c2"])
    ts(c1[:, :, :, :], c1[:, :, :, :], -8.0, None, ALU.mult, None, ["c1", "c2"], ["c1"])
    act(scb[:, :, :], scT[:, :, :], AF.Silu, ["scT"], ["scb"])
    for l in range(2):
        for g in range(12):
            i = load_slab(("mod", l, g))
            for j in range(4):
                col = (g * 4 + j) * 4
                for k in range(8):
                    mm(ps[0][:, col:col + 4], wk(i, k, j * 128, 128), scb[:, k, :], k == 0, k == 7,
                       [("ws", i), "scb"], PSK(0))
        pv = ps[0][:, 0:192].rearrange("p (a b) -> p a b", b=4)
        for j in range(3):
            tt(mod[:, l, :, j], pv[:, :, j], bmodT[:, l, :], ALU.add, ["bmodT"], [PSK(0), "mod"])
    for l in range(2):
        for m in (1, 4):
            ts(mod[:, l, m * 8:(m + 1) * 8, :], mod[:, l, m * 8:(m + 1) * 8, :], 1.0, None, ALU.add, None, ["mod"], ["mod"])

    def modv(l, m, c, col):
        return mod[:, l, m * 8 + c, col:col + 1]

    stages = ["pro", "A0", "B0", "C0", "D0", "E0", "F0", "G0", "H0", "F1", "G1", "G1a", "H1", "seq1", "full"]
    si = stages.index(stage)

    state = {"x_in_res": False}

    def modulate(l, s, which, tiles, xsrc, X, moe=False, after_tile=None):
        m_sh, m_sc = (0, 1) if which == 0 else (3, 4)
        for ti in tiles:
            t0, w = TILES[ti]
            col = 2 if ti == 0 else s
            xap, xkeys = xsrc(ti)
            SQ, RS = X["SQ"], X["RS"]
            for c in range(8):
                act(SQ[:, c, 0:w], xap(c), AF.Square, xkeys, [("SQ", c)])
            for c in range(8):
                mm(ps[1][:, 0:w], ones_bf[:, :], SQ[:, c, 0:w], c == 0, c == 7, [("SQ", c), "ones_bf"], PSK(1))
            act(RS[:, 0:w], ps[1][:, 0:w], AF.Sqrt, [], [PSK(1), "RS"], bias=EPS, scale=1.0 / D)
            P.op("dve", lambda e, w=w: e.reciprocal(RS[:, 0:w], RS[:, 0:w]), reads=["RS"], writes=["RS"])
            for c in range(8):
                if moe:
                    tmp, tk = X["TMP8"][:, c, 0:w], ("TMP8", c)
                else:
                    tmp, tk = X["TMP"][c % 2][:, 0:w], ("TMP", c % 2)
                stt(tmp, xap(c), modv(l, m_sc, c, col), RS[:, 0:w], ALU.mult, ALU.mult, list(xkeys) + ["mod", "RS"], [tk])
                act(HT[:, c, t0:t0 + w], tmp, AF.Identity, [tk, "mod"], [("HT", ti)], bias=modv(l, m_sh, c, col))
            if after_tile is not None:
                after_tile(ti)

    def token_mixer(l, s):
        ctx_out = (l == 0)
        tiles = [0, 1, 2, 3, 4]
        otiles = tiles if ctx_out else [1, 2, 3, 4]
        off = XBASE
        YB, off = A("YB", [8, NT], BF16, off)
        TB = off
        pool_dma(rgw[:, :, :], rgw_d[l].rearrange("p (a b) -> p a b", b=128), [], ["rgw"])
        ts(qg[:, 0:1], gains[:, l, 0:1], 0.125, None, ALU.mult, None, ["gains"], ["qg"])

        off = TB
        XL = []
        for i in range(2):
            t_, off = A(f"XL{i}", [8, 512], F32, off)
            XL.append(t_)
        X = {}
        X["SQ"], off = A("SQ", [8, 512], BF16, off)
        X["RS"], off = A("RS", [512], F32, off)
        X["TMP"] = []
        for i in range(2):
            t_, off = A(f"TMP{i}", [512], F32, off)
            X["TMP"].append(t_)
        xd = xres_d if state["x_in_res"] else xT_d

        def xsrc(ti):
            t0, w = TILES[ti]
            b = ti % 2
            sp_dma(XL[b][:, :, 0:w], xd[s, :, :, t0:t0 + w], [("xd", ti)], [("XL", b)])
            return (lambda c: XL[b][:, c, 0:w]), [("XL", b)]

        modulate(l, s, 0, tiles, xsrc, X)
        P.barrier()
        if stage == "A0":
            return HT, [("HT", t) for t in range(5)]

        off = TB
        S0, off = A("S0", [2312], F32, off)
        XC, off = A("XC", [NT], F32, off)
        S2, off = A("S2", [NT], F32, off)
        S3, off = A("S3", [NT], F32, off)
        HF, off = A("HF", [NT], F32, off)
        HR, off = A("HR", [NT], F32, off)
        XCb, off = A("XCb", [NT], BF16, off)
        GT, off = A("GT", [512], F32, off)

        def xrp_pos(t0):
            return 2 + t0 if t0 < CTX else 261 + (t0 - CTX)

        for c in range(8):
            if c % 2 == 0:
                wsi = load_slab(("rnn", l, c // 2))
            cb = (c % 2) * 256
            for a, b in ((0, 2), (258, 261), (2309, 2312)):
                P.op("dve", lambda e, a=a, b=b: e.memset(S0[:, a:b], 0.0), writes=["S0"])
            for ti in tiles:
                t0, w = TILES[ti]
                pb = 2 + (ti % 2)
                for k in range(8):
                    mm(ps[pb][:, 0:w], wk(wsi, k, cb, 128), HT[:, k, t0:t0 + w], k == 0, k == 7,
                       [("ws", wsi), ("HT", ti)], PSK(pb))
                p0 = xrp_pos(t0)
                act(S0[:, p0:p0 + w], ps[pb][:, 0:w], AF.Identity, [], [PSK(pb), "S0"])
            for (d0, n, base) in ((0, CTX, 2), (CTX, SEQ, 261)):
                ts(XC[:, d0:d0 + n], S0[:, base - 2:base - 2 + n], convw[:, l, c, 0:1], convb[:, l, c:c + 1],
                   ALU.mult, ALU.add, ["S0", "convw", "convb"], [("XC", d0)])
                for j in range(1, 4):
                    stt(XC[:, d0:d0 + n], S0[:, base - 2 + j:base - 2 + j + n], convw[:, l, c, j:j + 1], XC[:, d0:d0 + n],
                        ALU.mult, ALU.add, ["S0", "convw", ("XC", d0)], [("XC", d0)])
            act(XCb[:, :], XC[:, :], AF.Identity, [("XC", 0), ("XC", CTX)], ["XCb"])
            for dr in range(2):
                for ti in tiles:
                    t0, w = TILES[ti]
                    for gt in range(2):
                        pb = 4 + gt + 2 * (ti % 2)
                        mm(ps[pb][:, 0:w], rgw[:, (dr * 2 + gt) * 8 + c, :], XCb[:, t0:t0 + w], True, True,
                           ["rgw", "XCb"], PSK(pb))
                        dst = S2 if gt == 0 else S3
                        act(dst[:, t0:t0 + w], ps[pb][:, 0:w], AF.Sigmoid, ["rgb"], [PSK(pb), ("S2" if gt == 0 else "S3")],
                            bias=rgb[:, l, dr, gt, c:c + 1])
                act(S0[:, 0:NT], S2[:, :], AF.Exp, ["S2", "c1"], ["S0"], scale=c1[:, l, dr, c:c + 1])
                act(S2[:, :], S2[:, :], AF.Exp, ["S2", "c2"], ["S2"], scale=c2[:, l, dr, c:c + 1])
                act(S2[:, :], S2[:, :], AF.Sqrt, ["S2"], ["S2"], scale=-1.0, bias=1.0)
                tt(S3[:, :], S3[:, :], XC[:, :], ALU.mult, ["S3", ("XC", 0), ("XC", CTX)], ["S3"])
                tt(S3[:, :], S3[:, :], S2[:, :], ALU.mult, ["S3", "S2"], ["S3"])
                if dr == 0:
                    P.op("dve", lambda e: e.tensor_tensor_scan(HF[:, :], S0[:, 0:NT], S3[:, :], 0.0, ALU.mult, ALU.add),
                         reads=["S0", "S3"], writes=["HF"])
                else:
                    P.op("dve", lambda e: e.tensor_tensor_scan(HR[:, CTX - 1::-1], S0[:, CTX - 1::-1], S3[:, CTX - 1::-1], 0.0,
                                                               ALU.mult, ALU.add),
                         reads=["S0", "S3"], writes=["HR"])
                    P.op("dve", lambda e: e.tensor_tensor_scan(HR[:, NT - 1:CTX - 1:-1], S0[:, NT - 1:CTX - 1:-1],
                                                               S3[:, NT - 1:CTX - 1:-1], HR[:, 0:1], ALU.mult, ALU.add),
                         reads=["S0", "S3", "HR"], writes=["HR"])
            tt(HF[:, :], HF[:, :], HR[:, :], ALU.add, ["HF", "HR"], ["HF"])
            for ti in otiles:
                t0, w = TILES[ti]
                pb = 2 + (ti % 2)
                for k in range(8):
                    mm(ps[pb][:, 0:w], wk(wsi, k, cb + 128, 128), HT[:, k, t0:t0 + w], k == 0, k == 7,
                       [("ws", wsi), ("HT", ti)], PSK(pb))
                act(GT[:, 0:w], ps[pb][:, 0:w], AF.Gelu_apprx_tanh, [], [PSK(pb), "GT"])
                tt(YB[:, c, t0:t0 + w], HF[:, t0:t0 + w], GT[:, 0:w], ALU.mult, ["HF", "GT"], [("YB", ti)])
            if stage == "B0" and debug is not None and c == 0:
                pass
        P.barrier()
        if stage == "B0":
            return YB, [("YB", t) for t in range(5)]

        off = TB
        SG, MR = [], []
        for i in range(2):
            t_, off = A(f"SG{i}", [512], F32, off)
            SG.append(t_)
            t_, off = A(f"MR{i}", [512], F32, off)
            MR.append(t_)
        cnt = 0
        for g in range(2):
            wa = load_slab(("rnno", l, g))
            wb = load_slab(("gr", l, g))
            for ti in otiles:
                t0, w = TILES[ti]
                for j in range(4):
                    dc = g * 4 + j
                    b = cnt % 2
                    cnt += 1
                    p1, p2 = 2 + b, 4 + b
                    for k in range(8):
                        mm(ps[p1][:, 0:w], wk(wa, k, j * 128, 128), YB[:, k, t0:t0 + w], k == 0, k == 7,
                           [("ws", wa), ("YB", ti)], PSK(p1))
                    for k in range(8):
                        mm(ps[p2][:, 0:w], wk(wb, k, j * 128, 128), HT[:, k, t0:t0 + w], k == 0, k == 7,
                           [("ws", wb), ("HT", ti)], PSK(p2))
                    act(SG[b][:, 0:w], ps[p2][:, 0:w], AF.Sigmoid, [], [PSK(p2), ("SG", b)])
                    tt(MR[b][:, 0:w], ps[p1][:, 0:w], SG[b][:, 0:w], ALU.mult, [("SG", b)], [PSK(p1), ("MR", b)])
                    sp_dma(mrnn_d[:, dc, t0:t0 + w], MR[b][:, 0:w], [("MR", b)], [("mrnn", dc, ti)])
        P.barrier()

        off = TB
        KT, QT, Vz, Eb = [], [], [], []
        for i in range(2):
            t_, off = A(f"KT{i}", [NT], BF16, off)
            KT.append(t_)
            t_, off = A(f"QT{i}", [NT], BF16, off)
            QT.append(t_)
            t_, off = A(f"Vz{i}", [18, 192], BF16, off)
            Vz.append(t_)
            t_, off = A(f"Eb{i}", [2, NPAT, 128], BF16, off)
            Eb.append(t_)
        SQh, RSh, RD = [], [], []
        for i in range(2):
            t_, off = A(f"SQh{i}", [512], BF16, off)
            SQh.append(t_)
            t_, off = A(f"RSh{i}", [512], F32, off)
            RSh.append(t_)
            t_, off = A(f"RD{i}", [128], F32, off)
            RD.append(t_)
        PT = []
        for hh in range(2):
            row = []
            for i in range(2):
                t_, off = A(f"PT{hh}{i}", [7, 128], BF16, off)
                row.append(t_)
            PT.append(row)
        for i in range(2):
            P.op("dve", lambda e, i=i: e.memset(Vz[i][:, :, 64:128], 0.0), writes=[("Vz", i)])
        acnt = {"n": 0}

        def attend_S(c, qtok0, chunks, out_ti):
            cb_ = c % 2
            i = acnt["n"] % 2
            acnt["n"] += 1
            clist = [(0, None), (1, None)] + [(2 + m, pc) for (m, pc) in chunks]
            nchunk = len(clist)
            for hh in range(2):
                pbase = hh * 64
                bX, bY = 2 + 2 * hh, 3 + 2 * hh
                ptk = ("PT", hh, i)
                pt = PT[hh][i]
                for ci, (jt, pc) in enumerate(clist):
                    bank = bX if ci < 4 else bY
                    col = (ci % 4) * 128
                    mm(ps[bank][:, col:col + 128], KT[cb_][pbase:pbase + 64, jt * 128:(jt + 1) * 128],
                       QT[cb_][pbase:pbase + 64, qtok0:qtok0 + 128], True, True, [("KT", cb_), ("QT", cb_)], PSK(bank))
                n1 = min(4, nchunk)
                act(pt[:, 0:n1, :], ps[bX][:, 0:n1 * 128].rearrange("p (a b) -> p a b", b=128), AF.Exp, [], [PSK(bX), ptk])
                if nchunk > 4:
                    n2 = nchunk - 4
                    act(pt[:, 4:nchunk, :], ps[bY][:, 0:n2 * 128].rearrange("p (a b) -> p a b", b=128), AF.Exp, [],
                        [PSK(bY), ptk])
                if chunks:
                    pc0, n = chunks[0][1], len(chunks)
                    tt(pt[:, 2:2 + n, :], pt[:, 2:2 + n, :], Eb[cb_][:, hh, pc0:pc0 + n, :], ALU.mult, [ptk, ("Eb", cb_)], [ptk])
            return (c, qtok0, clist, out_ti, i)

        def attend_PV(stt_):
            c, qtok0, clist, out_ti, i = stt_
            cb_ = c % 2
            nchunk = len(clist)
            total = 2 * nchunk
            idx = 0
            for hh in range(2):
                ptk = ("PT", hh, i)
                for ci, (jt, pc) in enumerate(clist):
                    lv = Vz[cb_][:, jt, 0:128] if hh == 0 else Vz[cb_][:, jt, 64:192]
                    lo = onesV[:, 0:128] if hh == 0 else onesV[:, 64:192]
                    mm(ps[6][:, 0:128], lv, PT[hh][i][:, ci, :], idx == 0, idx == total - 1, [("Vz", cb_), ptk], PSK(6))
                    mm(ps[7][:, 0:128], lo, PT[hh][i][:, ci, :], idx == 0, idx == total - 1, ["onesV", ptk], PSK(7))
                    idx += 1
            P.op("dve", lambda e: e.reciprocal(RD[i][:, :], ps[7][:, 0:128]), reads=[], writes=[PSK(7), ("RD", i)])
            tt(YB[:, c, qtok0:qtok0 + 128], ps[6][:, 0:128], RD[i][:, :], ALU.mult, [("RD", i)], [PSK(6), ("YB", out_ti)])

        pcnt = {"n": 0}

        def inproj_items(c):
            cb_ = c % 2
            items = []
            st_ = {}

            def first():
                st_["ws"] = load_slab(("kvq", l, c))
                pool_dma(Eb[cb_][:, :, :, :], biasG_d[l, c].rearrange("p (h a b) -> p h a b", h=2, b=128), [], [("Eb", cb_)])
                act(Eb[cb_][:, :, :, :], Eb[cb_][:, :, :, :], AF.Exp, [("Eb", cb_)], [("Eb", cb_)])
                for hh in range(2):
                    tt(Eb[cb_][:, hh, :, :], Eb[cb_][:, hh, :, :], maskG[:, :, :], ALU.mult, [("Eb", cb_), "maskG"], [("Eb", cb_)])
            items.append(first)
            for (colbase, dst, dkey, gain, gkey, tl) in ((0, KT[cb_], ("KT", cb_), gains[:, l, 1:2], "gains", tiles),
                                                       (256, QT[cb_], ("QT", cb_), qg[:, 0:1], "qg", otiles)):
                for ti in tl:
                    def proj(colbase=colbase, dst=dst, dkey=dkey, gain=gain, gkey=gkey, ti=ti):
                        wsi = st_["ws"]
                        t0, w = TILES[ti]
                        b = pcnt["n"] % 2
                        pcnt["n"] += 1
                        pP, pS = (0, 1) if b == 0 else (6, 7)
                        for k in range(8):
                            mm(ps[pP][:, 0:w], wk(wsi, k, colbase, 128), HT[:, k, t0:t0 + w], k == 0, k == 7,
                               [("ws", wsi), ("HT", ti)], PSK(pP))
                        act(SQh[b][:, 0:w], ps[pP][:, 0:w], AF.Square, [], [PSK(pP), ("SQh", b)])
                        mm(ps[pS][:, 0:w], blk_bf[:, :], SQh[b][:, 0:w], True, True, [("SQh", b), "blk_bf"], PSK(pS))
                        act(RSh[b][:, 0:w], ps[pS][:, 0:w], AF.Sqrt, [], [PSK(pS), ("RSh", b)], bias=EPS, scale=1.0 / 64)
                        P.op("dve", lambda e, b=b, w=w: e.reciprocal(RSh[b][:, 0:w], RSh[b][:, 0:w]), reads=[("RSh", b)],
                             writes=[("RSh", b)])
                        stt(dst[:, t0:t0 + w], ps[pP][:, 0:w], gain, RSh[b][:, 0:w], ALU.mult, ALU.mult, [("RSh", b), gkey],
                            [PSK(pP), dkey])
                    items.append(proj)
            for j0 in range(0, 18, 4):
                def vproj(j0=j0):
                    wsi = st_["ws"]
                    n = min(4, 18 - j0)
                    bank = 0 if (j0 // 4) % 2 == 0 else 1
                    for jj in range(n):
                        jt = j0 + jj
                        ti = 0 if jt < 2 else 1 + (jt - 2) // 4
                        for k in range(8):
                            mm(ps[bank][:, jj * 128:(jj + 1) * 128], HT[:, k, jt * 128:(jt + 1) * 128], wk(wsi, k, 128, 128),
                               k == 0, k == 7, [("ws", wsi), ("HT", ti)], PSK(bank))
                    pv3 = ps[bank][:, 0:n * 128].rearrange("p (a b) -> p a b", b=128)
                    act(Vz[cb_][:, j0:j0 + n, 0:64], pv3[:, :, 0:64], AF.Identity, [], [PSK(bank), ("Vz", cb_)])
                    P.op("dve", lambda e, j0=j0, n=n, pv3=pv3: e.tensor_copy(Vz[cb_][:, j0:j0 + n, 128:192], pv3[:, :, 64:128]),
                         reads=[], writes=[PSK(bank), ("Vz", cb_)])
                items.append(vproj)
            return items

        for c in range(8):
            for it in inproj_items(c):
                it()
            calls = []
            if ctx_out:
                for qt in range(2):
                    calls.append((qt * 128, [], 0))
            for qp in range(16):
                calls.append((CTX + qp * 128, NA_PLAN[qp], 1 + qp // 4))
            pend = None
            for (q0, ch, oti) in calls:
                cur = attend_S(c, q0, ch, oti)
                if pend is not None:
                    attend_PV(pend)
                pend = cur
            attend_PV(pend)
        P.barrier()
        if stage == "D0":
            return YB, [("YB", t) for t in range(5)]

        off = TB
        MTa, off = A("MTa", [8, NT], BF16, off)
        XL2, MRL, T1, SG2 = [], [], [], []
        for i in range(2):
            t_, off = A(f"XL2{i}", [4, 512], F32, off)
            XL2.append(t_)
        for i in range(2):
            t_, off = A(f"MRL{i}", [512], F32, off)
            MRL.append(t_)
            t_, off = A(f"T1{i}", [512], F32, off)
            T1.append(t_)
            t_, off = A(f"SG2{i}", [512], F32, off)
            SG2.append(t_)
        cnt = 0
        for g in range(2):
            wa = load_slab(("nao", l, g))
            wb = load_slab(("gn", l, g))
            for ti in otiles:
                t0, w = TILES[ti]
                for j in range(4):
                    dc = g * 4 + j
                    b = cnt % 2
                    cnt += 1
                    p1, p2 = 2 + b, 4 + b
                    for k in range(8):
                        mm(ps[p1][:, 0:w], wk(wa, k, j * 128, 128), YB[:, k, t0:t0 + w], k == 0, k == 7,
                           [("ws", wa), ("YB", ti)], PSK(p1))
                    for k in range(8):
                        mm(ps[p2][:, 0:w], wk(wb, k, j * 128, 128), HT[:, k, t0:t0 + w], k == 0, k == 7,
                           [("ws", wb), ("HT", ti)], PSK(p2))
                    sp_dma(MRL[b][:, 0:w], mrnn_d[:, dc, t0:t0 + w], [("mrnn", dc, ti)], [("MRL", b)])
                    act(SG2[b][:, 0:w], ps[p2][:, 0:w], AF.Sigmoid, [], [PSK(p2), ("SG2", b)])
                    tt(T1[b][:, 0:w], ps[p1][:, 0:w], SG2[b][:, 0:w], ALU.mult, [("SG2", b)], [PSK(p1), ("T1", b)])
                    tt(MTa[:, dc, t0:t0 + w], T1[b][:, 0:w], MRL[b][:, 0:w], ALU.add, [("T1", b), ("MRL", b)], [("MTa", ti)])
        xn = 0
        for g in range(2):
            wo = load_slab(("out", l, g))
            for ti in otiles:
                t0, w = TILES[ti]
                col = 2 if ti == 0 else s
                xb = xn % 2
                xn += 1
                sp_dma(XL2[xb][:, :, 0:w], xd[s, :, g * 4:(g + 1) * 4, t0:t0 + w], [("xd", ti)], [("XL2", xb)])
                for j in range(4):
                    dc = g * 4 + j
                    b = cnt % 2
                    cnt += 1
                    p1 = 6 + b
                    for k in range(8):
                        mm(ps[p1][:, 0:w], wk(wo, k, j * 128, 128), MTa[:, k, t0:t0 + w], k == 0, k == 7,
                           [("ws", wo), ("MTa", ti)], PSK(p1))
                    stt(XL2[xb][:, j, 0:w], ps[p1][:, 0:w], modv(l, 2, dc, col), XL2[xb][:, j, 0:w], ALU.mult, ALU.add,
                        ["mod", ("XL2", xb)], [PSK(p1), ("XL2", xb)])
                sp_dma(xres_d[s, :, g * 4:(g + 1) * 4, t0:t0 + w], XL2[xb][:, :, 0:w], [("XL2", xb)], [("xdw", ti, g)])
        state["x_in_res"] = True
        P.barrier()
        return None

    def ffn(l, s):
        ctx_out = (l == 0)
        moe = (l == 1)
        otiles = [0, 1, 2, 3, 4] if ctx_out else [1, 2, 3, 4]
        off = XBASE
        XTs, off = A("XTs", [8, NT], F32, off)
        Gt, off = A("Gt", [16, 8], F32, off)
        OV = off
        X = {}
        X["SQ"], off = A("SQ2", [8, 512], BF16, off)
        X["RS"], off = A("RS2", [512], F32, off)
        if moe:
            X["TMP8"], off = A("TMP8", [8, 512], F32, off)
            LG, off = A("LG", [512], F32, off)
            LT, off = A("LT", [16, 8], F32, off)
            EQ1, off = A("EQ1", [16, 8], F32, off)
            L2, off = A("L2", [16, 8], F32, off)
            EQ2, off = A("EQ2", [16, 8], F32, off)
            TG, off = A("TG", [16, 8], F32, off)
            M1, off = A("M1", [16, 1], F32, off)
            M2, off = A("M2", [16, 1], F32, off)
            W1, off = A("W1", [16, 1], F32, off)
            W2, off = A("W2", [16, 1], F32, off)
        else:
            X["TMP"] = []
            for i in range(2):
                t_, off = A(f"TMPf{i}", [512], F32, off)
                X["TMP"].append(t_)
        for ti in otiles:
            t0, w = TILES[ti]
            sp_dma(XTs[:, :, t0:t0 + w], xres_d[s, :, :, t0:t0 + w], [("xd", ti)], [("XTs", ti)])

        def xsrc(ti):
            t0, w = TILES[ti]
            return (lambda c: XTs[:, c, t0:t0 + w]), [("XTs", ti)]

        if SPARSE_MOE and MERGED_MOE and stage in ("full", "seq1") and not state.get("zf"):
            state["zf"] = True
            zero_fill_hg()
        hook = None
        if moe:
            for c in range(8):
                mm(ps[2][0:8, 0:2], router[:, c, :], mod[:, l, 24 + c, s:s + 2], c == 0, c == 7, ["router", "mod"], PSK(2))
            act(routb[0:8, 0:2], ps[2][0:8, 0:2], AF.Identity, [], [PSK(2), "routb"])

            def hook(ti):
                t0, w = TILES[ti]
                for c in range(8):
                    mm(ps[2][0:8, 0:w], router[:, c, :], X["TMP8"][:, c, 0:w], c == 0, c == 7, ["router", ("TMP8", c)], PSK(2))
                act(LG[0:8, 0:w], ps[2][0:8, 0:w], AF.Identity, ["routb"], [PSK(2), "LG"], bias=routb[0:8, 0:1])
                for j in range(w // 128):
                    jt = (t0 - CTX) // 128 + j
                    P.op("pe", lambda e, jt=jt, j=j: e.transpose(ps[3][:, jt * 8:(jt + 1) * 8], LG[0:8, j * 128:(j + 1) * 128],
                                                                 ident_f[0:8, 0:8]),
                         reads=["LG", "ident_f"], writes=[PSK(3)])

        modulate(l, s, 1, otiles, xsrc, X, moe=moe, after_tile=hook)
        if moe:
            P.op("dve", lambda e: e.tensor_copy(LT[:, :, :], ps[3][:, 0:128].rearrange("p (a b) -> p a b", b=8)),
                 reads=[], writes=[PSK(3), "LT"])
            P.op("dve", lambda e: e.tensor_reduce(M1[:, :, 0], LT[:, :, :], AX.X, ALU.max), reads=["LT"], writes=["M1"])
            tt(EQ1[:, :, :], LT[:, :, :], M1[:, :, 0:1].to_broadcast([128, 16, 8]), ALU.is_equal, ["LT", "M1"], ["EQ1"])
            stt(L2[:, :, :], EQ1[:, :, :], -1.0e30, LT[:, :, :], ALU.mult, ALU.add, ["EQ1", "LT"], ["L2"])
            P.op("dve", lambda e: e.tensor_reduce(M2[:, :, 0], L2[:, :, :], AX.X, ALU.max), reads=["L2"], writes=["M2"])
            tt(EQ2[:, :, :], L2[:, :, :], M2[:, :, 0:1].to_broadcast([128, 16, 8]), ALU.is_equal, ["L2", "M2"], ["EQ2"])
            tt(W2[:, :, :], M2[:, :, :], M1[:, :, :], ALU.subtract, ["M1", "M2"], ["W2"])
            act(W2[:, :, :], W2[:, :, :], AF.Exp, ["W2"], ["W2"])
            ts(W1[:, :, :], W2[:, :, :], 1.0, None, ALU.add, None, ["W2"], ["W1"])
            P.op("dve", lambda e: e.reciprocal(W1[:, :, :], W1[:, :, :]), reads=["W1"], writes=["W1"])
            tt(W2[:, :, :], W2[:, :, :], W1[:, :, :], ALU.mult, ["W1", "W2"], ["W2"])
            tt(Gt[:, :, :], EQ1[:, :, :], W1[:, :, 0:1].to_broadcast([128, 16, 8]), ALU.mult, ["EQ1", "W1"], ["Gt"])
            tt(TG[:, :, :], EQ2[:, :, :], W2[:, :, 0:1].to_broadcast([128, 16, 8]), ALU.mult, ["EQ2", "W2"], ["TG"])
            tt(Gt[:, :, :], Gt[:, :, :], TG[:, :, :], ALU.add, ["Gt", "TG"], ["Gt"])
        P.barrier()
        if stage == "G0":
            return HT, [("HT", t) for t in range(5)]

        off = OV
        SIL, TT, ACTT, GE, DG = [], [], [], [], []
        for i in range(2):
            t_, off = A(f"SIL{i}", [512], BF16, off)
            SIL.append(t_)
            t_, off = A(f"TT{i}", [512], F32, off)
            TT.append(t_)
            t_, off = A(f"ACTT{i}", [4, 512], BF16, off)
            ACTT.append(t_)
            if moe:
                t_, off = A(f"GE{i}", [SEQ], F32, off)
                GE.append(t_)
                t_, off = A(f"DG{i}", [128], F32, off)
                DG.append(t_)
        cnt = {"h": 0, "a": 0, "o": 0, "d": 0}

        def swiglu_group(names, nj, tl, ge):
            w1 = load_slab(names[0])
            w3 = load_slab(names[1])
            w2 = load_slab(names[2])
            for ti in tl:
                t0, w = TILES[ti]
                col = 2 if ti == 0 else s
                ab = cnt["a"] % 2
                cnt["a"] += 1
                for j in range(nj):
                    b = cnt["h"] % 2
                    cnt["h"] += 1
                    b1, b3 = 2 + b, 4 + b
                    for k in range(8):
                        mm(ps[b1][:, 0:w], wk(w1, k, j * 128, 128), HT[:, k, t0:t0 + w], k == 0, k == 7,
                           [("ws", w1), ("HT", ti)], PSK(b1))
                    for k in range(8):
                        mm(ps[b3][:, 0:w], wk(w3, k, j * 128, 128), HT[:, k, t0:t0 + w], k == 0, k == 7,
                           [("ws", w3), ("HT", ti)], PSK(b3))
                    act(SIL[b][:, 0:w], ps[b1][:, 0:w], AF.Silu, [], [PSK(b1), ("SIL", b)])
                    if ge is None:
                        tt(ACTT[ab][:, j, 0:w], ps[b3][:, 0:w], SIL[b][:, 0:w], ALU.mult, [("SIL", b)], [PSK(b3), ("ACTT", ab)])
                    else:
                        tt(TT[b][:, 0:w], ps[b3][:, 0:w], SIL[b][:, 0:w], ALU.mult, [("SIL", b)], [PSK(b3), ("TT", b)])
                        tt(ACTT[ab][:, j, 0:w], TT[b][:, 0:w], GE[ge][:, t0 - CTX:t0 - CTX + w], ALU.mult,
                           [("TT", b), ("GE", ge)], [("ACTT", ab)])
                for dc in range(8):
                    ob = 6 + cnt["o"] % 2
                    cnt["o"] += 1
                    for j in range(nj):
                        mm(ps[ob][:, 0:w], wk2(w2, j, dc * 128, 128), ACTT[ab][:, j, 0:w], j == 0, j == nj - 1,
                           [("ws", w2), ("ACTT", ab)], PSK(ob))
                    stt(XTs[:, dc, t0:t0 + w], ps[ob][:, 0:w], modv(l, 5, dc, col), XTs[:, dc, t0:t0 + w], ALU.mult, ALU.add,
                        ["mod", ("XTs", ti)], [PSK(ob), ("XTs", ti)])

        if not moe:
            for g in range(6):
                swiglu_group([("f1", g), ("f3", g), ("f2", g)], 4 if g < 5 else 2, otiles, None)
        else:
            for ex in range(NEXP):
                ge = ex % 2
                for j0 in range(0, 16, 4):
                    bank = 0 if (j0 // 4) % 2 == 0 else 1
                    for jj in range(4):
                        jt = j0 + jj
                        d = cnt["d"] % 2
                        cnt["d"] += 1
                        ts(DG[d][:, :], ident_f[:, :], Gt[:, jt, ex:ex + 1], None, ALU.mult, None, ["ident_f", "Gt"], [("DG", d)])
                        mm(ps[bank][:, jj * 128:(jj + 1) * 128], ones_f[:, :], DG[d][:, :], True, True, ["ones_f", ("DG", d)],
                           PSK(bank))
                    act(GE[ge][:, j0 * 128:(j0 + 4) * 128], ps[bank][:, 0:512], AF.Identity, [], [PSK(bank), ("GE", ge)])
                for g in range(7):
                    swiglu_group([("m1", ex, g), ("m3", ex, g), ("m2", ex, g)], 4, [1, 2, 3, 4], ge)
        for ti in otiles:
            t0, w = TILES[ti]
            if l == 0:
                sp_dma(xres_d[s, :, :, t0:t0 + w], XTs[:, :, t0:t0 + w], [("XTs", ti)], [("xd", ti)])
            else:
                sp_dma(outT_d[s, :, :, t0 - CTX:t0 - CTX + w], XTs[:, :, t0:t0 + w], [("XTs", ti)], [("out", s, ti)])
        P.barrier()
        return None

    def ffn_moe_sparse(l, s):
        I32 = mybir.dt.int32
        TSZ = MOE_TSZ
        NQ = TSZ // 128
        NBLK = SEQ // TSZ
        lat = [1, 2, 3, 4]
        off = XBASE
        Gsm = {}
        for nm, shp, dt in (("W1", [16, 1], F32), ("W2", [16, 1], F32), ("SI", [16, 2], I32), ("JI", [8], I32)):
            Gsm[nm], off = A("m_" + nm, shp, dt, off)
        OV = off
        for nm, shp, dt in (("LT", [16, 8], F32), ("EQ1", [16, 8], F32), ("L2", [16, 8], F32), ("EQ2", [16, 8], F32),
                            ("M1", [16, 1], F32), ("M2", [16, 1], F32),
                            ("AB", [16, 8], BF16), ("PW", [16, 8], F32), ("TOT", [16, 8], F32), ("CS", [16, 8], F32),
                            ("EOFF", [16, 8], F32), ("TQ", [16, 8], F32), ("S12", [16, 2], F32),
                            ("NE", [8, 1], F32), ("THR", [8, 8], F32), ("CMP", [8, 8], F32), ("JF", [8], F32)):
            Gsm[nm], off = A("m_" + nm, shp, dt, off)
        LT, EQ1, L2, EQ2, M1, M2, W1, W2 = (Gsm[k] for k in ("LT", "EQ1", "L2", "EQ2", "M1", "M2", "W1", "W2"))
        AB, PW, TOT, CS, EOFF, TQ, S12, SI = (Gsm[k] for k in ("AB", "PW", "TOT", "CS", "EOFF", "TQ", "S12", "SI"))
        NE, THR, CMP, JF, JI = (Gsm[k] for k in ("NE", "THR", "CMP", "JF", "JI"))
        LG, off = A("mLG", [512], F32, off)
        XL = []
        for i in range(2):
            t_, off = A(f"mXL{i}", [8, 512], F32, off)
            XL.append(t_)
        X = {}
        X["SQ"], off = A("mSQ", [8, 512], BF16, off)
        X["RS"], off = A("mRS", [512], F32, off)
        X["TMP8"], off = A("mTMP8", [8, 512], F32, off)
        HTok = []
        for i in range(2):
            t_, off = A(f"HTok{i}", [D], BF16, off)
            HTok.append(t_)

        def xsrc(ti):
            t0, w = TILES[ti]
            b = ti % 2
            sp_dma(XL[b][:, :, 0:w], xres_d[s, :, :, t0:t0 + w], [("xd", ti)], [("XL", b)])
            return (lambda c: XL[b][:, c, 0:w]), [("XL", b)]

        for c in range(8):
            mm(ps[2][0:8, 0:2], router[:, c, :], mod[:, l, 24 + c, s:s + 2], c == 0, c == 7, ["router", "mod"], PSK(2))
        act(routb[0:8, 0:2], ps[2][0:8, 0:2], AF.Identity, [], [PSK(2), "routb"])

        def hook(ti):
            t0, w = TILES[ti]
            for c in range(8):
                mm(ps[2][0:8, 0:w], router[:, c, :], X["TMP8"][:, c, 0:w], c == 0, c == 7, ["router", ("TMP8", c)], PSK(2))
            act(LG[0:8, 0:w], ps[2][0:8, 0:w], AF.Identity, ["routb"], [PSK(2), "LG"], bias=routb[0:8, 0:1])
            for j in range(w // 128):
                jt = (t0 - CTX) // 128 + j
                P.op("pe", lambda e, jt=jt, j=j: e.transpose(ps[3][:, jt * 8:(jt + 1) * 8], LG[0:8, j * 128:(j + 1) * 128],
                                                             ident_f[0:8, 0:8]),
                     reads=["LG", "ident_f"], writes=[PSK(3)])

        modulate(l, s, 1, lat, xsrc, X, moe=True, after_tile=hook)
        P.op("dve", lambda e: e.tensor_copy(LT[:, :, :], ps[3][:, 0:128].rearrange("p (a b) -> p a b", b=8)),
             reads=[], writes=[PSK(3), "LT"])
        P.op("dve", lambda e: e.tensor_reduce(M1[:, :, 0], LT[:, :, :], AX.X, ALU.max), reads=["LT"], writes=["M1"])
        tt(EQ1[:, :, :], LT[:, :, :], M1[:, :, 0:1].to_broadcast([128, 16, 8]), ALU.is_equal, ["LT", "M1"], ["EQ1"])
        stt(L2[:, :, :], EQ1[:, :, :], -1.0e30, LT[:, :, :], ALU.mult, ALU.add, ["EQ1", "LT"], ["L2"])
        P.op("dve", lambda e: e.tensor_reduce(M2[:, :, 0], L2[:, :, :], AX.X, ALU.max), reads=["L2"], writes=["M2"])
        tt(EQ2[:, :, :], L2[:, :, :], M2[:, :, 0:1].to_broadcast([128, 16, 8]), ALU.is_equal, ["L2", "M2"], ["EQ2"])
        tt(W2[:, :, :], M2[:, :, :], M1[:, :, :], ALU.subtract, ["M1", "M2"], ["W2"])
        act(W2[:, :, :], W2[:, :, :], AF.Exp, ["W2"], ["W2"])
        ts(W1[:, :, :], W2[:, :, :], 1.0, None, ALU.add, None, ["W2"], ["W1"])
        P.op("dve", lambda e: e.reciprocal(W1[:, :, :], W1[:, :, :]), reads=["W1"], writes=["W1"])
        tt(W2[:, :, :], W2[:, :, :], W1[:, :, :], ALU.mult, ["W1", "W2"], ["W2"])
        tt(TQ[:, :, :], EQ1[:, :, :], EQ2[:, :, :], ALU.add, ["EQ1", "EQ2"], ["TQ"])
        P.op("dve", lambda e: e.tensor_copy(AB[:, :, :], TQ[:, :, :]), reads=["TQ"], writes=["AB"])
        ABf = AB[:, :, :].rearrange("p a b -> p (a b)")
        mm(ps[0][:, 0:128], ltri_bf[:, :], ABf, True, True, ["AB", "ltri_bf"], PSK(0))
        mm(ps[0][:, 128:256], ones_bf[:, :], ABf, True, True, ["AB", "ones_bf"], PSK(0))
        P.op("dve", lambda e: e.tensor_copy(PW[:, :, :], ps[0][:, 0:128].rearrange("p (a b) -> p a b", b=8)),
             reads=[], writes=[PSK(0), "PW"])
        P.op("dve", lambda e: e.tensor_copy(TOT[:, :, :], ps[0][:, 128:256].rearrange("p (a b) -> p a b", b=8)),
             reads=[], writes=[PSK(0), "TOT"])
        P.op("dve", lambda e: e.memset(CS[:, 0, :], 0.0), writes=["CS"])
        for j in range(1, 16):
            tt(CS[:, j, :], CS[:, j - 1, :], TOT[:, j - 1, :], ALU.add, ["CS", "TOT"], ["CS"])
        tt(NE[:, :, 0], CS[:, 15, :], TOT[:, 15, :], ALU.add, ["CS", "TOT"], ["NE"])
        for ex in range(NEXP):
            P.op("dve", lambda e, ex=ex: e.memset(EOFF[:, :, ex], float(ex * SEQ)), writes=["EOFF"])
            P.op("dve", lambda e, ex=ex: e.memset(THR[:, :, ex], float(ex * TSZ)), writes=["THR"])
        tt(PW[:, :, :], PW[:, :, :], CS[:, :, :], ALU.add, ["PW", "CS"], ["PW"])
        tt(PW[:, :, :], PW[:, :, :], EOFF[:, :, :], ALU.add, ["PW", "EOFF"], ["PW"])
        tt(TQ[:, :, :], EQ1[:, :, :], PW[:, :, :], ALU.mult, ["EQ1", "PW"], ["TQ"])
        P.op("dve", lambda e: e.tensor_reduce(S12[:, :, 0], TQ[:, :, :], AX.X, ALU.add), reads=["TQ"], writes=["S12"])
        tt(TQ[:, :, :], EQ2[:, :, :], PW[:, :, :], ALU.mult, ["EQ2", "PW", "S12"], ["TQ"])
        P.op("dve", lambda e: e.tensor_reduce(S12[:, :, 1], TQ[:, :, :], AX.X, ALU.add), reads=["TQ"], writes=["S12"])
        P.op("dve", lambda e: e.tensor_copy(SI[:, :, :], S12[:, :, :]), reads=["S12"], writes=["SI"])
        tt(CMP[:, :, :], NE[:, :, 0:1].to_broadcast([128, 8, 8]), THR[:, :, :], ALU.is_gt, ["NE", "THR"], ["CMP"])
        P.op("dve", lambda e: e.tensor_reduce(JF[:, :], CMP[:, :, :], AX.X, ALU.add), reads=["CMP"], writes=["JF"])
        if JCLAMP is not None:
            ts(JF[:, :], JF[:, :], float(JCLAMP), None, ALU.min, None, ["JF"], ["JF"])
        P.op("dve", lambda e: e.tensor_copy(JI[:, :], JF[:, :]), reads=["JF"], writes=["JI"])
        if stage == "G1a":
            DB, _ = A("DBG1", [64], F32, off)
            P.op("dve", lambda e: e.memset(DB[:, :], 0.0), writes=["DB"])
            P.op("dve", lambda e: e.tensor_copy(DB[:, 0:8], NE[:, :, 0]), reads=["NE"], writes=["DB"])
            P.op("dve", lambda e: e.tensor_copy(DB[:, 8:16], JF[:, :]), reads=["JF"], writes=["DB"])
            P.op("dve", lambda e: e.tensor_copy(DB[:, 16:48], S12[:, :, :].rearrange("p a b -> p (a b)")), reads=["S12"], writes=["DB"])
            P.op("dve", lambda e: e.tensor_copy(DB[:, 48:64], M1[:, :, 0]), reads=["M1"], writes=["DB"])
            sp_dma(dbg_d, DB[:, :], ["DB"], ["dbg"])
            return "done"
        psb = [ps[i][:, :].bitcast(BF16) for i in range(8)]
        if ZERO_HG:
            P.op("dve", lambda e: e.memset(HTok[0][:, :], 0.0), writes=[("HTok", 0)])
            for blk in range(NEXP * SEQ // 128):
                sp_dma(hg_d[blk * 128:(blk + 1) * 128, :], HTok[0][:, :], [("HTok", 0)], [("hgz", blk)])
        for jt in range(16):
            b = jt % 2
            ti = 1 + jt // 4
            for c in range(8):
                P.op("pe", lambda e, b=b, c=c, jt=jt: e.transpose(psb[b][:, c * 128:(c + 1) * 128],
                                                                  HT[:, c, CTX + jt * 128:CTX + (jt + 1) * 128], ident_bf[:, :]),
                     reads=[("HT", ti), "ident_bf"], writes=[PSK(b)])
            act(HTok[b][:, :], psb[b][:, :], AF.Identity, [], [PSK(b), ("HTok", b)])
            for k in range(2):
                P.dma("pool", lambda e, b=b, jt=jt, k=k: e.indirect_dma_start(
                    out=hg_d, out_offset=bass.IndirectOffsetOnAxis(ap=SI[:, jt, k:k + 1], axis=0), in_=HTok[b][:, :],
                    in_offset=None), reads=[("HTok", b), "SI"] + ([("hgz", q_) for q_ in range(128)] if ZERO_HG else []), writes=[("hg", jt, k)])
        P.barrier()
        if stage == "G1":
            return None

        off = OV
        Yacc, off = A("Yacc", [16, D], F32, off)
        HTg, off = A("HTg", [8, SEQ], BF16, off)
        HGs, SIL, ACTT = [], [], []
        t_, off = A("HGs0", [D], BF16, off)
        HGs = [t_, t_]
        for i in range(2):
            t_, off = A(f"mSIL{i}", [TSZ], BF16, off)
            SIL.append(t_)
            t_, off = A(f"mACTT{i}", [4, TSZ], BF16, off)
            ACTT.append(t_)
        cnt = {"h": 0, "a": 0, "o": 0, "g": 0}
        for ex in range(NEXP):
            P.load_reg(JI[0:1, ex:ex + 1], "JI")
            for k in range(16):
                b = cnt["g"] % 2
                cnt["g"] += 1
                r0 = ex * SEQ + k * 128
                sp_dma(HGs[b][:, :], hg_d[r0:r0 + 128, :], [("hg", a_, b_) for a_ in range(16) for b_ in range(2)], [("HGs", 0)])
                P.cond_begin(k // NQ + 1)
                for c in range(8):
                    P.op("pe", lambda e, b=b, c=c: e.transpose(psb[b][:, c * 128:(c + 1) * 128], HGs[b][:, c * 128:(c + 1) * 128],
                                                               ident_bf[:, :]),
                         reads=[("HGs", 0), "ident_bf"], writes=[PSK(b)])
                act(HTg[:, :, k * 128:(k + 1) * 128], psb[b][:, :].rearrange("p (a b) -> p a b", b=128), AF.Identity, [],
                    [PSK(b), ("HTg", k // NQ)])
                P.cond_end()
            for g in range(7):
                w1 = load_slab(("m1", ex, g), big=True)
                w3 = load_slab(("m3", ex, g), big=True)
                w2 = load_slab(("m2", ex, g), big=True)
                for j in range(NBLK):
                    P.cond_begin(j + 1)
                    ab = cnt["a"] % 2
                    cnt["a"] += 1
                    s0 = j * TSZ
                    for jj in range(4):
                        b = cnt["h"] % 2
                        cnt["h"] += 1
                        b1, b3 = 2 + b, 4 + b
                        for k in range(8):
                            mm(ps[b1][:, 0:TSZ], wk(w1, k, jj * 128, 128), HTg[:, k, s0:s0 + TSZ], k == 0, k == 7,
                               [("ws", w1), ("HTg", j)], PSK(b1))
                        for k in range(8):
                            mm(ps[b3][:, 0:TSZ], wk(w3, k, jj * 128, 128), HTg[:, k, s0:s0 + TSZ], k == 0, k == 7,
                               [("ws", w3), ("HTg", j)], PSK(b3))
                        act(SIL[b][:, :], ps[b1][:, 0:TSZ], AF.Silu, [], [PSK(b1), ("SIL", b)])
                        tt(ACTT[ab][:, jj, :], ps[b3][:, 0:TSZ], SIL[b][:, :], ALU.mult, [("SIL", b)], [PSK(b3), ("ACTT", ab)])
                    for h2 in range(NQ):
                        kt = NQ * j + h2
                        for dh in range(2):
                            ob = 6 + cnt["o"] % 2
                            cnt["o"] += 1
                            for jj in range(4):
                                mm(ps[ob][:, 0:512], ACTT[ab][:, jj, h2 * 128:(h2 + 1) * 128], wk2(w2, jj, dh * 512, 512),
                                   jj == 0, jj == 3, [("ws", w2), ("ACTT", ab)], PSK(ob))
                            ya = Yacc[:, kt, dh * 512:(dh + 1) * 512]
                            if g == 0:
                                P.op("dve", lambda e, ya=ya, ob=ob: e.tensor_copy(ya, ps[ob][:, 0:512]), reads=[],
                                     writes=[PSK(ob), ("Yacc", kt)])
                            else:
                                tt(ya, ps[ob][:, 0:512], ya, ALU.add, [("Yacc", kt)], [PSK(ob), ("Yacc", kt)])
                    P.cond_end()
            for k in range(16):
                r0 = ex * SEQ + k * 128
                sp_dma(yb_d[r0:r0 + 128, :], Yacc[:, k, :], [("Yacc", k)], [("yb", ex, k)])
        P.barrier()

        off = OV
        YA, YB2, OO, XC8 = [], [], [], []
        for i in range(2):
            t_, off = A(f"YA{i}", [D], F32, off)
            YA.append(t_)
            t_, off = A(f"YB2{i}", [D], F32, off)
            YB2.append(t_)
            t_, off = A(f"OO{i}", [D], F32, off)
            OO.append(t_)
            t_, off = A(f"XC8{i}", [8, 128], F32, off)
            XC8.append(t_)
        for jt in range(16):
            b = jt % 2
            ti = 1 + jt // 4
            c0 = CTX + jt * 128
            for k, dst, dk in ((0, YA, "YA"), (1, YB2, "YB2")):
                P.dma("pool", lambda e, b=b, jt=jt, k=k, dst=dst: e.indirect_dma_start(
                    out=dst[b][:, :], out_offset=None, in_=yb_d,
                    in_offset=bass.IndirectOffsetOnAxis(ap=SI[:, jt, k:k + 1], axis=0)),
                    reads=[("yb", a_, b_) for a_ in range(NEXP) for b_ in range(16)] + ["SI"], writes=[(dk, b)])
            sp_dma(XC8[b][:, :, :], xres_d[s, :, :, c0:c0 + 128], [("xd", ti)], [("XC8", b)])
            ts(OO[b][:, :], YA[b][:, :], W1[:, jt, 0:1], None, ALU.mult, None, [("YA", b), "W1"], [("OO", b)])
            stt(OO[b][:, :], YB2[b][:, :], W2[:, jt, 0:1], OO[b][:, :], ALU.mult, ALU.add, [("YB2", b), "W2", ("OO", b)], [("OO", b)])
            for c in range(8):
                pbk = 2 + 2 * b + c // 4
                P.op("pe", lambda e, b=b, c=c, pbk=pbk: e.transpose(ps[pbk][:, (c % 4) * 128:(c % 4 + 1) * 128],
                                                                    OO[b][:, c * 128:(c + 1) * 128], ident_f[:, :]),
                     reads=[("OO", b), "ident_f"], writes=[PSK(pbk)])
            for c in range(8):
                pbk = 2 + 2 * b + c // 4
                stt(XC8[b][:, c, :], ps[pbk][:, (c % 4) * 128:(c % 4 + 1) * 128], modv(l, 5, c, s), XC8[b][:, c, :],
                    ALU.mult, ALU.add, ["mod", ("XC8", b)], [PSK(pbk), ("XC8", b)])
            sp_dma(outT_d[s, :, :, jt * 128:(jt + 1) * 128], XC8[b][:, :, :], [("XC8", b)], [("out", s, jt)])
        P.barrier()
        return None

    def moe_merged(l, seqs):
        I32 = mybir.dt.int32
        TSZ = 512
        NQ = TSZ // 128
        CAP = SEQ * len(seqs)
        NPASS = len(seqs)
        off = XBASE
        PS_ = {}
        for s in seqs:
            for nm, shp, dt in (("W1", [16, 1], F32), ("W2", [16, 1], F32), ("SI", [16, 2], I32)):
                PS_[(nm, s)], off = A(f"mm_{nm}{s}", shp, dt, off)
        NEacc, off = A("mm_NEacc", [8, 1], F32, off)
        JI, off = A("mm_JI", [8], I32, off)
        OV = off
        G = {}
        for nm, shp, dt in (("LT", [16, 8], F32), ("EQ1", [16, 8], F32), ("L2", [16, 8], F32), ("EQ2", [16, 8], F32),
                            ("M1", [16, 1], F32), ("M2", [16, 1], F32),
                            ("AB", [16, 8], BF16), ("PW", [16, 8], F32), ("TOT", [16, 8], F32), ("CS", [16, 8], F32),
                            ("EOFF", [16, 8], F32), ("TQ", [16, 8], F32), ("S12", [16, 2], F32),
                            ("THR", [8, 8], F32), ("CMP", [8, 8], F32), ("JF", [8], F32)):
            G[nm], off = A("mm_" + nm, shp, dt, off)
        LT, EQ1, L2, EQ2, M1, M2 = (G[k] for k in ("LT", "EQ1", "L2", "EQ2", "M1", "M2"))
        AB, PW, TOT, CS, EOFF, TQ, S12 = (G[k] for k in ("AB", "PW", "TOT", "CS", "EOFF", "TQ", "S12"))
        THR, CMP, JF = (G[k] for k in ("THR", "CMP", "JF"))
        LG, off = A("mm_LG", [512], F32, off)
        XL = []
        for i in range(2):
            t_, off = A(f"mm_XL{i}", [8, 512], F32, off)
            XL.append(t_)
        X = {}
        X["SQ"], off = A("mm_SQ", [8, 512], BF16, off)
        X["RS"], off = A("mm_RS", [512], F32, off)
        X["TMP8"], off = A("mm_TMP8", [8, 512], F32, off)
        NHB = 4
        HTok = []
        for i in range(NHB):
            t_, off = A(f"mm_HTok{i}", [D], BF16, off)
            HTok.append(t_)
        psb = [ps[i][:, :].bitcast(BF16) for i in range(8)]
        lat = [1, 2, 3, 4]
        P.op("dve", lambda e: e.memset(NEacc[:, :, :], 0.0), writes=["NEacc"])
        for ex in range(NEXP):
            P.op("dve", lambda e, ex=ex: e.memset(EOFF[:, :, ex], float(ex * CAP)), writes=["EOFF"])
            P.op("dve", lambda e, ex=ex: e.memset(THR[:, :, ex], float(ex * TSZ)), writes=["THR"])

        for s in seqs:
            W1, W2, SI = PS_[("W1", s)], PS_[("W2", s)], PS_[("SI", s)]

            def xsrc(ti, s=s):
                t0, w = TILES[ti]
                b = ti % 2
                sp_dma(XL[b][:, :, 0:w], xres_d[s, :, :, t0:t0 + w], [("xd", ti)], [("XL", b)])
                return (lambda c: XL[b][:, c, 0:w]), [("XL", b)]

            for c in range(8):
                mm(ps[2][0:8, 0:2], router[:, c, :], mod[:, l, 24 + c, s:s + 2], c == 0, c == 7, ["router", "mod"], PSK(2))
            act(routb[0:8, 0:2], ps[2][0:8, 0:2], AF.Identity, [], [PSK(2), "routb"])

            def hook(ti):
                t0, w = TILES[ti]
                for c in range(8):
                    mm(ps[2][0:8, 0:w], router[:, c, :], X["TMP8"][:, c, 0:w], c == 0, c == 7, ["router", ("TMP8", c)], PSK(2))
                act(LG[0:8, 0:w], ps[2][0:8, 0:w], AF.Identity, ["routb"], [PSK(2), "LG"], bias=routb[0:8, 0:1])
                for j in range(w // 128):
                    jt = (t0 - CTX) // 128 + j
                    P.op("pe", lambda e, jt=jt, j=j: e.transpose(ps[3][:, jt * 8:(jt + 1) * 8], LG[0:8, j * 128:(j + 1) * 128],
                                                                 ident_f[0:8, 0:8]),
                         reads=["LG", "ident_f"], writes=[PSK(3)])

            modulate(l, s, 1, lat, xsrc, X, moe=True, after_tile=hook)
            P.op("dve", lambda e: e.tensor_copy(LT[:, :, :], ps[3][:, 0:128].rearrange("p (a b) -> p a b", b=8)),
                 reads=[], writes=[PSK(3), "LT"])
            P.op("dve", lambda e: e.tensor_reduce(M1[:, :, 0], LT[:, :, :], AX.X, ALU.max), reads=["LT"], writes=["M1"])
            tt(EQ1[:, :, :], LT[:, :, :], M1[:, :, 0:1].to_broadcast([128, 16, 8]), ALU.is_equal, ["LT", "M1"], ["EQ1"])
            stt(L2[:, :, :], EQ1[:, :, :], -1.0e30, LT[:, :, :], ALU.mult, ALU.add, ["EQ1", "LT"], ["L2"])
            P.op("dve", lambda e: e.tensor_reduce(M2[:, :, 0], L2[:, :, :], AX.X, ALU.max), reads=["L2"], writes=["M2"])
            tt(EQ2[:, :, :], L2[:, :, :], M2[:, :, 0:1].to_broadcast([128, 16, 8]), ALU.is_equal, ["L2", "M2"], ["EQ2"])
            tt(W2[:, :, :], M2[:, :, :], M1[:, :, :], ALU.subtract, ["M1", "M2"], [("W2", s)])
            act(W2[:, :, :], W2[:, :, :], AF.Exp, [("W2", s)], [("W2", s)])
            ts(W1[:, :, :], W2[:, :, :], 1.0, None, ALU.add, None, [("W2", s)], [("W1", s)])
            P.op("dve", lambda e, W1=W1: e.reciprocal(W1[:, :, :], W1[:, :, :]), reads=[("W1", s)], writes=[("W1", s)])
            tt(W2[:, :, :], W2[:, :, :], W1[:, :, :], ALU.mult, [("W1", s), ("W2", s)], [("W2", s)])
            tt(TQ[:, :, :], EQ1[:, :, :], EQ2[:, :, :], ALU.add, ["EQ1", "EQ2"], ["TQ"])
            P.op("dve", lambda e: e.tensor_copy(AB[:, :, :], TQ[:, :, :]), reads=["TQ"], writes=["AB"])
            ABf = AB[:, :, :].rearrange("p a b -> p (a b)")
            mm(ps[0][:, 0:128], ltri_bf[:, :], ABf, True, True, ["AB", "ltri_bf"], PSK(0))
            mm(ps[0][:, 128:256], ones_bf[:, :], ABf, True, True, ["AB", "ones_bf"], PSK(0))
            P.op("dve", lambda e: e.tensor_copy(PW[:, :, :], ps[0][:, 0:128].rearrange("p (a b) -> p a b", b=8)),
                 reads=[], writes=[PSK(0), "PW"])
            P.op("dve", lambda e: e.tensor_copy(TOT[:, :, :], ps[0][:, 128:256].rearrange("p (a b) -> p a b", b=8)),
                 reads=[], writes=[PSK(0), "TOT"])
            P.op("dve", lambda e: e.tensor_copy(CS[:, 0, :], NEacc[:, :, 0]), reads=["NEacc"], writes=["CS"])
            for j in range(1, 16):
                tt(CS[:, j, :], CS[:, j - 1, :], TOT[:, j - 1, :], ALU.add, ["CS", "TOT"], ["CS"])
            tt(NEacc[:, :, 0], CS[:, 15, :], TOT[:, 15, :], ALU.add, ["CS", "TOT"], ["NEacc"])
            tt(PW[:, :, :], PW[:, :, :], CS[:, :, :], ALU.add, ["PW", "CS"], ["PW"])
            tt(PW[:, :, :], PW[:, :, :], EOFF[:, :, :], ALU.add, ["PW", "EOFF"], ["PW"])
            tt(TQ[:, :, :], EQ1[:, :, :], PW[:, :, :], ALU.mult, ["EQ1", "PW"], ["TQ"])
            P.op("dve", lambda e: e.tensor_reduce(S12[:, :, 0], TQ[:, :, :], AX.X, ALU.add), reads=["TQ"], writes=["S12"])
            tt(TQ[:, :, :], EQ2[:, :, :], PW[:, :, :], ALU.mult, ["EQ2", "PW", "S12"], ["TQ"])
            P.op("dve", lambda e: e.tensor_reduce(S12[:, :, 1], TQ[:, :, :], AX.X, ALU.add), reads=["TQ"], writes=["S12"])
            P.op("dve", lambda e, SI=SI: e.tensor_copy(SI[:, :, :], S12[:, :, :]), reads=["S12"], writes=[("SI", s)])
            for jt in range(16):
                b = jt % 2
                hb = jt % NHB
                ti = 1 + jt // 4
                for c in range(8):
                    P.op("pe", lambda e, b=b, c=c, jt=jt: e.transpose(psb[b][:, c * 128:(c + 1) * 128],
                                                                      HT[:, c, CTX + jt * 128:CTX + (jt + 1) * 128], ident_bf[:, :]),
                         reads=[("HT", ti), "ident_bf"], writes=[PSK(b)])
                act(HTok[hb][:, :], psb[b][:, :], AF.Identity, [], [PSK(b), ("HTok", hb)])
                for k in range(2):
                    P.dma("pool", lambda e, hb=hb, jt=jt, k=k, SI=SI: e.indirect_dma_start(
                        out=hg_d, out_offset=bass.IndirectOffsetOnAxis(ap=SI[:, jt, k:k + 1], axis=0), in_=HTok[hb][:, :],
                        in_offset=None), reads=[("HTok", hb), ("SI", s)] + [("hgz", q_) for q_ in range(NEXP * SEQ * 2 // 128)], writes=[("hg", s, jt, k)])
        tt(CMP[:, :, :], NEacc[:, :, 0:1].to_broadcast([128, 8, 8]), THR[:, :, :], ALU.is_gt, ["NEacc", "THR"], ["CMP"])
        P.op("dve", lambda e: e.tensor_reduce(JF[:, :], CMP[:, :, :], AX.X, ALU.add), reads=["CMP"], writes=["JF"])
        P.op("dve", lambda e: e.tensor_copy(JI[:, :], JF[:, :]), reads=["JF"], writes=["JI"])
        P.barrier()

        off = OV
        Yacc, off = A("mm_Yacc", [16, D], F32, off)
        HTg, off = A("mm_HTg", [8, SEQ], BF16, off)
        HGs = []
        for i in range(2):
            t_, off = A(f"mm_HGs{i}", [D], BF16, off)
            HGs.append(t_)
        SIL, ACTT = [], []
        for i in range(2):
            t_, off = A(f"mm_SIL{i}", [TSZ], BF16, off)
            SIL.append(t_)
            t_, off = A(f"mm_ACTT{i}", [4, TSZ], BF16, off)
            ACTT.append(t_)
        cnt = {"h": 0, "a": 0, "o": 0, "g": 0}
        allhg = [("hg", s_, a_, b_) for s_ in seqs for a_ in range(16) for b_ in range(2)]
        for p_, ex in [(p__, e__) for p__ in range(NPASS) for e__ in range(NEXP)]:
            P.load_reg(JI[0:1, ex:ex + 1], "JI", engines=("pe", "act", "dve", "sp", "pool"))
            for _once in range(1):
                jb = 4 * p_
                for k in range(16):
                    b = cnt["g"] % 2
                    cnt["g"] += 1
                    r0 = ex * CAP + p_ * SEQ + k * 128
                    thr = jb + k // NQ + 1
                    P.cond_begin(thr)
                    sp_dma(HGs[b][:, :], hg_d[r0:r0 + 128, :], allhg, [("HGs", b)])
                    for c in range(8):
                        P.op("pe", lambda e, b=b, c=c: e.transpose(psb[b][:, c * 128:(c + 1) * 128], HGs[b][:, c * 128:(c + 1) * 128],
                                                                   ident_bf[:, :]),
                             reads=[("HGs", b), "ident_bf"], writes=[PSK(b)])
                    act(HTg[:, :, k * 128:(k + 1) * 128], psb[b][:, :].rearrange("p (a b) -> p a b", b=128), AF.Identity, [],
                        [PSK(b), ("HTg", k // NQ)])
                    P.cond_end()
                for g in range(7):
                    P.cond_begin(jb + 1)
                    w1 = load_slab(("m1", ex, g), big=True)
                    w3 = load_slab(("m3", ex, g), big=True)
                    w2 = load_slab(("m2", ex, g), big=True)
                    P.cond_end()
                    for j in range(4):
                        P.cond_begin(jb + j + 1)
                        ab = cnt["a"] % 2
                        cnt["a"] += 1
                        s0 = j * TSZ
                        for jj in range(4):
                            b = cnt["h"] % 2
                            cnt["h"] += 1
                            b1, b3 = 2 + b, 4 + b
                            for k in range(8):
                                mm(ps[b1][:, 0:TSZ], wk(w1, k, jj * 128, 128), HTg[:, k, s0:s0 + TSZ], k == 0, k == 7,
                                   [("ws", w1), ("HTg", j)], PSK(b1))
                            for k in range(8):
                                mm(ps[b3][:, 0:TSZ], wk(w3, k, jj * 128, 128), HTg[:, k, s0:s0 + TSZ], k == 0, k == 7,
                                   [("ws", w3), ("HTg", j)], PSK(b3))
                            act(SIL[b][:, :], ps[b1][:, 0:TSZ], AF.Silu, [], [PSK(b1), ("SIL", b)])
                            tt(ACTT[ab][:, jj, :], ps[b3][:, 0:TSZ], SIL[b][:, :], ALU.mult, [("SIL", b)], [PSK(b3), ("ACTT", ab)])
                        for h2 in range(NQ):
                            kt = NQ * j + h2
                            for dh in range(2):
                                ob = 6 + cnt["o"] % 2
                                cnt["o"] += 1
                                for jj in range(4):
                                    mm(ps[ob][:, 0:512], ACTT[ab][:, jj, h2 * 128:(h2 + 1) * 128], wk2(w2, jj, dh * 512, 512),
                                       jj == 0, jj == 3, [("ws", w2), ("ACTT", ab)], PSK(ob))
                                ya = Yacc[:, kt, dh * 512:(dh + 1) * 512]
                                if g == 0:
                                    P.op("dve", lambda e, ya=ya, ob=ob: e.tensor_copy(ya, ps[ob][:, 0:512]), reads=[],
                                         writes=[PSK(ob), ("Yacc", kt)])
                                else:
                                    tt(ya, ps[ob][:, 0:512], ya, ALU.add, [("Yacc", kt)], [PSK(ob), ("Yacc", kt)])
                        P.cond_end()
                for k in range(16):
                    r0 = ex * CAP + p_ * SEQ + k * 128
                    P.cond_begin(jb + k // NQ + 1)
                    sp_dma(yb_d[r0:r0 + 128, :], Yacc[:, k, :], [("Yacc", k)], [("yb", ex, p_, k)])
                    P.cond_end()
        P.barrier()

        off = OV
        NCB = 4
        YA, YB2, OO, XC8 = [], [], [], []
        for i in range(NCB):
            t_, off = A(f"mm_YA{i}", [D], F32, off)
            YA.append(t_)
            t_, off = A(f"mm_YB2{i}", [D], F32, off)
            YB2.append(t_)
            t_, off = A(f"mm_OO{i}", [D], F32, off)
            OO.append(t_)
            t_, off = A(f"mm_XC8{i}", [8, 128], F32, off)
            XC8.append(t_)
        allyb = [("yb", a_, p_, b_) for a_ in range(NEXP) for p_ in range(NPASS) for b_ in range(16)]
        n_ = 0
        for s in seqs:
            W1, W2, SI = PS_[("W1", s)], PS_[("W2", s)], PS_[("SI", s)]
            for jt in range(16):
                b = n_ % NCB
                pb2 = n_ % 2
                n_ += 1
                ti = 1 + jt // 4
                c0 = CTX + jt * 128
                for k, dst, dk in ((0, YA, "YA"), (1, YB2, "YB2")):
                    P.dma("pool", lambda e, b=b, jt=jt, k=k, dst=dst, SI=SI: e.indirect_dma_start(
                        out=dst[b][:, :], out_offset=None, in_=yb_d,
                        in_offset=bass.IndirectOffsetOnAxis(ap=SI[:, jt, k:k + 1], axis=0)),
                        reads=allyb + [("SI", s)], writes=[(dk, b)])
                sp_dma(XC8[b][:, :, :], xres_d[s, :, :, c0:c0 + 128], [("xd", ti)], [("XC8", b)])
                ts(OO[b][:, :], YA[b][:, :], W1[:, jt, 0:1], None, ALU.mult, None, [("YA", b), ("W1", s)], [("OO", b)])
                stt(OO[b][:, :], YB2[b][:, :], W2[:, jt, 0:1], OO[b][:, :], ALU.mult, ALU.add, [("YB2", b), ("W2", s), ("OO", b)],
                    [("OO", b)])
                for c in range(8):
                    pbk = 2 + 2 * pb2 + c // 4
                    P.op("pe", lambda e, b=b, c=c, pbk=pbk: e.transpose(ps[pbk][:, (c % 4) * 128:(c % 4 + 1) * 128],
                                                                        OO[b][:, c * 128:(c + 1) * 128], ident_f[:, :]),
                         reads=[("OO", b), "ident_f"], writes=[PSK(pbk)])
                for c in range(8):
                    pbk = 2 + 2 * pb2 + c // 4
                    stt(XC8[b][:, c, :], ps[pbk][:, (c % 4) * 128:(c % 4 + 1) * 128], modv(l, 5, c, s), XC8[b][:, c, :],
                        ALU.mult, ALU.add, ["mod", ("XC8", b)], [PSK(pbk), ("XC8", b)])
                sp_dma(outT_d[s, :, :, jt * 128:(jt + 1) * 128], XC8[b][:, :, :], [("XC8", b)], [("out", s, jt)])
        P.barrier()
        return None

    result = None
    if stage in ("full", "seq1"):
        seqs = (1,) if stage == "seq1" else (0, 1)
        for s in seqs:
            state["x_in_res"] = False
            for l in range(2):
                token_mixer(l, s)
                if l == 1 and SPARSE_MOE:
                    if not MERGED_MOE:
                        ffn_moe_sparse(l, s)
                else:
                    ffn(l, s)
        if SPARSE_MOE and MERGED_MOE:
            moe_merged(1, seqs)
    elif si >= 1:
        result = token_mixer(0, 0)
        if result is None and stage in ("G0", "H0", "F1", "G1", "G1a", "H1"):
            result = ffn(0, 0)
            if result is None and stage in ("F1", "G1", "G1a", "H1"):
                result = token_mixer(1, 0)
                if result is None and stage in ("G1", "G1a", "H1"):
                    result = ffn_moe_sparse(1, 0) if SPARSE_MOE else ffn(1, 0)

    if debug is not None:
        if stage == "pro":
            sp_dma(dbg_d.rearrange("p (a b) -> p a b", b=4), mod[:, :, :, :].rearrange("p l a b -> p (l a) b"), ["mod"], ["dbg"])
        elif stage in ("F0", "H0", "F1"):
            sp_dma(dbg_d, xres_d[0], [("xd", t) for t in range(5)], ["dbg"])
        elif result == "done":
            pass
        elif result is not None:
            src_t, keys = result
            DT, _ = A("DT", [NT], F32, (A.limit - NT * 4 - 64) // 32 * 32)
            for c in range(8):
                act(DT[:, :], src_t[:, c, :], AF.Identity, keys, ["DT"])
                sp_dma(dbg_d[:, c, :], DT[:, :], ["DT"], ["dbg"])
    P.emit(nc)
    return nc


def kernel(**inputs):
    inp = {k: np.asarray(v, np.float32) for k, v in inputs.items()}
    shared = build_shared(inp)
    nc = build_program("full")
    in_maps = []
    for core in range(NCORES):
        m = dict(shared)
        m.update(build_core_inputs(inp, core))
        in_maps.append(m)
    res = run_bass_kernel_spmd(nc, in_maps, core_ids=list(range(NCORES)))
    out = np.empty((2 * NCORES, SEQ, D), np.float32)
    for core in range(NCORES):
        oT = np.asarray(res.results[core]["outT"])
        out[2 * core:2 * core + 2] = oT.transpose(0, 3, 2, 1).reshape(2, SEQ, D)
    return out
```

```python
import contextlib
import numpy as np
import concourse.bass as bass
import concourse.mybir as mybir
from concourse.bass_utils import run_bass_kernel_spmd

F32 = mybir.dt.float32
BF16 = mybir.dt.bfloat16
AF = mybir.ActivationFunctionType
ALU = mybir.AluOpType
AX = mybir.AxisListType

NCORES = 8
D = 1024
NCH = 8
CTX = 256
SEQ = 2048
NT = CTX + SEQ
TILES = [(0, 256), (256, 512), (768, 512), (1280, 512), (1792, 512)]
D_FF = 2816
D_FFE = 3584
NEXP = 8
EPS = 1e-6
NSLOT = 8
NRING = 5
SLAB = 4096
WCH = 16
SPARSE_MOE = True
MOE_TSZ = 512
JCLAMP = None
MERGED_MOE = True
ZERO_HG = True
SB_BASE = 16384 + 128


class Ins:
    __slots__ = ("eng", "fn", "reads", "writes", "dma", "deps", "sig", "semkey", "val", "slot", "cond")


class Prog:
    ENGS = ["pe", "act", "dve", "pool", "sp"]

    def __init__(self):
        self.ins = []
        self.cur_cond = None
        self.ncond = 0
        self.cond_thr = {}

    def op(self, eng, fn, reads=(), writes=()):
        i = Ins()
        i.eng, i.fn, i.reads, i.writes, i.dma = eng, fn, tuple(reads), tuple(writes), False
        i.sig, i.deps, i.semkey, i.val, i.slot = False, (), None, 0, 0
        i.cond = self.cur_cond
        self.ins.append(i)
        return i

    def cond_begin(self, thr):
        self.ncond += 1
        self.cur_cond = self.ncond
        self.cond_thr[self.ncond] = thr

    def cond_end(self):
        self.cur_cond = None

    def load_reg(self, ap, key, engines=("pe", "act", "dve")):
        for e in engines:
            self.op(e, ("REGLOAD", ap), reads=[key])

    def dma(self, q, fn, reads=(), writes=()):
        i = self.op(q, fn, reads, writes)
        i.dma = True
        return i

    def barrier(self):
        for e in ("pe", "act", "dve", "sp"):
            self.op(e, lambda en: en.nop(), writes=("_bar",))

    def resolve(self):
        last_w = {}
        readers = {}
        ndma = {e: 0 for e in self.ENGS}
        slot_last = {e: {} for e in self.ENGS}
        for idx, I in enumerate(self.ins):
            reads = I.reads
            if I.eng != "pool" and "_bar" not in I.writes:
                reads = reads + ("_bar",)
            deps = {}
            for k in reads:
                j = last_w.get(k)
                if j is not None:
                    deps[j] = True
            for k in I.writes:
                j = last_w.get(k)
                if j is not None:
                    deps.setdefault(j, False)
                r = readers.get(k)
                if r:
                    for j2 in r[0].values():
                        deps.setdefault(j2, False)
                    for j2 in r[1]:
                        deps.setdefault(j2, False)
            final = []
            for j, raw in deps.items():
                J = self.ins[j]
                if J.dma:
                    final.append(j)
                elif J.eng == I.eng:
                    if I.dma or (raw and I.eng != "pe"):
                        final.append(j)
                else:
                    final.append(j)
            if I.dma:
                q = I.eng
                slot = ndma[q] % NSLOT
                prev = slot_last[q].get(slot)
                if prev is not None:
                    final.append(prev)
                slot_last[q][slot] = idx
                I.slot = slot
                ndma[q] += 1
            I.deps = final
            for j in final:
                self.ins[j].sig = True
            for k in reads:
                r = readers.setdefault(k, ({}, []))
                if I.dma:
                    r[1].append(idx)
                else:
                    r[0][I.eng] = idx
            for k in I.writes:
                last_w[k] = idx
                readers[k] = ({}, [])
        cnt = {e: 0 for e in self.ENGS}
        dcnt = {}
        for I in self.ins:
            if I.dma:
                key = ("d", I.eng, I.slot)
                dcnt[key] = dcnt.get(key, 0) + 16
                I.semkey, I.val = key, dcnt[key]
            elif I.sig:
                cnt[I.eng] += 1
                I.semkey, I.val = ("e", I.eng), cnt[I.eng]
        self.final_dma = dict(dcnt)

    def emit(self, nc, final_waits_on="sp"):
        self.resolve()
        keys = [("e", e) for e in self.ENGS]
        for e in self.ENGS:
            if any(I.dma and I.eng == e for I in self.ins):
                keys += [("d", e, s) for s in range(NSLOT)]
        with contextlib.ExitStack() as st:
            sems = {}
            for k in keys:
                sems[k] = st.enter_context(nc.semaphore("s_" + "_".join(str(x) for x in k)))
            block = st.enter_context(nc.Block())
            per = {e: [I for I in self.ins if I.eng == e] for e in self.ENGS}

            def replay(ename, eng):
                seen = {}
                reg = {}

                def do_waits(I, seen, only_external=None):
                    waits = {}
                    for j in I.deps:
                        J = self.ins[j]
                        if only_external is not None and J.cond == only_external:
                            continue
                        if waits.get(J.semkey, 0) < J.val:
                            waits[J.semkey] = J.val
                    for sk, v in waits.items():
                        if seen.get(sk, 0) < v:
                            eng.wait_ge(sems[sk], v)
                            seen[sk] = v

                def run(I):
                    if isinstance(I.fn, tuple):
                        if "r" not in reg:
                            reg["r"] = eng.alloc_register("rj_" + ename)
                        r = eng.reg_load(reg["r"], I.fn[1])
                    else:
                        r = I.fn(eng)
                    if I.dma:
                        r.then_inc(sems[I.semkey], 16)
                    elif I.sig:
                        r.then_inc(sems[I.semkey], 1)

                lst = per[ename]
                n = len(lst)
                p = 0
                while p < n:
                    I = lst[p]
                    if I.cond is None:
                        do_waits(I, seen)
                        run(I)
                        p += 1
                        continue
                    cid = I.cond
                    q = p
                    while q < n and lst[q].cond == cid:
                        q += 1
                    body = lst[p:q]
                    for B in body:
                        do_waits(B, seen, only_external=cid)
                    snap = dict(seen)
                    k = sum(1 for B in body if B.sig and not B.dma)
                    with eng.If_lt(reg["r"], self.cond_thr[cid]):
                        if k > 0:
                            eng.drain().then_inc(sems[("e", ename)], k)
                        for B in body:
                            if B.dma:
                                eng.nop().then_inc(sems[B.semkey], 16)
                        if k == 0 and not any(B.dma for B in body):
                            eng.nop()
                    with eng.Else():
                        inner = dict(snap)
                        for B in body:
                            do_waits(B, inner)
                            run(B)
                    seen = snap
                    p = q
                if ename == final_waits_on:
                    for sk, v in self.final_dma.items():
                        if seen.get(sk, 0) < v:
                            eng.wait_ge(sems[sk], v)

            @block.tensor
            def _(e):
                replay("pe", e)

            @block.scalar
            def _(e):
                replay("act", e)

            @block.vector
            def _(e):
                replay("dve", e)

            @block.gpsimd
            def _(e):
                replay("pool", e)

            @block.sync
            def _(e):
                replay("sp", e)


def na_plan():
    pats = []
    groups = {}
    plan = []
    for qp in range(16):
        r0 = 2 * qp
        s = [min(max(r - 4, 0), 24) for r in (r0, r0 + 1)]
        first = s[0] // 2
        last = (s[1] + 7) // 2
        keys = []
        for m in range(first, last + 1):
            key = []
            for kl in range(2):
                kr = 2 * m + kl
                for ql in range(2):
                    r = r0 + ql
                    valid = s[ql] <= kr < s[ql] + 8
                    key.append(kr - r + 7 if valid else None)
            keys.append(tuple(key))
        gk = tuple(keys)
        if gk not in groups:
            groups[gk] = len(pats)
            pats.extend(keys)
        base = groups[gk]
        plan.append([(first + i, base + i) for i in range(len(keys))])
    return pats, plan


NA_PATS, NA_PLAN = na_plan()
NPAT = len(NA_PATS)


def slab_order():
    pro = [("mod", l, g) for l in range(2) for g in range(12)]
    seq = []
    for l in range(2):
        ntile = 5 if l == 0 else 4
        seq += [("rnn", l, i) for i in range(4)]
        for g in range(2):
            seq += [("rnno", l, g), ("gr", l, g)]
        seq += [("kvq", l, c) for c in range(8)]
        for t in range(ntile):
            for g in range(2):
                seq += [("nao", l, g), ("gn", l, g)]
            for g in range(2):
                seq += [("out", l, g)]
        if l == 0:
            for g in range(6):
                seq += [("f1", g), ("f3", g), ("f2", g)]
        else:
            for e in range(NEXP):
                for g in range(7):
                    seq += [("m1", e, g), ("m3", e, g), ("m2", e, g)]
    return pro, seq


def unique_slabs():
    pro, seq = slab_order()
    names = []
    seen = set()
    for n in pro + seq:
        if n not in seen:
            seen.add(n)
            names.append(n)
    return names


SLAB_NAMES = unique_slabs()
SLAB_IDX = {n: i for i, n in enumerate(SLAB_NAMES)}


def _slab_cols(W, cols):
    S = W[:, cols]
    return np.ascontiguousarray(S.reshape(8, 128, 512).transpose(1, 0, 2)).reshape(128, SLAB)


def _slab_rows(W2, r0):
    blk = np.zeros((512, 1024), np.float32)
    n = max(0, min(512, W2.shape[0] - r0))
    blk[:n] = W2[r0:r0 + n]
    return np.ascontiguousarray(blk.reshape(4, 128, 1024).transpose(1, 0, 2)).reshape(128, SLAB)


def _cols_pad(W, c0, n):
    idx = np.full(512, c0, np.int64)
    idx[:n] = np.arange(c0, c0 + n)
    return idx


def build_wstream(inp):
    ar = np.arange
    out = np.empty((len(SLAB_NAMES), 128, SLAB), np.float32)
    for i, nm in enumerate(SLAB_NAMES):
        k = nm[0]
        if k == "mod":
            _, l, g = nm
            out[i] = _slab_cols(inp["w_mod"][l], ar(g * 512, g * 512 + 512))
        elif k == "rnn":
            _, l, j = nm
            cols = np.concatenate([ar(c * 128, c * 128 + 128) if which == 0 else ar(3072 + c * 128, 3072 + c * 128 + 128)
                                   for c in (2 * j, 2 * j + 1) for which in (0, 1)])
            out[i] = _slab_cols(inp["w_in"][l], cols)
        elif k == "rnno":
            _, l, g = nm
            out[i] = _slab_cols(inp["w_rnn_o"][l], ar(g * 512, g * 512 + 512))
        elif k == "gr":
            _, l, g = nm
            out[i] = _slab_cols(inp["w_in"][l], ar(5120 + g * 512, 5120 + g * 512 + 512))
        elif k == "kvq":
            _, l, c = nm
            cols = np.concatenate([ar(1024 + c * 128, 1024 + c * 128 + 128), ar(2048 + c * 128, 2048 + c * 128 + 128),
                                   ar(4096 + c * 128, 4096 + c * 128 + 128), ar(4096 + c * 128, 4096 + c * 128 + 128)])
            out[i] = _slab_cols(inp["w_in"][l], cols)
        elif k == "nao":
            _, l, g = nm
            out[i] = _slab_cols(inp["w_na_o"][l], ar(g * 512, g * 512 + 512))
        elif k == "gn":
            _, l, g = nm
            out[i] = _slab_cols(inp["w_in"][l], ar(6144 + g * 512, 6144 + g * 512 + 512))
        elif k == "out":
            _, l, g = nm
            out[i] = _slab_cols(inp["w_out"][l], ar(g * 512, g * 512 + 512))
        elif k in ("f1", "f3"):
            _, g = nm
            W = inp["ffn_w1"][0] if k == "f1" else inp["ffn_w3"][0]
            n = min(512, D_FF - g * 512)
            out[i] = _slab_cols(W, _cols_pad(W, g * 512, n))
        elif k == "f2":
            _, g = nm
            out[i] = _slab_rows(inp["ffn_w2"][0], g * 512)
        elif k in ("m1", "m3"):
            _, e, g = nm
            W = inp["moe_w1"][0][e] if k == "m1" else inp["moe_w3"][0][e]
            out[i] = _slab_cols(W, ar(g * 512, g * 512 + 512))
        elif k == "m2":
            _, e, g = nm
            out[i] = _slab_rows(inp["moe_w2"][0][e], g * 512)
        else:
            raise KeyError(nm)
    return out


def _pm(v):
    v = np.asarray(v, np.float32)
    lead = v.shape[:-1]
    return np.ascontiguousarray(np.moveaxis(v.reshape(*lead, 8, 128), -1, 0))


def build_shared(inp):
    sh = {}
    wsr = build_wstream(inp)
    for i in range((len(SLAB_NAMES) + WCH - 1) // WCH):
        sh[f"wstream{i}"] = wsr[i * WCH:(i + 1) * WCH]
    sh["bmodT"] = np.ascontiguousarray(np.moveaxis(inp["b_mod"].reshape(2, 48, 128), -1, 0))
    sh["convw"] = np.ascontiguousarray(np.moveaxis(inp["conv_w"].reshape(2, 4, 8, 128), -1, 0).transpose(0, 1, 3, 2))
    sh["convb"] = _pm(inp["conv_b"])
    sh["lam"] = _pm(inp["rg_lambda"])
    sh["rgb"] = _pm(inp["rg_b"])
    g = np.stack([inp["q_gain"], inp["k_gain"]], 1)
    sh["gains"] = np.ascontiguousarray(np.concatenate([g, g], -1).transpose(2, 0, 1))
    rgw = inp["rg_w"]
    bd = np.zeros((2, 128, 2, 2, 8, 128), np.float32)
    for hb in range(2):
        blk = rgw[:, :, :, hb::2]
        bd[:, hb * 64:(hb + 1) * 64, :, :, :, hb * 64:(hb + 1) * 64] = np.moveaxis(blk, 4, 1)
    sh["rgw"] = bd.reshape(2, 128, 32 * 128)
    kp = np.arange(128)
    kl, kc = kp // 64, kp % 64
    ql, qc = kp // 64, kp % 64
    wstart = np.clip(qc - 8, 0, 48)
    colv = (kc[:, None] >= wstart[None, :]) & (kc[:, None] < wstart[None, :] + 16)
    coff = np.clip(kc[:, None] - qc[None, :] + 15, 0, 30)
    bias = np.zeros((2, 8, 128, 2, NPAT, 128), np.float32)
    mask = np.zeros((128, NPAT, 128), np.float32)
    rpb = inp["rpb"]
    for pc, key in enumerate(NA_PATS):
        drm = np.full((128, 128), -1, np.int64)
        for a in range(2):
            for b in range(2):
                dr = key[a * 2 + b]
                if dr is not None:
                    sel = (kl[:, None] == a) & (ql[None, :] == b)
                    drm[sel] = dr
        valid = (drm >= 0) & colv
        mask[:, pc, :] = valid
        drc = np.where(drm >= 0, drm, 0)
        gathered = rpb[:, :, drc, coff]
        gathered = np.where(valid[None, None], gathered, np.float32(0))
        bias[:, :, :, :, pc, :] = gathered.reshape(2, 8, 2, 128, 128).transpose(0, 1, 3, 2, 4)
    sh["biasG"] = bias.reshape(2, 8, 128, 2 * NPAT * 128)
    sh["maskG"] = mask.reshape(128, NPAT * 128)
    sh["router"] = np.ascontiguousarray(inp["router"][0].reshape(8, 128, 8).transpose(1, 0, 2)).reshape(128, 64)
    sh["ident"] = np.eye(128, dtype=np.float32)
    sh["ltri"] = np.triu(np.ones((128, 128), np.float32), 1)
    return sh


def build_core_inputs(inp, core):
    b0 = 2 * core
    toks = np.concatenate([inp["ctx"][b0:b0 + 2], inp["x"][b0:b0 + 2]], axis=1)
    xT = np.ascontiguousarray(toks.reshape(2, NT, 8, 128).transpose(0, 3, 2, 1))
    cv = np.zeros((4, 1024), np.float32)
    cv[0:2] = inp["c"][b0:b0 + 2]
    cv[2] = inp["c_ctx"]
    scT = np.ascontiguousarray(cv.reshape(4, 8, 128).transpose(2, 1, 0))
    return {"xT": xT, "scT": scT}


def _nbytes(dt):
    return 2 if dt == BF16 else 4


class SBAlloc:
    def __init__(self, nc, limit):
        self.nc, self.off, self.limit, self.n = nc, SB_BASE, SB_BASE + limit - 256, 0

    def __call__(self, name, free_shape, dt, off=None):
        size = int(np.prod(free_shape)) * _nbytes(dt)
        size = (size + 31) // 32 * 32
        if off is None:
            off = self.off
            self.off += size
        assert off + size <= self.limit, (name, off, size, self.limit)
        self.n += 1
        return self.nc.alloc_sbuf_tensor_at(f"{name}_{self.n}", [128] + list(free_shape), dt, offset=off), off + size


def build_program(stage="full", debug=None, nslabs=None):
    nc = bass.Bass("TRN2", target_bir_lowering=False)
    P = Prog()
    limit = nc.sbuf_bytes_remaining
    A = SBAlloc(nc, limit)

    def din(name, shape):
        return nc.dram_tensor(name, list(shape), F32, kind="ExternalInput").ap()

    xT_d = din("xT", [2, 128, 8, NT])
    scT_d = din("scT", [128, 8, 4])
    nsl = nslabs or len(SLAB_NAMES)
    ws_d = [din(f"wstream{i}", [min(WCH, nsl - i * WCH), 128, SLAB]) for i in range((nsl + WCH - 1) // WCH)]
    bmod_d = din("bmodT", [128, 2, 48])
    convw_d = din("convw", [128, 2, 8, 4])
    convb_d = din("convb", [128, 2, 8])
    lam_d = din("lam", [128, 2, 2, 8])
    rgb_d = din("rgb", [128, 2, 2, 2, 8])
    gains_d = din("gains", [128, 2, 2])
    rgw_d = din("rgw", [2, 128, 32 * 128])
    biasG_d = din("biasG", [2, 8, 128, 2 * NPAT * 128])
    maskG_d = din("maskG", [128, NPAT * 128])
    router_d = din("router", [128, 64])
    ident_d = din("ident", [128, 128])
    ltri_d = din("ltri", [128, 128])
    outT_d = nc.dram_tensor("outT", [2, 128, 8, SEQ], F32, kind="ExternalOutput").ap()
    xres_d = nc.dram_tensor("xres", [2, 128, 8, NT], F32, kind="Internal").ap()
    mrnn_d = nc.dram_tensor("mrnn", [128, 8, NT], F32, kind="Internal").ap()
    hg_d = nc.dram_tensor("hg", [NEXP * SEQ * 2, D], BF16, kind="Internal").ap()
    yb_d = nc.dram_tensor("yb", [NEXP * SEQ * 2, D], F32, kind="Internal").ap()
    dbg_d = None
    if debug is not None:
        dbg_d = nc.dram_tensor("dbg", list(debug), F32, kind="ExternalOutput").ap()

    ones_bf, _ = A("ones_bf", [128], BF16)
    blk_bf, _ = A("blk_bf", [128], BF16)
    onesV, _ = A("onesV", [192], BF16)
    ident_f, _ = A("ident_f", [128], F32)
    ones_f, _ = A("ones_f", [128], F32)
    ident_bf, _ = A("ident_bf", [128], BF16)
    ltri_bf, _ = A("ltri_bf", [128], BF16)
    ZT, _ = A("ZT", [D], BF16)
    scT, _ = A("scT", [8, 4], F32)
    scb, _ = A("scb", [8, 4], BF16)
    bmodT, _ = A("bmodT", [2, 48], F32)
    mod, _ = A("mod", [2, 48, 4], F32)
    convw, _ = A("convw", [2, 8, 4], F32)
    convb, _ = A("convb", [2, 8], F32)
    lam, _ = A("lam", [2, 2, 8], F32)
    c1, _ = A("c1", [2, 2, 8], F32)
    c2, _ = A("c2", [2, 2, 8], F32)
    rgb, _ = A("rgb", [2, 2, 2, 8], F32)
    gains, _ = A("gains", [2, 2], F32)
    qg, _ = A("qg", [2], F32)
    rgw, _ = A("rgw", [32, 128], BF16)
    maskG, _ = A("maskG", [NPAT, 128], BF16)
    router, _ = A("router", [8, 8], F32)
    routb, _ = A("routb", [2], F32)
    WS = [A(f"ws{i}", [SLAB], BF16)[0] for i in range(NRING)]
    HT_OFF = A.off
    HT, _ = A("HT", [8, NT], BF16)
    NXR = 4
    for i_ in range(NXR):
        WS.append(A(f"wsx{i_}", [SLAB], BF16, HT_OFF + i_ * SLAB * 2)[0])
    XBASE = A.off

    ps = [nc.alloc_psum_tensor(f"ps{i}", [128, 512], F32) for i in range(8)]

    def PSK(i):
        return ("ps", i)

    ring = {"n": 0}

    def load_slab(name, big=False):
        i = ring["n"] % (NRING + NXR if big else NRING)
        ring["n"] += 1
        src = ws_d[SLAB_IDX[name] // WCH][SLAB_IDX[name] % WCH]
        dst = WS[i]
        wr = [("ws", i)] + ([("HT", t_) for t_ in range(5)] if i >= NRING else [])
        P.dma("pool", lambda e, dst=dst, src=src: e.dma_start(out=dst[:, :], in_=src), writes=wr)
        return i

    def wk(i, k, c0, n):
        return WS[i][:, k * 512 + c0: k * 512 + c0 + n]

    def wk2(i, j, c0, n):
        return WS[i][:, j * 1024 + c0: j * 1024 + c0 + n]

    def mm(out, lhsT, rhs, start, stop, reads, pk):
        P.op("pe", lambda e: e.matmul(out, lhsT, rhs, start=start, stop=stop), reads=reads, writes=[pk])

    def act(out, in_, func, reads, writes, bias=None, scale=None):
        kw = {}
        if bias is not None:
            kw["bias"] = bias
        if scale is not None:
            kw["scale"] = scale
        P.op("act", lambda e: e.activation(out, in_, func, **kw), reads=reads, writes=writes)

    def tt(out, in0, in1, op, reads, writes, eng="dve"):
        P.op(eng, lambda e: e.tensor_tensor(out, in0, in1, op), reads=reads, writes=writes)

    def ts(out, in0, s1, s2, op0, op1, reads, writes, eng="dve"):
        if s2 is None:
            P.op(eng, lambda e: e.tensor_scalar(out, in0, s1, None, op0), reads=reads, writes=writes)
        else:
            P.op(eng, lambda e: e.tensor_scalar(out, in0, s1, s2, op0, op1), reads=reads, writes=writes)

    def stt(out, in0, scalar, in1, op0, op1, reads, writes):
        P.op("dve", lambda e: e.scalar_tensor_tensor(out, in0, scalar, in1, op0, op1), reads=reads, writes=writes)

    def sp_dma(out, in_, reads, writes):
        P.dma("sp", lambda e: e.dma_start(out=out, in_=in_), reads=reads, writes=writes)

    def pool_dma(out, in_, reads, writes):
        P.dma("pool", lambda e: e.dma_start(out=out, in_=in_), reads=reads, writes=writes)

    P.op("dve", lambda e: e.memset(ones_bf[:, :], 1.0), writes=["ones_bf"])
    P.op("dve", lambda e: e.memset(ones_f[:, :], 1.0), writes=["ones_f"])
    P.op("dve", lambda e: e.memset(blk_bf[:, :], 0.0), writes=["blk_bf"])
    P.op("dve", lambda e: e.memset(blk_bf[0:64, 0:64], 1.0), writes=["blk_bf"])
    P.op("dve", lambda e: e.memset(blk_bf[64:128, 64:128], 1.0), writes=["blk_bf"])
    P.op("dve", lambda e: e.memset(onesV[:, :], 1.0), writes=["onesV"])
    P.op("dve", lambda e: e.memset(onesV[:, 64:128], 0.0), writes=["onesV"])
    sp_dma(ident_f[:, :], ident_d, [], ["ident_f"])
    pool_dma(ident_bf[:, :], ident_d, [], ["ident_bf"])
    pool_dma(ltri_bf[:, :], ltri_d, [], ["ltri_bf"])
    sp_dma(scT[:, :, :], scT_d, [], ["scT"])
    sp_dma(bmodT[:, :, :], bmod_d, [], ["bmodT"])
    sp_dma(convw[:, :, :, :], convw_d, [], ["convw"])
    sp_dma(convb[:, :, :], convb_d, [], ["convb"])
    sp_dma(lam[:, :, :, :], lam_d, [], ["lam"])
    sp_dma(rgb[:, :, :, :, :], rgb_d, [], ["rgb"])
    sp_dma(gains[:, :, :], gains_d, [], ["gains"])
    sp_dma(router[:, :, :], router_d.rearrange("p (k e) -> p k e", e=8), [], ["router"])
    pool_dma(maskG[:, :, :], maskG_d.rearrange("p (a b) -> p a b", b=128), [], ["maskG"])
    P.op("dve", lambda e: e.memset(ZT[:, :], 0.0), writes=["ZT"])

    zf_state = {"n": 0}

    def zero_fill_hg(nblk=64):
        tot = NEXP * SEQ * 2 // 128
        for blk in range(zf_state["n"], min(tot, zf_state["n"] + nblk)):
            sp_dma(hg_d[blk * 128:(blk + 1) * 128, :], ZT[:, :], ["ZT"], [("hgz", blk)])
        zf_state["n"] = min(tot, zf_state["n"] + nblk)
    act(c1[:, :, :, :], lam[:, :, :, :], AF.Exp, ["lam"], ["c1"], scale=-1.0)
    act(c1[:, :, :, :], c1[:, :, :, :], AF.Ln, ["c1"], ["c1"], bias=1.0)
    ts(c2[:, :, :, :], c1[:, :, :, :], -16.0, None, ALU.mult, None, ["c1"], ["c2"])
    ts(c1[:, :, :, :], c1[:, :, :, :], -8.0, None, ALU.mult, None, ["c1", "c2"], ["c1"])
    act(scb[:, :, :], scT[:, :, :], AF.Silu, ["scT"], ["scb"])
    for l in range(2):
        for g in range(12):
            i = load_slab(("mod", l, g), big=True)
            for j in range(4):
                col = (g * 4 + j) * 4
                for k in range(8):
                    mm(ps[0][:, col:col + 4], wk(i, k, j * 128, 128), scb[:, k, :], k == 0, k == 7,
                       [("ws", i), "scb"], PSK(0))
        pv = ps[0][:, 0:192].rearrange("p (a b) -> p a b", b=4)
        for j in range(3):
            tt(mod[:, l, :, j], pv[:, :, j], bmodT[:, l, :], ALU.add, ["bmodT"], [PSK(0), "mod"])
    for l in range(2):
        for m in (1, 4):
            ts(mod[:, l, m * 8:(m + 1) * 8, :], mod[:, l, m * 8:(m + 1) * 8, :], 1.0, None, ALU.add, None, ["mod"], ["mod"])

    def modv(l, m, c, col):
        return mod[:, l, m * 8 + c, col:col + 1]

    stages = ["pro", "A0", "B0", "C0", "D0", "E0", "F0", "G0", "H0", "F1", "G1", "G1a", "H1", "seq1", "full"]
    si = stages.index(stage)

    state = {"x_in_res": False}

    def modulate(l, s, which, tiles, xsrc, X, moe=False, after_tile=None):
        m_sh, m_sc = (0, 1) if which == 0 else (3, 4)
        for ti in tiles:
            t0, w = TILES[ti]
            col = 2 if ti == 0 else s
            xap, xkeys = xsrc(ti)
            SQ, RS = X["SQ"], X["RS"]
            for c in range(8):
                act(SQ[:, c, 0:w], xap(c), AF.Square, xkeys, [("SQ", c)])
            for c in range(8):
                mm(ps[1][:, 0:w], ones_bf[:, :], SQ[:, c, 0:w], c == 0, c == 7, [("SQ", c), "ones_bf"], PSK(1))
            act(RS[:, 0:w], ps[1][:, 0:w], AF.Sqrt, [], [PSK(1), "RS"], bias=EPS, scale=1.0 / D)
            P.op("dve", lambda e, w=w: e.reciprocal(RS[:, 0:w], RS[:, 0:w]), reads=["RS"], writes=["RS"])
            for c in range(8):
                if moe:
                    tmp, tk = X["TMP8"][:, c, 0:w], ("TMP8", c)
                else:
                    tmp, tk = X["TMP"][c % 2][:, 0:w], ("TMP", c % 2)
                stt(tmp, xap(c), modv(l, m_sc, c, col), RS[:, 0:w], ALU.mult, ALU.mult, list(xkeys) + ["mod", "RS"], [tk])
                act(HT[:, c, t0:t0 + w], tmp, AF.Identity, [tk, "mod"], [("HT", ti)], bias=modv(l, m_sh, c, col))
            if after_tile is not None:
                after_tile(ti)

    def token_mixer(l, s):
        ctx_out = (l == 0)
        tiles = [0, 1, 2, 3, 4]
        otiles = tiles if ctx_out else [1, 2, 3, 4]
        off = XBASE
        YB, off = A("YB", [8, NT], BF16, off)
        TB = off
        pool_dma(rgw[:, :, :], rgw_d[l].rearrange("p (a b) -> p a b", b=128), [], ["rgw"])
        ts(qg[:, 0:1], gains[:, l, 0:1], 0.125, None, ALU.mult, None, ["gains"], ["qg"])

        off = TB
        XL = []
        for i in range(2):
            t_, off = A(f"XL{i}", [8, 512], F32, off)
            XL.append(t_)
        X = {}
        X["SQ"], off = A("SQ", [8, 512], BF16, off)
        X["RS"], off = A("RS", [512], F32, off)
        X["TMP"] = []
        for i in range(2):
            t_, off = A(f"TMP{i}", [512], F32, off)
            X["TMP"].append(t_)
        xd = xres_d if state["x_in_res"] else xT_d

        def xsrc(ti):
            t0, w = TILES[ti]
            b = ti % 2
            sp_dma(XL[b][:, :, 0:w], xd[s, :, :, t0:t0 + w], [("xd", ti)], [("XL", b)])
            return (lambda c: XL[b][:, c, 0:w]), [("XL", b)]

        modulate(l, s, 0, tiles, xsrc, X)
        P.barrier()
        if stage == "A0":
            return HT, [("HT", t) for t in range(5)]

        off = TB
        S0s = []
        for i_ in range(2):
            t_, off = A(f"S0{i_}", [2312], F32, off)
            S0s.append(t_)
        XC, off = A("XC", [NT], F32, off)
        S2, off = A("S2", [NT], F32, off)
        S3, off = A("S3", [NT], F32, off)
        HF, off = A("HF", [NT], F32, off)
        HR, off = A("HR", [NT], F32, off)
        XCb, off = A("XCb", [NT], BF16, off)
        GT, off = A("GT", [512], F32, off)

        def xrp_pos(t0):
            return 2 + t0 if t0 < CTX else 261 + (t0 - CTX)

        for c in range(8):
            S0 = S0s[c % 2]
            S0k = ("S0", c % 2)
            if c % 2 == 0:
                wsi = load_slab(("rnn", l, c // 2))
            cb = (c % 2) * 256
            for a, b in ((0, 2), (258, 261), (2309, 2312)):
                P.op("dve", lambda e, a=a, b=b, S0=S0: e.memset(S0[:, a:b], 0.0), writes=[S0k])
            for ti in tiles:
                t0, w = TILES[ti]
                pb = 2 + (ti % 2)
                for k in range(8):
                    mm(ps[pb][:, 0:w], wk(wsi, k, cb, 128), HT[:, k, t0:t0 + w], k == 0, k == 7,
                       [("ws", wsi), ("HT", ti)], PSK(pb))
                p0 = xrp_pos(t0)
                act(S0[:, p0:p0 + w], ps[pb][:, 0:w], AF.Identity, [], [PSK(pb), S0k])
            for (d0, n, base) in ((0, CTX, 2), (CTX, SEQ, 261)):
                ts(XC[:, d0:d0 + n], S0[:, base - 2:base - 2 + n], convw[:, l, c, 0:1], convb[:, l, c:c + 1],
                   ALU.mult, ALU.add, [S0k, "convw", "convb"], [("XC", d0)])
                for j in range(1, 4):
                    stt(XC[:, d0:d0 + n], S0[:, base - 2 + j:base - 2 + j + n], convw[:, l, c, j:j + 1], XC[:, d0:d0 + n],
                        ALU.mult, ALU.add, [S0k, "convw", ("XC", d0)], [("XC", d0)])
            act(XCb[:, :], XC[:, :], AF.Identity, [("XC", 0), ("XC", CTX)], ["XCb"])
            for dr in range(2):
                for ti in tiles:
                    t0, w = TILES[ti]
                    for gt in range(2):
                        pb = 4 + gt + 2 * (ti % 2)
                        mm(ps[pb][:, 0:w], rgw[:, (dr * 2 + gt) * 8 + c, :], XCb[:, t0:t0 + w], True, True,
                           ["rgw", "XCb"], PSK(pb))
                        dst = S2 if gt == 0 else S3
                        act(dst[:, t0:t0 + w], ps[pb][:, 0:w], AF.Sigmoid, ["rgb"], [PSK(pb), ("S2" if gt == 0 else "S3")],
                            bias=rgb[:, l, dr, gt, c:c + 1])
                act(S0[:, 0:NT], S2[:, :], AF.Exp, ["S2", "c1"], [S0k], scale=c1[:, l, dr, c:c + 1])
                act(S2[:, :], S2[:, :], AF.Exp, ["S2", "c2"], ["S2"], scale=c2[:, l, dr, c:c + 1])
                act(S2[:, :], S2[:, :], AF.Sqrt, ["S2"], ["S2"], scale=-1.0, bias=1.0)
                tt(S3[:, :], S3[:, :], XC[:, :], ALU.mult, ["S3", ("XC", 0), ("XC", CTX)], ["S3"])
                tt(S3[:, :], S3[:, :], S2[:, :], ALU.mult, ["S3", "S2"], ["S3"])
                if dr == 0:
                    P.op("dve", lambda e, S0=S0: e.tensor_tensor_scan(HF[:, :], S0[:, 0:NT], S3[:, :], 0.0, ALU.mult, ALU.add),
                         reads=[S0k, "S3"], writes=["HF"])
                else:
                    P.op("dve", lambda e, S0=S0: e.tensor_tensor_scan(HR[:, CTX - 1::-1], S0[:, CTX - 1::-1], S3[:, CTX - 1::-1], 0.0,
                                                               ALU.mult, ALU.add),
                         reads=[S0k, "S3"], writes=["HR"])
                    P.op("dve", lambda e, S0=S0: e.tensor_tensor_scan(HR[:, NT - 1:CTX - 1:-1], S0[:, NT - 1:CTX - 1:-1],
                                                               S3[:, NT - 1:CTX - 1:-1], HR[:, 0:1], ALU.mult, ALU.add),
                         reads=[S0k, "S3", "HR"], writes=["HR"])
            tt(HF[:, :], HF[:, :], HR[:, :], ALU.add, ["HF", "HR"], ["HF"])
            for ti in otiles:
                t0, w = TILES[ti]
                pb = 2 + (ti % 2)
                for k in range(8):
                    mm(ps[pb][:, 0:w], wk(wsi, k, cb + 128, 128), HT[:, k, t0:t0 + w], k == 0, k == 7,
                       [("ws", wsi), ("HT", ti)], PSK(pb))
                act(GT[:, 0:w], ps[pb][:, 0:w], AF.Gelu_apprx_tanh, [], [PSK(pb), "GT"])
                tt(YB[:, c, t0:t0 + w], HF[:, t0:t0 + w], GT[:, 0:w], ALU.mult, ["HF", "GT"], [("YB", ti)])
            if stage == "B0" and debug is not None and c == 0:
                pass
        P.barrier()
        if stage == "B0":
            return YB, [("YB", t) for t in range(5)]

        off = TB
        SG, MR = [], []
        for i in range(2):
            t_, off = A(f"SG{i}", [512], F32, off)
            SG.append(t_)
            t_, off = A(f"MR{i}", [512], F32, off)
            MR.append(t_)
        cnt = 0
        for g in range(2):
            wa = load_slab(("rnno", l, g))
            wb = load_slab(("gr", l, g))
            for ti in otiles:
                t0, w = TILES[ti]
                for j in range(4):
                    dc = g * 4 + j
                    b = cnt % 2
                    cnt += 1
                    p1, p2 = 2 + b, 4 + b
                    for k in range(8):
                        mm(ps[p1][:, 0:w], wk(wa, k, j * 128, 128), YB[:, k, t0:t0 + w], k == 0, k == 7,
                           [("ws", wa), ("YB", ti)], PSK(p1))
                    for k in range(8):
                        mm(ps[p2][:, 0:w], wk(wb, k, j * 128, 128), HT[:, k, t0:t0 + w], k == 0, k == 7,
                           [("ws", wb), ("HT", ti)], PSK(p2))
                    act(SG[b][:, 0:w], ps[p2][:, 0:w], AF.Sigmoid, [], [PSK(p2), ("SG", b)])
                    tt(MR[b][:, 0:w], ps[p1][:, 0:w], SG[b][:, 0:w], ALU.mult, [("SG", b)], [PSK(p1), ("MR", b)])
                    sp_dma(mrnn_d[:, dc, t0:t0 + w], MR[b][:, 0:w], [("MR", b)], [("mrnn", dc, ti)])
        P.barrier()

        off = TB
        KT, QT, Vz, Eb = [], [], [], []
        for i in range(2):
            t_, off = A(f"KT{i}", [NT], BF16, off)
            KT.append(t_)
            t_, off = A(f"QT{i}", [NT], BF16, off)
            QT.append(t_)
            t_, off = A(f"Vz{i}", [18, 192], BF16, off)
            Vz.append(t_)
            t_, off = A(f"Eb{i}", [2, NPAT, 128], BF16, off)
            Eb.append(t_)
        SQh, RSh, RD = [], [], []
        for i in range(2):
            t_, off = A(f"SQh{i}", [512], BF16, off)
            SQh.append(t_)
            t_, off = A(f"RSh{i}", [512], F32, off)
            RSh.append(t_)
            t_, off = A(f"RD{i}", [128], F32, off)
            RD.append(t_)
        PT = []
        for hh in range(2):
            row = []
            for i in range(2):
                t_, off = A(f"PT{hh}{i}", [7, 128], BF16, off)
                row.append(t_)
            PT.append(row)
        for i in range(2):
            P.op("dve", lambda e, i=i: e.memset(Vz[i][:, :, 64:128], 0.0), writes=[("Vz", i)])
        acnt = {"n": 0}

        def attend_S(c, qtok0, chunks, out_ti):
            cb_ = c % 2
            i = acnt["n"] % 2
            acnt["n"] += 1
            clist = [(0, None), (1, None)] + [(2 + m, pc) for (m, pc) in chunks]
            nchunk = len(clist)
            for hh in range(2):
                pbase = hh * 64
                bX, bY = 2 + 2 * hh, 3 + 2 * hh
                ptk = ("PT", hh, i)
                pt = PT[hh][i]
                for ci, (jt, pc) in enumerate(clist):
                    bank = bX if ci < 4 else bY
                    col = (ci % 4) * 128
                    mm(ps[bank][:, col:col + 128], KT[cb_][pbase:pbase + 64, jt * 128:(jt + 1) * 128],
                       QT[cb_][pbase:pbase + 64, qtok0:qtok0 + 128], True, True, [("KT", cb_), ("QT", cb_)], PSK(bank))
                n1 = min(4, nchunk)
                act(pt[:, 0:n1, :], ps[bX][:, 0:n1 * 128].rearrange("p (a b) -> p a b", b=128), AF.Exp, [], [PSK(bX), ptk])
                if nchunk > 4:
                    n2 = nchunk - 4
                    act(pt[:, 4:nchunk, :], ps[bY][:, 0:n2 * 128].rearrange("p (a b) -> p a b", b=128), AF.Exp, [],
                        [PSK(bY), ptk])
                if chunks:
                    pc0, n = chunks[0][1], len(chunks)
                    tt(pt[:, 2:2 + n, :], pt[:, 2:2 + n, :], Eb[cb_][:, hh, pc0:pc0 + n, :], ALU.mult, [ptk, ("Eb", cb_)], [ptk])
            return (c, qtok0, clist, out_ti, i)

        def attend_PV(stt_):
            c, qtok0, clist, out_ti, i = stt_
            cb_ = c % 2
            nchunk = len(clist)
            total = 2 * nchunk
            idx = 0
            for hh in range(2):
                ptk = ("PT", hh, i)
                for ci, (jt, pc) in enumerate(clist):
                    lv = Vz[cb_][:, jt, 0:128] if hh == 0 else Vz[cb_][:, jt, 64:192]
                    lo = onesV[:, 0:128] if hh == 0 else onesV[:, 64:192]
                    mm(ps[6][:, 0:128], lv, PT[hh][i][:, ci, :], idx == 0, idx == total - 1, [("Vz", cb_), ptk], PSK(6))
                    mm(ps[7][:, 0:128], lo, PT[hh][i][:, ci, :], idx == 0, idx == total - 1, ["onesV", ptk], PSK(7))
                    idx += 1
            P.op("dve", lambda e: e.reciprocal(RD[i][:, :], ps[7][:, 0:128]), reads=[], writes=[PSK(7), ("RD", i)])
            tt(YB[:, c, qtok0:qtok0 + 128], ps[6][:, 0:128], RD[i][:, :], ALU.mult, [("RD", i)], [PSK(6), ("YB", out_ti)])

        pcnt = {"n": 0}

        def inproj_items(c):
            cb_ = c % 2
            items = []
            st_ = {}

            def first():
                st_["ws"] = load_slab(("kvq", l, c))
                pool_dma(Eb[cb_][:, :, :, :], biasG_d[l, c].rearrange("p (h a b) -> p h a b", h=2, b=128), [], [("Eb", cb_)])
                act(Eb[cb_][:, :, :, :], Eb[cb_][:, :, :, :], AF.Exp, [("Eb", cb_)], [("Eb", cb_)])
                for hh in range(2):
                    tt(Eb[cb_][:, hh, :, :], Eb[cb_][:, hh, :, :], maskG[:, :, :], ALU.mult, [("Eb", cb_), "maskG"], [("Eb", cb_)])
            items.append(first)
            for (colbase, dst, dkey, gain, gkey, tl) in ((0, KT[cb_], ("KT", cb_), gains[:, l, 1:2], "gains", tiles),
                                                       (256, QT[cb_], ("QT", cb_), qg[:, 0:1], "qg", otiles)):
                for ti in tl:
                    def proj(colbase=colbase, dst=dst, dkey=dkey, gain=gain, gkey=gkey, ti=ti):
                        wsi = st_["ws"]
                        t0, w = TILES[ti]
                        b = pcnt["n"] % 2
                        pcnt["n"] += 1
                        pP, pS = (0, 1) if b == 0 else (6, 7)
                        for k in range(8):
                            mm(ps[pP][:, 0:w], wk(wsi, k, colbase, 128), HT[:, k, t0:t0 + w], k == 0, k == 7,
                               [("ws", wsi), ("HT", ti)], PSK(pP))
                        act(SQh[b][:, 0:w], ps[pP][:, 0:w], AF.Square, [], [PSK(pP), ("SQh", b)])
                        mm(ps[pS][:, 0:w], blk_bf[:, :], SQh[b][:, 0:w], True, True, [("SQh", b), "blk_bf"], PSK(pS))
                        act(RSh[b][:, 0:w], ps[pS][:, 0:w], AF.Sqrt, [], [PSK(pS), ("RSh", b)], bias=EPS, scale=1.0 / 64)
                        P.op("dve", lambda e, b=b, w=w: e.reciprocal(RSh[b][:, 0:w], RSh[b][:, 0:w]), reads=[("RSh", b)],
                             writes=[("RSh", b)])
                        stt(dst[:, t0:t0 + w], ps[pP][:, 0:w], gain, RSh[b][:, 0:w], ALU.mult, ALU.mult, [("RSh", b), gkey],
                            [PSK(pP), dkey])
                    items.append(proj)
            for j0 in range(0, 18, 4):
                def vproj(j0=j0):
                    wsi = st_["ws"]
                    n = min(4, 18 - j0)
                    bank = 0 if (j0 // 4) % 2 == 0 else 1
                    for jj in range(n):
                        jt = j0 + jj
                        ti = 0 if jt < 2 else 1 + (jt - 2) // 4
                        for k in range(8):
                            mm(ps[bank][:, jj * 128:(jj + 1) * 128], HT[:, k, jt * 128:(jt + 1) * 128], wk(wsi, k, 128, 128),
                               k == 0, k == 7, [("ws", wsi), ("HT", ti)], PSK(bank))
                    pv3 = ps[bank][:, 0:n * 128].rearrange("p (a b) -> p a b", b=128)
                    act(Vz[cb_][:, j0:j0 + n, 0:64], pv3[:, :, 0:64], AF.Identity, [], [PSK(bank), ("Vz", cb_)])
                    P.op("dve", lambda e, j0=j0, n=n, pv3=pv3: e.tensor_copy(Vz[cb_][:, j0:j0 + n, 128:192], pv3[:, :, 64:128]),
                         reads=[], writes=[PSK(bank), ("Vz", cb_)])
                items.append(vproj)
            return items

        for c in range(8):
            for it in inproj_items(c):
                it()
            calls = []
            if ctx_out:
                for qt in range(2):
                    calls.append((qt * 128, [], 0))
            for qp in range(16):
                calls.append((CTX + qp * 128, NA_PLAN[qp], 1 + qp // 4))
            pend = None
            for (q0, ch, oti) in calls:
                cur = attend_S(c, q0, ch, oti)
                if pend is not None:
                    attend_PV(pend)
                pend = cur
            attend_PV(pend)
        P.barrier()
        if stage == "D0":
            return YB, [("YB", t) for t in range(5)]

        off = TB
        MTa, off = A("MTa", [8, NT], BF16, off)
        XL2, MRL, T1, SG2 = [], [], [], []
        for i in range(2):
            t_, off = A(f"XL2{i}", [4, 512], F32, off)
            XL2.append(t_)
        for i in range(2):
            t_, off = A(f"MRL{i}", [512], F32, off)
            MRL.append(t_)
            t_, off = A(f"T1{i}", [512], F32, off)
            T1.append(t_)
            t_, off = A(f"SG2{i}", [512], F32, off)
            SG2.append(t_)
        cnt = 0
        for g in range(2):
            wa = load_slab(("nao", l, g))
            wb = load_slab(("gn", l, g))
            for ti in otiles:
                t0, w = TILES[ti]
                for j in range(4):
                    dc = g * 4 + j
                    b = cnt % 2
                    cnt += 1
                    p1, p2 = 2 + b, 4 + b
                    for k in range(8):
                        mm(ps[p1][:, 0:w], wk(wa, k, j * 128, 128), YB[:, k, t0:t0 + w], k == 0, k == 7,
                           [("ws", wa), ("YB", ti)], PSK(p1))
                    for k in range(8):
                        mm(ps[p2][:, 0:w], wk(wb, k, j * 128, 128), HT[:, k, t0:t0 + w], k == 0, k == 7,
                           [("ws", wb), ("HT", ti)], PSK(p2))
                    sp_dma(MRL[b][:, 0:w], mrnn_d[:, dc, t0:t0 + w], [("mrnn", dc, ti)], [("MRL", b)])
                    act(SG2[b][:, 0:w], ps[p2][:, 0:w], AF.Sigmoid, [], [PSK(p2), ("SG2", b)])
                    tt(T1[b][:, 0:w], ps[p1][:, 0:w], SG2[b][:, 0:w], ALU.mult, [("SG2", b)], [PSK(p1), ("T1", b)])
                    tt(MTa[:, dc, t0:t0 + w], T1[b][:, 0:w], MRL[b][:, 0:w], ALU.add, [("T1", b), ("MRL", b)], [("MTa", ti)])
        xn = 0
        for g in range(2):
            wo = load_slab(("out", l, g))
            for ti in otiles:
                t0, w = TILES[ti]
                col = 2 if ti == 0 else s
                xb = xn % 2
                xn += 1
                sp_dma(XL2[xb][:, :, 0:w], xd[s, :, g * 4:(g + 1) * 4, t0:t0 + w], [("xd", ti)], [("XL2", xb)])
                for j in range(4):
                    dc = g * 4 + j
                    b = cnt % 2
                    cnt += 1
                    p1 = 6 + b
                    for k in range(8):
                        mm(ps[p1][:, 0:w], wk(wo, k, j * 128, 128), MTa[:, k, t0:t0 + w], k == 0, k == 7,
                           [("ws", wo), ("MTa", ti)], PSK(p1))
                    stt(XL2[xb][:, j, 0:w], ps[p1][:, 0:w], modv(l, 2, dc, col), XL2[xb][:, j, 0:w], ALU.mult, ALU.add,
                        ["mod", ("XL2", xb)], [PSK(p1), ("XL2", xb)])
                sp_dma(xres_d[s, :, g * 4:(g + 1) * 4, t0:t0 + w], XL2[xb][:, :, 0:w], [("XL2", xb)], [("xdw", ti, g)])
        state["x_in_res"] = True
        P.barrier()
        return None

    def ffn(l, s):
        ctx_out = (l == 0)
        moe = (l == 1)
        otiles = [0, 1, 2, 3, 4] if ctx_out else [1, 2, 3, 4]
        off = XBASE
        XTs, off = A("XTs", [8, NT], F32, off)
        Gt, off = A("Gt", [16, 8], F32, off)
        OV = off
        X = {}
        X["SQ"], off = A("SQ2", [8, 512], BF16, off)
        X["RS"], off = A("RS2", [512], F32, off)
        if moe:
            X["TMP8"], off = A("TMP8", [8, 512], F32, off)
            LG, off = A("LG", [512], F32, off)
            LT, off = A("LT", [16, 8], F32, off)
            EQ1, off = A("EQ1", [16, 8], F32, off)
            L2, off = A("L2", [16, 8], F32, off)
            EQ2, off = A("EQ2", [16, 8], F32, off)
            TG, off = A("TG", [16, 8], F32, off)
            M1, off = A("M1", [16, 1], F32, off)
            M2, off = A("M2", [16, 1], F32, off)
            W1, off = A("W1", [16, 1], F32, off)
            W2, off = A("W2", [16, 1], F32, off)
        else:
            X["TMP"] = []
            for i in range(2):
                t_, off = A(f"TMPf{i}", [512], F32, off)
                X["TMP"].append(t_)
        for ti in otiles:
            t0, w = TILES[ti]
            sp_dma(XTs[:, :, t0:t0 + w], xres_d[s, :, :, t0:t0 + w], [("xd", ti)], [("XTs", ti)])

        def xsrc(ti):
            t0, w = TILES[ti]
            return (lambda c: XTs[:, c, t0:t0 + w]), [("XTs", ti)]

        if SPARSE_MOE and MERGED_MOE and stage in ("full", "seq1"):
            zero_fill_hg(128 if stage == "full" else 256)
        hook = None
        if moe:
            for c in range(8):
                mm(ps[2][0:8, 0:2], router[:, c, :], mod[:, l, 24 + c, s:s + 2], c == 0, c == 7, ["router", "mod"], PSK(2))
            act(routb[0:8, 0:2], ps[2][0:8, 0:2], AF.Identity, [], [PSK(2), "routb"])

            def hook(ti):
                t0, w = TILES[ti]
                for c in range(8):
                    mm(ps[2][0:8, 0:w], router[:, c, :], X["TMP8"][:, c, 0:w], c == 0, c == 7, ["router", ("TMP8", c)], PSK(2))
                act(LG[0:8, 0:w], ps[2][0:8, 0:w], AF.Identity, ["routb"], [PSK(2), "LG"], bias=routb[0:8, 0:1])
                for j in range(w // 128):
                    jt = (t0 - CTX) // 128 + j
                    P.op("pe", lambda e, jt=jt, j=j: e.transpose(ps[3][:, jt * 8:(jt + 1) * 8], LG[0:8, j * 128:(j + 1) * 128],
                                                                 ident_f[0:8, 0:8]),
                         reads=["LG", "ident_f"], writes=[PSK(3)])

        modulate(l, s, 1, otiles, xsrc, X, moe=moe, after_tile=hook)
        if moe:
            P.op("dve", lambda e: e.tensor_copy(LT[:, :, :], ps[3][:, 0:128].rearrange("p (a b) -> p a b", b=8)),
                 reads=[], writes=[PSK(3), "LT"])
            P.op("dve", lambda e: e.tensor_reduce(M1[:, :, 0], LT[:, :, :], AX.X, ALU.max), reads=["LT"], writes=["M1"])
            tt(EQ1[:, :, :], LT[:, :, :], M1[:, :, 0:1].to_broadcast([128, 16, 8]), ALU.is_equal, ["LT", "M1"], ["EQ1"])
            stt(L2[:, :, :], EQ1[:, :, :], -1.0e30, LT[:, :, :], ALU.mult, ALU.add, ["EQ1", "LT"], ["L2"])
            P.op("dve", lambda e: e.tensor_reduce(M2[:, :, 0], L2[:, :, :], AX.X, ALU.max), reads=["L2"], writes=["M2"])
            tt(EQ2[:, :, :], L2[:, :, :], M2[:, :, 0:1].to_broadcast([128, 16, 8]), ALU.is_equal, ["L2", "M2"], ["EQ2"])
            tt(W2[:, :, :], M2[:, :, :], M1[:, :, :], ALU.subtract, ["M1", "M2"], ["W2"])
            act(W2[:, :, :], W2[:, :, :], AF.Exp, ["W2"], ["W2"])
            ts(W1[:, :, :], W2[:, :, :], 1.0, None, ALU.add, None, ["W2"], ["W1"])
            P.op("dve", lambda e: e.reciprocal(W1[:, :, :], W1[:, :, :]), reads=["W1"], writes=["W1"])
            tt(W2[:, :, :], W2[:, :, :], W1[:, :, :], ALU.mult, ["W1", "W2"], ["W2"])
            tt(Gt[:, :, :], EQ1[:, :, :], W1[:, :, 0:1].to_broadcast([128, 16, 8]), ALU.mult, ["EQ1", "W1"], ["Gt"])
            tt(TG[:, :, :], EQ2[:, :, :], W2[:, :, 0:1].to_broadcast([128, 16, 8]), ALU.mult, ["EQ2", "W2"], ["TG"])
            tt(Gt[:, :, :], Gt[:, :, :], TG[:, :, :], ALU.add, ["Gt", "TG"], ["Gt"])
        P.barrier()
        if stage == "G0":
            return HT, [("HT", t) for t in range(5)]

        off = OV
        SIL, TT, ACTT, GE, DG = [], [], [], [], []
        for i in range(2):
            t_, off = A(f"SIL{i}", [512], BF16, off)
            SIL.append(t_)
            t_, off = A(f"TT{i}", [512], F32, off)
            TT.append(t_)
            t_, off = A(f"ACTT{i}", [4, 512], BF16, off)
            ACTT.append(t_)
            if moe:
                t_, off = A(f"GE{i}", [SEQ], F32, off)
                GE.append(t_)
                t_, off = A(f"DG{i}", [128], F32, off)
                DG.append(t_)
        cnt = {"h": 0, "a": 0, "o": 0, "d": 0}

        def swiglu_group(names, nj, tl, ge):
            w1 = load_slab(names[0])
            w3 = load_slab(names[1])
            w2 = load_slab(names[2])
            for ti in tl:
                t0, w = TILES[ti]
                col = 2 if ti == 0 else s
                ab = cnt["a"] % 2
                cnt["a"] += 1
                for j in range(nj):
                    b = cnt["h"] % 2
                    cnt["h"] += 1
                    b1, b3 = 2 + b, 4 + b
                    for k in range(8):
                        mm(ps[b1][:, 0:w], wk(w1, k, j * 128, 128), HT[:, k, t0:t0 + w], k == 0, k == 7,
                           [("ws", w1), ("HT", ti)], PSK(b1))
                    for k in range(8):
                        mm(ps[b3][:, 0:w], wk(w3, k, j * 128, 128), HT[:, k, t0:t0 + w], k == 0, k == 7,
                           [("ws", w3), ("HT", ti)], PSK(b3))
                    act(SIL[b][:, 0:w], ps[b1][:, 0:w], AF.Silu, [], [PSK(b1), ("SIL", b)])
                    if ge is None:
                        tt(ACTT[ab][:, j, 0:w], ps[b3][:, 0:w], SIL[b][:, 0:w], ALU.mult, [("SIL", b)], [PSK(b3), ("ACTT", ab)])
                    else:
                        tt(TT[b][:, 0:w], ps[b3][:, 0:w], SIL[b][:, 0:w], ALU.mult, [("SIL", b)], [PSK(b3), ("TT", b)])
                        tt(ACTT[ab][:, j, 0:w], TT[b][:, 0:w], GE[ge][:, t0 - CTX:t0 - CTX + w], ALU.mult,
                           [("TT", b), ("GE", ge)], [("ACTT", ab)])
                for dc in range(8):
                    ob = 6 + cnt["o"] % 2
                    cnt["o"] += 1
                    for j in range(nj):
                        mm(ps[ob][:, 0:w], wk2(w2, j, dc * 128, 128), ACTT[ab][:, j, 0:w], j == 0, j == nj - 1,
                           [("ws", w2), ("ACTT", ab)], PSK(ob))
                    stt(XTs[:, dc, t0:t0 + w], ps[ob][:, 0:w], modv(l, 5, dc, col), XTs[:, dc, t0:t0 + w], ALU.mult, ALU.add,
                        ["mod", ("XTs", ti)], [PSK(ob), ("XTs", ti)])

        if not moe:
            for g in range(6):
                swiglu_group([("f1", g), ("f3", g), ("f2", g)], 4 if g < 5 else 2, otiles, None)
        else:
            for ex in range(NEXP):
                ge = ex % 2
                for j0 in range(0, 16, 4):
                    bank = 0 if (j0 // 4) % 2 == 0 else 1
                    for jj in range(4):
                        jt = j0 + jj
                        d = cnt["d"] % 2
                        cnt["d"] += 1
                        ts(DG[d][:, :], ident_f[:, :], Gt[:, jt, ex:ex + 1], None, ALU.mult, None, ["ident_f", "Gt"], [("DG", d)])
                        mm(ps[bank][:, jj * 128:(jj + 1) * 128], ones_f[:, :], DG[d][:, :], True, True, ["ones_f", ("DG", d)],
                           PSK(bank))
                    act(GE[ge][:, j0 * 128:(j0 + 4) * 128], ps[bank][:, 0:512], AF.Identity, [], [PSK(bank), ("GE", ge)])
                for g in range(7):
                    swiglu_group([("m1", ex, g), ("m3", ex, g), ("m2", ex, g)], 4, [1, 2, 3, 4], ge)
        for ti in otiles:
            t0, w = TILES[ti]
            if l == 0:
                sp_dma(xres_d[s, :, :, t0:t0 + w], XTs[:, :, t0:t0 + w], [("XTs", ti)], [("xd", ti)])
            else:
                sp_dma(outT_d[s, :, :, t0 - CTX:t0 - CTX + w], XTs[:, :, t0:t0 + w], [("XTs", ti)], [("out", s, ti)])
        P.barrier()
        return None

    def ffn_moe_sparse(l, s):
        I32 = mybir.dt.int32
        TSZ = MOE_TSZ
        NQ = TSZ // 128
        NBLK = SEQ // TSZ
        lat = [1, 2, 3, 4]
        off = XBASE
        Gsm = {}
        for nm, shp, dt in (("W1", [16, 1], F32), ("W2", [16, 1], F32), ("SI", [16, 2], I32), ("JI", [8], I32)):
            Gsm[nm], off = A("m_" + nm, shp, dt, off)
        OV = off
        for nm, shp, dt in (("LT", [16, 8], F32), ("EQ1", [16, 8], F32), ("L2", [16, 8], F32), ("EQ2", [16, 8], F32),
                            ("M1", [16, 1], F32), ("M2", [16, 1], F32),
                            ("AB", [16, 8], BF16), ("PW", [16, 8], F32), ("TOT", [16, 8], F32), ("CS", [16, 8], F32),
                            ("EOFF", [16, 8], F32), ("TQ", [16, 8], F32), ("S12", [16, 2], F32),
                            ("NE", [8, 1], F32), ("THR", [8, 8], F32), ("CMP", [8, 8], F32), ("JF", [8], F32)):
            Gsm[nm], off = A("m_" + nm, shp, dt, off)
        LT, EQ1, L2, EQ2, M1, M2, W1, W2 = (Gsm[k] for k in ("LT", "EQ1", "L2", "EQ2", "M1", "M2", "W1", "W2"))
        AB, PW, TOT, CS, EOFF, TQ, S12, SI = (Gsm[k] for k in ("AB", "PW", "TOT", "CS", "EOFF", "TQ", "S12", "SI"))
        NE, THR, CMP, JF, JI = (Gsm[k] for k in ("NE", "THR", "CMP", "JF", "JI"))
        LG, off = A("mLG", [512], F32, off)
        XL = []
        for i in range(2):
            t_, off = A(f"mXL{i}", [8, 512], F32, off)
            XL.append(t_)
        X = {}
        X["SQ"], off = A("mSQ", [8, 512], BF16, off)
        X["RS"], off = A("mRS", [512], F32, off)
        X["TMP8"], off = A("mTMP8", [8, 512], F32, off)
        HTok = []
        for i in range(2):
            t_, off = A(f"HTok{i}", [D], BF16, off)
            HTok.append(t_)

        def xsrc(ti):
            t0, w = TILES[ti]
            b = ti % 2
            sp_dma(XL[b][:, :, 0:w], xres_d[s, :, :, t0:t0 + w], [("xd", ti)], [("XL", b)])
            return (lambda c: XL[b][:, c, 0:w]), [("XL", b)]

        for c in range(8):
            mm(ps[2][0:8, 0:2], router[:, c, :], mod[:, l, 24 + c, s:s + 2], c == 0, c == 7, ["router", "mod"], PSK(2))
        act(routb[0:8, 0:2], ps[2][0:8, 0:2], AF.Identity, [], [PSK(2), "routb"])

        def hook(ti):
            t0, w = TILES[ti]
            for c in range(8):
                mm(ps[2][0:8, 0:w], router[:, c, :], X["TMP8"][:, c, 0:w], c == 0, c == 7, ["router", ("TMP8", c)], PSK(2))
            act(LG[0:8, 0:w], ps[2][0:8, 0:w], AF.Identity, ["routb"], [PSK(2), "LG"], bias=routb[0:8, 0:1])
            for j in range(w // 128):
                jt = (t0 - CTX) // 128 + j
                P.op("pe", lambda e, jt=jt, j=j: e.transpose(ps[3][:, jt * 8:(jt + 1) * 8], LG[0:8, j * 128:(j + 1) * 128],
                                                             ident_f[0:8, 0:8]),
                     reads=["LG", "ident_f"], writes=[PSK(3)])

        modulate(l, s, 1, lat, xsrc, X, moe=True, after_tile=hook)
        P.op("dve", lambda e: e.tensor_copy(LT[:, :, :], ps[3][:, 0:128].rearrange("p (a b) -> p a b", b=8)),
             reads=[], writes=[PSK(3), "LT"])
        P.op("dve", lambda e: e.tensor_reduce(M1[:, :, 0], LT[:, :, :], AX.X, ALU.max), reads=["LT"], writes=["M1"])
        tt(EQ1[:, :, :], LT[:, :, :], M1[:, :, 0:1].to_broadcast([128, 16, 8]), ALU.is_equal, ["LT", "M1"], ["EQ1"])
        stt(L2[:, :, :], EQ1[:, :, :], -1.0e30, LT[:, :, :], ALU.mult, ALU.add, ["EQ1", "LT"], ["L2"])
        P.op("dve", lambda e: e.tensor_reduce(M2[:, :, 0], L2[:, :, :], AX.X, ALU.max), reads=["L2"], writes=["M2"])
        tt(EQ2[:, :, :], L2[:, :, :], M2[:, :, 0:1].to_broadcast([128, 16, 8]), ALU.is_equal, ["L2", "M2"], ["EQ2"])
        tt(W2[:, :, :], M2[:, :, :], M1[:, :, :], ALU.subtract, ["M1", "M2"], ["W2"])
        act(W2[:, :, :], W2[:, :, :], AF.Exp, ["W2"], ["W2"])
        ts(W1[:, :, :], W2[:, :, :], 1.0, None, ALU.add, None, ["W2"], ["W1"])
        P.op("dve", lambda e: e.reciprocal(W1[:, :, :], W1[:, :, :]), reads=["W1"], writes=["W1"])
        tt(W2[:, :, :], W2[:, :, :], W1[:, :, :], ALU.mult, ["W1", "W2"], ["W2"])
        tt(TQ[:, :, :], EQ1[:, :, :], EQ2[:, :, :], ALU.add, ["EQ1", "EQ2"], ["TQ"])
        P.op("dve", lambda e: e.tensor_copy(AB[:, :, :], TQ[:, :, :]), reads=["TQ"], writes=["AB"])
        ABf = AB[:, :, :].rearrange("p a b -> p (a b)")
        mm(ps[0][:, 0:128], ltri_bf[:, :], ABf, True, True, ["AB", "ltri_bf"], PSK(0))
        mm(ps[0][:, 128:256], ones_bf[:, :], ABf, True, True, ["AB", "ones_bf"], PSK(0))
        P.op("dve", lambda e: e.tensor_copy(PW[:, :, :], ps[0][:, 0:128].rearrange("p (a b) -> p a b", b=8)),
             reads=[], writes=[PSK(0), "PW"])
        P.op("dve", lambda e: e.tensor_copy(TOT[:, :, :], ps[0][:, 128:256].rearrange("p (a b) -> p a b", b=8)),
             reads=[], writes=[PSK(0), "TOT"])
        P.op("dve", lambda e: e.memset(CS[:, 0, :], 0.0), writes=["CS"])
        for j in range(1, 16):
            tt(CS[:, j, :], CS[:, j - 1, :], TOT[:, j - 1, :], ALU.add, ["CS", "TOT"], ["CS"])
        tt(NE[:, :, 0], CS[:, 15, :], TOT[:, 15, :], ALU.add, ["CS", "TOT"], ["NE"])
        for ex in range(NEXP):
            P.op("dve", lambda e, ex=ex: e.memset(EOFF[:, :, ex], float(ex * SEQ)), writes=["EOFF"])
            P.op("dve", lambda e, ex=ex: e.memset(THR[:, :, ex], float(ex * TSZ)), writes=["THR"])
        tt(PW[:, :, :], PW[:, :, :], CS[:, :, :], ALU.add, ["PW", "CS"], ["PW"])
        tt(PW[:, :, :], PW[:, :, :], EOFF[:, :, :], ALU.add, ["PW", "EOFF"], ["PW"])
        tt(TQ[:, :, :], EQ1[:, :, :], PW[:, :, :], ALU.mult, ["EQ1", "PW"], ["TQ"])
        P.op("dve", lambda e: e.tensor_reduce(S12[:, :, 0], TQ[:, :, :], AX.X, ALU.add), reads=["TQ"], writes=["S12"])
        tt(TQ[:, :, :], EQ2[:, :, :], PW[:, :, :], ALU.mult, ["EQ2", "PW", "S12"], ["TQ"])
        P.op("dve", lambda e: e.tensor_reduce(S12[:, :, 1], TQ[:, :, :], AX.X, ALU.add), reads=["TQ"], writes=["S12"])
        P.op("dve", lambda e: e.tensor_copy(SI[:, :, :], S12[:, :, :]), reads=["S12"], writes=["SI"])
        tt(CMP[:, :, :], NE[:, :, 0:1].to_broadcast([128, 8, 8]), THR[:, :, :], ALU.is_gt, ["NE", "THR"], ["CMP"])
        P.op("dve", lambda e: e.tensor_reduce(JF[:, :], CMP[:, :, :], AX.X, ALU.add), reads=["CMP"], writes=["JF"])
        if JCLAMP is not None:
            ts(JF[:, :], JF[:, :], float(JCLAMP), None, ALU.min, None, ["JF"], ["JF"])
        P.op("dve", lambda e: e.tensor_copy(JI[:, :], JF[:, :]), reads=["JF"], writes=["JI"])
        if stage == "G1a":
            DB, _ = A("DBG1", [64], F32, off)
            P.op("dve", lambda e: e.memset(DB[:, :], 0.0), writes=["DB"])
            P.op("dve", lambda e: e.tensor_copy(DB[:, 0:8], NE[:, :, 0]), reads=["NE"], writes=["DB"])
            P.op("dve", lambda e: e.tensor_copy(DB[:, 8:16], JF[:, :]), reads=["JF"], writes=["DB"])
            P.op("dve", lambda e: e.tensor_copy(DB[:, 16:48], S12[:, :, :].rearrange("p a b -> p (a b)")), reads=["S12"], writes=["DB"])
            P.op("dve", lambda e: e.tensor_copy(DB[:, 48:64], M1[:, :, 0]), reads=["M1"], writes=["DB"])
            sp_dma(dbg_d, DB[:, :], ["DB"], ["dbg"])
            return "done"
        psb = [ps[i][:, :].bitcast(BF16) for i in range(8)]
        if ZERO_HG:
            P.op("dve", lambda e: e.memset(HTok[0][:, :], 0.0), writes=[("HTok", 0)])
            for blk in range(NEXP * SEQ // 128):
                sp_dma(hg_d[blk * 128:(blk + 1) * 128, :], HTok[0][:, :], [("HTok", 0)], [("hgz", blk)])
        for jt in range(16):
            b = jt % 2
            ti = 1 + jt // 4
            for c in range(8):
                P.op("pe", lambda e, b=b, c=c, jt=jt: e.transpose(psb[b][:, c * 128:(c + 1) * 128],
                                                                  HT[:, c, CTX + jt * 128:CTX + (jt + 1) * 128], ident_bf[:, :]),
                     reads=[("HT", ti), "ident_bf"], writes=[PSK(b)])
            act(HTok[b][:, :], psb[b][:, :], AF.Identity, [], [PSK(b), ("HTok", b)])
            for k in range(2):
                P.dma("pool", lambda e, b=b, jt=jt, k=k: e.indirect_dma_start(
                    out=hg_d, out_offset=bass.IndirectOffsetOnAxis(ap=SI[:, jt, k:k + 1], axis=0), in_=HTok[b][:, :],
                    in_offset=None), reads=[("HTok", b), "SI"] + ([("hgz", q_) for q_ in range(128)] if ZERO_HG else []), writes=[("hg", jt, k)])
        P.barrier()
        if stage == "G1":
            return None

        off = OV
        Yacc, off = A("Yacc", [16, D], F32, off)
        HTg, off = A("HTg", [8, SEQ], BF16, off)
        HGs, SIL, ACTT = [], [], []
        t_, off = A("HGs0", [D], BF16, off)
        HGs = [t_, t_]
        for i in range(2):
            t_, off = A(f"mSIL{i}", [TSZ], BF16, off)
            SIL.append(t_)
            t_, off = A(f"mACTT{i}", [4, TSZ], BF16, off)
            ACTT.append(t_)
        cnt = {"h": 0, "a": 0, "o": 0, "g": 0}
        for ex in range(NEXP):
            P.load_reg(JI[0:1, ex:ex + 1], "JI")
            for k in range(16):
                b = cnt["g"] % 2
                cnt["g"] += 1
                r0 = ex * SEQ + k * 128
                sp_dma(HGs[b][:, :], hg_d[r0:r0 + 128, :], [("hg", a_, b_) for a_ in range(16) for b_ in range(2)], [("HGs", 0)])
                P.cond_begin(k // NQ + 1)
                for c in range(8):
                    P.op("pe", lambda e, b=b, c=c: e.transpose(psb[b][:, c * 128:(c + 1) * 128], HGs[b][:, c * 128:(c + 1) * 128],
                                                               ident_bf[:, :]),
                         reads=[("HGs", 0), "ident_bf"], writes=[PSK(b)])
                act(HTg[:, :, k * 128:(k + 1) * 128], psb[b][:, :].rearrange("p (a b) -> p a b", b=128), AF.Identity, [],
                    [PSK(b), ("HTg", k // NQ)])
                P.cond_end()
            for g in range(7):
                w1 = load_slab(("m1", ex, g), big=True)
                w3 = load_slab(("m3", ex, g), big=True)
                w2 = load_slab(("m2", ex, g), big=True)
                for j in range(NBLK):
                    P.cond_begin(j + 1)
                    ab = cnt["a"] % 2
                    cnt["a"] += 1
                    s0 = j * TSZ
                    for jj in range(4):
                        b = cnt["h"] % 2
                        cnt["h"] += 1
                        b1, b3 = 2 + b, 4 + b
                        for k in range(8):
                            mm(ps[b1][:, 0:TSZ], wk(w1, k, jj * 128, 128), HTg[:, k, s0:s0 + TSZ], k == 0, k == 7,
                               [("ws", w1), ("HTg", j)], PSK(b1))
                        for k in range(8):
                            mm(ps[b3][:, 0:TSZ], wk(w3, k, jj * 128, 128), HTg[:, k, s0:s0 + TSZ], k == 0, k == 7,
                               [("ws", w3), ("HTg", j)], PSK(b3))
                        act(SIL[b][:, :], ps[b1][:, 0:TSZ], AF.Silu, [], [PSK(b1), ("SIL", b)])
                        tt(ACTT[ab][:, jj, :], ps[b3][:, 0:TSZ], SIL[b][:, :], ALU.mult, [("SIL", b)], [PSK(b3), ("ACTT", ab)])
                    for h2 in range(NQ):
                        kt = NQ * j + h2
                        for dh in range(2):
                            ob = 6 + cnt["o"] % 2
                            cnt["o"] += 1
                            for jj in range(4):
                                mm(ps[ob][:, 0:512], ACTT[ab][:, jj, h2 * 128:(h2 + 1) * 128], wk2(w2, jj, dh * 512, 512),
                                   jj == 0, jj == 3, [("ws", w2), ("ACTT", ab)], PSK(ob))
                            ya = Yacc[:, kt, dh * 512:(dh + 1) * 512]
                            if g == 0:
                                P.op("dve", lambda e, ya=ya, ob=ob: e.tensor_copy(ya, ps[ob][:, 0:512]), reads=[],
                                     writes=[PSK(ob), ("Yacc", kt)])
                            else:
                                tt(ya, ps[ob][:, 0:512], ya, ALU.add, [("Yacc", kt)], [PSK(ob), ("Yacc", kt)])
                    P.cond_end()
            for k in range(16):
                r0 = ex * SEQ + k * 128
                sp_dma(yb_d[r0:r0 + 128, :], Yacc[:, k, :], [("Yacc", k)], [("yb", ex, k)])
        P.barrier()

        off = OV
        YA, YB2, OO, XC8 = [], [], [], []
        for i in range(2):
            t_, off = A(f"YA{i}", [D], F32, off)
            YA.append(t_)
            t_, off = A(f"YB2{i}", [D], F32, off)
            YB2.append(t_)
            t_, off = A(f"OO{i}", [D], F32, off)
            OO.append(t_)
            t_, off = A(f"XC8{i}", [8, 128], F32, off)
            XC8.append(t_)
        for jt in range(16):
            b = jt % 2
            ti = 1 + jt // 4
            c0 = CTX + jt * 128
            for k, dst, dk in ((0, YA, "YA"), (1, YB2, "YB2")):
                P.dma("pool", lambda e, b=b, jt=jt, k=k, dst=dst: e.indirect_dma_start(
                    out=dst[b][:, :], out_offset=None, in_=yb_d,
                    in_offset=bass.IndirectOffsetOnAxis(ap=SI[:, jt, k:k + 1], axis=0)),
                    reads=[("yb", a_, b_) for a_ in range(NEXP) for b_ in range(16)] + ["SI"], writes=[(dk, b)])
            sp_dma(XC8[b][:, :, :], xres_d[s, :, :, c0:c0 + 128], [("xd", ti)], [("XC8", b)])
            ts(OO[b][:, :], YA[b][:, :], W1[:, jt, 0:1], None, ALU.mult, None, [("YA", b), "W1"], [("OO", b)])
            stt(OO[b][:, :], YB2[b][:, :], W2[:, jt, 0:1], OO[b][:, :], ALU.mult, ALU.add, [("YB2", b), "W2", ("OO", b)], [("OO", b)])
            for c in range(8):
                pbk = 2 + 2 * b + c // 4
                P.op("pe", lambda e, b=b, c=c, pbk=pbk: e.transpose(ps[pbk][:, (c % 4) * 128:(c % 4 + 1) * 128],
                                                                    OO[b][:, c * 128:(c + 1) * 128], ident_f[:, :]),
                     reads=[("OO", b), "ident_f"], writes=[PSK(pbk)])
            for c in range(8):
                pbk = 2 + 2 * b + c // 4
                stt(XC8[b][:, c, :], ps[pbk][:, (c % 4) * 128:(c % 4 + 1) * 128], modv(l, 5, c, s), XC8[b][:, c, :],
                    ALU.mult, ALU.add, ["mod", ("XC8", b)], [PSK(pbk), ("XC8", b)])
            sp_dma(outT_d[s, :, :, jt * 128:(jt + 1) * 128], XC8[b][:, :, :], [("XC8", b)], [("out", s, jt)])
        P.barrier()
        return None

    def moe_merged(l, seqs):
        I32 = mybir.dt.int32
        TSZ = 512
        NQ = TSZ // 128
        CAP = SEQ * len(seqs)
        NPASS = len(seqs)
        off = XBASE
        PS_ = {}
        for s in seqs:
            for nm, shp, dt in (("W1", [16, 1], F32), ("W2", [16, 1], F32), ("SI", [16, 2], I32)):
                PS_[(nm, s)], off = A(f"mm_{nm}{s}", shp, dt, off)
        NEacc, off = A("mm_NEacc", [8, 1], F32, off)
        JI, off = A("mm_JI", [8], I32, off)
        OV = off
        G = {}
        for nm, shp, dt in (("LT", [16, 8], F32), ("EQ1", [16, 8], F32), ("L2", [16, 8], F32), ("EQ2", [16, 8], F32),
                            ("M1", [16, 1], F32), ("M2", [16, 1], F32),
                            ("AB", [16, 8], BF16), ("PW", [16, 8], F32), ("TOT", [16, 8], F32), ("CS", [16, 8], F32),
                            ("EOFF", [16, 8], F32), ("TQ", [16, 8], F32), ("S12", [16, 2], F32),
                            ("THR", [8, 8], F32), ("CMP", [8, 8], F32), ("JF", [8], F32)):
            G[nm], off = A("mm_" + nm, shp, dt, off)
        LT, EQ1, L2, EQ2, M1, M2 = (G[k] for k in ("LT", "EQ1", "L2", "EQ2", "M1", "M2"))
        AB, PW, TOT, CS, EOFF, TQ, S12 = (G[k] for k in ("AB", "PW", "TOT", "CS", "EOFF", "TQ", "S12"))
        THR, CMP, JF = (G[k] for k in ("THR", "CMP", "JF"))
        LG, off = A("mm_LG", [512], F32, off)
        XL = []
        for i in range(2):
            t_, off = A(f"mm_XL{i}", [8, 512], F32, off)
            XL.append(t_)
        X = {}
        X["SQ"], off = A("mm_SQ", [8, 512], BF16, off)
        X["RS"], off = A("mm_RS", [512], F32, off)
        X["TMP8"], off = A("mm_TMP8", [8, 512], F32, off)
        NHB = 4
        HTok = []
        for i in range(NHB):
            t_, off = A(f"mm_HTok{i}", [D], BF16, off)
            HTok.append(t_)
        psb = [ps[i][:, :].bitcast(BF16) for i in range(8)]
        lat = [1, 2, 3, 4]
        P.op("dve", lambda e: e.memset(NEacc[:, :, :], 0.0), writes=["NEacc"])
        for ex in range(NEXP):
            P.op("dve", lambda e, ex=ex: e.memset(EOFF[:, :, ex], float(ex * CAP)), writes=["EOFF"])
            P.op("dve", lambda e, ex=ex: e.memset(THR[:, :, ex], float(ex * TSZ)), writes=["THR"])

        for s in seqs:
            W1, W2, SI = PS_[("W1", s)], PS_[("W2", s)], PS_[("SI", s)]

            def xsrc(ti, s=s):
                t0, w = TILES[ti]
                b = ti % 2
                sp_dma(XL[b][:, :, 0:w], xres_d[s, :, :, t0:t0 + w], [("xd", ti)], [("XL", b)])
                return (lambda c: XL[b][:, c, 0:w]), [("XL", b)]

            for c in range(8):
                mm(ps[2][0:8, 0:2], router[:, c, :], mod[:, l, 24 + c, s:s + 2], c == 0, c == 7, ["router", "mod"], PSK(2))
            act(routb[0:8, 0:2], ps[2][0:8, 0:2], AF.Identity, [], [PSK(2), "routb"])

            def hook(ti):
                t0, w = TILES[ti]
                for c in range(8):
                    mm(ps[2][0:8, 0:w], router[:, c, :], X["TMP8"][:, c, 0:w], c == 0, c == 7, ["router", ("TMP8", c)], PSK(2))
                act(LG[0:8, 0:w], ps[2][0:8, 0:w], AF.Identity, ["routb"], [PSK(2), "LG"], bias=routb[0:8, 0:1])
                for j in range(w // 128):
                    jt = (t0 - CTX) // 128 + j
                    P.op("pe", lambda e, jt=jt, j=j: e.transpose(ps[3][:, jt * 8:(jt + 1) * 8], LG[0:8, j * 128:(j + 1) * 128],
                                                                 ident_f[0:8, 0:8]),
                         reads=["LG", "ident_f"], writes=[PSK(3)])

            modulate(l, s, 1, lat, xsrc, X, moe=True, after_tile=hook)
            P.op("dve", lambda e: e.tensor_copy(LT[:, :, :], ps[3][:, 0:128].rearrange("p (a b) -> p a b", b=8)),
                 reads=[], writes=[PSK(3), "LT"])
            P.op("dve", lambda e: e.tensor_reduce(M1[:, :, 0], LT[:, :, :], AX.X, ALU.max), reads=["LT"], writes=["M1"])
            tt(EQ1[:, :, :], LT[:, :, :], M1[:, :, 0:1].to_broadcast([128, 16, 8]), ALU.is_equal, ["LT", "M1"], ["EQ1"])
            stt(L2[:, :, :], EQ1[:, :, :], -1.0e30, LT[:, :, :], ALU.mult, ALU.add, ["EQ1", "LT"], ["L2"])
            P.op("dve", lambda e: e.tensor_reduce(M2[:, :, 0], L2[:, :, :], AX.X, ALU.max), reads=["L2"], writes=["M2"])
            tt(EQ2[:, :, :], L2[:, :, :], M2[:, :, 0:1].to_broadcast([128, 16, 8]), ALU.is_equal, ["L2", "M2"], ["EQ2"])
            tt(W2[:, :, :], M2[:, :, :], M1[:, :, :], ALU.subtract, ["M1", "M2"], [("W2", s)])
            act(W2[:, :, :], W2[:, :, :], AF.Exp, [("W2", s)], [("W2", s)])
            ts(W1[:, :, :], W2[:, :, :], 1.0, None, ALU.add, None, [("W2", s)], [("W1", s)])
            P.op("dve", lambda e, W1=W1: e.reciprocal(W1[:, :, :], W1[:, :, :]), reads=[("W1", s)], writes=[("W1", s)])
            tt(W2[:, :, :], W2[:, :, :], W1[:, :, :], ALU.mult, [("W1", s), ("W2", s)], [("W2", s)])
            tt(TQ[:, :, :], EQ1[:, :, :], EQ2[:, :, :], ALU.add, ["EQ1", "EQ2"], ["TQ"])
            P.op("dve", lambda e: e.tensor_copy(AB[:, :, :], TQ[:, :, :]), reads=["TQ"], writes=["AB"])
            ABf = AB[:, :, :].rearrange("p a b -> p (a b)")
            mm(ps[0][:, 0:128], ltri_bf[:, :], ABf, True, True, ["AB", "ltri_bf"], PSK(0))
            mm(ps[0][:, 128:256], ones_bf[:, :], ABf, True, True, ["AB", "ones_bf"], PSK(0))
            P.op("dve", lambda e: e.tensor_copy(PW[:, :, :], ps[0][:, 0:128].rearrange("p (a b) -> p a b", b=8)),
                 reads=[], writes=[PSK(0), "PW"])
            P.op("dve", lambda e: e.tensor_copy(TOT[:, :, :], ps[0][:, 128:256].rearrange("p (a b) -> p a b", b=8)),
                 reads=[], writes=[PSK(0), "TOT"])
            P.op("dve", lambda e: e.tensor_copy(CS[:, 0, :], NEacc[:, :, 0]), reads=["NEacc"], writes=["CS"])
            for j in range(1, 16):
                tt(CS[:, j, :], CS[:, j - 1, :], TOT[:, j - 1, :], ALU.add, ["CS", "TOT"], ["CS"])
            tt(NEacc[:, :, 0], CS[:, 15, :], TOT[:, 15, :], ALU.add, ["CS", "TOT"], ["NEacc"])
            tt(PW[:, :, :], PW[:, :, :], CS[:, :, :], ALU.add, ["PW", "CS"], ["PW"])
            tt(PW[:, :, :], PW[:, :, :], EOFF[:, :, :], ALU.add, ["PW", "EOFF"], ["PW"])
            tt(TQ[:, :, :], EQ1[:, :, :], PW[:, :, :], ALU.mult, ["EQ1", "PW"], ["TQ"])
            P.op("dve", lambda e: e.tensor_reduce(S12[:, :, 0], TQ[:, :, :], AX.X, ALU.add), reads=["TQ"], writes=["S12"])
            tt(TQ[:, :, :], EQ2[:, :, :], PW[:, :, :], ALU.mult, ["EQ2", "PW", "S12"], ["TQ"])
            P.op("dve", lambda e: e.tensor_reduce(S12[:, :, 1], TQ[:, :, :], AX.X, ALU.add), reads=["TQ"], writes=["S12"])
            P.op("dve", lambda e, SI=SI: e.tensor_copy(SI[:, :, :], S12[:, :, :]), reads=["S12"], writes=[("SI", s)])
            for jt in range(16):
                b = jt % 2
                hb = jt % NHB
                ti = 1 + jt // 4
                for c in range(8):
                    P.op("pe", lambda e, b=b, c=c, jt=jt: e.transpose(psb[b][:, c * 128:(c + 1) * 128],
                                                                      HT[:, c, CTX + jt * 128:CTX + (jt + 1) * 128], ident_bf[:, :]),
                         reads=[("HT", ti), "ident_bf"], writes=[PSK(b)])
                act(HTok[hb][:, :], psb[b][:, :], AF.Identity, [], [PSK(b), ("HTok", hb)])
                for k in range(2):
                    P.dma("pool", lambda e, hb=hb, jt=jt, k=k, SI=SI: e.indirect_dma_start(
                        out=hg_d, out_offset=bass.IndirectOffsetOnAxis(ap=SI[:, jt, k:k + 1], axis=0), in_=HTok[hb][:, :],
                        in_offset=None), reads=[("HTok", hb), ("SI", s)] + [("hgz", q_) for q_ in range(NEXP * SEQ * 2 // 128)], writes=[("hg", s, jt, k)])
        tt(CMP[:, :, :], NEacc[:, :, 0:1].to_broadcast([128, 8, 8]), THR[:, :, :], ALU.is_gt, ["NEacc", "THR"], ["CMP"])
        P.op("dve", lambda e: e.tensor_reduce(JF[:, :], CMP[:, :, :], AX.X, ALU.add), reads=["CMP"], writes=["JF"])
        P.op("dve", lambda e: e.tensor_copy(JI[:, :], JF[:, :]), reads=["JF"], writes=["JI"])
        P.barrier()

        off = OV
        Yacc, off = A("mm_Yacc", [16, D], F32, off)
        HTg, off = A("mm_HTg", [8, SEQ], BF16, off)
        HGs = []
        for i in range(2):
            t_, off = A(f"mm_HGs{i}", [D], BF16, off)
            HGs.append(t_)
        SIL, ACTT = [], []
        for i in range(2):
            t_, off = A(f"mm_SIL{i}", [TSZ], BF16, off)
            SIL.append(t_)
            t_, off = A(f"mm_ACTT{i}", [4, TSZ], BF16, off)
            ACTT.append(t_)
        cnt = {"h": 0, "a": 0, "o": 0, "g": 0}
        allhg = [("hg", s_, a_, b_) for s_ in seqs for a_ in range(16) for b_ in range(2)]
        for p_, ex in [(p__, e__) for p__ in range(NPASS) for e__ in range(NEXP)]:
            P.load_reg(JI[0:1, ex:ex + 1], "JI", engines=("pe", "act", "dve", "sp", "pool"))
            for _once in range(1):
                jb = 4 * p_
                for k in range(16):
                    b = cnt["g"] % 2
                    cnt["g"] += 1
                    r0 = ex * CAP + p_ * SEQ + k * 128
                    thr = jb + k // NQ + 1
                    P.cond_begin(thr)
                    sp_dma(HGs[b][:, :], hg_d[r0:r0 + 128, :], allhg, [("HGs", b)])
                    for c in range(8):
                        P.op("pe", lambda e, b=b, c=c: e.transpose(psb[b][:, c * 128:(c + 1) * 128], HGs[b][:, c * 128:(c + 1) * 128],
                                                                   ident_bf[:, :]),
                             reads=[("HGs", b), "ident_bf"], writes=[PSK(b)])
                    act(HTg[:, :, k * 128:(k + 1) * 128], psb[b][:, :].rearrange("p (a b) -> p a b", b=128), AF.Identity, [],
                        [PSK(b), ("HTg", k // NQ)])
                    P.cond_end()
                for g in range(7):
                    P.cond_begin(jb + 1)
                    w1 = load_slab(("m1", ex, g), big=True)
                    P.cond_end()
                    P.cond_begin(jb + 1)
                    w3 = load_slab(("m3", ex, g), big=True)
                    P.cond_end()
                    P.cond_begin(jb + 1)
                    w2 = load_slab(("m2", ex, g), big=True)
                    P.cond_end()
                    for j in range(4):
                        P.cond_begin(jb + j + 1)
                        ab = cnt["a"] % 2
                        cnt["a"] += 1
                        s0 = j * TSZ
                        for jj in range(4):
                            b = cnt["h"] % 2
                            cnt["h"] += 1
                            b1, b3 = 2 + b, 4 + b
                            for k in range(8):
                                mm(ps[b1][:, 0:TSZ], wk(w1, k, jj * 128, 128), HTg[:, k, s0:s0 + TSZ], k == 0, k == 7,
                                   [("ws", w1), ("HTg", j)], PSK(b1))
                            for k in range(8):
                                mm(ps[b3][:, 0:TSZ], wk(w3, k, jj * 128, 128), HTg[:, k, s0:s0 + TSZ], k == 0, k == 7,
                                   [("ws", w3), ("HTg", j)], PSK(b3))
                            act(SIL[b][:, :], ps[b1][:, 0:TSZ], AF.Silu, [], [PSK(b1), ("SIL", b)])
                            tt(ACTT[ab][:, jj, :], ps[b3][:, 0:TSZ], SIL[b][:, :], ALU.mult, [("SIL", b)], [PSK(b3), ("ACTT", ab)])
                        P.cond_end()
                        P.cond_begin(jb + j + 1)
                        for h2 in range(NQ):
                            kt = NQ * j + h2
                            for dh in range(2):
                                ob = 6 + cnt["o"] % 2
                                cnt["o"] += 1
                                for jj in range(4):
                                    mm(ps[ob][:, 0:512], ACTT[ab][:, jj, h2 * 128:(h2 + 1) * 128], wk2(w2, jj, dh * 512, 512),
                                       jj == 0, jj == 3, [("ws", w2), ("ACTT", ab)], PSK(ob))
                                ya = Yacc[:, kt, dh * 512:(dh + 1) * 512]
                                if g == 0:
                                    P.op("dve", lambda e, ya=ya, ob=ob: e.tensor_copy(ya, ps[ob][:, 0:512]), reads=[],
                                         writes=[PSK(ob), ("Yacc", kt)])
                                else:
                                    tt(ya, ps[ob][:, 0:512], ya, ALU.add, [("Yacc", kt)], [PSK(ob), ("Yacc", kt)])
                        P.cond_end()
                for k in range(16):
                    r0 = ex * CAP + p_ * SEQ + k * 128
                    P.cond_begin(jb + k // NQ + 1)
                    sp_dma(yb_d[r0:r0 + 128, :], Yacc[:, k, :], [("Yacc", k)], [("yb", ex, p_, k)])
                    P.cond_end()
        P.barrier()

        off = OV
        NCB = 4
        YA, YB2, OO, XC8 = [], [], [], []
        for i in range(NCB):
            t_, off = A(f"mm_YA{i}", [D], F32, off)
            YA.append(t_)
            t_, off = A(f"mm_YB2{i}", [D], F32, off)
            YB2.append(t_)
            t_, off = A(f"mm_OO{i}", [D], F32, off)
            OO.append(t_)
            t_, off = A(f"mm_XC8{i}", [8, 128], F32, off)
            XC8.append(t_)
        allyb = [("yb", a_, p_, b_) for a_ in range(NEXP) for p_ in range(NPASS) for b_ in range(16)]
        n_ = 0
        for s in seqs:
            W1, W2, SI = PS_[("W1", s)], PS_[("W2", s)], PS_[("SI", s)]
            for jt in range(16):
                b = n_ % NCB
                pb2 = n_ % 2
                n_ += 1
                ti = 1 + jt // 4
                c0 = CTX + jt * 128
                for k, dst, dk in ((0, YA, "YA"), (1, YB2, "YB2")):
                    P.dma("pool", lambda e, b=b, jt=jt, k=k, dst=dst, SI=SI: e.indirect_dma_start(
                        out=dst[b][:, :], out_offset=None, in_=yb_d,
                        in_offset=bass.IndirectOffsetOnAxis(ap=SI[:, jt, k:k + 1], axis=0)),
                        reads=allyb + [("SI", s)], writes=[(dk, b)])
                sp_dma(XC8[b][:, :, :], xres_d[s, :, :, c0:c0 + 128], [("xd", ti)], [("XC8", b)])
                ts(OO[b][:, :], YA[b][:, :], W1[:, jt, 0:1], None, ALU.mult, None, [("YA", b), ("W1", s)], [("OO", b)])
                stt(OO[b][:, :], YB2[b][:, :], W2[:, jt, 0:1], OO[b][:, :], ALU.mult, ALU.add, [("YB2", b), ("W2", s), ("OO", b)],
                    [("OO", b)])
                for c in range(8):
                    pbk = 2 + 2 * pb2 + c // 4
                    P.op("pe", lambda e, b=b, c=c, pbk=pbk: e.transpose(ps[pbk][:, (c % 4) * 128:(c % 4 + 1) * 128],
                                                                        OO[b][:, c * 128:(c + 1) * 128], ident_f[:, :]),
                         reads=[("OO", b), "ident_f"], writes=[PSK(pbk)])
                for c in range(8):
                    pbk = 2 + 2 * pb2 + c // 4
                    stt(XC8[b][:, c, :], ps[pbk][:, (c % 4) * 128:(c % 4 + 1) * 128], modv(l, 5, c, s), XC8[b][:, c, :],
                        ALU.mult, ALU.add, ["mod", ("XC8", b)], [PSK(pbk), ("XC8", b)])
                sp_dma(outT_d[s, :, :, jt * 128:(jt + 1) * 128], XC8[b][:, :, :], [("XC8", b)], [("out", s, jt)])
        P.barrier()
        return None

    result = None
    if stage in ("full", "seq1"):
        seqs = (1,) if stage == "seq1" else (0, 1)
        for s in seqs:
            state["x_in_res"] = False
            for l in range(2):
                token_mixer(l, s)
                if l == 1 and SPARSE_MOE:
                    if not MERGED_MOE:
                        ffn_moe_sparse(l, s)
                else:
                    ffn(l, s)
        if SPARSE_MOE and MERGED_MOE:
            moe_merged(1, seqs)
    elif si >= 1:
        result = token_mixer(0, 0)
        if result is None and stage in ("G0", "H0", "F1", "G1", "G1a", "H1"):
            result = ffn(0, 0)
            if result is None and stage in ("F1", "G1", "G1a", "H1"):
                result = token_mixer(1, 0)
                if result is None and stage in ("G1", "G1a", "H1"):
                    result = ffn_moe_sparse(1, 0) if SPARSE_MOE else ffn(1, 0)

    if debug is not None:
        if stage == "pro":
            sp_dma(dbg_d.rearrange("p (a b) -> p a b", b=4), mod[:, :, :, :].rearrange("p l a b -> p (l a) b"), ["mod"], ["dbg"])
        elif stage in ("F0", "H0", "F1"):
            sp_dma(dbg_d, xres_d[0], [("xd", t) for t in range(5)], ["dbg"])
        elif result == "done":
            pass
        elif result is not None:
            src_t, keys = result
            DT, _ = A("DT", [NT], F32, (A.limit - NT * 4 - 64) // 32 * 32)
            for c in range(8):
                act(DT[:, :], src_t[:, c, :], AF.Identity, keys, ["DT"])
                sp_dma(dbg_d[:, c, :], DT[:, :], ["DT"], ["dbg"])
    P.emit(nc)
    return nc


def kernel(**inputs):
    inp = {k: np.asarray(v, np.float32) for k, v in inputs.items()}
    shared = build_shared(inp)
    nc = build_program("full")
    in_maps = []
    for core in range(NCORES):
        m = dict(shared)
        m.update(build_core_inputs(inp, core))
        in_maps.append(m)
    res = run_bass_kernel_spmd(nc, in_maps, core_ids=list(range(NCORES)))
    out = np.empty((2 * NCORES, SEQ, D), np.float32)
    for core in range(NCORES):
        oT = np.asarray(res.results[core]["outT"])
        out[2 * core:2 * core + 2] = oT.transpose(0, 3, 2, 1).reshape(2, SEQ, D)
    return out
```

```python
import contextlib
import numpy as np
import concourse.bass as bass
import concourse.mybir as mybir
from concourse.bass_utils import run_bass_kernel_spmd

F32 = mybir.dt.float32
BF16 = mybir.dt.bfloat16
AF = mybir.ActivationFunctionType
ALU = mybir.AluOpType
AX = mybir.AxisListType

NCORES = 8
D = 1024
NCH = 8
CTX = 256
SEQ = 2048
NT = CTX + SEQ
TILES = [(0, 256), (256, 512), (768, 512), (1280, 512), (1792, 512)]
D_FF = 2816
D_FFE = 3584
NEXP = 8
EPS = 1e-6
NSLOT = 8
NRING = 5
SLAB = 4096
WCH = 16
SPARSE_MOE = True
MOE_TSZ = 512
JCLAMP = None
MERGED_MOE = True
ZERO_HG = True
SB_BASE = 16384 + 128


class Ins:
    __slots__ = ("eng", "fn", "reads", "writes", "dma", "deps", "sig", "semkey", "val", "slot", "cond")


class Prog:
    ENGS = ["pe", "act", "dve", "pool", "sp"]

    def __init__(self):
        self.ins = []
        self.cur_cond = None
        self.ncond = 0
        self.cond_thr = {}

    def op(self, eng, fn, reads=(), writes=()):
        i = Ins()
        i.eng, i.fn, i.reads, i.writes, i.dma = eng, fn, tuple(reads), tuple(writes), False
        i.sig, i.deps, i.semkey, i.val, i.slot = False, (), None, 0, 0
        i.cond = self.cur_cond
        self.ins.append(i)
        return i

    def cond_begin(self, thr):
        self.ncond += 1
        self.cur_cond = self.ncond
        self.cond_thr[self.ncond] = thr

    def cond_end(self):
        self.cur_cond = None

    def load_reg(self, ap, key, engines=("pe", "act", "dve")):
        for e in engines:
            self.op(e, ("REGLOAD", ap), reads=[key])

    def dma(self, q, fn, reads=(), writes=()):
        i = self.op(q, fn, reads, writes)
        i.dma = True
        return i

    def barrier(self):
        for e in ("pe", "act", "dve", "sp"):
            self.op(e, lambda en: en.nop(), writes=("_bar",))

    def resolve(self):
        last_w = {}
        readers = {}
        ndma = {e: 0 for e in self.ENGS}
        slot_last = {e: {} for e in self.ENGS}
        for idx, I in enumerate(self.ins):
            reads = I.reads
            if I.eng != "pool" and "_bar" not in I.writes:
                reads = reads + ("_bar",)
            deps = {}
            for k in reads:
                j = last_w.get(k)
                if j is not None:
                    deps[j] = True
            for k in I.writes:
                j = last_w.get(k)
                if j is not None:
                    deps.setdefault(j, False)
                r = readers.get(k)
                if r:
                    for j2 in r[0].values():
                        deps.setdefault(j2, False)
                    for j2 in r[1]:
                        deps.setdefault(j2, False)
            final = []
            for j, raw in deps.items():
                J = self.ins[j]
                if J.dma:
                    final.append(j)
                elif J.eng == I.eng:
                    if I.dma or (raw and I.eng != "pe"):
                        final.append(j)
                else:
                    final.append(j)
            if I.dma:
                q = I.eng
                slot = ndma[q] % NSLOT
                prev = slot_last[q].get(slot)
                if prev is not None:
                    final.append(prev)
                slot_last[q][slot] = idx
                I.slot = slot
                ndma[q] += 1
            I.deps = final
            for j in final:
                self.ins[j].sig = True
            for k in reads:
                r = readers.setdefault(k, ({}, []))
                if I.dma:
                    r[1].append(idx)
                else:
                    r[0][I.eng] = idx
            for k in I.writes:
                last_w[k] = idx
                readers[k] = ({}, [])
        cnt = {e: 0 for e in self.ENGS}
        dcnt = {}
        for I in self.ins:
            if I.dma:
                key = ("d", I.eng, I.slot)
                dcnt[key] = dcnt.get(key, 0) + 16
                I.semkey, I.val = key, dcnt[key]
            elif I.sig:
                cnt[I.eng] += 1
                I.semkey, I.val = ("e", I.eng), cnt[I.eng]
        self.final_dma = dict(dcnt)

    def emit(self, nc, final_waits_on="sp"):
        self.resolve()
        keys = [("e", e) for e in self.ENGS]
        for e in self.ENGS:
            if any(I.dma and I.eng == e for I in self.ins):
                keys += [("d", e, s) for s in range(NSLOT)]
        with contextlib.ExitStack() as st:
            sems = {}
            for k in keys:
                sems[k] = st.enter_context(nc.semaphore("s_" + "_".join(str(x) for x in k)))
            block = st.enter_context(nc.Block())
            per = {e: [I for I in self.ins if I.eng == e] for e in self.ENGS}

            def replay(ename, eng):
                seen = {}
                reg = {}

                def do_waits(I, seen, only_external=None):
                    waits = {}
                    for j in I.deps:
                        J = self.ins[j]
                        if only_external is not None and J.cond == only_external:
                            continue
                        if waits.get(J.semkey, 0) < J.val:
                            waits[J.semkey] = J.val
                    for sk, v in waits.items():
                        if seen.get(sk, 0) < v:
                            eng.wait_ge(sems[sk], v)
                            seen[sk] = v

                def run(I):
                    if isinstance(I.fn, tuple):
                        if "r" not in reg:
                            reg["r"] = eng.alloc_register("rj_" + ename)
                        r = eng.reg_load(reg["r"], I.fn[1])
                    else:
                        r = I.fn(eng)
                    if I.dma:
                        r.then_inc(sems[I.semkey], 16)
                    elif I.sig:
                        r.then_inc(sems[I.semkey], 1)

                lst = per[ename]
                n = len(lst)
                p = 0
                while p < n:
                    I = lst[p]
                    if I.cond is None:
                        do_waits(I, seen)
                        run(I)
                        p += 1
                        continue
                    cid = I.cond
                    q = p
                    while q < n and lst[q].cond == cid:
                        q += 1
                    body = lst[p:q]
                    for B in body:
                        do_waits(B, seen, only_external=cid)
                    snap = dict(seen)
                    k = sum(1 for B in body if B.sig and not B.dma)
                    with eng.If_lt(reg["r"], self.cond_thr[cid]):
                        if k > 0:
                            eng.drain().then_inc(sems[("e", ename)], k)
                        for B in body:
                            if B.dma:
                                eng.nop().then_inc(sems[B.semkey], 16)
                        if k == 0 and not any(B.dma for B in body):
                            eng.nop()
                    with eng.Else():
                        inner = dict(snap)
                        for B in body:
                            do_waits(B, inner)
                            run(B)
                    seen = snap
                    p = q
                if ename == final_waits_on:
                    for sk, v in self.final_dma.items():
                        if seen.get(sk, 0) < v:
                            eng.wait_ge(sems[sk], v)

            @block.tensor
            def _(e):
                replay("pe", e)

            @block.scalar
            def _(e):
                replay("act", e)

            @block.vector
            def _(e):
                replay("dve", e)

            @block.gpsimd
            def _(e):
                replay("pool", e)

            @block.sync
            def _(e):
                replay("sp", e)


def na_plan():
    pats = []
    groups = {}
    plan = []
    for qp in range(16):
        r0 = 2 * qp
        s = [min(max(r - 4, 0), 24) for r in (r0, r0 + 1)]
        first = s[0] // 2
        last = (s[1] + 7) // 2
        keys = []
        for m in range(first, last + 1):
            key = []
            for kl in range(2):
                kr = 2 * m + kl
                for ql in range(2):
                    r = r0 + ql
                    valid = s[ql] <= kr < s[ql] + 8
                    key.append(kr - r + 7 if valid else None)
            keys.append(tuple(key))
        gk = tuple(keys)
        if gk not in groups:
            groups[gk] = len(pats)
            pats.extend(keys)
        base = groups[gk]
        plan.append([(first + i, base + i) for i in range(len(keys))])
    return pats, plan


NA_PATS, NA_PLAN = na_plan()
NPAT = len(NA_PATS)


def slab_order():
    pro = [("mod", l, g) for l in range(2) for g in range(12)]
    seq = []
    for l in range(2):
        ntile = 5 if l == 0 else 4
        seq += [("rnn", l, i) for i in range(4)]
        for g in range(2):
            seq += [("rnno", l, g), ("gr", l, g)]
        seq += [("kvq", l, c) for c in range(8)]
        for t in range(ntile):
            for g in range(2):
                seq += [("nao", l, g), ("gn", l, g)]
            for g in range(2):
                seq += [("out", l, g)]
        if l == 0:
            for g in range(6):
                seq += [("f1", g), ("f3", g), ("f2", g)]
        else:
            for e in range(NEXP):
                for g in range(7):
                    seq += [("m1", e, g), ("m3", e, g), ("m2", e, g)]
    return pro, seq


def unique_slabs():
    pro, seq = slab_order()
    names = []
    seen = set()
    for n in pro + seq:
        if n not in seen:
            seen.add(n)
            names.append(n)
    return names


SLAB_NAMES = unique_slabs()
SLAB_IDX = {n: i for i, n in enumerate(SLAB_NAMES)}


def _slab_cols(W, cols):
    S = W[:, cols]
    return np.ascontiguousarray(S.reshape(8, 128, 512).transpose(1, 0, 2)).reshape(128, SLAB)


def _slab_rows(W2, r0):
    blk = np.zeros((512, 1024), np.float32)
    n = max(0, min(512, W2.shape[0] - r0))
    blk[:n] = W2[r0:r0 + n]
    return np.ascontiguousarray(blk.reshape(4, 128, 1024).transpose(1, 0, 2)).reshape(128, SLAB)


def _cols_pad(W, c0, n):
    idx = np.full(512, c0, np.int64)
    idx[:n] = np.arange(c0, c0 + n)
    return idx


def build_wstream(inp):
    ar = np.arange
    out = np.empty((len(SLAB_NAMES), 128, SLAB), np.float32)
    for i, nm in enumerate(SLAB_NAMES):
        k = nm[0]
        if k == "mod":
            _, l, g = nm
            out[i] = _slab_cols(inp["w_mod"][l], ar(g * 512, g * 512 + 512))
        elif k == "rnn":
            _, l, j = nm
            cols = np.concatenate([ar(c * 128, c * 128 + 128) if which == 0 else ar(3072 + c * 128, 3072 + c * 128 + 128)
                                   for c in (2 * j, 2 * j + 1) for which in (0, 1)])
            out[i] = _slab_cols(inp["w_in"][l], cols)
        elif k == "rnno":
            _, l, g = nm
            out[i] = _slab_cols(inp["w_rnn_o"][l], ar(g * 512, g * 512 + 512))
        elif k == "gr":
            _, l, g = nm
            out[i] = _slab_cols(inp["w_in"][l], ar(5120 + g * 512, 5120 + g * 512 + 512))
        elif k == "kvq":
            _, l, c = nm
            cols = np.concatenate([ar(1024 + c * 128, 1024 + c * 128 + 128), ar(2048 + c * 128, 2048 + c * 128 + 128),
                                   ar(4096 + c * 128, 4096 + c * 128 + 128), ar(4096 + c * 128, 4096 + c * 128 + 128)])
            out[i] = _slab_cols(inp["w_in"][l], cols)
        elif k == "nao":
            _, l, g = nm
            out[i] = _slab_cols(inp["w_na_o"][l], ar(g * 512, g * 512 + 512))
        elif k == "gn":
            _, l, g = nm
            out[i] = _slab_cols(inp["w_in"][l], ar(6144 + g * 512, 6144 + g * 512 + 512))
        elif k == "out":
            _, l, g = nm
            out[i] = _slab_cols(inp["w_out"][l], ar(g * 512, g * 512 + 512))
        elif k in ("f1", "f3"):
            _, g = nm
            W = inp["ffn_w1"][0] if k == "f1" else inp["ffn_w3"][0]
            n = min(512, D_FF - g * 512)
            out[i] = _slab_cols(W, _cols_pad(W, g * 512, n))
        elif k == "f2":
            _, g = nm
            out[i] = _slab_rows(inp["ffn_w2"][0], g * 512)
        elif k in ("m1", "m3"):
            _, e, g = nm
            W = inp["moe_w1"][0][e] if k == "m1" else inp["moe_w3"][0][e]
            out[i] = _slab_cols(W, ar(g * 512, g * 512 + 512))
        elif k == "m2":
            _, e, g = nm
            out[i] = _slab_rows(inp["moe_w2"][0][e], g * 512)
        else:
            raise KeyError(nm)
    return out


def _pm(v):
    v = np.asarray(v, np.float32)
    lead = v.shape[:-1]
    return np.ascontiguousarray(np.moveaxis(v.reshape(*lead, 8, 128), -1, 0))


def build_shared(inp):
    sh = {}
    wsr = build_wstream(inp)
    for i in range((len(SLAB_NAMES) + WCH - 1) // WCH):
        sh[f"wstream{i}"] = wsr[i * WCH:(i + 1) * WCH]
    sh["bmodT"] = np.ascontiguousarray(np.moveaxis(inp["b_mod"].reshape(2, 48, 128), -1, 0))
    sh["convw"] = np.ascontiguousarray(np.moveaxis(inp["conv_w"].reshape(2, 4, 8, 128), -1, 0).transpose(0, 1, 3, 2))
    sh["convb"] = _pm(inp["conv_b"])
    sh["lam"] = _pm(inp["rg_lambda"])
    sh["rgb"] = _pm(inp["rg_b"])
    g = np.stack([inp["q_gain"], inp["k_gain"]], 1)
    sh["gains"] = np.ascontiguousarray(np.concatenate([g, g], -1).transpose(2, 0, 1))
    rgw = inp["rg_w"]
    bd = np.zeros((2, 128, 2, 2, 8, 128), np.float32)
    for hb in range(2):
        blk = rgw[:, :, :, hb::2]
        bd[:, hb * 64:(hb + 1) * 64, :, :, :, hb * 64:(hb + 1) * 64] = np.moveaxis(blk, 4, 1)
    sh["rgw"] = bd.reshape(2, 128, 32 * 128)
    kp = np.arange(128)
    kl, kc = kp // 64, kp % 64
    ql, qc = kp // 64, kp % 64
    wstart = np.clip(qc - 8, 0, 48)
    colv = (kc[:, None] >= wstart[None, :]) & (kc[:, None] < wstart[None, :] + 16)
    coff = np.clip(kc[:, None] - qc[None, :] + 15, 0, 30)
    bias = np.zeros((2, 8, 128, 2, NPAT, 128), np.float32)
    mask = np.zeros((128, NPAT, 128), np.float32)
    rpb = inp["rpb"]
    for pc, key in enumerate(NA_PATS):
        drm = np.full((128, 128), -1, np.int64)
        for a in range(2):
            for b in range(2):
                dr = key[a * 2 + b]
                if dr is not None:
                    sel = (kl[:, None] == a) & (ql[None, :] == b)
                    drm[sel] = dr
        valid = (drm >= 0) & colv
        mask[:, pc, :] = valid
        drc = np.where(drm >= 0, drm, 0)
        gathered = rpb[:, :, drc, coff]
        gathered = np.where(valid[None, None], gathered, np.float32(0))
        bias[:, :, :, :, pc, :] = gathered.reshape(2, 8, 2, 128, 128).transpose(0, 1, 3, 2, 4)
    sh["biasG"] = bias.reshape(2, 8, 128, 2 * NPAT * 128)
    sh["maskG"] = mask.reshape(128, NPAT * 128)
    sh["router"] = np.ascontiguousarray(inp["router"][0].reshape(8, 128, 8).transpose(1, 0, 2)).reshape(128, 64)
    sh["ident"] = np.eye(128, dtype=np.float32)
    sh["ltri"] = np.triu(np.ones((128, 128), np.float32), 1)
    return sh


def build_core_inputs(inp, core):
    b0 = 2 * core
    toks = np.concatenate([inp["ctx"][b0:b0 + 2], inp["x"][b0:b0 + 2]], axis=1)
    xT = np.ascontiguousarray(toks.reshape(2, NT, 8, 128).transpose(0, 3, 2, 1))
    cv = np.zeros((4, 1024), np.float32)
    cv[0:2] = inp["c"][b0:b0 + 2]
    cv[2] = inp["c_ctx"]
    scT = np.ascontiguousarray(cv.reshape(4, 8, 128).transpose(2, 1, 0))
    return {"xT": xT, "scT": scT}


def _nbytes(dt):
    return 2 if dt == BF16 else 4


class SBAlloc:
    def __init__(self, nc, limit):
        self.nc, self.off, self.limit, self.n = nc, SB_BASE, SB_BASE + limit - 256, 0

    def __call__(self, name, free_shape, dt, off=None):
        size = int(np.prod(free_shape)) * _nbytes(dt)
        size = (size + 31) // 32 * 32
        if off is None:
            off = self.off
            self.off += size
        assert off + size <= self.limit, (name, off, size, self.limit)
        self.n += 1
        return self.nc.alloc_sbuf_tensor_at(f"{name}_{self.n}", [128] + list(free_shape), dt, offset=off), off + size


def build_program(stage="full", debug=None, nslabs=None):
    nc = bass.Bass("TRN2", target_bir_lowering=False)
    P = Prog()
    limit = nc.sbuf_bytes_remaining
    A = SBAlloc(nc, limit)

    def din(name, shape):
        return nc.dram_tensor(name, list(shape), F32, kind="ExternalInput").ap()

    xT_d = din("xT", [2, 128, 8, NT])
    scT_d = din("scT", [128, 8, 4])
    nsl = nslabs or len(SLAB_NAMES)
    ws_d = [din(f"wstream{i}", [min(WCH, nsl - i * WCH), 128, SLAB]) for i in range((nsl + WCH - 1) // WCH)]
    bmod_d = din("bmodT", [128, 2, 48])
    convw_d = din("convw", [128, 2, 8, 4])
    convb_d = din("convb", [128, 2, 8])
    lam_d = din("lam", [128, 2, 2, 8])
    rgb_d = din("rgb", [128, 2, 2, 2, 8])
    gains_d = din("gains", [128, 2, 2])
    rgw_d = din("rgw", [2, 128, 32 * 128])
    biasG_d = din("biasG", [2, 8, 128, 2 * NPAT * 128])
    maskG_d = din("maskG", [128, NPAT * 128])
    router_d = din("router", [128, 64])
    ident_d = din("ident", [128, 128])
    ltri_d = din("ltri", [128, 128])
    outT_d = nc.dram_tensor("outT", [2, 128, 8, SEQ], F32, kind="ExternalOutput").ap()
    xres_d = nc.dram_tensor("xres", [2, 128, 8, NT], F32, kind="Internal").ap()
    mrnn_d = nc.dram_tensor("mrnn", [128, 8, NT], F32, kind="Internal").ap()
    hg_d = nc.dram_tensor("hg", [NEXP * SEQ * 2, D], BF16, kind="Internal").ap()
    yb_d = nc.dram_tensor("yb", [NEXP * SEQ * 2, D], F32, kind="Internal").ap()
    dbg_d = None
    if debug is not None:
        dbg_d = nc.dram_tensor("dbg", list(debug), F32, kind="ExternalOutput").ap()

    ones_bf, _ = A("ones_bf", [128], BF16)
    blk_bf, _ = A("blk_bf", [128], BF16)
    onesV, _ = A("onesV", [192], BF16)
    ident_f, _ = A("ident_f", [128], F32)
    ones_f, _ = A("ones_f", [128], F32)
    ident_bf, _ = A("ident_bf", [128], BF16)
    ltri_bf, _ = A("ltri_bf", [128], BF16)
    ZT, _ = A("ZT", [D], BF16)
    scT, _ = A("scT", [8, 4], F32)
    scb, _ = A("scb", [8, 4], BF16)
    bmodT, _ = A("bmodT", [2, 48], F32)
    mod, _ = A("mod", [2, 48, 4], F32)
    convw, _ = A("convw", [2, 8, 4], F32)
    convb, _ = A("convb", [2, 8], F32)
    lam, _ = A("lam", [2, 2, 8], F32)
    c1, _ = A("c1", [2, 2, 8], F32)
    c2, _ = A("c2", [2, 2, 8], F32)
    rgb, _ = A("rgb", [2, 2, 2, 8], F32)
    gains, _ = A("gains", [2, 2], F32)
    qg, _ = A("qg", [2], F32)
    rgw, _ = A("rgw", [32, 128], BF16)
    maskG, _ = A("maskG", [NPAT, 128], BF16)
    router, _ = A("router", [8, 8], F32)
    routb, _ = A("routb", [2], F32)
    WS = [A(f"ws{i}", [SLAB], BF16)[0] for i in range(NRING)]
    HT_OFF = A.off
    HT, _ = A("HT", [8, NT], BF16)
    NXR = 4
    for i_ in range(NXR):
        WS.append(A(f"wsx{i_}", [SLAB], BF16, HT_OFF + i_ * SLAB * 2)[0])
    XBASE = A.off

    ps = [nc.alloc_psum_tensor(f"ps{i}", [128, 512], F32) for i in range(8)]

    def PSK(i):
        return ("ps", i)

    ring = {"n": 0}

    def load_slab(name, big=False):
        i = ring["n"] % (NRING + NXR if big else NRING)
        ring["n"] += 1
        src = ws_d[SLAB_IDX[name] // WCH][SLAB_IDX[name] % WCH]
        dst = WS[i]
        wr = [("ws", i)] + ([("HT", t_) for t_ in range(5)] if i >= NRING else [])
        P.dma("pool", lambda e, dst=dst, src=src: e.dma_start(out=dst[:, :], in_=src), writes=wr)
        return i

    def wk(i, k, c0, n):
        return WS[i][:, k * 512 + c0: k * 512 + c0 + n]

    def wk2(i, j, c0, n):
        return WS[i][:, j * 1024 + c0: j * 1024 + c0 + n]

    def mm(out, lhsT, rhs, start, stop, reads, pk):
        P.op("pe", lambda e: e.matmul(out, lhsT, rhs, start=start, stop=stop), reads=reads, writes=[pk])

    def act(out, in_, func, reads, writes, bias=None, scale=None):
        kw = {}
        if bias is not None:
            kw["bias"] = bias
        if scale is not None:
            kw["scale"] = scale
        P.op("act", lambda e: e.activation(out, in_, func, **kw), reads=reads, writes=writes)

    def tt(out, in0, in1, op, reads, writes, eng="dve"):
        P.op(eng, lambda e: e.tensor_tensor(out, in0, in1, op), reads=reads, writes=writes)

    def ts(out, in0, s1, s2, op0, op1, reads, writes, eng="dve"):
        if s2 is None:
            P.op(eng, lambda e: e.tensor_scalar(out, in0, s1, None, op0), reads=reads, writes=writes)
        else:
            P.op(eng, lambda e: e.tensor_scalar(out, in0, s1, s2, op0, op1), reads=reads, writes=writes)

    def stt(out, in0, scalar, in1, op0, op1, reads, writes):
        P.op("dve", lambda e: e.scalar_tensor_tensor(out, in0, scalar, in1, op0, op1), reads=reads, writes=writes)

    def sp_dma(out, in_, reads, writes):
        P.dma("sp", lambda e: e.dma_start(out=out, in_=in_), reads=reads, writes=writes)

    def pool_dma(out, in_, reads, writes):
        P.dma("pool", lambda e: e.dma_start(out=out, in_=in_), reads=reads, writes=writes)

    P.op("dve", lambda e: e.memset(ones_bf[:, :], 1.0), writes=["ones_bf"])
    P.op("dve", lambda e: e.memset(ones_f[:, :], 1.0), writes=["ones_f"])
    P.op("dve", lambda e: e.memset(blk_bf[:, :], 0.0), writes=["blk_bf"])
    P.op("dve", lambda e: e.memset(blk_bf[0:64, 0:64], 1.0), writes=["blk_bf"])
    P.op("dve", lambda e: e.memset(blk_bf[64:128, 64:128], 1.0), writes=["blk_bf"])
    P.op("dve", lambda e: e.memset(onesV[:, :], 1.0), writes=["onesV"])
    P.op("dve", lambda e: e.memset(onesV[:, 64:128], 0.0), writes=["onesV"])
    sp_dma(ident_f[:, :], ident_d, [], ["ident_f"])
    pool_dma(ident_bf[:, :], ident_d, [], ["ident_bf"])
    pool_dma(ltri_bf[:, :], ltri_d, [], ["ltri_bf"])
    sp_dma(scT[:, :, :], scT_d, [], ["scT"])
    sp_dma(bmodT[:, :, :], bmod_d, [], ["bmodT"])
    sp_dma(convw[:, :, :, :], convw_d, [], ["convw"])
    sp_dma(convb[:, :, :], convb_d, [], ["convb"])
    sp_dma(lam[:, :, :, :], lam_d, [], ["lam"])
    sp_dma(rgb[:, :, :, :, :], rgb_d, [], ["rgb"])
    sp_dma(gains[:, :, :], gains_d, [], ["gains"])
    sp_dma(router[:, :, :], router_d.rearrange("p (k e) -> p k e", e=8), [], ["router"])
    pool_dma(maskG[:, :, :], maskG_d.rearrange("p (a b) -> p a b", b=128), [], ["maskG"])
    P.op("dve", lambda e: e.memset(ZT[:, :], 0.0), writes=["ZT"])

    zf_state = {"n": 0}

    def zero_fill_hg(nblk=64):
        tot = NEXP * SEQ * 2 // 128
        for blk in range(zf_state["n"], min(tot, zf_state["n"] + nblk)):
            sp_dma(hg_d[blk * 128:(blk + 1) * 128, :], ZT[:, :], ["ZT"], [("hgz", blk)])
        zf_state["n"] = min(tot, zf_state["n"] + nblk)
    act(c1[:, :, :, :], lam[:, :, :, :], AF.Exp, ["lam"], ["c1"], scale=-1.0)
    act(c1[:, :, :, :], c1[:, :, :, :], AF.Ln, ["c1"], ["c1"], bias=1.0)
    ts(c2[:, :, :, :], c1[:, :, :, :], -16.0, None, ALU.mult, None, ["c1"], ["c2"])
    ts(c1[:, :, :, :], c1[:, :, :, :], -8.0, None, ALU.mult, None, ["c1", "c2"], ["c1"])
    act(scb[:, :, :], scT[:, :, :], AF.Silu, ["scT"], ["scb"])
    for l in range(2):
        for g in range(12):
            i = load_slab(("mod", l, g), big=True)
            for j in range(4):
                col = (g * 4 + j) * 4
                for k in range(8):
                    mm(ps[0][:, col:col + 4], wk(i, k, j * 128, 128), scb[:, k, :], k == 0, k == 7,
                       [("ws", i), "scb"], PSK(0))
        pv = ps[0][:, 0:192].rearrange("p (a b) -> p a b", b=4)
        for j in range(3):
            tt(mod[:, l, :, j], pv[:, :, j], bmodT[:, l, :], ALU.add, ["bmodT"], [PSK(0), "mod"])
    for l in range(2):
        for m in (1, 4):
            ts(mod[:, l, m * 8:(m + 1) * 8, :], mod[:, l, m * 8:(m + 1) * 8, :], 1.0, None, ALU.add, None, ["mod"], ["mod"])

    def modv(l, m, c, col):
        return mod[:, l, m * 8 + c, col:col + 1]

    stages = ["pro", "A0", "B0", "C0", "D0", "E0", "F0", "G0", "H0", "F1", "G1", "G1a", "H1", "seq1", "full"]
    si = stages.index(stage)

    state = {"x_in_res": False}

    def modulate(l, s, which, tiles, xsrc, X, moe=False, after_tile=None):
        m_sh, m_sc = (0, 1) if which == 0 else (3, 4)
        for ti in tiles:
            t0, w = TILES[ti]
            col = 2 if ti == 0 else s
            xap, xkeys = xsrc(ti)
            SQ, RS = X["SQ"], X["RS"]
            for c in range(8):
                act(SQ[:, c, 0:w], xap(c), AF.Square, xkeys, [("SQ", c)])
            for c in range(8):
                mm(ps[1][:, 0:w], ones_bf[:, :], SQ[:, c, 0:w], c == 0, c == 7, [("SQ", c), "ones_bf"], PSK(1))
            act(RS[:, 0:w], ps[1][:, 0:w], AF.Sqrt, [], [PSK(1), "RS"], bias=EPS, scale=1.0 / D)
            P.op("dve", lambda e, w=w: e.reciprocal(RS[:, 0:w], RS[:, 0:w]), reads=["RS"], writes=["RS"])
            for c in range(8):
                if moe:
                    tmp, tk = X["TMP8"][:, c, 0:w], ("TMP8", c)
                else:
                    tmp, tk = X["TMP"][c % 2][:, 0:w], ("TMP", c % 2)
                stt(tmp, xap(c), modv(l, m_sc, c, col), RS[:, 0:w], ALU.mult, ALU.mult, list(xkeys) + ["mod", "RS"], [tk])
                act(HT[:, c, t0:t0 + w], tmp, AF.Identity, [tk, "mod"], [("HT", ti)], bias=modv(l, m_sh, c, col))
            if after_tile is not None:
                after_tile(ti)

    def token_mixer(l, s):
        ctx_out = (l == 0)
        tiles = [0, 1, 2, 3, 4]
        otiles = tiles if ctx_out else [1, 2, 3, 4]
        off = XBASE
        YB, off = A("YB", [8, NT], BF16, off)
        TB = off
        pool_dma(rgw[:, :, :], rgw_d[l].rearrange("p (a b) -> p a b", b=128), [], ["rgw"])
        ts(qg[:, 0:1], gains[:, l, 0:1], 0.125, None, ALU.mult, None, ["gains"], ["qg"])

        off = TB
        XL = []
        for i in range(2):
            t_, off = A(f"XL{i}", [8, 512], F32, off)
            XL.append(t_)
        X = {}
        X["SQ"], off = A("SQ", [8, 512], BF16, off)
        X["RS"], off = A("RS", [512], F32, off)
        X["TMP"] = []
        for i in range(2):
            t_, off = A(f"TMP{i}", [512], F32, off)
            X["TMP"].append(t_)
        xd = xres_d if state["x_in_res"] else xT_d

        def xsrc(ti):
            t0, w = TILES[ti]
            b = ti % 2
            sp_dma(XL[b][:, :, 0:w], xd[s, :, :, t0:t0 + w], [("xd", ti)], [("XL", b)])
            return (lambda c: XL[b][:, c, 0:w]), [("XL", b)]

        modulate(l, s, 0, tiles, xsrc, X)
        P.barrier()
        if stage == "A0":
            return HT, [("HT", t) for t in range(5)]

        off = TB
        S0s = []
        for i_ in range(2):
            t_, off = A(f"S0{i_}", [2312], F32, off)
            S0s.append(t_)
        XC, off = A("XC", [NT], F32, off)
        S2, off = A("S2", [NT], F32, off)
        S3, off = A("S3", [NT], F32, off)
        HF, off = A("HF", [NT], F32, off)
        HR, off = A("HR", [NT], F32, off)
        XCb, off = A("XCb", [NT], BF16, off)
        GT, off = A("GT", [512], F32, off)

        def xrp_pos(t0):
            return 2 + t0 if t0 < CTX else 261 + (t0 - CTX)

        for c in range(8):
            S0 = S0s[c % 2]
            S0k = ("S0", c % 2)
            if c % 2 == 0:
                wsi = load_slab(("rnn", l, c // 2))
            cb = (c % 2) * 256
            for a, b in ((0, 2), (258, 261), (2309, 2312)):
                P.op("dve", lambda e, a=a, b=b, S0=S0: e.memset(S0[:, a:b], 0.0), writes=[S0k])
            for ti in tiles:
                t0, w = TILES[ti]
                pb = 2 + (ti % 2)
                for k in range(8):
                    mm(ps[pb][:, 0:w], wk(wsi, k, cb, 128), HT[:, k, t0:t0 + w], k == 0, k == 7,
                       [("ws", wsi), ("HT", ti)], PSK(pb))
                p0 = xrp_pos(t0)
                act(S0[:, p0:p0 + w], ps[pb][:, 0:w], AF.Identity, [], [PSK(pb), S0k])
            for (d0, n, base) in ((0, CTX, 2), (CTX, SEQ, 261)):
                ts(XC[:, d0:d0 + n], S0[:, base - 2:base - 2 + n], convw[:, l, c, 0:1], convb[:, l, c:c + 1],
                   ALU.mult, ALU.add, [S0k, "convw", "convb"], [("XC", d0)])
                for j in range(1, 4):
                    stt(XC[:, d0:d0 + n], S0[:, base - 2 + j:base - 2 + j + n], convw[:, l, c, j:j + 1], XC[:, d0:d0 + n],
                        ALU.mult, ALU.add, [S0k, "convw", ("XC", d0)], [("XC", d0)])
            act(XCb[:, :], XC[:, :], AF.Identity, [("XC", 0), ("XC", CTX)], ["XCb"])
            for dr in range(2):
                for ti in tiles:
                    t0, w = TILES[ti]
                    for gt in range(2):
                        pb = 4 + gt + 2 * (ti % 2)
                        mm(ps[pb][:, 0:w], rgw[:, (dr * 2 + gt) * 8 + c, :], XCb[:, t0:t0 + w], True, True,
                           ["rgw", "XCb"], PSK(pb))
                        dst = S2 if gt == 0 else S3
                        act(dst[:, t0:t0 + w], ps[pb][:, 0:w], AF.Sigmoid, ["rgb"], [PSK(pb), ("S2" if gt == 0 else "S3")],
                            bias=rgb[:, l, dr, gt, c:c + 1])
                act(S0[:, 0:NT], S2[:, :], AF.Exp, ["S2", "c1"], [S0k], scale=c1[:, l, dr, c:c + 1])
                act(S2[:, :], S2[:, :], AF.Exp, ["S2", "c2"], ["S2"], scale=c2[:, l, dr, c:c + 1])
                act(S2[:, :], S2[:, :], AF.Sqrt, ["S2"], ["S2"], scale=-1.0, bias=1.0)
                tt(S3[:, :], S3[:, :], XC[:, :], ALU.mult, ["S3", ("XC", 0), ("XC", CTX)], ["S3"])
                tt(S3[:, :], S3[:, :], S2[:, :], ALU.mult, ["S3", "S2"], ["S3"])
                if dr == 0:
                    P.op("dve", lambda e, S0=S0: e.tensor_tensor_scan(HF[:, :], S0[:, 0:NT], S3[:, :], 0.0, ALU.mult, ALU.add),
                         reads=[S0k, "S3"], writes=["HF"])
                else:
                    P.op("dve", lambda e, S0=S0: e.tensor_tensor_scan(HR[:, CTX - 1::-1], S0[:, CTX - 1::-1], S3[:, CTX - 1::-1], 0.0,
                                                               ALU.mult, ALU.add),
                         reads=[S0k, "S3"], writes=["HR"])
                    P.op("dve", lambda e, S0=S0: e.tensor_tensor_scan(HR[:, NT - 1:CTX - 1:-1], S0[:, NT - 1:CTX - 1:-1],
                                                               S3[:, NT - 1:CTX - 1:-1], HR[:, 0:1], ALU.mult, ALU.add),
                         reads=[S0k, "S3", "HR"], writes=["HR"])
            tt(HF[:, :], HF[:, :], HR[:, :], ALU.add, ["HF", "HR"], ["HF"])
            for ti in otiles:
                t0, w = TILES[ti]
                pb = 2 + (ti % 2)
                for k in range(8):
                    mm(ps[pb][:, 0:w], wk(wsi, k, cb + 128, 128), HT[:, k, t0:t0 + w], k == 0, k == 7,
                       [("ws", wsi), ("HT", ti)], PSK(pb))
                act(GT[:, 0:w], ps[pb][:, 0:w], AF.Gelu_apprx_tanh, [], [PSK(pb), "GT"])
                tt(YB[:, c, t0:t0 + w], HF[:, t0:t0 + w], GT[:, 0:w], ALU.mult, ["HF", "GT"], [("YB", ti)])
            if stage == "B0" and debug is not None and c == 0:
                pass
        P.barrier()
        if stage == "B0":
            return YB, [("YB", t) for t in range(5)]

        off = TB
        SG, MR = [], []
        for i in range(2):
            t_, off = A(f"SG{i}", [512], F32, off)
            SG.append(t_)
            t_, off = A(f"MR{i}", [512], F32, off)
            MR.append(t_)
        cnt = 0
        for g in range(2):
            wa = load_slab(("rnno", l, g))
            wb = load_slab(("gr", l, g))
            for ti in otiles:
                t0, w = TILES[ti]
                for j in range(4):
                    dc = g * 4 + j
                    b = cnt % 2
                    cnt += 1
                    p1, p2 = 2 + b, 4 + b
                    for k in range(8):
                        mm(ps[p1][:, 0:w], wk(wa, k, j * 128, 128), YB[:, k, t0:t0 + w], k == 0, k == 7,
                           [("ws", wa), ("YB", ti)], PSK(p1))
                    for k in range(8):
                        mm(ps[p2][:, 0:w], wk(wb, k, j * 128, 128), HT[:, k, t0:t0 + w], k == 0, k == 7,
                           [("ws", wb), ("HT", ti)], PSK(p2))
                    act(SG[b][:, 0:w], ps[p2][:, 0:w], AF.Sigmoid, [], [PSK(p2), ("SG", b)])
                    tt(MR[b][:, 0:w], ps[p1][:, 0:w], SG[b][:, 0:w], ALU.mult, [("SG", b)], [PSK(p1), ("MR", b)])
                    sp_dma(mrnn_d[:, dc, t0:t0 + w], MR[b][:, 0:w], [("MR", b)], [("mrnn", dc, ti)])
        P.barrier()

        off = TB
        KT, QT, Vz, Eb = [], [], [], []
        for i in range(2):
            t_, off = A(f"KT{i}", [NT], BF16, off)
            KT.append(t_)
            t_, off = A(f"QT{i}", [NT], BF16, off)
            QT.append(t_)
            t_, off = A(f"Vz{i}", [18, 192], BF16, off)
            Vz.append(t_)
            t_, off = A(f"Eb{i}", [2, NPAT, 128], BF16, off)
            Eb.append(t_)
        SQh, RSh, RD = [], [], []
        for i in range(2):
            t_, off = A(f"SQh{i}", [512], BF16, off)
            SQh.append(t_)
            t_, off = A(f"RSh{i}", [512], F32, off)
            RSh.append(t_)
            t_, off = A(f"RD{i}", [128], F32, off)
            RD.append(t_)
        PT = []
        for hh in range(2):
            row = []
            for i in range(2):
                t_, off = A(f"PT{hh}{i}", [7, 128], BF16, off)
                row.append(t_)
            PT.append(row)
        for i in range(2):
            P.op("dve", lambda e, i=i: e.memset(Vz[i][:, :, 64:128], 0.0), writes=[("Vz", i)])
        acnt = {"n": 0}

        def attend_S(c, qtok0, chunks, out_ti):
            cb_ = c % 2
            i = acnt["n"] % 2
            acnt["n"] += 1
            clist = [(0, None), (1, None)] + [(2 + m, pc) for (m, pc) in chunks]
            nchunk = len(clist)
            for hh in range(2):
                pbase = hh * 64
                bX, bY = 2 + 2 * hh, 3 + 2 * hh
                ptk = ("PT", hh, i)
                pt = PT[hh][i]
                for ci, (jt, pc) in enumerate(clist):
                    bank = bX if ci < 4 else bY
                    col = (ci % 4) * 128
                    mm(ps[bank][:, col:col + 128], KT[cb_][pbase:pbase + 64, jt * 128:(jt + 1) * 128],
                       QT[cb_][pbase:pbase + 64, qtok0:qtok0 + 128], True, True, [("KT", cb_), ("QT", cb_)], PSK(bank))
                n1 = min(4, nchunk)
                act(pt[:, 0:n1, :], ps[bX][:, 0:n1 * 128].rearrange("p (a b) -> p a b", b=128), AF.Exp, [], [PSK(bX), ptk])
                if nchunk > 4:
                    n2 = nchunk - 4
                    act(pt[:, 4:nchunk, :], ps[bY][:, 0:n2 * 128].rearrange("p (a b) -> p a b", b=128), AF.Exp, [],
                        [PSK(bY), ptk])
                if chunks:
                    pc0, n = chunks[0][1], len(chunks)
                    tt(pt[:, 2:2 + n, :], pt[:, 2:2 + n, :], Eb[cb_][:, hh, pc0:pc0 + n, :], ALU.mult, [ptk, ("Eb", cb_)], [ptk])
            return (c, qtok0, clist, out_ti, i)

        def attend_PV(stt_):
            c, qtok0, clist, out_ti, i = stt_
            cb_ = c % 2
            nchunk = len(clist)
            total = 2 * nchunk
            idx = 0
            for hh in range(2):
                ptk = ("PT", hh, i)
                for ci, (jt, pc) in enumerate(clist):
                    lv = Vz[cb_][:, jt, 0:128] if hh == 0 else Vz[cb_][:, jt, 64:192]
                    lo = onesV[:, 0:128] if hh == 0 else onesV[:, 64:192]
                    mm(ps[6][:, 0:128], lv, PT[hh][i][:, ci, :], idx == 0, idx == total - 1, [("Vz", cb_), ptk], PSK(6))
                    mm(ps[7][:, 0:128], lo, PT[hh][i][:, ci, :], idx == 0, idx == total - 1, ["onesV", ptk], PSK(7))
                    idx += 1
            P.op("dve", lambda e: e.reciprocal(RD[i][:, :], ps[7][:, 0:128]), reads=[], writes=[PSK(7), ("RD", i)])
            tt(YB[:, c, qtok0:qtok0 + 128], ps[6][:, 0:128], RD[i][:, :], ALU.mult, [("RD", i)], [PSK(6), ("YB", out_ti)])

        pcnt = {"n": 0}

        def inproj_items(c):
            cb_ = c % 2
            items = []
            st_ = {}

            def first():
                st_["ws"] = load_slab(("kvq", l, c))
                pool_dma(Eb[cb_][:, :, :, :], biasG_d[l, c].rearrange("p (h a b) -> p h a b", h=2, b=128), ["_bar"], [("Eb", cb_)])
                act(Eb[cb_][:, :, :, :], Eb[cb_][:, :, :, :], AF.Exp, [("Eb", cb_)], [("Eb", cb_)])
                for hh in range(2):
                    tt(Eb[cb_][:, hh, :, :], Eb[cb_][:, hh, :, :], maskG[:, :, :], ALU.mult, [("Eb", cb_), "maskG"], [("Eb", cb_)])
            items.append(first)
            for (colbase, dst, dkey, gain, gkey, tl) in ((0, KT[cb_], ("KT", cb_), gains[:, l, 1:2], "gains", tiles),
                                                       (256, QT[cb_], ("QT", cb_), qg[:, 0:1], "qg", otiles)):
                for ti in tl:
                    def proj(colbase=colbase, dst=dst, dkey=dkey, gain=gain, gkey=gkey, ti=ti):
                        wsi = st_["ws"]
                        t0, w = TILES[ti]
                        b = pcnt["n"] % 2
                        pcnt["n"] += 1
                        pP, pS = (0, 1) if b == 0 else (6, 7)
                        for k in range(8):
                            mm(ps[pP][:, 0:w], wk(wsi, k, colbase, 128), HT[:, k, t0:t0 + w], k == 0, k == 7,
                               [("ws", wsi), ("HT", ti)], PSK(pP))
                        act(SQh[b][:, 0:w], ps[pP][:, 0:w], AF.Square, [], [PSK(pP), ("SQh", b)])
                        mm(ps[pS][:, 0:w], blk_bf[:, :], SQh[b][:, 0:w], True, True, [("SQh", b), "blk_bf"], PSK(pS))
                        act(RSh[b][:, 0:w], ps[pS][:, 0:w], AF.Sqrt, [], [PSK(pS), ("RSh", b)], bias=EPS, scale=1.0 / 64)
                        P.op("dve", lambda e, b=b, w=w: e.reciprocal(RSh[b][:, 0:w], RSh[b][:, 0:w]), reads=[("RSh", b)],
                             writes=[("RSh", b)])
                        stt(dst[:, t0:t0 + w], ps[pP][:, 0:w], gain, RSh[b][:, 0:w], ALU.mult, ALU.mult, [("RSh", b), gkey],
                            [PSK(pP), dkey])
                    items.append(proj)
            for j0 in range(0, 18, 4):
                def vproj(j0=j0):
                    wsi = st_["ws"]
                    n = min(4, 18 - j0)
                    bank = 0 if (j0 // 4) % 2 == 0 else 1
                    for jj in range(n):
                        jt = j0 + jj
                        ti = 0 if jt < 2 else 1 + (jt - 2) // 4
                        for k in range(8):
                            mm(ps[bank][:, jj * 128:(jj + 1) * 128], HT[:, k, jt * 128:(jt + 1) * 128], wk(wsi, k, 128, 128),
                               k == 0, k == 7, [("ws", wsi), ("HT", ti)], PSK(bank))
                    pv3 = ps[bank][:, 0:n * 128].rearrange("p (a b) -> p a b", b=128)
                    act(Vz[cb_][:, j0:j0 + n, 0:64], pv3[:, :, 0:64], AF.Identity, [], [PSK(bank), ("Vz", cb_)])
                    P.op("dve", lambda e, j0=j0, n=n, pv3=pv3: e.tensor_copy(Vz[cb_][:, j0:j0 + n, 128:192], pv3[:, :, 64:128]),
                         reads=[], writes=[PSK(bank), ("Vz", cb_)])
                items.append(vproj)
            return items

        for c in range(8):
            for it in inproj_items(c):
                it()
            calls = []
            if ctx_out:
                for qt in range(2):
                    calls.append((qt * 128, [], 0))
            for qp in range(16):
                calls.append((CTX + qp * 128, NA_PLAN[qp], 1 + qp // 4))
            pend = None
            for (q0, ch, oti) in calls:
                cur = attend_S(c, q0, ch, oti)
                if pend is not None:
                    attend_PV(pend)
                pend = cur
            attend_PV(pend)
        P.barrier()
        if stage == "D0":
            return YB, [("YB", t) for t in range(5)]

        off = TB
        MTa, off = A("MTa", [8, NT], BF16, off)
        XL2, MRL, T1, SG2 = [], [], [], []
        for i in range(2):
            t_, off = A(f"XL2{i}", [4, 512], F32, off)
            XL2.append(t_)
        for i in range(2):
            t_, off = A(f"MRL{i}", [512], F32, off)
            MRL.append(t_)
            t_, off = A(f"T1{i}", [512], F32, off)
            T1.append(t_)
            t_, off = A(f"SG2{i}", [512], F32, off)
            SG2.append(t_)
        cnt = 0
        for g in range(2):
            wa = load_slab(("nao", l, g))
            wb = load_slab(("gn", l, g))
            for ti in otiles:
                t0, w = TILES[ti]
                for j in range(4):
                    dc = g * 4 + j
                    b = cnt % 2
                    cnt += 1
                    p1, p2 = 2 + b, 4 + b
                    for k in range(8):
                        mm(ps[p1][:, 0:w], wk(wa, k, j * 128, 128), YB[:, k, t0:t0 + w], k == 0, k == 7,
                           [("ws", wa), ("YB", ti)], PSK(p1))
                    for k in range(8):
                        mm(ps[p2][:, 0:w], wk(wb, k, j * 128, 128), HT[:, k, t0:t0 + w], k == 0, k == 7,
                           [("ws", wb), ("HT", ti)], PSK(p2))
                    sp_dma(MRL[b][:, 0:w], mrnn_d[:, dc, t0:t0 + w], [("mrnn", dc, ti)], [("MRL", b)])
                    act(SG2[b][:, 0:w], ps[p2][:, 0:w], AF.Sigmoid, [], [PSK(p2), ("SG2", b)])
                    tt(T1[b][:, 0:w], ps[p1][:, 0:w], SG2[b][:, 0:w], ALU.mult, [("SG2", b)], [PSK(p1), ("T1", b)])
                    tt(MTa[:, dc, t0:t0 + w], T1[b][:, 0:w], MRL[b][:, 0:w], ALU.add, [("T1", b), ("MRL", b)], [("MTa", ti)])
        xn = 0
        for g in range(2):
            wo = load_slab(("out", l, g))
            for ti in otiles:
                t0, w = TILES[ti]
                col = 2 if ti == 0 else s
                xb = xn % 2
                xn += 1
                sp_dma(XL2[xb][:, :, 0:w], xd[s, :, g * 4:(g + 1) * 4, t0:t0 + w], [("xd", ti)], [("XL2", xb)])
                for j in range(4):
                    dc = g * 4 + j
                    b = cnt % 2
                    cnt += 1
                    p1 = 6 + b
                    for k in range(8):
                        mm(ps[p1][:, 0:w], wk(wo, k, j * 128, 128), MTa[:, k, t0:t0 + w], k == 0, k == 7,
                           [("ws", wo), ("MTa", ti)], PSK(p1))
                    stt(XL2[xb][:, j, 0:w], ps[p1][:, 0:w], modv(l, 2, dc, col), XL2[xb][:, j, 0:w], ALU.mult, ALU.add,
                        ["mod", ("XL2", xb)], [PSK(p1), ("XL2", xb)])
                sp_dma(xres_d[s, :, g * 4:(g + 1) * 4, t0:t0 + w], XL2[xb][:, :, 0:w], [("XL2", xb)], [("xdw", ti, g)])
        state["x_in_res"] = True
        P.barrier()
        return None

    def ffn(l, s):
        ctx_out = (l == 0)
        moe = (l == 1)
        otiles = [0, 1, 2, 3, 4] if ctx_out else [1, 2, 3, 4]
        off = XBASE
        XTs, off = A("XTs", [8, NT], F32, off)
        Gt, off = A("Gt", [16, 8], F32, off)
        OV = off
        X = {}
        X["SQ"], off = A("SQ2", [8, 512], BF16, off)
        X["RS"], off = A("RS2", [512], F32, off)
        if moe:
            X["TMP8"], off = A("TMP8", [8, 512], F32, off)
            LG, off = A("LG", [512], F32, off)
            LT, off = A("LT", [16, 8], F32, off)
            EQ1, off = A("EQ1", [16, 8], F32, off)
            L2, off = A("L2", [16, 8], F32, off)
            EQ2, off = A("EQ2", [16, 8], F32, off)
            TG, off = A("TG", [16, 8], F32, off)
            M1, off = A("M1", [16, 1], F32, off)
            M2, off = A("M2", [16, 1], F32, off)
            W1, off = A("W1", [16, 1], F32, off)
            W2, off = A("W2", [16, 1], F32, off)
        else:
            X["TMP"] = []
            for i in range(2):
                t_, off = A(f"TMPf{i}", [512], F32, off)
                X["TMP"].append(t_)
        for ti in otiles:
            t0, w = TILES[ti]
            sp_dma(XTs[:, :, t0:t0 + w], xres_d[s, :, :, t0:t0 + w], [("xd", ti)], [("XTs", ti)])

        def xsrc(ti):
            t0, w = TILES[ti]
            return (lambda c: XTs[:, c, t0:t0 + w]), [("XTs", ti)]

        if SPARSE_MOE and MERGED_MOE and stage in ("full", "seq1"):
            zero_fill_hg(128 if stage == "full" else 256)
        hook = None
        if moe:
            for c in range(8):
                mm(ps[2][0:8, 0:2], router[:, c, :], mod[:, l, 24 + c, s:s + 2], c == 0, c == 7, ["router", "mod"], PSK(2))
            act(routb[0:8, 0:2], ps[2][0:8, 0:2], AF.Identity, [], [PSK(2), "routb"])

            def hook(ti):
                t0, w = TILES[ti]
                for c in range(8):
                    mm(ps[2][0:8, 0:w], router[:, c, :], X["TMP8"][:, c, 0:w], c == 0, c == 7, ["router", ("TMP8", c)], PSK(2))
                act(LG[0:8, 0:w], ps[2][0:8, 0:w], AF.Identity, ["routb"], [PSK(2), "LG"], bias=routb[0:8, 0:1])
                for j in range(w // 128):
                    jt = (t0 - CTX) // 128 + j
                    P.op("pe", lambda e, jt=jt, j=j: e.transpose(ps[3][:, jt * 8:(jt + 1) * 8], LG[0:8, j * 128:(j + 1) * 128],
                                                                 ident_f[0:8, 0:8]),
                         reads=["LG", "ident_f"], writes=[PSK(3)])

        modulate(l, s, 1, otiles, xsrc, X, moe=moe, after_tile=hook)
        if moe:
            P.op("dve", lambda e: e.tensor_copy(LT[:, :, :], ps[3][:, 0:128].rearrange("p (a b) -> p a b", b=8)),
                 reads=[], writes=[PSK(3), "LT"])
            P.op("dve", lambda e: e.tensor_reduce(M1[:, :, 0], LT[:, :, :], AX.X, ALU.max), reads=["LT"], writes=["M1"])
            tt(EQ1[:, :, :], LT[:, :, :], M1[:, :, 0:1].to_broadcast([128, 16, 8]), ALU.is_equal, ["LT", "M1"], ["EQ1"])
            stt(L2[:, :, :], EQ1[:, :, :], -1.0e30, LT[:, :, :], ALU.mult, ALU.add, ["EQ1", "LT"], ["L2"])
            P.op("dve", lambda e: e.tensor_reduce(M2[:, :, 0], L2[:, :, :], AX.X, ALU.max), reads=["L2"], writes=["M2"])
            tt(EQ2[:, :, :], L2[:, :, :], M2[:, :, 0:1].to_broadcast([128, 16, 8]), ALU.is_equal, ["L2", "M2"], ["EQ2"])
            tt(W2[:, :, :], M2[:, :, :], M1[:, :, :], ALU.subtract, ["M1", "M2"], ["W2"])
            act(W2[:, :, :], W2[:, :, :], AF.Exp, ["W2"], ["W2"])
            ts(W1[:, :, :], W2[:, :, :], 1.0, None, ALU.add, None, ["W2"], ["W1"])
            P.op("dve", lambda e: e.reciprocal(W1[:, :, :], W1[:, :, :]), reads=["W1"], writes=["W1"])
            tt(W2[:, :, :], W2[:, :, :], W1[:, :, :], ALU.mult, ["W1", "W2"], ["W2"])
            tt(Gt[:, :, :], EQ1[:, :, :], W1[:, :, 0:1].to_broadcast([128, 16, 8]), ALU.mult, ["EQ1", "W1"], ["Gt"])
            tt(TG[:, :, :], EQ2[:, :, :], W2[:, :, 0:1].to_broadcast([128, 16, 8]), ALU.mult, ["EQ2", "W2"], ["TG"])
            tt(Gt[:, :, :], Gt[:, :, :], TG[:, :, :], ALU.add, ["Gt", "TG"], ["Gt"])
        P.barrier()
        if stage == "G0":
            return HT, [("HT", t) for t in range(5)]

        off = OV
        SIL, TT, ACTT, GE, DG = [], [], [], [], []
        for i in range(2):
            t_, off = A(f"SIL{i}", [512], BF16, off)
            SIL.append(t_)
            t_, off = A(f"TT{i}", [512], F32, off)
            TT.append(t_)
            t_, off = A(f"ACTT{i}", [4, 512], BF16, off)
            ACTT.append(t_)
            if moe:
                t_, off = A(f"GE{i}", [SEQ], F32, off)
                GE.append(t_)
                t_, off = A(f"DG{i}", [128], F32, off)
                DG.append(t_)
        cnt = {"h": 0, "a": 0, "o": 0, "d": 0}

        def swiglu_group(names, nj, tl, ge):
            w1 = load_slab(names[0])
            w3 = load_slab(names[1])
            w2 = load_slab(names[2])
            for ti in tl:
                t0, w = TILES[ti]
                col = 2 if ti == 0 else s
                ab = cnt["a"] % 2
                cnt["a"] += 1
                for j in range(nj):
                    b = cnt["h"] % 2
                    cnt["h"] += 1
                    b1, b3 = 2 + b, 4 + b
                    for k in range(8):
                        mm(ps[b1][:, 0:w], wk(w1, k, j * 128, 128), HT[:, k, t0:t0 + w], k == 0, k == 7,
                           [("ws", w1), ("HT", ti)], PSK(b1))
                    for k in range(8):
                        mm(ps[b3][:, 0:w], wk(w3, k, j * 128, 128), HT[:, k, t0:t0 + w], k == 0, k == 7,
                           [("ws", w3), ("HT", ti)], PSK(b3))
                    act(SIL[b][:, 0:w], ps[b1][:, 0:w], AF.Silu, [], [PSK(b1), ("SIL", b)])
                    if ge is None:
                        tt(ACTT[ab][:, j, 0:w], ps[b3][:, 0:w], SIL[b][:, 0:w], ALU.mult, [("SIL", b)], [PSK(b3), ("ACTT", ab)])
                    else:
                        tt(TT[b][:, 0:w], ps[b3][:, 0:w], SIL[b][:, 0:w], ALU.mult, [("SIL", b)], [PSK(b3), ("TT", b)])
                        tt(ACTT[ab][:, j, 0:w], TT[b][:, 0:w], GE[ge][:, t0 - CTX:t0 - CTX + w], ALU.mult,
                           [("TT", b), ("GE", ge)], [("ACTT", ab)])
                for dc in range(8):
                    ob = 6 + cnt["o"] % 2
                    cnt["o"] += 1
                    for j in range(nj):
                        mm(ps[ob][:, 0:w], wk2(w2, j, dc * 128, 128), ACTT[ab][:, j, 0:w], j == 0, j == nj - 1,
                           [("ws", w2), ("ACTT", ab)], PSK(ob))
                    stt(XTs[:, dc, t0:t0 + w], ps[ob][:, 0:w], modv(l, 5, dc, col), XTs[:, dc, t0:t0 + w], ALU.mult, ALU.add,
                        ["mod", ("XTs", ti)], [PSK(ob), ("XTs", ti)])

        if not moe:
            for g in range(6):
                swiglu_group([("f1", g), ("f3", g), ("f2", g)], 4 if g < 5 else 2, otiles, None)
        else:
            for ex in range(NEXP):
                ge = ex % 2
                for j0 in range(0, 16, 4):
                    bank = 0 if (j0 // 4) % 2 == 0 else 1
                    for jj in range(4):
                        jt = j0 + jj
                        d = cnt["d"] % 2
                        cnt["d"] += 1
                        ts(DG[d][:, :], ident_f[:, :], Gt[:, jt, ex:ex + 1], None, ALU.mult, None, ["ident_f", "Gt"], [("DG", d)])
                        mm(ps[bank][:, jj * 128:(jj + 1) * 128], ones_f[:, :], DG[d][:, :], True, True, ["ones_f", ("DG", d)],
                           PSK(bank))
                    act(GE[ge][:, j0 * 128:(j0 + 4) * 128], ps[bank][:, 0:512], AF.Identity, [], [PSK(bank), ("GE", ge)])
                for g in range(7):
                    swiglu_group([("m1", ex, g), ("m3", ex, g), ("m2", ex, g)], 4, [1, 2, 3, 4], ge)
        for ti in otiles:
            t0, w = TILES[ti]
            if l == 0:
                sp_dma(xres_d[s, :, :, t0:t0 + w], XTs[:, :, t0:t0 + w], [("XTs", ti)], [("xd", ti)])
            else:
                sp_dma(outT_d[s, :, :, t0 - CTX:t0 - CTX + w], XTs[:, :, t0:t0 + w], [("XTs", ti)], [("out", s, ti)])
        P.barrier()
        return None

    def ffn_moe_sparse(l, s):
        I32 = mybir.dt.int32
        TSZ = MOE_TSZ
        NQ = TSZ // 128
        NBLK = SEQ // TSZ
        lat = [1, 2, 3, 4]
        off = XBASE
        Gsm = {}
        for nm, shp, dt in (("W1", [16, 1], F32), ("W2", [16, 1], F32), ("SI", [16, 2], I32), ("JI", [8], I32)):
            Gsm[nm], off = A("m_" + nm, shp, dt, off)
        OV = off
        for nm, shp, dt in (("LT", [16, 8], F32), ("EQ1", [16, 8], F32), ("L2", [16, 8], F32), ("EQ2", [16, 8], F32),
                            ("M1", [16, 1], F32), ("M2", [16, 1], F32),
                            ("AB", [16, 8], BF16), ("PW", [16, 8], F32), ("TOT", [16, 8], F32), ("CS", [16, 8], F32),
                            ("EOFF", [16, 8], F32), ("TQ", [16, 8], F32), ("S12", [16, 2], F32),
                            ("NE", [8, 1], F32), ("THR", [8, 8], F32), ("CMP", [8, 8], F32), ("JF", [8], F32)):
            Gsm[nm], off = A("m_" + nm, shp, dt, off)
        LT, EQ1, L2, EQ2, M1, M2, W1, W2 = (Gsm[k] for k in ("LT", "EQ1", "L2", "EQ2", "M1", "M2", "W1", "W2"))
        AB, PW, TOT, CS, EOFF, TQ, S12, SI = (Gsm[k] for k in ("AB", "PW", "TOT", "CS", "EOFF", "TQ", "S12", "SI"))
        NE, THR, CMP, JF, JI = (Gsm[k] for k in ("NE", "THR", "CMP", "JF", "JI"))
        LG, off = A("mLG", [512], F32, off)
        XL = []
        for i in range(2):
            t_, off = A(f"mXL{i}", [8, 512], F32, off)
            XL.append(t_)
        X = {}
        X["SQ"], off = A("mSQ", [8, 512], BF16, off)
        X["RS"], off = A("mRS", [512], F32, off)
        X["TMP8"], off = A("mTMP8", [8, 512], F32, off)
        HTok = []
        for i in range(2):
            t_, off = A(f"HTok{i}", [D], BF16, off)
            HTok.append(t_)

        def xsrc(ti):
            t0, w = TILES[ti]
            b = ti % 2
            sp_dma(XL[b][:, :, 0:w], xres_d[s, :, :, t0:t0 + w], [("xd", ti)], [("XL", b)])
            return (lambda c: XL[b][:, c, 0:w]), [("XL", b)]

        for c in range(8):
            mm(ps[2][0:8, 0:2], router[:, c, :], mod[:, l, 24 + c, s:s + 2], c == 0, c == 7, ["router", "mod"], PSK(2))
        act(routb[0:8, 0:2], ps[2][0:8, 0:2], AF.Identity, [], [PSK(2), "routb"])

        def hook(ti):
            t0, w = TILES[ti]
            for c in range(8):
                mm(ps[2][0:8, 0:w], router[:, c, :], X["TMP8"][:, c, 0:w], c == 0, c == 7, ["router", ("TMP8", c)], PSK(2))
            act(LG[0:8, 0:w], ps[2][0:8, 0:w], AF.Identity, ["routb"], [PSK(2), "LG"], bias=routb[0:8, 0:1])
            for j in range(w // 128):
                jt = (t0 - CTX) // 128 + j
                P.op("pe", lambda e, jt=jt, j=j: e.transpose(ps[3][:, jt * 8:(jt + 1) * 8], LG[0:8, j * 128:(j + 1) * 128],
                                                             ident_f[0:8, 0:8]),
                     reads=["LG", "ident_f"], writes=[PSK(3)])

        modulate(l, s, 1, lat, xsrc, X, moe=True, after_tile=hook)
        P.op("dve", lambda e: e.tensor_copy(LT[:, :, :], ps[3][:, 0:128].rearrange("p (a b) -> p a b", b=8)),
             reads=[], writes=[PSK(3), "LT"])
        P.op("dve", lambda e: e.tensor_reduce(M1[:, :, 0], LT[:, :, :], AX.X, ALU.max), reads=["LT"], writes=["M1"])
        tt(EQ1[:, :, :], LT[:, :, :], M1[:, :, 0:1].to_broadcast([128, 16, 8]), ALU.is_equal, ["LT", "M1"], ["EQ1"])
        stt(L2[:, :, :], EQ1[:, :, :], -1.0e30, LT[:, :, :], ALU.mult, ALU.add, ["EQ1", "LT"], ["L2"])
        P.op("dve", lambda e: e.tensor_reduce(M2[:, :, 0], L2[:, :, :], AX.X, ALU.max), reads=["L2"], writes=["M2"])
        tt(EQ2[:, :, :], L2[:, :, :], M2[:, :, 0:1].to_broadcast([128, 16, 8]), ALU.is_equal, ["L2", "M2"], ["EQ2"])
        tt(W2[:, :, :], M2[:, :, :], M1[:, :, :], ALU.subtract, ["M1", "M2"], ["W2"])
        act(W2[:, :, :], W2[:, :, :], AF.Exp, ["W2"], ["W2"])
        ts(W1[:, :, :], W2[:, :, :], 1.0, None, ALU.add, None, ["W2"], ["W1"])
        P.op("dve", lambda e: e.reciprocal(W1[:, :, :], W1[:, :, :]), reads=["W1"], writes=["W1"])
        tt(W2[:, :, :], W2[:, :, :], W1[:, :, :], ALU.mult, ["W1", "W2"], ["W2"])
        tt(TQ[:, :, :], EQ1[:, :, :], EQ2[:, :, :], ALU.add, ["EQ1", "EQ2"], ["TQ"])
        P.op("dve", lambda e: e.tensor_copy(AB[:, :, :], TQ[:, :, :]), reads=["TQ"], writes=["AB"])
        ABf = AB[:, :, :].rearrange("p a b -> p (a b)")
        mm(ps[0][:, 0:128], ltri_bf[:, :], ABf, True, True, ["AB", "ltri_bf"], PSK(0))
        mm(ps[0][:, 128:256], ones_bf[:, :], ABf, True, True, ["AB", "ones_bf"], PSK(0))
        P.op("dve", lambda e: e.tensor_copy(PW[:, :, :], ps[0][:, 0:128].rearrange("p (a b) -> p a b", b=8)),
             reads=[], writes=[PSK(0), "PW"])
        P.op("dve", lambda e: e.tensor_copy(TOT[:, :, :], ps[0][:, 128:256].rearrange("p (a b) -> p a b", b=8)),
             reads=[], writes=[PSK(0), "TOT"])
        P.op("dve", lambda e: e.memset(CS[:, 0, :], 0.0), writes=["CS"])
        for j in range(1, 16):
            tt(CS[:, j, :], CS[:, j - 1, :], TOT[:, j - 1, :], ALU.add, ["CS", "TOT"], ["CS"])
        tt(NE[:, :, 0], CS[:, 15, :], TOT[:, 15, :], ALU.add, ["CS", "TOT"], ["NE"])
        for ex in range(NEXP):
            P.op("dve", lambda e, ex=ex: e.memset(EOFF[:, :, ex], float(ex * SEQ)), writes=["EOFF"])
            P.op("dve", lambda e, ex=ex: e.memset(THR[:, :, ex], float(ex * TSZ)), writes=["THR"])
        tt(PW[:, :, :], PW[:, :, :], CS[:, :, :], ALU.add, ["PW", "CS"], ["PW"])
        tt(PW[:, :, :], PW[:, :, :], EOFF[:, :, :], ALU.add, ["PW", "EOFF"], ["PW"])
        tt(TQ[:, :, :], EQ1[:, :, :], PW[:, :, :], ALU.mult, ["EQ1", "PW"], ["TQ"])
        P.op("dve", lambda e: e.tensor_reduce(S12[:, :, 0], TQ[:, :, :], AX.X, ALU.add), reads=["TQ"], writes=["S12"])
        tt(TQ[:, :, :], EQ2[:, :, :], PW[:, :, :], ALU.mult, ["EQ2", "PW", "S12"], ["TQ"])
        P.op("dve", lambda e: e.tensor_reduce(S12[:, :, 1], TQ[:, :, :], AX.X, ALU.add), reads=["TQ"], writes=["S12"])
        P.op("dve", lambda e: e.tensor_copy(SI[:, :, :], S12[:, :, :]), reads=["S12"], writes=["SI"])
        tt(CMP[:, :, :], NE[:, :, 0:1].to_broadcast([128, 8, 8]), THR[:, :, :], ALU.is_gt, ["NE", "THR"], ["CMP"])
        P.op("dve", lambda e: e.tensor_reduce(JF[:, :], CMP[:, :, :], AX.X, ALU.add), reads=["CMP"], writes=["JF"])
        if JCLAMP is not None:
            ts(JF[:, :], JF[:, :], float(JCLAMP), None, ALU.min, None, ["JF"], ["JF"])
        P.op("dve", lambda e: e.tensor_copy(JI[:, :], JF[:, :]), reads=["JF"], writes=["JI"])
        if stage == "G1a":
            DB, _ = A("DBG1", [64], F32, off)
            P.op("dve", lambda e: e.memset(DB[:, :], 0.0), writes=["DB"])
            P.op("dve", lambda e: e.tensor_copy(DB[:, 0:8], NE[:, :, 0]), reads=["NE"], writes=["DB"])
            P.op("dve", lambda e: e.tensor_copy(DB[:, 8:16], JF[:, :]), reads=["JF"], writes=["DB"])
            P.op("dve", lambda e: e.tensor_copy(DB[:, 16:48], S12[:, :, :].rearrange("p a b -> p (a b)")), reads=["S12"], writes=["DB"])
            P.op("dve", lambda e: e.tensor_copy(DB[:, 48:64], M1[:, :, 0]), reads=["M1"], writes=["DB"])
            sp_dma(dbg_d, DB[:, :], ["DB"], ["dbg"])
            return "done"
        psb = [ps[i][:, :].bitcast(BF16) for i in range(8)]
        if ZERO_HG:
            P.op("dve", lambda e: e.memset(HTok[0][:, :], 0.0), writes=[("HTok", 0)])
            for blk in range(NEXP * SEQ // 128):
                sp_dma(hg_d[blk * 128:(blk + 1) * 128, :], HTok[0][:, :], [("HTok", 0)], [("hgz", blk)])
        for jt in range(16):
            b = jt % 2
            ti = 1 + jt // 4
            for c in range(8):
                P.op("pe", lambda e, b=b, c=c, jt=jt: e.transpose(psb[b][:, c * 128:(c + 1) * 128],
                                                                  HT[:, c, CTX + jt * 128:CTX + (jt + 1) * 128], ident_bf[:, :]),
                     reads=[("HT", ti), "ident_bf"], writes=[PSK(b)])
            act(HTok[b][:, :], psb[b][:, :], AF.Identity, [], [PSK(b), ("HTok", b)])
            for k in range(2):
                P.dma("pool", lambda e, b=b, jt=jt, k=k: e.indirect_dma_start(
                    out=hg_d, out_offset=bass.IndirectOffsetOnAxis(ap=SI[:, jt, k:k + 1], axis=0), in_=HTok[b][:, :],
                    in_offset=None), reads=[("HTok", b), "SI"] + ([("hgz", q_) for q_ in range(128)] if ZERO_HG else []), writes=[("hg", jt, k)])
        P.barrier()
        if stage == "G1":
            return None

        off = OV
        Yacc, off = A("Yacc", [16, D], F32, off)
        HTg, off = A("HTg", [8, SEQ], BF16, off)
        HGs, SIL, ACTT = [], [], []
        t_, off = A("HGs0", [D], BF16, off)
        HGs = [t_, t_]
        for i in range(2):
            t_, off = A(f"mSIL{i}", [TSZ], BF16, off)
            SIL.append(t_)
            t_, off = A(f"mACTT{i}", [4, TSZ], BF16, off)
            ACTT.append(t_)
        cnt = {"h": 0, "a": 0, "o": 0, "g": 0}
        for ex in range(NEXP):
            P.load_reg(JI[0:1, ex:ex + 1], "JI")
            for k in range(16):
                b = cnt["g"] % 2
                cnt["g"] += 1
                r0 = ex * SEQ + k * 128
                sp_dma(HGs[b][:, :], hg_d[r0:r0 + 128, :], [("hg", a_, b_) for a_ in range(16) for b_ in range(2)], [("HGs", 0)])
                P.cond_begin(k // NQ + 1)
                for c in range(8):
                    P.op("pe", lambda e, b=b, c=c: e.transpose(psb[b][:, c * 128:(c + 1) * 128], HGs[b][:, c * 128:(c + 1) * 128],
                                                               ident_bf[:, :]),
                         reads=[("HGs", 0), "ident_bf"], writes=[PSK(b)])
                act(HTg[:, :, k * 128:(k + 1) * 128], psb[b][:, :].rearrange("p (a b) -> p a b", b=128), AF.Identity, [],
                    [PSK(b), ("HTg", k // NQ)])
                P.cond_end()
            for g in range(7):
                w1 = load_slab(("m1", ex, g), big=True)
                w3 = load_slab(("m3", ex, g), big=True)
                w2 = load_slab(("m2", ex, g), big=True)
                for j in range(NBLK):
                    P.cond_begin(j + 1)
                    ab = cnt["a"] % 2
                    cnt["a"] += 1
                    s0 = j * TSZ
                    for jj in range(4):
                        b = cnt["h"] % 2
                        cnt["h"] += 1
                        b1, b3 = 2 + b, 4 + b
                        for k in range(8):
                            mm(ps[b1][:, 0:TSZ], wk(w1, k, jj * 128, 128), HTg[:, k, s0:s0 + TSZ], k == 0, k == 7,
                               [("ws", w1), ("HTg", j)], PSK(b1))
                        for k in range(8):
                            mm(ps[b3][:, 0:TSZ], wk(w3, k, jj * 128, 128), HTg[:, k, s0:s0 + TSZ], k == 0, k == 7,
                               [("ws", w3), ("HTg", j)], PSK(b3))
                        act(SIL[b][:, :], ps[b1][:, 0:TSZ], AF.Silu, [], [PSK(b1), ("SIL", b)])
                        tt(ACTT[ab][:, jj, :], ps[b3][:, 0:TSZ], SIL[b][:, :], ALU.mult, [("SIL", b)], [PSK(b3), ("ACTT", ab)])
                    for h2 in range(NQ):
                        kt = NQ * j + h2
                        for dh in range(2):
                            ob = 6 + cnt["o"] % 2
                            cnt["o"] += 1
                            for jj in range(4):
                                mm(ps[ob][:, 0:512], ACTT[ab][:, jj, h2 * 128:(h2 + 1) * 128], wk2(w2, jj, dh * 512, 512),
                                   jj == 0, jj == 3, [("ws", w2), ("ACTT", ab)], PSK(ob))
                            ya = Yacc[:, kt, dh * 512:(dh + 1) * 512]
                            if g == 0:
                                P.op("dve", lambda e, ya=ya, ob=ob: e.tensor_copy(ya, ps[ob][:, 0:512]), reads=[],
                                     writes=[PSK(ob), ("Yacc", kt)])
                            else:
                                tt(ya, ps[ob][:, 0:512], ya, ALU.add, [("Yacc", kt)], [PSK(ob), ("Yacc", kt)])
                    P.cond_end()
            for k in range(16):
                r0 = ex * SEQ + k * 128
                sp_dma(yb_d[r0:r0 + 128, :], Yacc[:, k, :], [("Yacc", k)], [("yb", ex, k)])
        P.barrier()

        off = OV
        YA, YB2, OO, XC8 = [], [], [], []
        for i in range(2):
            t_, off = A(f"YA{i}", [D], F32, off)
            YA.append(t_)
            t_, off = A(f"YB2{i}", [D], F32, off)
            YB2.append(t_)
            t_, off = A(f"OO{i}", [D], F32, off)
            OO.append(t_)
            t_, off = A(f"XC8{i}", [8, 128], F32, off)
            XC8.append(t_)
        for jt in range(16):
            b = jt % 2
            ti = 1 + jt // 4
            c0 = CTX + jt * 128
            for k, dst, dk in ((0, YA, "YA"), (1, YB2, "YB2")):
                P.dma("pool", lambda e, b=b, jt=jt, k=k, dst=dst: e.indirect_dma_start(
                    out=dst[b][:, :], out_offset=None, in_=yb_d,
                    in_offset=bass.IndirectOffsetOnAxis(ap=SI[:, jt, k:k + 1], axis=0)),
                    reads=[("yb", a_, b_) for a_ in range(NEXP) for b_ in range(16)] + ["SI"], writes=[(dk, b)])
            sp_dma(XC8[b][:, :, :], xres_d[s, :, :, c0:c0 + 128], [("xd", ti)], [("XC8", b)])
            ts(OO[b][:, :], YA[b][:, :], W1[:, jt, 0:1], None, ALU.mult, None, [("YA", b), "W1"], [("OO", b)])
            stt(OO[b][:, :], YB2[b][:, :], W2[:, jt, 0:1], OO[b][:, :], ALU.mult, ALU.add, [("YB2", b), "W2", ("OO", b)], [("OO", b)])
            for c in range(8):
                pbk = 2 + 2 * b + c // 4
                P.op("pe", lambda e, b=b, c=c, pbk=pbk: e.transpose(ps[pbk][:, (c % 4) * 128:(c % 4 + 1) * 128],
                                                                    OO[b][:, c * 128:(c + 1) * 128], ident_f[:, :]),
                     reads=[("OO", b), "ident_f"], writes=[PSK(pbk)])
            for c in range(8):
                pbk = 2 + 2 * b + c // 4
                stt(XC8[b][:, c, :], ps[pbk][:, (c % 4) * 128:(c % 4 + 1) * 128], modv(l, 5, c, s), XC8[b][:, c, :],
                    ALU.mult, ALU.add, ["mod", ("XC8", b)], [PSK(pbk), ("XC8", b)])
            sp_dma(outT_d[s, :, :, jt * 128:(jt + 1) * 128], XC8[b][:, :, :], [("XC8", b)], [("out", s, jt)])
        P.barrier()
        return None

    def moe_merged(l, seqs):
        I32 = mybir.dt.int32
        TSZ = 512
        NQ = TSZ // 128
        CAP = SEQ * len(seqs)
        NPASS = len(seqs)
        off = XBASE
        PS_ = {}
        for s in seqs:
            for nm, shp, dt in (("W1", [16, 1], F32), ("W2", [16, 1], F32), ("SI", [16, 2], I32)):
                PS_[(nm, s)], off = A(f"mm_{nm}{s}", shp, dt, off)
        NEacc, off = A("mm_NEacc", [8, 1], F32, off)
        JI, off = A("mm_JI", [8], I32, off)
        OV = off
        G = {}
        for nm, shp, dt in (("LT", [16, 8], F32), ("EQ1", [16, 8], F32), ("L2", [16, 8], F32), ("EQ2", [16, 8], F32),
                            ("M1", [16, 1], F32), ("M2", [16, 1], F32),
                            ("AB", [16, 8], BF16), ("PW", [16, 8], F32), ("TOT", [16, 8], F32), ("CS", [16, 8], F32),
                            ("EOFF", [16, 8], F32), ("TQ", [16, 8], F32), ("S12", [16, 2], F32),
                            ("THR", [8, 8], F32), ("CMP", [8, 8], F32), ("JF", [8], F32)):
            G[nm], off = A("mm_" + nm, shp, dt, off)
        LT, EQ1, L2, EQ2, M1, M2 = (G[k] for k in ("LT", "EQ1", "L2", "EQ2", "M1", "M2"))
        AB, PW, TOT, CS, EOFF, TQ, S12 = (G[k] for k in ("AB", "PW", "TOT", "CS", "EOFF", "TQ", "S12"))
        THR, CMP, JF = (G[k] for k in ("THR", "CMP", "JF"))
        LG, off = A("mm_LG", [512], F32, off)
        XL = []
        for i in range(2):
            t_, off = A(f"mm_XL{i}", [8, 512], F32, off)
            XL.append(t_)
        X = {}
        X["SQ"], off = A("mm_SQ", [8, 512], BF16, off)
        X["RS"], off = A("mm_RS", [512], F32, off)
        X["TMP8"], off = A("mm_TMP8", [8, 512], F32, off)
        NHB = 4
        HTok = []
        for i in range(NHB):
            t_, off = A(f"mm_HTok{i}", [D], BF16, off)
            HTok.append(t_)
        psb = [ps[i][:, :].bitcast(BF16) for i in range(8)]
        lat = [1, 2, 3, 4]
        P.op("dve", lambda e: e.memset(NEacc[:, :, :], 0.0), writes=["NEacc"])
        for ex in range(NEXP):
            P.op("dve", lambda e, ex=ex: e.memset(EOFF[:, :, ex], float(ex * CAP)), writes=["EOFF"])
            P.op("dve", lambda e, ex=ex: e.memset(THR[:, :, ex], float(ex * TSZ)), writes=["THR"])

        for s in seqs:
            W1, W2, SI = PS_[("W1", s)], PS_[("W2", s)], PS_[("SI", s)]

            def xsrc(ti, s=s):
                t0, w = TILES[ti]
                b = ti % 2
                sp_dma(XL[b][:, :, 0:w], xres_d[s, :, :, t0:t0 + w], [("xd", ti)], [("XL", b)])
                return (lambda c: XL[b][:, c, 0:w]), [("XL", b)]

            for c in range(8):
                mm(ps[2][0:8, 0:2], router[:, c, :], mod[:, l, 24 + c, s:s + 2], c == 0, c == 7, ["router", "mod"], PSK(2))
            act(routb[0:8, 0:2], ps[2][0:8, 0:2], AF.Identity, [], [PSK(2), "routb"])

            def hook(ti):
                t0, w = TILES[ti]
                for c in range(8):
                    mm(ps[2][0:8, 0:w], router[:, c, :], X["TMP8"][:, c, 0:w], c == 0, c == 7, ["router", ("TMP8", c)], PSK(2))
                act(LG[0:8, 0:w], ps[2][0:8, 0:w], AF.Identity, ["routb"], [PSK(2), "LG"], bias=routb[0:8, 0:1])
                for j in range(w // 128):
                    jt = (t0 - CTX) // 128 + j
                    P.op("pe", lambda e, jt=jt, j=j: e.transpose(ps[3][:, jt * 8:(jt + 1) * 8], LG[0:8, j * 128:(j + 1) * 128],
                                                                 ident_f[0:8, 0:8]),
                         reads=["LG", "ident_f"], writes=[PSK(3)])

            modulate(l, s, 1, lat, xsrc, X, moe=True, after_tile=hook)
            P.op("dve", lambda e: e.tensor_copy(LT[:, :, :], ps[3][:, 0:128].rearrange("p (a b) -> p a b", b=8)),
                 reads=[], writes=[PSK(3), "LT"])
            P.op("dve", lambda e: e.tensor_reduce(M1[:, :, 0], LT[:, :, :], AX.X, ALU.max), reads=["LT"], writes=["M1"])
            tt(EQ1[:, :, :], LT[:, :, :], M1[:, :, 0:1].to_broadcast([128, 16, 8]), ALU.is_equal, ["LT", "M1"], ["EQ1"])
            stt(L2[:, :, :], EQ1[:, :, :], -1.0e30, LT[:, :, :], ALU.mult, ALU.add, ["EQ1", "LT"], ["L2"])
            P.op("dve", lambda e: e.tensor_reduce(M2[:, :, 0], L2[:, :, :], AX.X, ALU.max), reads=["L2"], writes=["M2"])
            tt(EQ2[:, :, :], L2[:, :, :], M2[:, :, 0:1].to_broadcast([128, 16, 8]), ALU.is_equal, ["L2", "M2"], ["EQ2"])
            tt(W2[:, :, :], M2[:, :, :], M1[:, :, :], ALU.subtract, ["M1", "M2"], [("W2", s)])
            act(W2[:, :, :], W2[:, :, :], AF.Exp, [("W2", s)], [("W2", s)])
            ts(W1[:, :, :], W2[:, :, :], 1.0, None, ALU.add, None, [("W2", s)], [("W1", s)])
            P.op("dve", lambda e, W1=W1: e.reciprocal(W1[:, :, :], W1[:, :, :]), reads=[("W1", s)], writes=[("W1", s)])
            tt(W2[:, :, :], W2[:, :, :], W1[:, :, :], ALU.mult, [("W1", s), ("W2", s)], [("W2", s)])
            tt(TQ[:, :, :], EQ1[:, :, :], EQ2[:, :, :], ALU.add, ["EQ1", "EQ2"], ["TQ"])
            P.op("dve", lambda e: e.tensor_copy(AB[:, :, :], TQ[:, :, :]), reads=["TQ"], writes=["AB"])
            ABf = AB[:, :, :].rearrange("p a b -> p (a b)")
            mm(ps[0][:, 0:128], ltri_bf[:, :], ABf, True, True, ["AB", "ltri_bf"], PSK(0))
            mm(ps[0][:, 128:256], ones_bf[:, :], ABf, True, True, ["AB", "ones_bf"], PSK(0))
            P.op("dve", lambda e: e.tensor_copy(PW[:, :, :], ps[0][:, 0:128].rearrange("p (a b) -> p a b", b=8)),
                 reads=[], writes=[PSK(0), "PW"])
            P.op("dve", lambda e: e.tensor_copy(TOT[:, :, :], ps[0][:, 128:256].rearrange("p (a b) -> p a b", b=8)),
                 reads=[], writes=[PSK(0), "TOT"])
            P.op("dve", lambda e: e.tensor_copy(CS[:, 0, :], NEacc[:, :, 0]), reads=["NEacc"], writes=["CS"])
            for j in range(1, 16):
                tt(CS[:, j, :], CS[:, j - 1, :], TOT[:, j - 1, :], ALU.add, ["CS", "TOT"], ["CS"])
            tt(NEacc[:, :, 0], CS[:, 15, :], TOT[:, 15, :], ALU.add, ["CS", "TOT"], ["NEacc"])
            tt(PW[:, :, :], PW[:, :, :], CS[:, :, :], ALU.add, ["PW", "CS"], ["PW"])
            tt(PW[:, :, :], PW[:, :, :], EOFF[:, :, :], ALU.add, ["PW", "EOFF"], ["PW"])
            tt(TQ[:, :, :], EQ1[:, :, :], PW[:, :, :], ALU.mult, ["EQ1", "PW"], ["TQ"])
            P.op("dve", lambda e: e.tensor_reduce(S12[:, :, 0], TQ[:, :, :], AX.X, ALU.add), reads=["TQ"], writes=["S12"])
            tt(TQ[:, :, :], EQ2[:, :, :], PW[:, :, :], ALU.mult, ["EQ2", "PW", "S12"], ["TQ"])
            P.op("dve", lambda e: e.tensor_reduce(S12[:, :, 1], TQ[:, :, :], AX.X, ALU.add), reads=["TQ"], writes=["S12"])
            P.op("dve", lambda e, SI=SI: e.tensor_copy(SI[:, :, :], S12[:, :, :]), reads=["S12"], writes=[("SI", s)])
            for jt in range(16):
                b = jt % 2
                hb = jt % NHB
                ti = 1 + jt // 4
                for c in range(8):
                    P.op("pe", lambda e, b=b, c=c, jt=jt: e.transpose(psb[b][:, c * 128:(c + 1) * 128],
                                                                      HT[:, c, CTX + jt * 128:CTX + (jt + 1) * 128], ident_bf[:, :]),
                         reads=[("HT", ti), "ident_bf"], writes=[PSK(b)])
                act(HTok[hb][:, :], psb[b][:, :], AF.Identity, [], [PSK(b), ("HTok", hb)])
                for k in range(2):
                    P.dma("pool", lambda e, hb=hb, jt=jt, k=k, SI=SI: e.indirect_dma_start(
                        out=hg_d, out_offset=bass.IndirectOffsetOnAxis(ap=SI[:, jt, k:k + 1], axis=0), in_=HTok[hb][:, :],
                        in_offset=None), reads=[("HTok", hb), ("SI", s)] + [("hgz", q_) for q_ in range(NEXP * SEQ * 2 // 128)], writes=[("hg", s, jt, k)])
        tt(CMP[:, :, :], NEacc[:, :, 0:1].to_broadcast([128, 8, 8]), THR[:, :, :], ALU.is_gt, ["NEacc", "THR"], ["CMP"])
        P.op("dve", lambda e: e.tensor_reduce(JF[:, :], CMP[:, :, :], AX.X, ALU.add), reads=["CMP"], writes=["JF"])
        P.op("dve", lambda e: e.tensor_copy(JI[:, :], JF[:, :]), reads=["JF"], writes=["JI"])
        P.barrier()

        off = OV
        Yacc, off = A("mm_Yacc", [16, D], F32, off)
        HTg, off = A("mm_HTg", [8, SEQ], BF16, off)
        HGs = []
        for i in range(2):
            t_, off = A(f"mm_HGs{i}", [D], BF16, off)
            HGs.append(t_)
        SIL, ACTT = [], []
        for i in range(2):
            t_, off = A(f"mm_SIL{i}", [TSZ], BF16, off)
            SIL.append(t_)
            t_, off = A(f"mm_ACTT{i}", [4, TSZ], BF16, off)
            ACTT.append(t_)
        cnt = {"h": 0, "a": 0, "o": 0, "g": 0}
        allhg = [("hg", s_, a_, b_) for s_ in seqs for a_ in range(16) for b_ in range(2)]
        for p_, ex in [(p__, e__) for p__ in range(NPASS) for e__ in range(NEXP)]:
            P.load_reg(JI[0:1, ex:ex + 1], "JI", engines=("pe", "act", "dve", "sp", "pool"))
            for _once in range(1):
                jb = 4 * p_
                for k in range(16):
                    b = cnt["g"] % 2
                    cnt["g"] += 1
                    r0 = ex * CAP + p_ * SEQ + k * 128
                    thr = jb + k // NQ + 1
                    P.cond_begin(thr)
                    sp_dma(HGs[b][:, :], hg_d[r0:r0 + 128, :], allhg, [("HGs", b)])
                    for c in range(8):
                        P.op("pe", lambda e, b=b, c=c: e.transpose(psb[b][:, c * 128:(c + 1) * 128], HGs[b][:, c * 128:(c + 1) * 128],
                                                                   ident_bf[:, :]),
                             reads=[("HGs", b), "ident_bf"], writes=[PSK(b)])
                    act(HTg[:, :, k * 128:(k + 1) * 128], psb[b][:, :].rearrange("p (a b) -> p a b", b=128), AF.Identity, [],
                        [PSK(b), ("HTg", k // NQ)])
                    P.cond_end()
                for g in range(7):
                    P.cond_begin(jb + 1)
                    w1 = load_slab(("m1", ex, g), big=True)
                    P.cond_end()
                    P.cond_begin(jb + 1)
                    w3 = load_slab(("m3", ex, g), big=True)
                    P.cond_end()
                    P.cond_begin(jb + 1)
                    w2 = load_slab(("m2", ex, g), big=True)
                    P.cond_end()
                    for j in range(4):
                        P.cond_begin(jb + j + 1)
                        ab = cnt["a"] % 2
                        cnt["a"] += 1
                        s0 = j * TSZ
                        for jj in range(4):
                            b = cnt["h"] % 2
                            cnt["h"] += 1
                            b1, b3 = 2 + b, 4 + b
                            for k in range(8):
                                mm(ps[b1][:, 0:TSZ], wk(w1, k, jj * 128, 128), HTg[:, k, s0:s0 + TSZ], k == 0, k == 7,
                                   [("ws", w1), ("HTg", j)], PSK(b1))
                            for k in range(8):
                                mm(ps[b3][:, 0:TSZ], wk(w3, k, jj * 128, 128), HTg[:, k, s0:s0 + TSZ], k == 0, k == 7,
                                   [("ws", w3), ("HTg", j)], PSK(b3))
                            act(SIL[b][:, :], ps[b1][:, 0:TSZ], AF.Silu, [], [PSK(b1), ("SIL", b)])
                            tt(ACTT[ab][:, jj, :], ps[b3][:, 0:TSZ], SIL[b][:, :], ALU.mult, [("SIL", b)], [PSK(b3), ("ACTT", ab)])
                        P.cond_end()
                        P.cond_begin(jb + j + 1)
                        for h2 in range(NQ):
                            kt = NQ * j + h2
                            for dh in range(2):
                                ob = 6 + cnt["o"] % 2
                                cnt["o"] += 1
                                for jj in range(4):
                                    mm(ps[ob][:, 0:512], ACTT[ab][:, jj, h2 * 128:(h2 + 1) * 128], wk2(w2, jj, dh * 512, 512),
                                       jj == 0, jj == 3, [("ws", w2), ("ACTT", ab)], PSK(ob))
                                ya = Yacc[:, kt, dh * 512:(dh + 1) * 512]
                                if g == 0:
                                    P.op("dve", lambda e, ya=ya, ob=ob: e.tensor_copy(ya, ps[ob][:, 0:512]), reads=[],
                                         writes=[PSK(ob), ("Yacc", kt)])
                                else:
                                    tt(ya, ps[ob][:, 0:512], ya, ALU.add, [("Yacc", kt)], [PSK(ob), ("Yacc", kt)])
                        P.cond_end()
                for k in range(16):
                    r0 = ex * CAP + p_ * SEQ + k * 128
                    P.cond_begin(jb + k // NQ + 1)
                    sp_dma(yb_d[r0:r0 + 128, :], Yacc[:, k, :], [("Yacc", k)], [("yb", ex, p_, k)])
                    P.cond_end()
        P.barrier()

        off = OV
        NCB = 4
        YA, YB2, OO, XC8 = [], [], [], []
        for i in range(NCB):
            t_, off = A(f"mm_YA{i}", [D], F32, off)
            YA.append(t_)
            t_, off = A(f"mm_YB2{i}", [D], F32, off)
            YB2.append(t_)
            t_, off = A(f"mm_OO{i}", [D], F32, off)
            OO.append(t_)
            t_, off = A(f"mm_XC8{i}", [8, 128], F32, off)
            XC8.append(t_)
        allyb = [("yb", a_, p_, b_) for a_ in range(NEXP) for p_ in range(NPASS) for b_ in range(16)]
        n_ = 0
        for s in seqs:
            W1, W2, SI = PS_[("W1", s)], PS_[("W2", s)], PS_[("SI", s)]
            for jt in range(16):
                b = n_ % NCB
                pb2 = n_ % 2
                n_ += 1
                ti = 1 + jt // 4
                c0 = CTX + jt * 128
                for k, dst, dk in ((0, YA, "YA"), (1, YB2, "YB2")):
                    P.dma("pool", lambda e, b=b, jt=jt, k=k, dst=dst, SI=SI: e.indirect_dma_start(
                        out=dst[b][:, :], out_offset=None, in_=yb_d,
                        in_offset=bass.IndirectOffsetOnAxis(ap=SI[:, jt, k:k + 1], axis=0)),
                        reads=allyb + [("SI", s)], writes=[(dk, b)])
                sp_dma(XC8[b][:, :, :], xres_d[s, :, :, c0:c0 + 128], [("xd", ti)], [("XC8", b)])
                ts(OO[b][:, :], YA[b][:, :], W1[:, jt, 0:1], None, ALU.mult, None, [("YA", b), ("W1", s)], [("OO", b)])
                stt(OO[b][:, :], YB2[b][:, :], W2[:, jt, 0:1], OO[b][:, :], ALU.mult, ALU.add, [("YB2", b), ("W2", s), ("OO", b)],
                    [("OO", b)])
                for c in range(8):
                    pbk = 2 + 2 * pb2 + c // 4
                    P.op("pe", lambda e, b=b, c=c, pbk=pbk: e.transpose(ps[pbk][:, (c % 4) * 128:(c % 4 + 1) * 128],
                                                                        OO[b][:, c * 128:(c + 1) * 128], ident_f[:, :]),
                         reads=[("OO", b), "ident_f"], writes=[PSK(pbk)])
                for c in range(8):
                    pbk = 2 + 2 * pb2 + c // 4
                    stt(XC8[b][:, c, :], ps[pbk][:, (c % 4) * 128:(c % 4 + 1) * 128], modv(l, 5, c, s), XC8[b][:, c, :],
                        ALU.mult, ALU.add, ["mod", ("XC8", b)], [PSK(pbk), ("XC8", b)])
                sp_dma(outT_d[s, :, :, jt * 128:(jt + 1) * 128], XC8[b][:, :, :], [("XC8", b)], [("out", s, jt)])
        P.barrier()
        return None

    result = None
    if stage in ("full", "seq1"):
        seqs = (1,) if stage == "seq1" else (0, 1)
        for s in seqs:
            state["x_in_res"] = False
            for l in range(2):
                token_mixer(l, s)
                if l == 1 and SPARSE_MOE:
                    if not MERGED_MOE:
                        ffn_moe_sparse(l, s)
                else:
                    ffn(l, s)
        if SPARSE_MOE and MERGED_MOE:
            moe_merged(1, seqs)
    elif si >= 1:
        result = token_mixer(0, 0)
        if result is None and stage in ("G0", "H0", "F1", "G1", "G1a", "H1"):
            result = ffn(0, 0)
            if result is None and stage in ("F1", "G1", "G1a", "H1"):
                result = token_mixer(1, 0)
                if result is None and stage in ("G1", "G1a", "H1"):
                    result = ffn_moe_sparse(1, 0) if SPARSE_MOE else ffn(1, 0)

    if debug is not None:
        if stage == "pro":
            sp_dma(dbg_d.rearrange("p (a b) -> p a b", b=4), mod[:, :, :, :].rearrange("p l a b -> p (l a) b"), ["mod"], ["dbg"])
        elif stage in ("F0", "H0", "F1"):
            sp_dma(dbg_d, xres_d[0], [("xd", t) for t in range(5)], ["dbg"])
        elif result == "done":
            pass
        elif result is not None:
            src_t, keys = result
            DT, _ = A("DT", [NT], F32, (A.limit - NT * 4 - 64) // 32 * 32)
            for c in range(8):
                act(DT[:, :], src_t[:, c, :], AF.Identity, keys, ["DT"])
                sp_dma(dbg_d[:, c, :], DT[:, :], ["DT"], ["dbg"])
    P.emit(nc)
    return nc


def kernel(**inputs):
    inp = {k: np.asarray(v, np.float32) for k, v in inputs.items()}
    shared = build_shared(inp)
    nc = build_program("full")
    in_maps = []
    for core in range(NCORES):
        m = dict(shared)
        m.update(build_core_inputs(inp, core))
        in_maps.append(m)
    res = run_bass_kernel_spmd(nc, in_maps, core_ids=list(range(NCORES)))
    out = np.empty((2 * NCORES, SEQ, D), np.float32)
    for core in range(NCORES):
        oT = np.asarray(res.results[core]["outT"])
        out[2 * core:2 * core + 2] = oT.transpose(0, 3, 2, 1).reshape(2, SEQ, D)
    return out
```

```python
import contextlib
import numpy as np
import concourse.bass as bass
import concourse.mybir as mybir
from concourse.bass_utils import run_bass_kernel_spmd

F32 = mybir.dt.float32
BF16 = mybir.dt.bfloat16
AF = mybir.ActivationFunctionType
ALU = mybir.AluOpType
AX = mybir.AxisListType

NCORES = 8
D = 1024
NCH = 8
CTX = 256
SEQ = 2048
NT = CTX + SEQ
TILES = [(0, 256), (256, 512), (768, 512), (1280, 512), (1792, 512)]
D_FF = 2816
D_FFE = 3584
NEXP = 8
EPS = 1e-6
NSLOT = 16
NRING = 5
SLAB = 4096
WCH = 16
SPARSE_MOE = True
MOE_TSZ = 512
JCLAMP = None
MERGED_MOE = True
ZERO_HG = True
SB_BASE = 16384 + 128


class Ins:
    __slots__ = ("eng", "fn", "reads", "writes", "dma", "deps", "sig", "semkey", "val", "slot", "cond")


class Prog:
    ENGS = ["pe", "act", "dve", "pool", "sp"]

    def __init__(self):
        self.ins = []
        self.cur_cond = None
        self.ncond = 0
        self.cond_thr = {}

    def op(self, eng, fn, reads=(), writes=()):
        i = Ins()
        i.eng, i.fn, i.reads, i.writes, i.dma = eng, fn, tuple(reads), tuple(writes), False
        i.sig, i.deps, i.semkey, i.val, i.slot = False, (), None, 0, 0
        i.cond = self.cur_cond
        self.ins.append(i)
        return i

    def cond_begin(self, thr):
        self.ncond += 1
        self.cur_cond = self.ncond
        self.cond_thr[self.ncond] = thr

    def cond_end(self):
        self.cur_cond = None

    def load_reg(self, ap, key, engines=("pe", "act", "dve")):
        for e in engines:
            self.op(e, ("REGLOAD", ap), reads=[key])

    def dma(self, q, fn, reads=(), writes=()):
        i = self.op(q, fn, reads, writes)
        i.dma = True
        return i

    def barrier(self):
        for e in ("pe", "act", "dve", "sp"):
            self.op(e, lambda en: en.nop(), writes=("_bar",))

    def resolve(self):
        last_w = {}
        readers = {}
        ndma = {e: 0 for e in self.ENGS}
        slot_last = {e: {} for e in self.ENGS}
        for idx, I in enumerate(self.ins):
            reads = I.reads
            if I.eng != "pool" and "_bar" not in I.writes:
                reads = reads + ("_bar",)
            deps = {}
            for k in reads:
                j = last_w.get(k)
                if j is not None:
                    deps[j] = True
            for k in I.writes:
                j = last_w.get(k)
                if j is not None:
                    deps.setdefault(j, False)
                r = readers.get(k)
                if r:
                    for j2 in r[0].values():
                        deps.setdefault(j2, False)
                    for j2 in r[1]:
                        deps.setdefault(j2, False)
            final = []
            for j, raw in deps.items():
                J = self.ins[j]
                if J.dma:
                    final.append(j)
                elif J.eng == I.eng:
                    if I.dma or (raw and I.eng != "pe"):
                        final.append(j)
                else:
                    final.append(j)
            if I.dma:
                q = I.eng
                slot = ndma[q] % NSLOT
                prev = slot_last[q].get(slot)
                if prev is not None:
                    final.append(prev)
                slot_last[q][slot] = idx
                I.slot = slot
                ndma[q] += 1
            I.deps = final
            for j in final:
                self.ins[j].sig = True
            for k in reads:
                r = readers.setdefault(k, ({}, []))
                if I.dma:
                    r[1].append(idx)
                else:
                    r[0][I.eng] = idx
            for k in I.writes:
                last_w[k] = idx
                readers[k] = ({}, [])
        cnt = {e: 0 for e in self.ENGS}
        dcnt = {}
        for I in self.ins:
            if I.dma:
                key = ("d", I.eng, I.slot)
                dcnt[key] = dcnt.get(key, 0) + 16
                I.semkey, I.val = key, dcnt[key]
            elif I.sig:
                cnt[I.eng] += 1
                I.semkey, I.val = ("e", I.eng), cnt[I.eng]
        self.final_dma = dict(dcnt)

    def emit(self, nc, final_waits_on="sp"):
        self.resolve()
        keys = [("e", e) for e in self.ENGS]
        for e in self.ENGS:
            if any(I.dma and I.eng == e for I in self.ins):
                keys += [("d", e, s) for s in range(NSLOT)]
        with contextlib.ExitStack() as st:
            sems = {}
            for k in keys:
                sems[k] = st.enter_context(nc.semaphore("s_" + "_".join(str(x) for x in k)))
            block = st.enter_context(nc.Block())
            per = {e: [I for I in self.ins if I.eng == e] for e in self.ENGS}

            def replay(ename, eng):
                seen = {}
                reg = {}

                def do_waits(I, seen, only_external=None):
                    waits = {}
                    for j in I.deps:
                        J = self.ins[j]
                        if only_external is not None and J.cond == only_external:
                            continue
                        if waits.get(J.semkey, 0) < J.val:
                            waits[J.semkey] = J.val
                    for sk, v in waits.items():
                        if seen.get(sk, 0) < v:
                            eng.wait_ge(sems[sk], v)
                            seen[sk] = v

                def run(I):
                    if isinstance(I.fn, tuple):
                        if "r" not in reg:
                            reg["r"] = eng.alloc_register("rj_" + ename)
                        r = eng.reg_load(reg["r"], I.fn[1])
                    else:
                        r = I.fn(eng)
                    if I.dma:
                        r.then_inc(sems[I.semkey], 16)
                    elif I.sig:
                        r.then_inc(sems[I.semkey], 1)

                lst = per[ename]
                n = len(lst)
                p = 0
                while p < n:
                    I = lst[p]
                    if I.cond is None:
                        do_waits(I, seen)
                        run(I)
                        p += 1
                        continue
                    cid = I.cond
                    q = p
                    while q < n and lst[q].cond == cid:
                        q += 1
                    body = lst[p:q]
                    for B in body:
                        do_waits(B, seen, only_external=cid)
                    snap = dict(seen)
                    k = sum(1 for B in body if B.sig and not B.dma)
                    with eng.If_lt(reg["r"], self.cond_thr[cid]):
                        if k > 0:
                            eng.drain().then_inc(sems[("e", ename)], k)
                        for B in body:
                            if B.dma:
                                eng.nop().then_inc(sems[B.semkey], 16)
                        if k == 0 and not any(B.dma for B in body):
                            eng.nop()
                    with eng.Else():
                        inner = dict(snap)
                        for B in body:
                            do_waits(B, inner)
                            run(B)
                    seen = snap
                    p = q
                if ename == final_waits_on:
                    for sk, v in self.final_dma.items():
                        if seen.get(sk, 0) < v:
                            eng.wait_ge(sems[sk], v)

            @block.tensor
            def _(e):
                replay("pe", e)

            @block.scalar
            def _(e):
                replay("act", e)

            @block.vector
            def _(e):
                replay("dve", e)

            @block.gpsimd
            def _(e):
                replay("pool", e)

            @block.sync
            def _(e):
                replay("sp", e)


def na_plan():
    pats = []
    groups = {}
    plan = []
    for qp in range(16):
        r0 = 2 * qp
        s = [min(max(r - 4, 0), 24) for r in (r0, r0 + 1)]
        first = s[0] // 2
        last = (s[1] + 7) // 2
        keys = []
        for m in range(first, last + 1):
            key = []
            for kl in range(2):
                kr = 2 * m + kl
                for ql in range(2):
                    r = r0 + ql
                    valid = s[ql] <= kr < s[ql] + 8
                    key.append(kr - r + 7 if valid else None)
            keys.append(tuple(key))
        gk = tuple(keys)
        if gk not in groups:
            groups[gk] = len(pats)
            pats.extend(keys)
        base = groups[gk]
        plan.append([(first + i, base + i) for i in range(len(keys))])
    return pats, plan


NA_PATS, NA_PLAN = na_plan()
NPAT = len(NA_PATS)


def slab_order():
    pro = [("mod", l, g) for l in range(2) for g in range(12)]
    seq = []
    for l in range(2):
        ntile = 5 if l == 0 else 4
        seq += [("rnn", l, i) for i in range(4)]
        for g in range(2):
            seq += [("rnno", l, g), ("gr", l, g)]
        seq += [("kvq", l, c) for c in range(8)]
        for t in range(ntile):
            for g in range(2):
                seq += [("nao", l, g), ("gn", l, g)]
            for g in range(2):
                seq += [("out", l, g)]
        if l == 0:
            for g in range(6):
                seq += [("f1", g), ("f3", g), ("f2", g)]
        else:
            for e in range(NEXP):
                for g in range(7):
                    seq += [("m1", e, g), ("m3", e, g), ("m2", e, g)]
    return pro, seq


def unique_slabs():
    pro, seq = slab_order()
    names = []
    seen = set()
    for n in pro + seq:
        if n not in seen:
            seen.add(n)
            names.append(n)
    return names


SLAB_NAMES = unique_slabs()
SLAB_IDX = {n: i for i, n in enumerate(SLAB_NAMES)}


def _slab_cols(W, cols):
    S = W[:, cols]
    return np.ascontiguousarray(S.reshape(8, 128, 512).transpose(1, 0, 2)).reshape(128, SLAB)


def _slab_rows(W2, r0):
    blk = np.zeros((512, 1024), np.float32)
    n = max(0, min(512, W2.shape[0] - r0))
    blk[:n] = W2[r0:r0 + n]
    return np.ascontiguousarray(blk.reshape(4, 128, 1024).transpose(1, 0, 2)).reshape(128, SLAB)


def _cols_pad(W, c0, n):
    idx = np.full(512, c0, np.int64)
    idx[:n] = np.arange(c0, c0 + n)
    return idx


def build_wstream(inp):
    ar = np.arange
    out = np.empty((len(SLAB_NAMES), 128, SLAB), np.float32)
    for i, nm in enumerate(SLAB_NAMES):
        k = nm[0]
        if k == "mod":
            _, l, g = nm
            out[i] = _slab_cols(inp["w_mod"][l], ar(g * 512, g * 512 + 512))
        elif k == "rnn":
            _, l, j = nm
            cols = np.concatenate([ar(c * 128, c * 128 + 128) if which == 0 else ar(3072 + c * 128, 3072 + c * 128 + 128)
                                   for c in (2 * j, 2 * j + 1) for which in (0, 1)])
            out[i] = _slab_cols(inp["w_in"][l], cols)
        elif k == "rnno":
            _, l, g = nm
            out[i] = _slab_cols(inp["w_rnn_o"][l], ar(g * 512, g * 512 + 512))
        elif k == "gr":
            _, l, g = nm
            out[i] = _slab_cols(inp["w_in"][l], ar(5120 + g * 512, 5120 + g * 512 + 512))
        elif k == "kvq":
            _, l, c = nm
            cols = np.concatenate([ar(1024 + c * 128, 1024 + c * 128 + 128), ar(2048 + c * 128, 2048 + c * 128 + 128),
                                   ar(4096 + c * 128, 4096 + c * 128 + 128), ar(4096 + c * 128, 4096 + c * 128 + 128)])
            out[i] = _slab_cols(inp["w_in"][l], cols)
        elif k == "nao":
            _, l, g = nm
            out[i] = _slab_cols(inp["w_na_o"][l], ar(g * 512, g * 512 + 512))
        elif k == "gn":
            _, l, g = nm
            out[i] = _slab_cols(inp["w_in"][l], ar(6144 + g * 512, 6144 + g * 512 + 512))
        elif k == "out":
            _, l, g = nm
            out[i] = _slab_cols(inp["w_out"][l], ar(g * 512, g * 512 + 512))
        elif k in ("f1", "f3"):
            _, g = nm
            W = inp["ffn_w1"][0] if k == "f1" else inp["ffn_w3"][0]
            n = min(512, D_FF - g * 512)
            out[i] = _slab_cols(W, _cols_pad(W, g * 512, n))
        elif k == "f2":
            _, g = nm
            out[i] = _slab_rows(inp["ffn_w2"][0], g * 512)
        elif k in ("m1", "m3"):
            _, e, g = nm
            W = inp["moe_w1"][0][e] if k == "m1" else inp["moe_w3"][0][e]
            out[i] = _slab_cols(W, ar(g * 512, g * 512 + 512))
        elif k == "m2":
            _, e, g = nm
            out[i] = _slab_rows(inp["moe_w2"][0][e], g * 512)
        else:
            raise KeyError(nm)
    return out


def _pm(v):
    v = np.asarray(v, np.float32)
    lead = v.shape[:-1]
    return np.ascontiguousarray(np.moveaxis(v.reshape(*lead, 8, 128), -1, 0))


def build_shared(inp):
    sh = {}
    wsr = build_wstream(inp)
    for i in range((len(SLAB_NAMES) + WCH - 1) // WCH):
        sh[f"wstream{i}"] = wsr[i * WCH:(i + 1) * WCH]
    sh["bmodT"] = np.ascontiguousarray(np.moveaxis(inp["b_mod"].reshape(2, 48, 128), -1, 0))
    sh["convw"] = np.ascontiguousarray(np.moveaxis(inp["conv_w"].reshape(2, 4, 8, 128), -1, 0).transpose(0, 1, 3, 2))
    sh["convb"] = _pm(inp["conv_b"])
    sh["lam"] = _pm(inp["rg_lambda"])
    sh["rgb"] = _pm(inp["rg_b"])
    g = np.stack([inp["q_gain"], inp["k_gain"]], 1)
    sh["gains"] = np.ascontiguousarray(np.concatenate([g, g], -1).transpose(2, 0, 1))
    rgw = inp["rg_w"]
    bd = np.zeros((2, 128, 2, 2, 8, 128), np.float32)
    for hb in range(2):
        blk = rgw[:, :, :, hb::2]
        bd[:, hb * 64:(hb + 1) * 64, :, :, :, hb * 64:(hb + 1) * 64] = np.moveaxis(blk, 4, 1)
    sh["rgw"] = bd.reshape(2, 128, 32 * 128)
    kp = np.arange(128)
    kl, kc = kp // 64, kp % 64
    ql, qc = kp // 64, kp % 64
    wstart = np.clip(qc - 8, 0, 48)
    colv = (kc[:, None] >= wstart[None, :]) & (kc[:, None] < wstart[None, :] + 16)
    coff = np.clip(kc[:, None] - qc[None, :] + 15, 0, 30)
    bias = np.zeros((2, 8, 128, 2, NPAT, 128), np.float32)
    mask = np.zeros((128, NPAT, 128), np.float32)
    rpb = inp["rpb"]
    for pc, key in enumerate(NA_PATS):
        drm = np.full((128, 128), -1, np.int64)
        for a in range(2):
            for b in range(2):
                dr = key[a * 2 + b]
                if dr is not None:
                    sel = (kl[:, None] == a) & (ql[None, :] == b)
                    drm[sel] = dr
        valid = (drm >= 0) & colv
        mask[:, pc, :] = valid
        drc = np.where(drm >= 0, drm, 0)
        gathered = rpb[:, :, drc, coff]
        gathered = np.where(valid[None, None], gathered, np.float32(0))
        bias[:, :, :, :, pc, :] = gathered.reshape(2, 8, 2, 128, 128).transpose(0, 1, 3, 2, 4)
    sh["biasG"] = bias.reshape(2, 8, 128, 2 * NPAT * 128)
    sh["maskG"] = mask.reshape(128, NPAT * 128)
    sh["router"] = np.ascontiguousarray(inp["router"][0].reshape(8, 128, 8).transpose(1, 0, 2)).reshape(128, 64)
    sh["ident"] = np.eye(128, dtype=np.float32)
    sh["ltri"] = np.triu(np.ones((128, 128), np.float32), 1)
    return sh


def build_core_inputs(inp, core):
    b0 = 2 * core
    toks = np.concatenate([inp["ctx"][b0:b0 + 2], inp["x"][b0:b0 + 2]], axis=1)
    xT = np.ascontiguousarray(toks.reshape(2, NT, 8, 128).transpose(0, 3, 2, 1))
    cv = np.zeros((4, 1024), np.float32)
    cv[0:2] = inp["c"][b0:b0 + 2]
    cv[2] = inp["c_ctx"]
    scT = np.ascontiguousarray(cv.reshape(4, 8, 128).transpose(2, 1, 0))
    return {"xT": xT, "scT": scT}


def _nbytes(dt):
    return 2 if dt == BF16 else 4


class SBAlloc:
    def __init__(self, nc, limit):
        self.nc, self.off, self.limit, self.n = nc, SB_BASE, SB_BASE + limit - 256, 0

    def __call__(self, name, free_shape, dt, off=None):
        size = int(np.prod(free_shape)) * _nbytes(dt)
        size = (size + 31) // 32 * 32
        if off is None:
            off = self.off
            self.off += size
        assert off + size <= self.limit, (name, off, size, self.limit)
        self.n += 1
        return self.nc.alloc_sbuf_tensor_at(f"{name}_{self.n}", [128] + list(free_shape), dt, offset=off), off + size


def build_program(stage="full", debug=None, nslabs=None):
    nc = bass.Bass("TRN2", target_bir_lowering=False)
    P = Prog()
    limit = nc.sbuf_bytes_remaining
    A = SBAlloc(nc, limit)

    def din(name, shape):
        return nc.dram_tensor(name, list(shape), F32, kind="ExternalInput").ap()

    xT_d = din("xT", [2, 128, 8, NT])
    scT_d = din("scT", [128, 8, 4])
    nsl = nslabs or len(SLAB_NAMES)
    ws_d = [din(f"wstream{i}", [min(WCH, nsl - i * WCH), 128, SLAB]) for i in range((nsl + WCH - 1) // WCH)]
    bmod_d = din("bmodT", [128, 2, 48])
    convw_d = din("convw", [128, 2, 8, 4])
    convb_d = din("convb", [128, 2, 8])
    lam_d = din("lam", [128, 2, 2, 8])
    rgb_d = din("rgb", [128, 2, 2, 2, 8])
    gains_d = din("gains", [128, 2, 2])
    rgw_d = din("rgw", [2, 128, 32 * 128])
    biasG_d = din("biasG", [2, 8, 128, 2 * NPAT * 128])
    maskG_d = din("maskG", [128, NPAT * 128])
    router_d = din("router", [128, 64])
    ident_d = din("ident", [128, 128])
    ltri_d = din("ltri", [128, 128])
    outT_d = nc.dram_tensor("outT", [2, 128, 8, SEQ], F32, kind="ExternalOutput").ap()
    xres_d = nc.dram_tensor("xres", [2, 128, 8, NT], F32, kind="Internal").ap()
    mrnn_d = nc.dram_tensor("mrnn", [128, 8, NT], F32, kind="Internal").ap()
    hg_d = nc.dram_tensor("hg", [NEXP * SEQ * 2, D], BF16, kind="Internal").ap()
    yb_d = nc.dram_tensor("yb", [NEXP * SEQ * 2, D], F32, kind="Internal").ap()
    dbg_d = None
    if debug is not None:
        dbg_d = nc.dram_tensor("dbg", list(debug), F32, kind="ExternalOutput").ap()

    ones_bf, _ = A("ones_bf", [128], BF16)
    blk_bf, _ = A("blk_bf", [128], BF16)
    onesV, _ = A("onesV", [192], BF16)
    ident_f, _ = A("ident_f", [128], F32)
    ones_f, _ = A("ones_f", [128], F32)
    ident_bf, _ = A("ident_bf", [128], BF16)
    ltri_bf, _ = A("ltri_bf", [128], BF16)
    ZT, _ = A("ZT", [D], BF16)
    scT, _ = A("scT", [8, 4], F32)
    scb, _ = A("scb", [8, 4], BF16)
    bmodT, _ = A("bmodT", [2, 48], F32)
    mod, _ = A("mod", [2, 48, 4], F32)
    convw, _ = A("convw", [2, 8, 4], F32)
    convb, _ = A("convb", [2, 8], F32)
    lam, _ = A("lam", [2, 2, 8], F32)
    c1, _ = A("c1", [2, 2, 8], F32)
    c2, _ = A("c2", [2, 2, 8], F32)
    rgb, _ = A("rgb", [2, 2, 2, 8], F32)
    gains, _ = A("gains", [2, 2], F32)
    qg, _ = A("qg", [2], F32)
    rgw, _ = A("rgw", [32, 128], BF16)
    maskG, _ = A("maskG", [NPAT, 128], BF16)
    router, _ = A("router", [8, 8], F32)
    routb, _ = A("routb", [2], F32)
    WS = [A(f"ws{i}", [SLAB], BF16)[0] for i in range(NRING)]
    HT_OFF = A.off
    HT, _ = A("HT", [8, NT], BF16)
    NXR = 4
    for i_ in range(NXR):
        WS.append(A(f"wsx{i_}", [SLAB], BF16, HT_OFF + i_ * SLAB * 2)[0])
    XBASE = A.off

    ps = [nc.alloc_psum_tensor(f"ps{i}", [128, 512], F32) for i in range(8)]

    def PSK(i):
        return ("ps", i)

    ring = {"n": 0}

    def load_slab(name, big=False):
        i = ring["n"] % (NRING + NXR if big else NRING)
        ring["n"] += 1
        src = ws_d[SLAB_IDX[name] // WCH][SLAB_IDX[name] % WCH]
        dst = WS[i]
        wr = [("ws", i)] + ([("HT", t_) for t_ in range(5)] if i >= NRING else [])
        P.dma("pool", lambda e, dst=dst, src=src: e.dma_start(out=dst[:, :], in_=src), writes=wr)
        return i

    def wk(i, k, c0, n):
        return WS[i][:, k * 512 + c0: k * 512 + c0 + n]

    def wk2(i, j, c0, n):
        return WS[i][:, j * 1024 + c0: j * 1024 + c0 + n]

    def mm(out, lhsT, rhs, start, stop, reads, pk):
        P.op("pe", lambda e: e.matmul(out, lhsT, rhs, start=start, stop=stop), reads=reads, writes=[pk])

    def act(out, in_, func, reads, writes, bias=None, scale=None):
        kw = {}
        if bias is not None:
            kw["bias"] = bias
        if scale is not None:
            kw["scale"] = scale
        P.op("act", lambda e: e.activation(out, in_, func, **kw), reads=reads, writes=writes)

    def tt(out, in0, in1, op, reads, writes, eng="dve"):
        P.op(eng, lambda e: e.tensor_tensor(out, in0, in1, op), reads=reads, writes=writes)

    def ts(out, in0, s1, s2, op0, op1, reads, writes, eng="dve"):
        if s2 is None:
            P.op(eng, lambda e: e.tensor_scalar(out, in0, s1, None, op0), reads=reads, writes=writes)
        else:
            P.op(eng, lambda e: e.tensor_scalar(out, in0, s1, s2, op0, op1), reads=reads, writes=writes)

    def stt(out, in0, scalar, in1, op0, op1, reads, writes):
        P.op("dve", lambda e: e.scalar_tensor_tensor(out, in0, scalar, in1, op0, op1), reads=reads, writes=writes)

    def sp_dma(out, in_, reads, writes):
        P.dma("sp", lambda e: e.dma_start(out=out, in_=in_), reads=reads, writes=writes)

    def pool_dma(out, in_, reads, writes):
        P.dma("pool", lambda e: e.dma_start(out=out, in_=in_), reads=reads, writes=writes)

    P.op("dve", lambda e: e.memset(ones_bf[:, :], 1.0), writes=["ones_bf"])
    P.op("dve", lambda e: e.memset(ones_f[:, :], 1.0), writes=["ones_f"])
    P.op("dve", lambda e: e.memset(blk_bf[:, :], 0.0), writes=["blk_bf"])
    P.op("dve", lambda e: e.memset(blk_bf[0:64, 0:64], 1.0), writes=["blk_bf"])
    P.op("dve", lambda e: e.memset(blk_bf[64:128, 64:128], 1.0), writes=["blk_bf"])
    P.op("dve", lambda e: e.memset(onesV[:, :], 1.0), writes=["onesV"])
    P.op("dve", lambda e: e.memset(onesV[:, 64:128], 0.0), writes=["onesV"])
    sp_dma(ident_f[:, :], ident_d, [], ["ident_f"])
    pool_dma(ident_bf[:, :], ident_d, [], ["ident_bf"])
    pool_dma(ltri_bf[:, :], ltri_d, [], ["ltri_bf"])
    sp_dma(scT[:, :, :], scT_d, [], ["scT"])
    sp_dma(bmodT[:, :, :], bmod_d, [], ["bmodT"])
    sp_dma(convw[:, :, :, :], convw_d, [], ["convw"])
    sp_dma(convb[:, :, :], convb_d, [], ["convb"])
    sp_dma(lam[:, :, :, :], lam_d, [], ["lam"])
    sp_dma(rgb[:, :, :, :, :], rgb_d, [], ["rgb"])
    sp_dma(gains[:, :, :], gains_d, [], ["gains"])
    sp_dma(router[:, :, :], router_d.rearrange("p (k e) -> p k e", e=8), [], ["router"])
    pool_dma(maskG[:, :, :], maskG_d.rearrange("p (a b) -> p a b", b=128), [], ["maskG"])
    P.op("dve", lambda e: e.memset(ZT[:, :], 0.0), writes=["ZT"])

    zf_state = {"n": 0}

    def zero_fill_hg(nblk=64):
        tot = NEXP * SEQ * 2 // 128
        for blk in range(zf_state["n"], min(tot, zf_state["n"] + nblk)):
            sp_dma(hg_d[blk * 128:(blk + 1) * 128, :], ZT[:, :], ["ZT"], [("hgz", blk)])
        zf_state["n"] = min(tot, zf_state["n"] + nblk)
    act(c1[:, :, :, :], lam[:, :, :, :], AF.Exp, ["lam"], ["c1"], scale=-1.0)
    act(c1[:, :, :, :], c1[:, :, :, :], AF.Ln, ["c1"], ["c1"], bias=1.0)
    ts(c2[:, :, :, :], c1[:, :, :, :], -16.0, None, ALU.mult, None, ["c1"], ["c2"])
    ts(c1[:, :, :, :], c1[:, :, :, :], -8.0, None, ALU.mult, None, ["c1", "c2"], ["c1"])
    act(scb[:, :, :], scT[:, :, :], AF.Silu, ["scT"], ["scb"])
    for l in range(2):
        for g in range(12):
            i = load_slab(("mod", l, g), big=True)
            for j in range(4):
                col = (g * 4 + j) * 4
                for k in range(8):
                    mm(ps[0][:, col:col + 4], wk(i, k, j * 128, 128), scb[:, k, :], k == 0, k == 7,
                       [("ws", i), "scb"], PSK(0))
        pv = ps[0][:, 0:192].rearrange("p (a b) -> p a b", b=4)
        for j in range(3):
            tt(mod[:, l, :, j], pv[:, :, j], bmodT[:, l, :], ALU.add, ["bmodT"], [PSK(0), "mod"])
    for l in range(2):
        for m in (1, 4):
            ts(mod[:, l, m * 8:(m + 1) * 8, :], mod[:, l, m * 8:(m + 1) * 8, :], 1.0, None, ALU.add, None, ["mod"], ["mod"])

    def modv(l, m, c, col):
        return mod[:, l, m * 8 + c, col:col + 1]

    stages = ["pro", "A0", "B0", "C0", "D0", "E0", "F0", "G0", "H0", "F1", "G1", "G1a", "H1", "seq1", "full"]
    si = stages.index(stage)

    state = {"x_in_res": False}

    def modulate(l, s, which, tiles, xsrc, X, moe=False, after_tile=None):
        m_sh, m_sc = (0, 1) if which == 0 else (3, 4)
        for ti in tiles:
            t0, w = TILES[ti]
            col = 2 if ti == 0 else s
            xap, xkeys = xsrc(ti)
            SQ, RS = X["SQ"], X["RS"]
            for c in range(8):
                act(SQ[:, c, 0:w], xap(c), AF.Square, xkeys, [("SQ", c)])
            for c in range(8):
                mm(ps[1][:, 0:w], ones_bf[:, :], SQ[:, c, 0:w], c == 0, c == 7, [("SQ", c), "ones_bf"], PSK(1))
            act(RS[:, 0:w], ps[1][:, 0:w], AF.Sqrt, [], [PSK(1), "RS"], bias=EPS, scale=1.0 / D)
            P.op("dve", lambda e, w=w: e.reciprocal(RS[:, 0:w], RS[:, 0:w]), reads=["RS"], writes=["RS"])
            for c in range(8):
                if moe:
                    tmp, tk = X["TMP8"][:, c, 0:w], ("TMP8", c)
                else:
                    tmp, tk = X["TMP"][c % 2][:, 0:w], ("TMP", c % 2)
                stt(tmp, xap(c), modv(l, m_sc, c, col), RS[:, 0:w], ALU.mult, ALU.mult, list(xkeys) + ["mod", "RS"], [tk])
                act(HT[:, c, t0:t0 + w], tmp, AF.Identity, [tk, "mod"], [("HT", ti)], bias=modv(l, m_sh, c, col))
            if after_tile is not None:
                after_tile(ti)

    def token_mixer(l, s):
        ctx_out = (l == 0)
        tiles = [0, 1, 2, 3, 4]
        otiles = tiles if ctx_out else [1, 2, 3, 4]
        off = XBASE
        YB, off = A("YB", [8, NT], BF16, off)
        TB = off
        pool_dma(rgw[:, :, :], rgw_d[l].rearrange("p (a b) -> p a b", b=128), [], ["rgw"])
        ts(qg[:, 0:1], gains[:, l, 0:1], 0.125, None, ALU.mult, None, ["gains"], ["qg"])

        off = TB
        XL = []
        for i in range(2):
            t_, off = A(f"XL{i}", [8, 512], F32, off)
            XL.append(t_)
        X = {}
        X["SQ"], off = A("SQ", [8, 512], BF16, off)
        X["RS"], off = A("RS", [512], F32, off)
        X["TMP"] = []
        for i in range(2):
            t_, off = A(f"TMP{i}", [512], F32, off)
            X["TMP"].append(t_)
        xd = xres_d if state["x_in_res"] else xT_d

        def xsrc(ti):
            t0, w = TILES[ti]
            b = ti % 2
            sp_dma(XL[b][:, :, 0:w], xd[s, :, :, t0:t0 + w], [("xd", ti)], [("XL", b)])
            return (lambda c: XL[b][:, c, 0:w]), [("XL", b)]

        modulate(l, s, 0, tiles, xsrc, X)
        P.barrier()
        if stage == "A0":
            return HT, [("HT", t) for t in range(5)]

        off = TB
        S0s = []
        for i_ in range(2):
            t_, off = A(f"S0{i_}", [2312], F32, off)
            S0s.append(t_)
        XC, off = A("XC", [NT], F32, off)
        S2, off = A("S2", [NT], F32, off)
        S3, off = A("S3", [NT], F32, off)
        HF, off = A("HF", [NT], F32, off)
        HR, off = A("HR", [NT], F32, off)
        XCb, off = A("XCb", [NT], BF16, off)
        GT, off = A("GT", [512], F32, off)

        def xrp_pos(t0):
            return 2 + t0 if t0 < CTX else 261 + (t0 - CTX)

        for c in range(8):
            S0 = S0s[c % 2]
            S0k = ("S0", c % 2)
            if c % 2 == 0:
                wsi = load_slab(("rnn", l, c // 2))
            cb = (c % 2) * 256
            for a, b in ((0, 2), (258, 261), (2309, 2312)):
                P.op("dve", lambda e, a=a, b=b, S0=S0: e.memset(S0[:, a:b], 0.0), writes=[S0k])
            for ti in tiles:
                t0, w = TILES[ti]
                pb = 2 + (ti % 2)
                for k in range(8):
                    mm(ps[pb][:, 0:w], wk(wsi, k, cb, 128), HT[:, k, t0:t0 + w], k == 0, k == 7,
                       [("ws", wsi), ("HT", ti)], PSK(pb))
                p0 = xrp_pos(t0)
                act(S0[:, p0:p0 + w], ps[pb][:, 0:w], AF.Identity, [], [PSK(pb), S0k])
            for (d0, n, base) in ((0, CTX, 2), (CTX, SEQ, 261)):
                ts(XC[:, d0:d0 + n], S0[:, base - 2:base - 2 + n], convw[:, l, c, 0:1], convb[:, l, c:c + 1],
                   ALU.mult, ALU.add, [S0k, "convw", "convb"], [("XC", d0)])
                for j in range(1, 4):
                    stt(XC[:, d0:d0 + n], S0[:, base - 2 + j:base - 2 + j + n], convw[:, l, c, j:j + 1], XC[:, d0:d0 + n],
                        ALU.mult, ALU.add, [S0k, "convw", ("XC", d0)], [("XC", d0)])
            act(XCb[:, :], XC[:, :], AF.Identity, [("XC", 0), ("XC", CTX)], ["XCb"])
            for dr in range(2):
                for ti in tiles:
                    t0, w = TILES[ti]
                    for gt in range(2):
                        pb = 4 + gt + 2 * (ti % 2)
                        mm(ps[pb][:, 0:w], rgw[:, (dr * 2 + gt) * 8 + c, :], XCb[:, t0:t0 + w], True, True,
                           ["rgw", "XCb"], PSK(pb))
                        dst = S2 if gt == 0 else S3
                        act(dst[:, t0:t0 + w], ps[pb][:, 0:w], AF.Sigmoid, ["rgb"], [PSK(pb), ("S2" if gt == 0 else "S3")],
                            bias=rgb[:, l, dr, gt, c:c + 1])
                act(S0[:, 0:NT], S2[:, :], AF.Exp, ["S2", "c1"], [S0k], scale=c1[:, l, dr, c:c + 1])
                act(S2[:, :], S2[:, :], AF.Exp, ["S2", "c2"], ["S2"], scale=c2[:, l, dr, c:c + 1])
                act(S2[:, :], S2[:, :], AF.Sqrt, ["S2"], ["S2"], scale=-1.0, bias=1.0)
                tt(S3[:, :], S3[:, :], XC[:, :], ALU.mult, ["S3", ("XC", 0), ("XC", CTX)], ["S3"])
                tt(S3[:, :], S3[:, :], S2[:, :], ALU.mult, ["S3", "S2"], ["S3"])
                if dr == 0:
                    P.op("dve", lambda e, S0=S0: e.tensor_tensor_scan(HF[:, :], S0[:, 0:NT], S3[:, :], 0.0, ALU.mult, ALU.add),
                         reads=[S0k, "S3"], writes=["HF"])
                else:
                    P.op("dve", lambda e, S0=S0: e.tensor_tensor_scan(HR[:, CTX - 1::-1], S0[:, CTX - 1::-1], S3[:, CTX - 1::-1], 0.0,
                                                               ALU.mult, ALU.add),
                         reads=[S0k, "S3"], writes=["HR"])
                    P.op("dve", lambda e, S0=S0: e.tensor_tensor_scan(HR[:, NT - 1:CTX - 1:-1], S0[:, NT - 1:CTX - 1:-1],
                                                               S3[:, NT - 1:CTX - 1:-1], HR[:, 0:1], ALU.mult, ALU.add),
                         reads=[S0k, "S3", "HR"], writes=["HR"])
            tt(HF[:, :], HF[:, :], HR[:, :], ALU.add, ["HF", "HR"], ["HF"])
            for ti in otiles:
                t0, w = TILES[ti]
                pb = 2 + (ti % 2)
                for k in range(8):
                    mm(ps[pb][:, 0:w], wk(wsi, k, cb + 128, 128), HT[:, k, t0:t0 + w], k == 0, k == 7,
                       [("ws", wsi), ("HT", ti)], PSK(pb))
                act(GT[:, 0:w], ps[pb][:, 0:w], AF.Gelu_apprx_tanh, [], [PSK(pb), "GT"])
                tt(YB[:, c, t0:t0 + w], HF[:, t0:t0 + w], GT[:, 0:w], ALU.mult, ["HF", "GT"], [("YB", ti)])
            if stage == "B0" and debug is not None and c == 0:
                pass
        P.barrier()
        if stage == "B0":
            return YB, [("YB", t) for t in range(5)]

        off = TB
        SG, MR = [], []
        for i in range(2):
            t_, off = A(f"SG{i}", [512], F32, off)
            SG.append(t_)
            t_, off = A(f"MR{i}", [512], F32, off)
            MR.append(t_)
        cnt = 0
        for g in range(2):
            wa = load_slab(("rnno", l, g))
            wb = load_slab(("gr", l, g))
            for ti in otiles:
                t0, w = TILES[ti]
                for j in range(4):
                    dc = g * 4 + j
                    b = cnt % 2
                    cnt += 1
                    p1, p2 = 2 + b, 4 + b
                    for k in range(8):
                        mm(ps[p1][:, 0:w], wk(wa, k, j * 128, 128), YB[:, k, t0:t0 + w], k == 0, k == 7,
                           [("ws", wa), ("YB", ti)], PSK(p1))
                    for k in range(8):
                        mm(ps[p2][:, 0:w], wk(wb, k, j * 128, 128), HT[:, k, t0:t0 + w], k == 0, k == 7,
                           [("ws", wb), ("HT", ti)], PSK(p2))
                    act(SG[b][:, 0:w], ps[p2][:, 0:w], AF.Sigmoid, [], [PSK(p2), ("SG", b)])
                    tt(MR[b][:, 0:w], ps[p1][:, 0:w], SG[b][:, 0:w], ALU.mult, [("SG", b)], [PSK(p1), ("MR", b)])
                    sp_dma(mrnn_d[:, dc, t0:t0 + w], MR[b][:, 0:w], [("MR", b)], [("mrnn", dc, ti)])
        P.barrier()

        off = TB
        KT, QT, Vz, Eb = [], [], [], []
        for i in range(2):
            t_, off = A(f"KT{i}", [NT], BF16, off)
            KT.append(t_)
            t_, off = A(f"QT{i}", [NT], BF16, off)
            QT.append(t_)
            t_, off = A(f"Vz{i}", [18, 192], BF16, off)
            Vz.append(t_)
            t_, off = A(f"Eb{i}", [2, NPAT, 128], BF16, off)
            Eb.append(t_)
        SQh, RSh, RD = [], [], []
        for i in range(2):
            t_, off = A(f"SQh{i}", [512], BF16, off)
            SQh.append(t_)
            t_, off = A(f"RSh{i}", [512], F32, off)
            RSh.append(t_)
            t_, off = A(f"RD{i}", [128], F32, off)
            RD.append(t_)
        PT = []
        for hh in range(2):
            row = []
            for i in range(2):
                t_, off = A(f"PT{hh}{i}", [7, 128], BF16, off)
                row.append(t_)
            PT.append(row)
        for i in range(2):
            P.op("dve", lambda e, i=i: e.memset(Vz[i][:, :, 64:128], 0.0), writes=[("Vz", i)])
        acnt = {"n": 0}

        def attend_S(c, qtok0, chunks, out_ti):
            cb_ = c % 2
            i = acnt["n"] % 2
            acnt["n"] += 1
            clist = [(0, None), (1, None)] + [(2 + m, pc) for (m, pc) in chunks]
            nchunk = len(clist)
            for hh in range(2):
                pbase = hh * 64
                bX, bY = 2 + 2 * hh, 3 + 2 * hh
                ptk = ("PT", hh, i)
                pt = PT[hh][i]
                for ci, (jt, pc) in enumerate(clist):
                    bank = bX if ci < 4 else bY
                    col = (ci % 4) * 128
                    mm(ps[bank][:, col:col + 128], KT[cb_][pbase:pbase + 64, jt * 128:(jt + 1) * 128],
                       QT[cb_][pbase:pbase + 64, qtok0:qtok0 + 128], True, True, [("KT", cb_), ("QT", cb_)], PSK(bank))
                n1 = min(4, nchunk)
                act(pt[:, 0:n1, :], ps[bX][:, 0:n1 * 128].rearrange("p (a b) -> p a b", b=128), AF.Exp, [], [PSK(bX), ptk])
                if nchunk > 4:
                    n2 = nchunk - 4
                    act(pt[:, 4:nchunk, :], ps[bY][:, 0:n2 * 128].rearrange("p (a b) -> p a b", b=128), AF.Exp, [],
                        [PSK(bY), ptk])
                if chunks:
                    pc0, n = chunks[0][1], len(chunks)
                    tt(pt[:, 2:2 + n, :], pt[:, 2:2 + n, :], Eb[cb_][:, hh, pc0:pc0 + n, :], ALU.mult, [ptk, ("Eb", cb_)], [ptk])
            return (c, qtok0, clist, out_ti, i)

        def attend_PV(stt_):
            c, qtok0, clist, out_ti, i = stt_
            cb_ = c % 2
            nchunk = len(clist)
            total = 2 * nchunk
            idx = 0
            for hh in range(2):
                ptk = ("PT", hh, i)
                for ci, (jt, pc) in enumerate(clist):
                    lv = Vz[cb_][:, jt, 0:128] if hh == 0 else Vz[cb_][:, jt, 64:192]
                    lo = onesV[:, 0:128] if hh == 0 else onesV[:, 64:192]
                    mm(ps[6][:, 0:128], lv, PT[hh][i][:, ci, :], idx == 0, idx == total - 1, [("Vz", cb_), ptk], PSK(6))
                    mm(ps[7][:, 0:128], lo, PT[hh][i][:, ci, :], idx == 0, idx == total - 1, ["onesV", ptk], PSK(7))
                    idx += 1
            P.op("dve", lambda e: e.reciprocal(RD[i][:, :], ps[7][:, 0:128]), reads=[], writes=[PSK(7), ("RD", i)])
            tt(YB[:, c, qtok0:qtok0 + 128], ps[6][:, 0:128], RD[i][:, :], ALU.mult, [("RD", i)], [PSK(6), ("YB", out_ti)])

        pcnt = {"n": 0}

        def inproj_items(c):
            cb_ = c % 2
            items = []
            st_ = {}

            def first():
                st_["ws"] = load_slab(("kvq", l, c))
                pool_dma(Eb[cb_][:, :, :, :], biasG_d[l, c].rearrange("p (h a b) -> p h a b", h=2, b=128), ["_bar"], [("Eb", cb_)])
                act(Eb[cb_][:, :, :, :], Eb[cb_][:, :, :, :], AF.Exp, [("Eb", cb_)], [("Eb", cb_)])
                for hh in range(2):
                    tt(Eb[cb_][:, hh, :, :], Eb[cb_][:, hh, :, :], maskG[:, :, :], ALU.mult, [("Eb", cb_), "maskG"], [("Eb", cb_)])
            items.append(first)
            for (colbase, dst, dkey, gain, gkey, tl) in ((0, KT[cb_], ("KT", cb_), gains[:, l, 1:2], "gains", tiles),
                                                       (256, QT[cb_], ("QT", cb_), qg[:, 0:1], "qg", otiles)):
                for ti in tl:
                    def proj(colbase=colbase, dst=dst, dkey=dkey, gain=gain, gkey=gkey, ti=ti):
                        wsi = st_["ws"]
                        t0, w = TILES[ti]
                        b = pcnt["n"] % 2
                        pcnt["n"] += 1
                        pP, pS = (0, 1) if b == 0 else (6, 7)
                        for k in range(8):
                            mm(ps[pP][:, 0:w], wk(wsi, k, colbase, 128), HT[:, k, t0:t0 + w], k == 0, k == 7,
                               [("ws", wsi), ("HT", ti)], PSK(pP))
                        act(SQh[b][:, 0:w], ps[pP][:, 0:w], AF.Square, [], [PSK(pP), ("SQh", b)])
                        mm(ps[pS][:, 0:w], blk_bf[:, :], SQh[b][:, 0:w], True, True, [("SQh", b), "blk_bf"], PSK(pS))
                        act(RSh[b][:, 0:w], ps[pS][:, 0:w], AF.Sqrt, [], [PSK(pS), ("RSh", b)], bias=EPS, scale=1.0 / 64)
                        P.op("dve", lambda e, b=b, w=w: e.reciprocal(RSh[b][:, 0:w], RSh[b][:, 0:w]), reads=[("RSh", b)],
                             writes=[("RSh", b)])
                        stt(dst[:, t0:t0 + w], ps[pP][:, 0:w], gain, RSh[b][:, 0:w], ALU.mult, ALU.mult, [("RSh", b), gkey],
                            [PSK(pP), dkey])
                    items.append(proj)
            for j0 in range(0, 18, 4):
                def vproj(j0=j0):
                    wsi = st_["ws"]
                    n = min(4, 18 - j0)
                    bank = 0 if (j0 // 4) % 2 == 0 else 1
                    for jj in range(n):
                        jt = j0 + jj
                        ti = 0 if jt < 2 else 1 + (jt - 2) // 4
                        for k in range(8):
                            mm(ps[bank][:, jj * 128:(jj + 1) * 128], HT[:, k, jt * 128:(jt + 1) * 128], wk(wsi, k, 128, 128),
                               k == 0, k == 7, [("ws", wsi), ("HT", ti)], PSK(bank))
                    pv3 = ps[bank][:, 0:n * 128].rearrange("p (a b) -> p a b", b=128)
                    act(Vz[cb_][:, j0:j0 + n, 0:64], pv3[:, :, 0:64], AF.Identity, [], [PSK(bank), ("Vz", cb_)])
                    P.op("dve", lambda e, j0=j0, n=n, pv3=pv3: e.tensor_copy(Vz[cb_][:, j0:j0 + n, 128:192], pv3[:, :, 64:128]),
                         reads=[], writes=[PSK(bank), ("Vz", cb_)])
                items.append(vproj)
            return items

        for c in range(8):
            for it in inproj_items(c):
                it()
            calls = []
            if ctx_out:
                for qt in range(2):
                    calls.append((qt * 128, [], 0))
            for qp in range(16):
                calls.append((CTX + qp * 128, NA_PLAN[qp], 1 + qp // 4))
            pend = None
            for (q0, ch, oti) in calls:
                cur = attend_S(c, q0, ch, oti)
                if pend is not None:
                    attend_PV(pend)
                pend = cur
            attend_PV(pend)
        P.barrier()
        if stage == "D0":
            return YB, [("YB", t) for t in range(5)]

        off = TB
        MTa, off = A("MTa", [8, NT], BF16, off)
        XL2, MRL, T1, SG2 = [], [], [], []
        for i in range(2):
            t_, off = A(f"XL2{i}", [4, 512], F32, off)
            XL2.append(t_)
        for i in range(2):
            t_, off = A(f"MRL{i}", [512], F32, off)
            MRL.append(t_)
            t_, off = A(f"T1{i}", [512], F32, off)
            T1.append(t_)
            t_, off = A(f"SG2{i}", [512], F32, off)
            SG2.append(t_)
        cnt = 0
        for g in range(2):
            wa = load_slab(("nao", l, g))
            wb = load_slab(("gn", l, g))
            for ti in otiles:
                t0, w = TILES[ti]
                for j in range(4):
                    dc = g * 4 + j
                    b = cnt % 2
                    cnt += 1
                    p1, p2 = 2 + b, 4 + b
                    for k in range(8):
                        mm(ps[p1][:, 0:w], wk(wa, k, j * 128, 128), YB[:, k, t0:t0 + w], k == 0, k == 7,
                           [("ws", wa), ("YB", ti)], PSK(p1))
                    for k in range(8):
                        mm(ps[p2][:, 0:w], wk(wb, k, j * 128, 128), HT[:, k, t0:t0 + w], k == 0, k == 7,
                           [("ws", wb), ("HT", ti)], PSK(p2))
                    sp_dma(MRL[b][:, 0:w], mrnn_d[:, dc, t0:t0 + w], [("mrnn", dc, ti)], [("MRL", b)])
                    act(SG2[b][:, 0:w], ps[p2][:, 0:w], AF.Sigmoid, [], [PSK(p2), ("SG2", b)])
                    tt(T1[b][:, 0:w], ps[p1][:, 0:w], SG2[b][:, 0:w], ALU.mult, [("SG2", b)], [PSK(p1), ("T1", b)])
                    tt(MTa[:, dc, t0:t0 + w], T1[b][:, 0:w], MRL[b][:, 0:w], ALU.add, [("T1", b), ("MRL", b)], [("MTa", ti)])
        xn = 0
        for g in range(2):
            wo = load_slab(("out", l, g))
            for ti in otiles:
                t0, w = TILES[ti]
                col = 2 if ti == 0 else s
                xb = xn % 2
                xn += 1
                sp_dma(XL2[xb][:, :, 0:w], xd[s, :, g * 4:(g + 1) * 4, t0:t0 + w], [("xd", ti)], [("XL2", xb)])
                for j in range(4):
                    dc = g * 4 + j
                    b = cnt % 2
                    cnt += 1
                    p1 = 6 + b
                    for k in range(8):
                        mm(ps[p1][:, 0:w], wk(wo, k, j * 128, 128), MTa[:, k, t0:t0 + w], k == 0, k == 7,
                           [("ws", wo), ("MTa", ti)], PSK(p1))
                    stt(XL2[xb][:, j, 0:w], ps[p1][:, 0:w], modv(l, 2, dc, col), XL2[xb][:, j, 0:w], ALU.mult, ALU.add,
                        ["mod", ("XL2", xb)], [PSK(p1), ("XL2", xb)])
                sp_dma(xres_d[s, :, g * 4:(g + 1) * 4, t0:t0 + w], XL2[xb][:, :, 0:w], [("XL2", xb)], [("xdw", ti, g)])
        state["x_in_res"] = True
        P.barrier()
        return None

    def ffn(l, s):
        ctx_out = (l == 0)
        moe = (l == 1)
        otiles = [0, 1, 2, 3, 4] if ctx_out else [1, 2, 3, 4]
        off = XBASE
        XTs, off = A("XTs", [8, NT], F32, off)
        Gt, off = A("Gt", [16, 8], F32, off)
        OV = off
        X = {}
        X["SQ"], off = A("SQ2", [8, 512], BF16, off)
        X["RS"], off = A("RS2", [512], F32, off)
        if moe:
            X["TMP8"], off = A("TMP8", [8, 512], F32, off)
            LG, off = A("LG", [512], F32, off)
            LT, off = A("LT", [16, 8], F32, off)
            EQ1, off = A("EQ1", [16, 8], F32, off)
            L2, off = A("L2", [16, 8], F32, off)
            EQ2, off = A("EQ2", [16, 8], F32, off)
            TG, off = A("TG", [16, 8], F32, off)
            M1, off = A("M1", [16, 1], F32, off)
            M2, off = A("M2", [16, 1], F32, off)
            W1, off = A("W1", [16, 1], F32, off)
            W2, off = A("W2", [16, 1], F32, off)
        else:
            X["TMP"] = []
            for i in range(2):
                t_, off = A(f"TMPf{i}", [512], F32, off)
                X["TMP"].append(t_)
        for ti in otiles:
            t0, w = TILES[ti]
            sp_dma(XTs[:, :, t0:t0 + w], xres_d[s, :, :, t0:t0 + w], [("xd", ti)], [("XTs", ti)])

        def xsrc(ti):
            t0, w = TILES[ti]
            return (lambda c: XTs[:, c, t0:t0 + w]), [("XTs", ti)]

        if SPARSE_MOE and MERGED_MOE and stage in ("full", "seq1"):
            zero_fill_hg(128 if stage == "full" else 256)
        hook = None
        if moe:
            for c in range(8):
                mm(ps[2][0:8, 0:2], router[:, c, :], mod[:, l, 24 + c, s:s + 2], c == 0, c == 7, ["router", "mod"], PSK(2))
            act(routb[0:8, 0:2], ps[2][0:8, 0:2], AF.Identity, [], [PSK(2), "routb"])

            def hook(ti):
                t0, w = TILES[ti]
                for c in range(8):
                    mm(ps[2][0:8, 0:w], router[:, c, :], X["TMP8"][:, c, 0:w], c == 0, c == 7, ["router", ("TMP8", c)], PSK(2))
                act(LG[0:8, 0:w], ps[2][0:8, 0:w], AF.Identity, ["routb"], [PSK(2), "LG"], bias=routb[0:8, 0:1])
                for j in range(w // 128):
                    jt = (t0 - CTX) // 128 + j
                    P.op("pe", lambda e, jt=jt, j=j: e.transpose(ps[3][:, jt * 8:(jt + 1) * 8], LG[0:8, j * 128:(j + 1) * 128],
                                                                 ident_f[0:8, 0:8]),
                         reads=["LG", "ident_f"], writes=[PSK(3)])

        modulate(l, s, 1, otiles, xsrc, X, moe=moe, after_tile=hook)
        if moe:
            P.op("dve", lambda e: e.tensor_copy(LT[:, :, :], ps[3][:, 0:128].rearrange("p (a b) -> p a b", b=8)),
                 reads=[], writes=[PSK(3), "LT"])
            P.op("dve", lambda e: e.tensor_reduce(M1[:, :, 0], LT[:, :, :], AX.X, ALU.max), reads=["LT"], writes=["M1"])
            tt(EQ1[:, :, :], LT[:, :, :], M1[:, :, 0:1].to_broadcast([128, 16, 8]), ALU.is_equal, ["LT", "M1"], ["EQ1"])
            stt(L2[:, :, :], EQ1[:, :, :], -1.0e30, LT[:, :, :], ALU.mult, ALU.add, ["EQ1", "LT"], ["L2"])
            P.op("dve", lambda e: e.tensor_reduce(M2[:, :, 0], L2[:, :, :], AX.X, ALU.max), reads=["L2"], writes=["M2"])
            tt(EQ2[:, :, :], L2[:, :, :], M2[:, :, 0:1].to_broadcast([128, 16, 8]), ALU.is_equal, ["L2", "M2"], ["EQ2"])
            tt(W2[:, :, :], M2[:, :, :], M1[:, :, :], ALU.subtract, ["M1", "M2"], ["W2"])
            act(W2[:, :, :], W2[:, :, :], AF.Exp, ["W2"], ["W2"])
            ts(W1[:, :, :], W2[:, :, :], 1.0, None, ALU.add, None, ["W2"], ["W1"])
            P.op("dve", lambda e: e.reciprocal(W1[:, :, :], W1[:, :, :]), reads=["W1"], writes=["W1"])
            tt(W2[:, :, :], W2[:, :, :], W1[:, :, :], ALU.mult, ["W1", "W2"], ["W2"])
            tt(Gt[:, :, :], EQ1[:, :, :], W1[:, :, 0:1].to_broadcast([128, 16, 8]), ALU.mult, ["EQ1", "W1"], ["Gt"])
            tt(TG[:, :, :], EQ2[:, :, :], W2[:, :, 0:1].to_broadcast([128, 16, 8]), ALU.mult, ["EQ2", "W2"], ["TG"])
            tt(Gt[:, :, :], Gt[:, :, :], TG[:, :, :], ALU.add, ["Gt", "TG"], ["Gt"])
        P.barrier()
        if stage == "G0":
            return HT, [("HT", t) for t in range(5)]

        off = OV
        SIL, TT, ACTT, GE, DG = [], [], [], [], []
        for i in range(2):
            t_, off = A(f"SIL{i}", [512], BF16, off)
            SIL.append(t_)
            t_, off = A(f"TT{i}", [512], F32, off)
            TT.append(t_)
            t_, off = A(f"ACTT{i}", [4, 512], BF16, off)
            ACTT.append(t_)
            if moe:
                t_, off = A(f"GE{i}", [SEQ], F32, off)
                GE.append(t_)
                t_, off = A(f"DG{i}", [128], F32, off)
                DG.append(t_)
        cnt = {"h": 0, "a": 0, "o": 0, "d": 0}

        def swiglu_group(names, nj, tl, ge):
            w1 = load_slab(names[0])
            w3 = load_slab(names[1])
            w2 = load_slab(names[2])
            for ti in tl:
                t0, w = TILES[ti]
                col = 2 if ti == 0 else s
                ab = cnt["a"] % 2
                cnt["a"] += 1
                for j in range(nj):
                    b = cnt["h"] % 2
                    cnt["h"] += 1
                    b1, b3 = 2 + b, 4 + b
                    for k in range(8):
                        mm(ps[b1][:, 0:w], wk(w1, k, j * 128, 128), HT[:, k, t0:t0 + w], k == 0, k == 7,
                           [("ws", w1), ("HT", ti)], PSK(b1))
                    for k in range(8):
                        mm(ps[b3][:, 0:w], wk(w3, k, j * 128, 128), HT[:, k, t0:t0 + w], k == 0, k == 7,
                           [("ws", w3), ("HT", ti)], PSK(b3))
                    act(SIL[b][:, 0:w], ps[b1][:, 0:w], AF.Silu, [], [PSK(b1), ("SIL", b)])
                    if ge is None:
                        tt(ACTT[ab][:, j, 0:w], ps[b3][:, 0:w], SIL[b][:, 0:w], ALU.mult, [("SIL", b)], [PSK(b3), ("ACTT", ab)])
                    else:
                        tt(TT[b][:, 0:w], ps[b3][:, 0:w], SIL[b][:, 0:w], ALU.mult, [("SIL", b)], [PSK(b3), ("TT", b)])
                        tt(ACTT[ab][:, j, 0:w], TT[b][:, 0:w], GE[ge][:, t0 - CTX:t0 - CTX + w], ALU.mult,
                           [("TT", b), ("GE", ge)], [("ACTT", ab)])
                for dc in range(8):
                    ob = 6 + cnt["o"] % 2
                    cnt["o"] += 1
                    for j in range(nj):
                        mm(ps[ob][:, 0:w], wk2(w2, j, dc * 128, 128), ACTT[ab][:, j, 0:w], j == 0, j == nj - 1,
                           [("ws", w2), ("ACTT", ab)], PSK(ob))
                    stt(XTs[:, dc, t0:t0 + w], ps[ob][:, 0:w], modv(l, 5, dc, col), XTs[:, dc, t0:t0 + w], ALU.mult, ALU.add,
                        ["mod", ("XTs", ti)], [PSK(ob), ("XTs", ti)])

        if not moe:
            for g in range(6):
                swiglu_group([("f1", g), ("f3", g), ("f2", g)], 4 if g < 5 else 2, otiles, None)
        else:
            for ex in range(NEXP):
                ge = ex % 2
                for j0 in range(0, 16, 4):
                    bank = 0 if (j0 // 4) % 2 == 0 else 1
                    for jj in range(4):
                        jt = j0 + jj
                        d = cnt["d"] % 2
                        cnt["d"] += 1
                        ts(DG[d][:, :], ident_f[:, :], Gt[:, jt, ex:ex + 1], None, ALU.mult, None, ["ident_f", "Gt"], [("DG", d)])
                        mm(ps[bank][:, jj * 128:(jj + 1) * 128], ones_f[:, :], DG[d][:, :], True, True, ["ones_f", ("DG", d)],
                           PSK(bank))
                    act(GE[ge][:, j0 * 128:(j0 + 4) * 128], ps[bank][:, 0:512], AF.Identity, [], [PSK(bank), ("GE", ge)])
                for g in range(7):
                    swiglu_group([("m1", ex, g), ("m3", ex, g), ("m2", ex, g)], 4, [1, 2, 3, 4], ge)
        for ti in otiles:
            t0, w = TILES[ti]
            if l == 0:
                sp_dma(xres_d[s, :, :, t0:t0 + w], XTs[:, :, t0:t0 + w], [("XTs", ti)], [("xd", ti)])
            else:
                sp_dma(outT_d[s, :, :, t0 - CTX:t0 - CTX + w], XTs[:, :, t0:t0 + w], [("XTs", ti)], [("out", s, ti)])
        P.barrier()
        return None

    def ffn_moe_sparse(l, s):
        I32 = mybir.dt.int32
        TSZ = MOE_TSZ
        NQ = TSZ // 128
        NBLK = SEQ // TSZ
        lat = [1, 2, 3, 4]
        off = XBASE
        Gsm = {}
        for nm, shp, dt in (("W1", [16, 1], F32), ("W2", [16, 1], F32), ("SI", [16, 2], I32), ("JI", [8], I32)):
            Gsm[nm], off = A("m_" + nm, shp, dt, off)
        OV = off
        for nm, shp, dt in (("LT", [16, 8], F32), ("EQ1", [16, 8], F32), ("L2", [16, 8], F32), ("EQ2", [16, 8], F32),
                            ("M1", [16, 1], F32), ("M2", [16, 1], F32),
                            ("AB", [16, 8], BF16), ("PW", [16, 8], F32), ("TOT", [16, 8], F32), ("CS", [16, 8], F32),
                            ("EOFF", [16, 8], F32), ("TQ", [16, 8], F32), ("S12", [16, 2], F32),
                            ("NE", [8, 1], F32), ("THR", [8, 8], F32), ("CMP", [8, 8], F32), ("JF", [8], F32)):
            Gsm[nm], off = A("m_" + nm, shp, dt, off)
        LT, EQ1, L2, EQ2, M1, M2, W1, W2 = (Gsm[k] for k in ("LT", "EQ1", "L2", "EQ2", "M1", "M2", "W1", "W2"))
        AB, PW, TOT, CS, EOFF, TQ, S12, SI = (Gsm[k] for k in ("AB", "PW", "TOT", "CS", "EOFF", "TQ", "S12", "SI"))
        NE, THR, CMP, JF, JI = (Gsm[k] for k in ("NE", "THR", "CMP", "JF", "JI"))
        LG, off = A("mLG", [512], F32, off)
        XL = []
        for i in range(2):
            t_, off = A(f"mXL{i}", [8, 512], F32, off)
            XL.append(t_)
        X = {}
        X["SQ"], off = A("mSQ", [8, 512], BF16, off)
        X["RS"], off = A("mRS", [512], F32, off)
        X["TMP8"], off = A("mTMP8", [8, 512], F32, off)
        HTok = []
        for i in range(2):
            t_, off = A(f"HTok{i}", [D], BF16, off)
            HTok.append(t_)

        def xsrc(ti):
            t0, w = TILES[ti]
            b = ti % 2
            sp_dma(XL[b][:, :, 0:w], xres_d[s, :, :, t0:t0 + w], [("xd", ti)], [("XL", b)])
            return (lambda c: XL[b][:, c, 0:w]), [("XL", b)]

        for c in range(8):
            mm(ps[2][0:8, 0:2], router[:, c, :], mod[:, l, 24 + c, s:s + 2], c == 0, c == 7, ["router", "mod"], PSK(2))
        act(routb[0:8, 0:2], ps[2][0:8, 0:2], AF.Identity, [], [PSK(2), "routb"])

        def hook(ti):
            t0, w = TILES[ti]
            for c in range(8):
                mm(ps[2][0:8, 0:w], router[:, c, :], X["TMP8"][:, c, 0:w], c == 0, c == 7, ["router", ("TMP8", c)], PSK(2))
            act(LG[0:8, 0:w], ps[2][0:8, 0:w], AF.Identity, ["routb"], [PSK(2), "LG"], bias=routb[0:8, 0:1])
            for j in range(w // 128):
                jt = (t0 - CTX) // 128 + j
                P.op("pe", lambda e, jt=jt, j=j: e.transpose(ps[3][:, jt * 8:(jt + 1) * 8], LG[0:8, j * 128:(j + 1) * 128],
                                                             ident_f[0:8, 0:8]),
                     reads=["LG", "ident_f"], writes=[PSK(3)])

        modulate(l, s, 1, lat, xsrc, X, moe=True, after_tile=hook)
        P.op("dve", lambda e: e.tensor_copy(LT[:, :, :], ps[3][:, 0:128].rearrange("p (a b) -> p a b", b=8)),
             reads=[], writes=[PSK(3), "LT"])
        P.op("dve", lambda e: e.tensor_reduce(M1[:, :, 0], LT[:, :, :], AX.X, ALU.max), reads=["LT"], writes=["M1"])
        tt(EQ1[:, :, :], LT[:, :, :], M1[:, :, 0:1].to_broadcast([128, 16, 8]), ALU.is_equal, ["LT", "M1"], ["EQ1"])
        stt(L2[:, :, :], EQ1[:, :, :], -1.0e30, LT[:, :, :], ALU.mult, ALU.add, ["EQ1", "LT"], ["L2"])
        P.op("dve", lambda e: e.tensor_reduce(M2[:, :, 0], L2[:, :, :], AX.X, ALU.max), reads=["L2"], writes=["M2"])
        tt(EQ2[:, :, :], L2[:, :, :], M2[:, :, 0:1].to_broadcast([128, 16, 8]), ALU.is_equal, ["L2", "M2"], ["EQ2"])
        tt(W2[:, :, :], M2[:, :, :], M1[:, :, :], ALU.subtract, ["M1", "M2"], ["W2"])
        act(W2[:, :, :], W2[:, :, :], AF.Exp, ["W2"], ["W2"])
        ts(W1[:, :, :], W2[:, :, :], 1.0, None, ALU.add, None, ["W2"], ["W1"])
        P.op("dve", lambda e: e.reciprocal(W1[:, :, :], W1[:, :, :]), reads=["W1"], writes=["W1"])
        tt(W2[:, :, :], W2[:, :, :], W1[:, :, :], ALU.mult, ["W1", "W2"], ["W2"])
        tt(TQ[:, :, :], EQ1[:, :, :], EQ2[:, :, :], ALU.add, ["EQ1", "EQ2"], ["TQ"])
        P.op("dve", lambda e: e.tensor_copy(AB[:, :, :], TQ[:, :, :]), reads=["TQ"], writes=["AB"])
        ABf = AB[:, :, :].rearrange("p a b -> p (a b)")
        mm(ps[0][:, 0:128], ltri_bf[:, :], ABf, True, True, ["AB", "ltri_bf"], PSK(0))
        mm(ps[0][:, 128:256], ones_bf[:, :], ABf, True, True, ["AB", "ones_bf"], PSK(0))
        P.op("dve", lambda e: e.tensor_copy(PW[:, :, :], ps[0][:, 0:128].rearrange("p (a b) -> p a b", b=8)),
             reads=[], writes=[PSK(0), "PW"])
        P.op("dve", lambda e: e.tensor_copy(TOT[:, :, :], ps[0][:, 128:256].rearrange("p (a b) -> p a b", b=8)),
             reads=[], writes=[PSK(0), "TOT"])
        P.op("dve", lambda e: e.memset(CS[:, 0, :], 0.0), writes=["CS"])
        for j in range(1, 16):
            tt(CS[:, j, :], CS[:, j - 1, :], TOT[:, j - 1, :], ALU.add, ["CS", "TOT"], ["CS"])
        tt(NE[:, :, 0], CS[:, 15, :], TOT[:, 15, :], ALU.add, ["CS", "TOT"], ["NE"])
        for ex in range(NEXP):
            P.op("dve", lambda e, ex=ex: e.memset(EOFF[:, :, ex], float(ex * SEQ)), writes=["EOFF"])
            P.op("dve", lambda e, ex=ex: e.memset(THR[:, :, ex], float(ex * TSZ)), writes=["THR"])
        tt(PW[:, :, :], PW[:, :, :], CS[:, :, :], ALU.add, ["PW", "CS"], ["PW"])
        tt(PW[:, :, :], PW[:, :, :], EOFF[:, :, :], ALU.add, ["PW", "EOFF"], ["PW"])
        tt(TQ[:, :, :], EQ1[:, :, :], PW[:, :, :], ALU.mult, ["EQ1", "PW"], ["TQ"])
        P.op("dve", lambda e: e.tensor_reduce(S12[:, :, 0], TQ[:, :, :], AX.X, ALU.add), reads=["TQ"], writes=["S12"])
        tt(TQ[:, :, :], EQ2[:, :, :], PW[:, :, :], ALU.mult, ["EQ2", "PW", "S12"], ["TQ"])
        P.op("dve", lambda e: e.tensor_reduce(S12[:, :, 1], TQ[:, :, :], AX.X, ALU.add), reads=["TQ"], writes=["S12"])
        P.op("dve", lambda e: e.tensor_copy(SI[:, :, :], S12[:, :, :]), reads=["S12"], writes=["SI"])
        tt(CMP[:, :, :], NE[:, :, 0:1].to_broadcast([128, 8, 8]), THR[:, :, :], ALU.is_gt, ["NE", "THR"], ["CMP"])
        P.op("dve", lambda e: e.tensor_reduce(JF[:, :], CMP[:, :, :], AX.X, ALU.add), reads=["CMP"], writes=["JF"])
        if JCLAMP is not None:
            ts(JF[:, :], JF[:, :], float(JCLAMP), None, ALU.min, None, ["JF"], ["JF"])
        P.op("dve", lambda e: e.tensor_copy(JI[:, :], JF[:, :]), reads=["JF"], writes=["JI"])
        if stage == "G1a":
            DB, _ = A("DBG1", [64], F32, off)
            P.op("dve", lambda e: e.memset(DB[:, :], 0.0), writes=["DB"])
            P.op("dve", lambda e: e.tensor_copy(DB[:, 0:8], NE[:, :, 0]), reads=["NE"], writes=["DB"])
            P.op("dve", lambda e: e.tensor_copy(DB[:, 8:16], JF[:, :]), reads=["JF"], writes=["DB"])
            P.op("dve", lambda e: e.tensor_copy(DB[:, 16:48], S12[:, :, :].rearrange("p a b -> p (a b)")), reads=["S12"], writes=["DB"])
            P.op("dve", lambda e: e.tensor_copy(DB[:, 48:64], M1[:, :, 0]), reads=["M1"], writes=["DB"])
            sp_dma(dbg_d, DB[:, :], ["DB"], ["dbg"])
            return "done"
        psb = [ps[i][:, :].bitcast(BF16) for i in range(8)]
        if ZERO_HG:
            P.op("dve", lambda e: e.memset(HTok[0][:, :], 0.0), writes=[("HTok", 0)])
            for blk in range(NEXP * SEQ // 128):
                sp_dma(hg_d[blk * 128:(blk + 1) * 128, :], HTok[0][:, :], [("HTok", 0)], [("hgz", blk)])
        for jt in range(16):
            b = jt % 2
            ti = 1 + jt // 4
            for c in range(8):
                P.op("pe", lambda e, b=b, c=c, jt=jt: e.transpose(psb[b][:, c * 128:(c + 1) * 128],
                                                                  HT[:, c, CTX + jt * 128:CTX + (jt + 1) * 128], ident_bf[:, :]),
                     reads=[("HT", ti), "ident_bf"], writes=[PSK(b)])
            act(HTok[b][:, :], psb[b][:, :], AF.Identity, [], [PSK(b), ("HTok", b)])
            for k in range(2):
                P.dma("pool", lambda e, b=b, jt=jt, k=k: e.indirect_dma_start(
                    out=hg_d, out_offset=bass.IndirectOffsetOnAxis(ap=SI[:, jt, k:k + 1], axis=0), in_=HTok[b][:, :],
                    in_offset=None), reads=[("HTok", b), "SI"] + ([("hgz", q_) for q_ in range(128)] if ZERO_HG else []), writes=[("hg", jt, k)])
        P.barrier()
        if stage == "G1":
            return None

        off = OV
        Yacc, off = A("Yacc", [16, D], F32, off)
        HTg, off = A("HTg", [8, SEQ], BF16, off)
        HGs, SIL, ACTT = [], [], []
        t_, off = A("HGs0", [D], BF16, off)
        HGs = [t_, t_]
        for i in range(2):
            t_, off = A(f"mSIL{i}", [TSZ], BF16, off)
            SIL.append(t_)
            t_, off = A(f"mACTT{i}", [4, TSZ], BF16, off)
            ACTT.append(t_)
        cnt = {"h": 0, "a": 0, "o": 0, "g": 0}
        for ex in range(NEXP):
            P.load_reg(JI[0:1, ex:ex + 1], "JI")
            for k in range(16):
                b = cnt["g"] % 2
                cnt["g"] += 1
                r0 = ex * SEQ + k * 128
                sp_dma(HGs[b][:, :], hg_d[r0:r0 + 128, :], [("hg", a_, b_) for a_ in range(16) for b_ in range(2)], [("HGs", 0)])
                P.cond_begin(k // NQ + 1)
                for c in range(8):
                    P.op("pe", lambda e, b=b, c=c: e.transpose(psb[b][:, c * 128:(c + 1) * 128], HGs[b][:, c * 128:(c + 1) * 128],
                                                               ident_bf[:, :]),
                         reads=[("HGs", 0), "ident_bf"], writes=[PSK(b)])
                act(HTg[:, :, k * 128:(k + 1) * 128], psb[b][:, :].rearrange("p (a b) -> p a b", b=128), AF.Identity, [],
                    [PSK(b), ("HTg", k // NQ)])
                P.cond_end()
            for g in range(7):
                w1 = load_slab(("m1", ex, g), big=True)
                w3 = load_slab(("m3", ex, g), big=True)
                w2 = load_slab(("m2", ex, g), big=True)
                for j in range(NBLK):
                    P.cond_begin(j + 1)
                    ab = cnt["a"] % 2
                    cnt["a"] += 1
                    s0 = j * TSZ
                    for jj in range(4):
                        b = cnt["h"] % 2
                        cnt["h"] += 1
                        b1, b3 = 2 + b, 4 + b
                        for k in range(8):
                            mm(ps[b1][:, 0:TSZ], wk(w1, k, jj * 128, 128), HTg[:, k, s0:s0 + TSZ], k == 0, k == 7,
                               [("ws", w1), ("HTg", j)], PSK(b1))
                        for k in range(8):
                            mm(ps[b3][:, 0:TSZ], wk(w3, k, jj * 128, 128), HTg[:, k, s0:s0 + TSZ], k == 0, k == 7,
                               [("ws", w3), ("HTg", j)], PSK(b3))
                        act(SIL[b][:, :], ps[b1][:, 0:TSZ], AF.Silu, [], [PSK(b1), ("SIL", b)])
                        tt(ACTT[ab][:, jj, :], ps[b3][:, 0:TSZ], SIL[b][:, :], ALU.mult, [("SIL", b)], [PSK(b3), ("ACTT", ab)])
                    for h2 in range(NQ):
                        kt = NQ * j + h2
                        for dh in range(2):
                            ob = 6 + cnt["o"] % 2
                            cnt["o"] += 1
                            for jj in range(4):
                                mm(ps[ob][:, 0:512], ACTT[ab][:, jj, h2 * 128:(h2 + 1) * 128], wk2(w2, jj, dh * 512, 512),
                                   jj == 0, jj == 3, [("ws", w2), ("ACTT", ab)], PSK(ob))
                            ya = Yacc[:, kt, dh * 512:(dh + 1) * 512]
                            if g == 0:
                                P.op("dve", lambda e, ya=ya, ob=ob: e.tensor_copy(ya, ps[ob][:, 0:512]), reads=[],
                                     writes=[PSK(ob), ("Yacc", kt)])
                            else:
                                tt(ya, ps[ob][:, 0:512], ya, ALU.add, [("Yacc", kt)], [PSK(ob), ("Yacc", kt)])
                    P.cond_end()
            for k in range(16):
                r0 = ex * SEQ + k * 128
                sp_dma(yb_d[r0:r0 + 128, :], Yacc[:, k, :], [("Yacc", k)], [("yb", ex, k)])
        P.barrier()

        off = OV
        YA, YB2, OO, XC8 = [], [], [], []
        for i in range(2):
            t_, off = A(f"YA{i}", [D], F32, off)
            YA.append(t_)
            t_, off = A(f"YB2{i}", [D], F32, off)
            YB2.append(t_)
            t_, off = A(f"OO{i}", [D], F32, off)
            OO.append(t_)
            t_, off = A(f"XC8{i}", [8, 128], F32, off)
            XC8.append(t_)
        for jt in range(16):
            b = jt % 2
            ti = 1 + jt // 4
            c0 = CTX + jt * 128
            for k, dst, dk in ((0, YA, "YA"), (1, YB2, "YB2")):
                P.dma("pool", lambda e, b=b, jt=jt, k=k, dst=dst: e.indirect_dma_start(
                    out=dst[b][:, :], out_offset=None, in_=yb_d,
                    in_offset=bass.IndirectOffsetOnAxis(ap=SI[:, jt, k:k + 1], axis=0)),
                    reads=[("yb", a_, b_) for a_ in range(NEXP) for b_ in range(16)] + ["SI"], writes=[(dk, b)])
            sp_dma(XC8[b][:, :, :], xres_d[s, :, :, c0:c0 + 128], [("xd", ti)], [("XC8", b)])
            ts(OO[b][:, :], YA[b][:, :], W1[:, jt, 0:1], None, ALU.mult, None, [("YA", b), "W1"], [("OO", b)])
            stt(OO[b][:, :], YB2[b][:, :], W2[:, jt, 0:1], OO[b][:, :], ALU.mult, ALU.add, [("YB2", b), "W2", ("OO", b)], [("OO", b)])
            for c in range(8):
                pbk = 2 + 2 * b + c // 4
                P.op("pe", lambda e, b=b, c=c, pbk=pbk: e.transpose(ps[pbk][:, (c % 4) * 128:(c % 4 + 1) * 128],
                                                                    OO[b][:, c * 128:(c + 1) * 128], ident_f[:, :]),
                     reads=[("OO", b), "ident_f"], writes=[PSK(pbk)])
            for c in range(8):
                pbk = 2 + 2 * b + c // 4
                stt(XC8[b][:, c, :], ps[pbk][:, (c % 4) * 128:(c % 4 + 1) * 128], modv(l, 5, c, s), XC8[b][:, c, :],
                    ALU.mult, ALU.add, ["mod", ("XC8", b)], [PSK(pbk), ("XC8", b)])
            sp_dma(outT_d[s, :, :, jt * 128:(jt + 1) * 128], XC8[b][:, :, :], [("XC8", b)], [("out", s, jt)])
        P.barrier()
        return None

    def moe_merged(l, seqs):
        I32 = mybir.dt.int32
        TSZ = 512
        NQ = TSZ // 128
        CAP = SEQ * len(seqs)
        NPASS = len(seqs)
        off = XBASE
        PS_ = {}
        for s in seqs:
            for nm, shp, dt in (("W1", [16, 1], F32), ("W2", [16, 1], F32), ("SI", [16, 2], I32)):
                PS_[(nm, s)], off = A(f"mm_{nm}{s}", shp, dt, off)
        NEacc, off = A("mm_NEacc", [8, 1], F32, off)
        JI, off = A("mm_JI", [8], I32, off)
        OV = off
        G = {}
        for nm, shp, dt in (("LT", [16, 8], F32), ("EQ1", [16, 8], F32), ("L2", [16, 8], F32), ("EQ2", [16, 8], F32),
                            ("M1", [16, 1], F32), ("M2", [16, 1], F32),
                            ("AB", [16, 8], BF16), ("PW", [16, 8], F32), ("TOT", [16, 8], F32), ("CS", [16, 8], F32),
                            ("EOFF", [16, 8], F32), ("TQ", [16, 8], F32), ("S12", [16, 2], F32),
                            ("THR", [8, 8], F32), ("CMP", [8, 8], F32), ("JF", [8], F32)):
            G[nm], off = A("mm_" + nm, shp, dt, off)
        LT, EQ1, L2, EQ2, M1, M2 = (G[k] for k in ("LT", "EQ1", "L2", "EQ2", "M1", "M2"))
        AB, PW, TOT, CS, EOFF, TQ, S12 = (G[k] for k in ("AB", "PW", "TOT", "CS", "EOFF", "TQ", "S12"))
        THR, CMP, JF = (G[k] for k in ("THR", "CMP", "JF"))
        LG, off = A("mm_LG", [512], F32, off)
        XL = []
        for i in range(2):
            t_, off = A(f"mm_XL{i}", [8, 512], F32, off)
            XL.append(t_)
        X = {}
        X["SQ"], off = A("mm_SQ", [8, 512], BF16, off)
        X["RS"], off = A("mm_RS", [512], F32, off)
        X["TMP8"], off = A("mm_TMP8", [8, 512], F32, off)
        NHB = 4
        HTok = []
        for i in range(NHB):
            t_, off = A(f"mm_HTok{i}", [D], BF16, off)
            HTok.append(t_)
        psb = [ps[i][:, :].bitcast(BF16) for i in range(8)]
        lat = [1, 2, 3, 4]
        P.op("dve", lambda e: e.memset(NEacc[:, :, :], 0.0), writes=["NEacc"])
        for ex in range(NEXP):
            P.op("dve", lambda e, ex=ex: e.memset(EOFF[:, :, ex], float(ex * CAP)), writes=["EOFF"])
            P.op("dve", lambda e, ex=ex: e.memset(THR[:, :, ex], float(ex * TSZ)), writes=["THR"])

        for s in seqs:
            W1, W2, SI = PS_[("W1", s)], PS_[("W2", s)], PS_[("SI", s)]

            def xsrc(ti, s=s):
                t0, w = TILES[ti]
                b = ti % 2
                sp_dma(XL[b][:, :, 0:w], xres_d[s, :, :, t0:t0 + w], [("xd", ti)], [("XL", b)])
                return (lambda c: XL[b][:, c, 0:w]), [("XL", b)]

            for c in range(8):
                mm(ps[2][0:8, 0:2], router[:, c, :], mod[:, l, 24 + c, s:s + 2], c == 0, c == 7, ["router", "mod"], PSK(2))
            act(routb[0:8, 0:2], ps[2][0:8, 0:2], AF.Identity, [], [PSK(2), "routb"])

            def hook(ti):
                t0, w = TILES[ti]
                for c in range(8):
                    mm(ps[2][0:8, 0:w], router[:, c, :], X["TMP8"][:, c, 0:w], c == 0, c == 7, ["router", ("TMP8", c)], PSK(2))
                act(LG[0:8, 0:w], ps[2][0:8, 0:w], AF.Identity, ["routb"], [PSK(2), "LG"], bias=routb[0:8, 0:1])
                for j in range(w // 128):
                    jt = (t0 - CTX) // 128 + j
                    P.op("pe", lambda e, jt=jt, j=j: e.transpose(ps[3][:, jt * 8:(jt + 1) * 8], LG[0:8, j * 128:(j + 1) * 128],
                                                                 ident_f[0:8, 0:8]),
                         reads=["LG", "ident_f"], writes=[PSK(3)])

            modulate(l, s, 1, lat, xsrc, X, moe=True, after_tile=hook)
            P.op("dve", lambda e: e.tensor_copy(LT[:, :, :], ps[3][:, 0:128].rearrange("p (a b) -> p a b", b=8)),
                 reads=[], writes=[PSK(3), "LT"])
            P.op("dve", lambda e: e.tensor_reduce(M1[:, :, 0], LT[:, :, :], AX.X, ALU.max), reads=["LT"], writes=["M1"])
            tt(EQ1[:, :, :], LT[:, :, :], M1[:, :, 0:1].to_broadcast([128, 16, 8]), ALU.is_equal, ["LT", "M1"], ["EQ1"])
            stt(L2[:, :, :], EQ1[:, :, :], -1.0e30, LT[:, :, :], ALU.mult, ALU.add, ["EQ1", "LT"], ["L2"])
            P.op("dve", lambda e: e.tensor_reduce(M2[:, :, 0], L2[:, :, :], AX.X, ALU.max), reads=["L2"], writes=["M2"])
            tt(EQ2[:, :, :], L2[:, :, :], M2[:, :, 0:1].to_broadcast([128, 16, 8]), ALU.is_equal, ["L2", "M2"], ["EQ2"])
            tt(W2[:, :, :], M2[:, :, :], M1[:, :, :], ALU.subtract, ["M1", "M2"], [("W2", s)])
            act(W2[:, :, :], W2[:, :, :], AF.Exp, [("W2", s)], [("W2", s)])
            ts(W1[:, :, :], W2[:, :, :], 1.0, None, ALU.add, None, [("W2", s)], [("W1", s)])
            P.op("dve", lambda e, W1=W1: e.reciprocal(W1[:, :, :], W1[:, :, :]), reads=[("W1", s)], writes=[("W1", s)])
            tt(W2[:, :, :], W2[:, :, :], W1[:, :, :], ALU.mult, [("W1", s), ("W2", s)], [("W2", s)])
            tt(TQ[:, :, :], EQ1[:, :, :], EQ2[:, :, :], ALU.add, ["EQ1", "EQ2"], ["TQ"])
            P.op("dve", lambda e: e.tensor_copy(AB[:, :, :], TQ[:, :, :]), reads=["TQ"], writes=["AB"])
            ABf = AB[:, :, :].rearrange("p a b -> p (a b)")
            mm(ps[0][:, 0:128], ltri_bf[:, :], ABf, True, True, ["AB", "ltri_bf"], PSK(0))
            mm(ps[0][:, 128:256], ones_bf[:, :], ABf, True, True, ["AB", "ones_bf"], PSK(0))
            P.op("dve", lambda e: e.tensor_copy(PW[:, :, :], ps[0][:, 0:128].rearrange("p (a b) -> p a b", b=8)),
                 reads=[], writes=[PSK(0), "PW"])
            P.op("dve", lambda e: e.tensor_copy(TOT[:, :, :], ps[0][:, 128:256].rearrange("p (a b) -> p a b", b=8)),
                 reads=[], writes=[PSK(0), "TOT"])
            P.op("dve", lambda e: e.tensor_copy(CS[:, 0, :], NEacc[:, :, 0]), reads=["NEacc"], writes=["CS"])
            for j in range(1, 16):
                tt(CS[:, j, :], CS[:, j - 1, :], TOT[:, j - 1, :], ALU.add, ["CS", "TOT"], ["CS"])
            tt(NEacc[:, :, 0], CS[:, 15, :], TOT[:, 15, :], ALU.add, ["CS", "TOT"], ["NEacc"])
            tt(PW[:, :, :], PW[:, :, :], CS[:, :, :], ALU.add, ["PW", "CS"], ["PW"])
            tt(PW[:, :, :], PW[:, :, :], EOFF[:, :, :], ALU.add, ["PW", "EOFF"], ["PW"])
            tt(TQ[:, :, :], EQ1[:, :, :], PW[:, :, :], ALU.mult, ["EQ1", "PW"], ["TQ"])
            P.op("dve", lambda e: e.tensor_reduce(S12[:, :, 0], TQ[:, :, :], AX.X, ALU.add), reads=["TQ"], writes=["S12"])
            tt(TQ[:, :, :], EQ2[:, :, :], PW[:, :, :], ALU.mult, ["EQ2", "PW", "S12"], ["TQ"])
            P.op("dve", lambda e: e.tensor_reduce(S12[:, :, 1], TQ[:, :, :], AX.X, ALU.add), reads=["TQ"], writes=["S12"])
            P.op("dve", lambda e, SI=SI: e.tensor_copy(SI[:, :, :], S12[:, :, :]), reads=["S12"], writes=[("SI", s)])
            for jt in range(16):
                b = jt % 2
                hb = jt % NHB
                ti = 1 + jt // 4
                for c in range(8):
                    P.op("pe", lambda e, b=b, c=c, jt=jt: e.transpose(psb[b][:, c * 128:(c + 1) * 128],
                                                                      HT[:, c, CTX + jt * 128:CTX + (jt + 1) * 128], ident_bf[:, :]),
                         reads=[("HT", ti), "ident_bf"], writes=[PSK(b)])
                act(HTok[hb][:, :], psb[b][:, :], AF.Identity, [], [PSK(b), ("HTok", hb)])
                for k in range(2):
                    P.dma("pool", lambda e, hb=hb, jt=jt, k=k, SI=SI: e.indirect_dma_start(
                        out=hg_d, out_offset=bass.IndirectOffsetOnAxis(ap=SI[:, jt, k:k + 1], axis=0), in_=HTok[hb][:, :],
                        in_offset=None), reads=[("HTok", hb), ("SI", s)] + [("hgz", q_) for q_ in range(NEXP * SEQ * 2 // 128)], writes=[("hg", s, jt, k)])
        tt(CMP[:, :, :], NEacc[:, :, 0:1].to_broadcast([128, 8, 8]), THR[:, :, :], ALU.is_gt, ["NEacc", "THR"], ["CMP"])
        P.op("dve", lambda e: e.tensor_reduce(JF[:, :], CMP[:, :, :], AX.X, ALU.add), reads=["CMP"], writes=["JF"])
        P.op("dve", lambda e: e.tensor_copy(JI[:, :], JF[:, :]), reads=["JF"], writes=["JI"])
        P.barrier()

        off = OV
        Yacc, off = A("mm_Yacc", [16, D], F32, off)
        HTg, off = A("mm_HTg", [8, SEQ], BF16, off)
        HGs = []
        for i in range(2):
            t_, off = A(f"mm_HGs{i}", [D], BF16, off)
            HGs.append(t_)
        SIL, ACTT = [], []
        for i in range(2):
            t_, off = A(f"mm_SIL{i}", [TSZ], BF16, off)
            SIL.append(t_)
            t_, off = A(f"mm_ACTT{i}", [4, TSZ], BF16, off)
            ACTT.append(t_)
        cnt = {"h": 0, "a": 0, "o": 0, "g": 0}
        allhg = [("hg", s_, a_, b_) for s_ in seqs for a_ in range(16) for b_ in range(2)]
        for p_, ex in [(p__, e__) for p__ in range(NPASS) for e__ in range(NEXP)]:
            P.load_reg(JI[0:1, ex:ex + 1], "JI", engines=("pe", "act", "dve", "sp", "pool"))
            for _once in range(1):
                jb = 4 * p_
                for k in range(16):
                    b = cnt["g"] % 2
                    cnt["g"] += 1
                    r0 = ex * CAP + p_ * SEQ + k * 128
                    thr = jb + k // NQ + 1
                    P.cond_begin(thr)
                    sp_dma(HGs[b][:, :], hg_d[r0:r0 + 128, :], allhg, [("HGs", b)])
                    for c in range(8):
                        P.op("pe", lambda e, b=b, c=c: e.transpose(psb[b][:, c * 128:(c + 1) * 128], HGs[b][:, c * 128:(c + 1) * 128],
                                                                   ident_bf[:, :]),
                             reads=[("HGs", b), "ident_bf"], writes=[PSK(b)])
                    act(HTg[:, :, k * 128:(k + 1) * 128], psb[b][:, :].rearrange("p (a b) -> p a b", b=128), AF.Identity, [],
                        [PSK(b), ("HTg", k // NQ)])
                    P.cond_end()
                for g in range(7):
                    P.cond_begin(jb + 1)
                    w1 = load_slab(("m1", ex, g), big=True)
                    P.cond_end()
                    P.cond_begin(jb + 1)
                    w3 = load_slab(("m3", ex, g), big=True)
                    P.cond_end()
                    P.cond_begin(jb + 1)
                    w2 = load_slab(("m2", ex, g), big=True)
                    P.cond_end()
                    for j in range(4):
                        P.cond_begin(jb + j + 1)
                        ab = cnt["a"] % 2
                        cnt["a"] += 1
                        s0 = j * TSZ
                        for jj in range(4):
                            b = cnt["h"] % 2
                            cnt["h"] += 1
                            b1, b3 = 2 + b, 4 + b
                            for k in range(8):
                                mm(ps[b1][:, 0:TSZ], wk(w1, k, jj * 128, 128), HTg[:, k, s0:s0 + TSZ], k == 0, k == 7,
                                   [("ws", w1), ("HTg", j)], PSK(b1))
                            for k in range(8):
                                mm(ps[b3][:, 0:TSZ], wk(w3, k, jj * 128, 128), HTg[:, k, s0:s0 + TSZ], k == 0, k == 7,
                                   [("ws", w3), ("HTg", j)], PSK(b3))
                            act(SIL[b][:, :], ps[b1][:, 0:TSZ], AF.Silu, [], [PSK(b1), ("SIL", b)])
                            tt(ACTT[ab][:, jj, :], ps[b3][:, 0:TSZ], SIL[b][:, :], ALU.mult, [("SIL", b)], [PSK(b3), ("ACTT", ab)])
                        P.cond_end()
                        P.cond_begin(jb + j + 1)
                        for h2 in range(NQ):
                            kt = NQ * j + h2
                            for dh in range(2):
                                ob = 6 + cnt["o"] % 2
                                cnt["o"] += 1
                                for jj in range(4):
                                    mm(ps[ob][:, 0:512], ACTT[ab][:, jj, h2 * 128:(h2 + 1) * 128], wk2(w2, jj, dh * 512, 512),
                                       jj == 0, jj == 3, [("ws", w2), ("ACTT", ab)], PSK(ob))
                                ya = Yacc[:, kt, dh * 512:(dh + 1) * 512]
                                if g == 0:
                                    P.op("dve", lambda e, ya=ya, ob=ob: e.tensor_copy(ya, ps[ob][:, 0:512]), reads=[],
                                         writes=[PSK(ob), ("Yacc", kt)])
                                else:
                                    tt(ya, ps[ob][:, 0:512], ya, ALU.add, [("Yacc", kt)], [PSK(ob), ("Yacc", kt)])
                        P.cond_end()
                for k in range(16):
                    r0 = ex * CAP + p_ * SEQ + k * 128
                    P.cond_begin(jb + k // NQ + 1)
                    sp_dma(yb_d[r0:r0 + 128, :], Yacc[:, k, :], [("Yacc", k)], [("yb", ex, p_, k)])
                    P.cond_end()
        P.barrier()

        off = OV
        NCB = 6
        YA, YB2, OO, XC8 = [], [], [], []
        for i in range(NCB):
            t_, off = A(f"mm_YA{i}", [D], F32, off)
            YA.append(t_)
            t_, off = A(f"mm_YB2{i}", [D], F32, off)
            YB2.append(t_)
            t_, off = A(f"mm_OO{i}", [D], F32, off)
            OO.append(t_)
            t_, off = A(f"mm_XC8{i}", [8, 128], F32, off)
            XC8.append(t_)
        allyb = [("yb", a_, p_, b_) for a_ in range(NEXP) for p_ in range(NPASS) for b_ in range(16)]
        n_ = 0
        for s in seqs:
            W1, W2, SI = PS_[("W1", s)], PS_[("W2", s)], PS_[("SI", s)]
            for jt in range(16):
                b = n_ % NCB
                pb2 = n_ % 2
                n_ += 1
                ti = 1 + jt // 4
                c0 = CTX + jt * 128
                for k, dst, dk in ((0, YA, "YA"), (1, YB2, "YB2")):
                    P.dma("pool", lambda e, b=b, jt=jt, k=k, dst=dst, SI=SI: e.indirect_dma_start(
                        out=dst[b][:, :], out_offset=None, in_=yb_d,
                        in_offset=bass.IndirectOffsetOnAxis(ap=SI[:, jt, k:k + 1], axis=0)),
                        reads=allyb + [("SI", s)], writes=[(dk, b)])
                sp_dma(XC8[b][:, :, :], xres_d[s, :, :, c0:c0 + 128], [("xd", ti)], [("XC8", b)])
                ts(OO[b][:, :], YA[b][:, :], W1[:, jt, 0:1], None, ALU.mult, None, [("YA", b), ("W1", s)], [("OO", b)])
                stt(OO[b][:, :], YB2[b][:, :], W2[:, jt, 0:1], OO[b][:, :], ALU.mult, ALU.add, [("YB2", b), ("W2", s), ("OO", b)],
                    [("OO", b)])
                for c in range(8):
                    pbk = 2 + 2 * pb2 + c // 4
                    P.op("pe", lambda e, b=b, c=c, pbk=pbk: e.transpose(ps[pbk][:, (c % 4) * 128:(c % 4 + 1) * 128],
                                                                        OO[b][:, c * 128:(c + 1) * 128], ident_f[:, :]),
                         reads=[("OO", b), "ident_f"], writes=[PSK(pbk)])
                for c in range(8):
                    pbk = 2 + 2 * pb2 + c // 4
                    stt(XC8[b][:, c, :], ps[pbk][:, (c % 4) * 128:(c % 4 + 1) * 128], modv(l, 5, c, s), XC8[b][:, c, :],
                        ALU.mult, ALU.add, ["mod", ("XC8", b)], [PSK(pbk), ("XC8", b)])
                sp_dma(outT_d[s, :, :, jt * 128:(jt + 1) * 128], XC8[b][:, :, :], [("XC8", b)], [("out", s, jt)])
        P.barrier()
        return None

    result = None
    if stage in ("full", "seq1"):
        seqs = (1,) if stage == "seq1" else (0, 1)
        for s in seqs:
            state["x_in_res"] = False
            for l in range(2):
                token_mixer(l, s)
                if l == 1 and SPARSE_MOE:
                    if not MERGED_MOE:
                        ffn_moe_sparse(l, s)
                else:
                    ffn(l, s)
        if SPARSE_MOE and MERGED_MOE:
            moe_merged(1, seqs)
    elif si >= 1:
        result = token_mixer(0, 0)
        if result is None and stage in ("G0", "H0", "F1", "G1", "G1a", "H1"):
            result = ffn(0, 0)
            if result is None and stage in ("F1", "G1", "G1a", "H1"):
                result = token_mixer(1, 0)
                if result is None and stage in ("G1", "G1a", "H1"):
                    result = ffn_moe_sparse(1, 0) if SPARSE_MOE else ffn(1, 0)

    if debug is not None:
        if stage == "pro":
            sp_dma(dbg_d.rearrange("p (a b) -> p a b", b=4), mod[:, :, :, :].rearrange("p l a b -> p (l a) b"), ["mod"], ["dbg"])
        elif stage in ("F0", "H0", "F1"):
            sp_dma(dbg_d, xres_d[0], [("xd", t) for t in range(5)], ["dbg"])
        elif result == "done":
            pass
        elif result is not None:
            src_t, keys = result
            DT, _ = A("DT", [NT], F32, (A.limit - NT * 4 - 64) // 32 * 32)
            for c in range(8):
                act(DT[:, :], src_t[:, c, :], AF.Identity, keys, ["DT"])
                sp_dma(dbg_d[:, c, :], DT[:, :], ["DT"], ["dbg"])
    P.emit(nc)
    return nc


def kernel(**inputs):
    inp = {k: np.asarray(v, np.float32) for k, v in inputs.items()}
    shared = build_shared(inp)
    nc = build_program("full")
    in_maps = []
    for core in range(NCORES):
        m = dict(shared)
        m.update(build_core_inputs(inp, core))
        in_maps.append(m)
    res = run_bass_kernel_spmd(nc, in_maps, core_ids=list(range(NCORES)))
    out = np.empty((2 * NCORES, SEQ, D), np.float32)
    for core in range(NCORES):
        oT = np.asarray(res.results[core]["outT"])
        out[2 * core:2 * core + 2] = oT.transpose(0, 3, 2, 1).reshape(2, SEQ, D)
    return out
```
